# Optimizing a Trainium2 kernel written in Bass

```python
import jax, jax.numpy as jnp
from jax import lax
import numpy as np

D_MODEL = 1024
BATCH = 8
SEQ = 2048
DEPTH = 1

D_CONV = D_MODEL // 2
CONV_WIDTH = 31
M_INNER = D_MODEL // 2
M_HEADS = 4
M_HEAD_DIM = M_INNER // M_HEADS
QK_CONV_WIDTH = 4
CHUNK = 64
N_GROUPS = 4
E_PER_GROUP = 8
N_EXPERTS = N_GROUPS * E_PER_GROUP
TOP_K = 2
D_EXPERT = D_MODEL // 2
EXPERT_ROWS = 128
RMS_EPS = 1e-6
LN_EPS = 1e-5
IN_WIDTHS = (D_CONV, D_CONV, M_INNER, M_INNER, M_INNER, M_INNER, M_HEADS, M_HEADS, D_MODEL, D_MODEL)
D_IN = 2 * D_CONV + 4 * M_INNER + 2 * M_HEADS + 2 * D_MODEL

kernel_name = "hybrid_conformer_mlstm_hmoe_adaln"


def rmsnorm(x, g):
    xf = x.astype(jnp.float32)
    y = xf * lax.rsqrt(jnp.mean(xf * xf, axis=-1, keepdims=True) + RMS_EPS)
    return (y * g.astype(jnp.float32)).astype(x.dtype)


def layernorm(x, g, b):
    xf = x.astype(jnp.float32)
    mu = jnp.mean(xf, axis=-1, keepdims=True)
    var = jnp.mean(jnp.square(xf - mu), axis=-1, keepdims=True)
    y = (xf - mu) * lax.rsqrt(var + LN_EPS)
    return (y * g.astype(jnp.float32) + b.astype(jnp.float32)).astype(x.dtype)


def modulate(h, shift, scale):
    return h * (1 + scale[:, None, :]) + shift[:, None, :]


def causal_dwconv(u, w, b):
    width = w.shape[0]
    y = lax.conv_general_dilated(
        u, w.astype(u.dtype)[:, None, :], window_strides=(1,), padding=[(width - 1, 0)],
        dimension_numbers=("NWC", "WIO", "NWC"), feature_group_count=u.shape[-1])
    return y + b.astype(u.dtype)


def mlstm_chunkwise(q, k, v, i_pre, f_pre):
    B, S, NH, DH = q.shape
    NC = S // CHUNK
    q = q * (DH ** -0.5)
    lf = jax.nn.log_sigmoid(f_pre)
    li = i_pre

    def to_chunks(a):
        return a.reshape(B, NC, CHUNK, NH, DH).transpose(1, 0, 3, 2, 4)

    def gates_to_chunks(a):
        return a.reshape(B, NC, CHUNK, NH).transpose(1, 0, 3, 2)

    causal = jnp.tril(jnp.ones((CHUNK, CHUNK), dtype=bool))

    def step(carry, xs):
        C, n, m = carry
        qc, kc, vc, lic, lfc = xs
        bcum = jnp.cumsum(lfc, axis=-1)
        dmat = bcum[..., :, None] - bcum[..., None, :] + lic[..., None, :]
        dmat = jnp.where(causal, dmat, -jnp.inf)
        inter = bcum + m[..., None]
        m_t = jnp.maximum(jnp.max(dmat, axis=-1), inter)
        wts = jnp.exp(dmat - m_t[..., None])
        s_inter = jnp.exp(inter - m_t)
        qk = jnp.einsum("bhtd,bhsd->bhts", qc, kc) * wts
        num = jnp.einsum("bhts,bhsd->bhtd", qk, vc) + s_inter[..., None] * jnp.einsum("bhtd,bhde->bhte", qc, C)
        den = jnp.sum(qk, axis=-1) + s_inter * jnp.einsum("bhtd,bhd->bht", qc, n)
        h = num / jnp.maximum(jnp.abs(den), jnp.exp(-m_t))[..., None]
        b_last = bcum[..., -1]
        a = b_last[..., None] - bcum + lic
        m_new = jnp.maximum(b_last + m, jnp.max(a, axis=-1))
        wk = jnp.exp(a - m_new[..., None])
        sc = jnp.exp(b_last + m - m_new)
        C_new = sc[..., None, None] * C + jnp.einsum("bhs,bhsd,bhse->bhde", wk, kc, vc)
        n_new = sc[..., None] * n + jnp.einsum("bhs,bhsd->bhd", wk, kc)
        return (C_new, n_new, m_new), h

    init = (jnp.zeros((B, NH, DH, DH), jnp.float32),
            jnp.zeros((B, NH, DH), jnp.float32),
            jnp.zeros((B, NH), jnp.float32))
    xs = (to_chunks(q), to_chunks(k), to_chunks(v), gates_to_chunks(li), gates_to_chunks(lf))
    _, h = lax.scan(step, init, xs)
    return h.transpose(1, 0, 3, 2, 4).reshape(B, S, NH, DH)


def hybrid_mixer(h, w_in, b_if, conv_dw_w, conv_dw_b, conv_ln_g, conv_ln_b, w_conv_out,
                 qk_conv_w, qk_conv_b, m_norm_g, w_m_out, w_out):
    B, S, _ = h.shape
    z = h @ w_in
    offs = np.cumsum(IN_WIDTHS)[:-1].tolist()
    glu_a, glu_b, q, k, v, o, i_pre, f_pre, g_a, g_b = jnp.split(z, offs, axis=-1)

    u = glu_a * jax.nn.sigmoid(glu_b)
    u = causal_dwconv(u, conv_dw_w, conv_dw_b)
    u = jax.nn.silu(layernorm(u, conv_ln_g, conv_ln_b))
    y_a = u @ w_conv_out

    qk = jax.nn.silu(causal_dwconv(jnp.concatenate([q, k], axis=-1), qk_conv_w, qk_conv_b))
    q, k = jnp.split(qk, 2, axis=-1)
    heads = lambda a: a.astype(jnp.float32).reshape(B, S, M_HEADS, M_HEAD_DIM)
    bif = b_if.astype(jnp.float32)
    i_g = i_pre.astype(jnp.float32) + bif[:M_HEADS]
    f_g = f_pre.astype(jnp.float32) + bif[M_HEADS:]
    hm = mlstm_chunkwise(heads(q), heads(k), heads(v), i_g, f_g)
    mu = jnp.mean(hm, axis=-1, keepdims=True)
    var = jnp.mean(jnp.square(hm - mu), axis=-1, keepdims=True)
    hm = (hm - mu) * lax.rsqrt(var + LN_EPS) * m_norm_g.astype(jnp.float32).reshape(M_HEADS, M_HEAD_DIM)
    hm = hm * jax.nn.sigmoid(heads(o))
    y_b = hm.reshape(B, S, M_INNER).astype(h.dtype) @ w_m_out

    merged = jax.nn.sigmoid(g_a) * y_a + jax.nn.sigmoid(g_b) * y_b
    return merged @ w_out


def hierarchical_moe(h, w_rg, b_rg, w_re, b_re, w_e_gate, w_e_up, w_e_down):
    T, D = h.shape
    hf = h.astype(jnp.float32)
    p_grp = jax.nn.softmax(hf @ w_rg.astype(jnp.float32) + b_rg.astype(jnp.float32), axis=-1)
    g_sel = jnp.argmax(p_grp, axis=-1)
    p_g = jnp.take_along_axis(p_grp, g_sel[:, None], axis=-1)
    le = (hf @ w_re.astype(jnp.float32) + b_re.astype(jnp.float32)).reshape(T, N_GROUPS, E_PER_GROUP)
    le_sel = jnp.take_along_axis(le, g_sel[:, None, None], axis=1)[:, 0]
    top_p, top_e = lax.top_k(jax.nn.softmax(le_sel, axis=-1), TOP_K)
    w_tok = p_g * top_p / jnp.sum(top_p, axis=-1, keepdims=True)
    e_idx = g_sel[:, None] * E_PER_GROUP + top_e

    A = T * TOP_K
    e_flat = e_idx.reshape(A).astype(jnp.int32)
    w_flat = w_tok.reshape(A)
    tok = jnp.arange(A, dtype=jnp.int32) // TOP_K
    order = jnp.argsort(e_flat)
    e_sorted = e_flat[order]
    tok_sorted = tok[order]
    counts = jnp.bincount(e_flat, length=N_EXPERTS)
    padded = (counts + EXPERT_ROWS - 1) // EXPERT_ROWS * EXPERT_ROWS
    starts = jnp.cumsum(counts) - counts
    pends = jnp.cumsum(padded)
    pstarts = pends - padded
    dest = pstarts[e_sorted] + jnp.arange(A, dtype=jnp.int32) - starts[e_sorted]
    n_groups_rows = -(-(A + N_EXPERTS * (EXPERT_ROWS - 1)) // EXPERT_ROWS)
    P = n_groups_rows * EXPERT_ROWS
    xs = jnp.zeros((P, D), h.dtype).at[dest].set(h[tok_sorted])
    grp_e = jnp.minimum(jnp.searchsorted(pends, jnp.arange(n_groups_rows) * EXPERT_ROWS, side="right"),
                        N_EXPERTS - 1)

    def expert_rows(args):
        xb, e = args
        return (jax.nn.silu(xb @ w_e_gate[e]) * (xb @ w_e_up[e])) @ w_e_down[e]

    ys = lax.map(expert_rows, (xs.reshape(n_groups_rows, EXPERT_ROWS, D), grp_e)).reshape(P, D)
    contrib = ys[dest].astype(jnp.float32) * w_flat[order][:, None]
    return jax.ops.segment_sum(contrib, tok_sorted, num_segments=T).astype(h.dtype)


def setup_inputs(seed: int = 0) -> dict:
    key = jax.random.key(seed)
    ks = jax.random.split(key, 32)
    L, D = DEPTH, D_MODEL

    def nrm(k, shape, scale):
        return jax.random.normal(k, shape, jnp.float32) * scale

    b_if = jnp.concatenate([
        nrm(ks[6], (L, M_HEADS), 0.1),
        jnp.linspace(3.0, 6.0, M_HEADS, dtype=jnp.float32)[None, :] + nrm(ks[7], (L, M_HEADS), 0.1)], axis=-1)
    return {
        "x": nrm(ks[0], (BATCH, SEQ, D), 1.0),
        "c": nrm(ks[1], (BATCH, D), 1.0),
        "w_ada": nrm(ks[2], (L, D, 6 * D), 0.5 * D ** -0.5),
        "b_ada": nrm(ks[3], (L, 6 * D), 0.02),
        "g_norm1": 1.0 + nrm(ks[4], (L, D), 0.02),
        "w_in": nrm(ks[5], (L, D, D_IN), D ** -0.5),
        "b_if": b_if,
        "conv_dw_w": nrm(ks[8], (L, CONV_WIDTH, D_CONV), CONV_WIDTH ** -0.5),
        "conv_dw_b": nrm(ks[9], (L, D_CONV), 0.02),
        "conv_ln_g": 1.0 + nrm(ks[10], (L, D_CONV), 0.02),
        "conv_ln_b": nrm(ks[11], (L, D_CONV), 0.02),
        "w_conv_out": nrm(ks[12], (L, D_CONV, D), D_CONV ** -0.5),
        "qk_conv_w": nrm(ks[13], (L, QK_CONV_WIDTH, 2 * M_INNER), QK_CONV_WIDTH ** -0.5),
        "qk_conv_b": nrm(ks[14], (L, 2 * M_INNER), 0.02),
        "m_norm_g": 1.0 + nrm(ks[15], (L, M_INNER), 0.02),
        "w_m_out": nrm(ks[16], (L, M_INNER, D), M_INNER ** -0.5),
        "w_out": nrm(ks[17], (L, D, D), D ** -0.5),
        "g_norm2": 1.0 + nrm(ks[18], (L, D), 0.02),
        "w_rg": nrm(ks[19], (L, D, N_GROUPS), D ** -0.5),
        "b_rg": nrm(ks[20], (L, N_GROUPS), 0.01),
        "w_re": nrm(ks[21], (L, D, N_EXPERTS), D ** -0.5),
        "b_re": nrm(ks[22], (L, N_EXPERTS), 0.01),
        "w_e_gate": nrm(ks[23], (L, N_EXPERTS, D, D_EXPERT), D ** -0.5),
        "w_e_up": nrm(ks[24], (L, N_EXPERTS, D, D_EXPERT), D ** -0.5),
        "w_e_down": nrm(ks[25], (L, N_EXPERTS, D_EXPERT, D), D_EXPERT ** -0.5),
        "g_final": 1.0 + nrm(ks[26], (D,), 0.02),
    }


def reference(x, c, w_ada, b_ada, g_norm1, w_in, b_if, conv_dw_w, conv_dw_b, conv_ln_g, conv_ln_b,
              w_conv_out, qk_conv_w, qk_conv_b, m_norm_g, w_m_out, w_out, g_norm2, w_rg, b_rg,
              w_re, b_re, w_e_gate, w_e_up, w_e_down, g_final):
    B, S, D = x.shape
    for l in range(DEPTH):
        mod = jax.nn.silu(c) @ w_ada[l] + b_ada[l]
        sh1, sc1, gt1, sh2, sc2, gt2 = jnp.split(mod, 6, axis=-1)
        h = modulate(rmsnorm(x, g_norm1[l]), sh1, sc1)
        y = hybrid_mixer(h, w_in[l], b_if[l], conv_dw_w[l], conv_dw_b[l], conv_ln_g[l], conv_ln_b[l],
                         w_conv_out[l], qk_conv_w[l], qk_conv_b[l], m_norm_g[l], w_m_out[l], w_out[l])
        x = x + gt1[:, None, :] * y
        h = modulate(rmsnorm(x, g_norm2[l]), sh2, sc2)
        y = hierarchical_moe(h.reshape(B * S, D), w_rg[l], b_rg[l], w_re[l], b_re[l],
                             w_e_gate[l], w_e_up[l], w_e_down[l]).reshape(B, S, D)
        x = x + gt2[:, None, :] * y
    return rmsnorm(x, g_final)
```

```python
import contextlib
import numpy as np
import concourse.bass as bass
import concourse.mybir as mybir
from concourse.bass_utils import run_bass_kernel_spmd

F32 = mybir.dt.float32
BF16 = mybir.dt.bfloat16
I32 = mybir.dt.int32
AF = mybir.ActivationFunctionType
ALU = mybir.AluOpType
AX = mybir.AxisListType

T = 2048
D = 1024
NT = 16
NB = 4
DIN = 5128
ENG_NAMES = ("pe", "act", "dve", "pool", "sp")


class Op:
    __slots__ = ("eng", "fn", "is_dma", "grp", "signal", "val", "idx", "deps")

    def __init__(self, eng, fn, is_dma, grp):
        self.eng = eng
        self.fn = fn
        self.is_dma = is_dma
        self.grp = grp
        self.signal = False
        self.val = None
        self.idx = None
        self.deps = []


def _reduce_ops(ops):
    latest = {}
    dm = {}
    for o in ops:
        if o.is_dma:
            if o.grp not in dm or dm[o.grp].idx < o.idx:
                dm[o.grp] = o
        else:
            if o.eng not in latest or latest[o.eng].idx < o.idx:
                latest[o.eng] = o
    return list(latest.values()) + list(dm.values())


class Prog:
    def __init__(self, nc):
        self.nc = nc
        self.ops = {e: [] for e in ENG_NAMES}
        self.all_ops = []
        self.last_writer = {}
        self.readers = {}
        self.dma_groups = {}
        self.buf_pred = {}
        self.keys_by_buf = {}
        self.wait_all_groups = set()

    def _touch(self, k):
        if k not in self.readers:
            self.readers[k] = list(self.buf_pred.get(k[0], ()))
            self.last_writer[k] = None
            self.keys_by_buf.setdefault(k[0], set()).add(k)

    def ops_touching(self, bufname):
        s = list(self.buf_pred.get(bufname, ()))
        for k in self.keys_by_buf.get(bufname, ()):
            w = self.last_writer.get(k)
            if w is not None:
                s.append(w)
            s.extend(self.readers.get(k, ()))
        return _reduce_ops(s)

    def add(self, eng, fn, reads=(), writes=(), dma=False, grp=None):
        op = Op(eng, fn, dma, grp)
        op.idx = len(self.all_ops)
        self.all_ops.append(op)
        self.ops[eng].append(op)
        if dma:
            assert grp is not None
            self.dma_groups.setdefault(grp, []).append(op)
        deps = []
        for k in reads:
            self._touch(k)
            w = self.last_writer[k]
            if w is not None:
                deps.append((w, "raw"))
            elif self.readers[k] and k[0] in self.buf_pred:
                pass
        for k in writes:
            self._touch(k)
            w = self.last_writer[k]
            if w is not None:
                deps.append((w, "waw"))
            for r in self.readers[k]:
                deps.append((r, "war"))
        for d, kind in deps:
            if d is op:
                continue
            if (not d.is_dma) and (not dma) and d.eng == eng:
                if eng == "pe":
                    continue
            op.deps.append(d)
        for k in reads:
            self.readers[k].append(op)
        for k in writes:
            self.last_writer[k] = op
            self.readers[k] = []
        return op

    def emit(self, final_wait_groups=()):
        nc = self.nc
        for op in self.all_ops:
            op.deps = _reduce_ops(op.deps)
            for d in op.deps:
                d.signal = True
        for e in ENG_NAMES:
            c = 0
            for op in self.ops[e]:
                if (not op.is_dma) and op.signal:
                    c += 1
                    op.val = c
        gtotal = {}
        for g, lst in self.dma_groups.items():
            c = 0
            for op in lst:
                c += 16
                op.val = c
            gtotal[g] = c
        with contextlib.ExitStack() as st:
            esem = {e: st.enter_context(nc.semaphore("s_" + e)) for e in ENG_NAMES}
            gsem = {g: st.enter_context(nc.semaphore("d_%d" % i))
                    for i, g in enumerate(self.dma_groups)}
            block = st.enter_context(nc.Block())

            def run(e, engobj):
                seen = {}
                for op in self.ops[e]:
                    for d in op.deps:
                        if d.is_dma:
                            key = ("g", d.grp)
                            sem = gsem[d.grp]
                            v = gtotal[d.grp] if d.grp in self.wait_all_groups else d.val
                        else:
                            key = ("e", d.eng)
                            sem = esem[d.eng]
                            v = d.val
                        if seen.get(key, 0) >= v:
                            continue
                        seen[key] = v
                        engobj.wait_ge(sem, v)
                    ins = op.fn(engobj)
                    if op.is_dma:
                        ins.then_inc(gsem[op.grp], 16)
                    elif op.signal:
                        ins.then_inc(esem[e], 1)
                if e == "sp":
                    for g in final_wait_groups:
                        engobj.wait_ge(gsem[g], gtotal[g])

            block.tensor(lambda eng: run("pe", eng))
            block.scalar(lambda eng: run("act", eng))
            block.vector(lambda eng: run("dve", eng))
            block.gpsimd(lambda eng: run("pool", eng))
            block.sync(lambda eng: run("sp", eng))


class Arena:
    def __init__(self, nc, prog, words):
        self.t = nc.alloc_sbuf_tensor("arena", [128, words], F32)
        self.P = prog
        self.free_list = [(0, words)]
        self.live = {}
        self.dead = []
        self.peak = 0

    def alloc(self, name, shape, dt, parts=128):
        n = int(np.prod(shape))
        esz = 2 if dt == BF16 else 4
        words = (n * esz + 31) // 32 * 8
        for i, (o, w) in enumerate(self.free_list):
            if w >= words:
                off = o
                if w == words:
                    self.free_list.pop(i)
                else:
                    self.free_list[i] = (o + words, w - words)
                break
        else:
            raise RuntimeError("SBUF arena full allocating %s (%d words); live=%s" % (
                name, words, {k: v[1] for k, v in self.live.items()}))
        self.live[name] = (off, words)
        self.peak = max(self.peak, off + words)
        preds = []
        for (o, w, nm) in self.dead:
            if o < off + words and off < o + w:
                preds.extend(self.P.ops_touching(nm))
        assert name not in self.P.keys_by_buf, name
        self.P.buf_pred[name] = _reduce_ops(preds)
        v = self.t[0:parts, off:off + words]
        if dt != F32:
            v = v.bitcast(dt)
        v = v[:, 0:n]
        if len(shape) == 2:
            v = v.rearrange("p (a b) -> p a b", b=shape[1])
        elif len(shape) == 3:
            v = v.rearrange("p (a b c) -> p a b c", b=shape[1], c=shape[2])
        return v

    def free(self, name):
        off, words = self.live.pop(name)
        self.dead.append((off, words, name))
        fl = self.free_list + [(off, words)]
        fl.sort()
        merged = []
        for o, w in fl:
            if merged and merged[-1][0] + merged[-1][1] == o:
                merged[-1] = (merged[-1][0], merged[-1][1] + w)
            else:
                merged.append((o, w))
        self.free_list = merged


def build(stage=99, dbg=()):
    nc = bass.Bass("TRN2", target_bir_lowering=False)
    P = Prog(nc)
    A = Arena(nc, P, 52992)

    def din(name, shape, dt=F32):
        return nc.dram_tensor(name, list(shape), dt, kind="ExternalInput").ap()

    x_d = din("x", [T, D])
    ccol_d = din("c_col", [128, 8])
    wada_d = din("w_ada", [D, 6 * D])
    bada_d = din("b_ada_col", [128, 48])
    g1_d = din("g1_col", [128, 8])
    g2_d = din("g2_col", [128, 8])
    win_d = din("w_in", [D, DIN])
    bif_d = din("b_if_bc", [128, 8])
    cw_d = din("conv_w_col", [128, 4, 31])
    cb_d = din("conv_b_col", [128, 4])
    clg_d = din("conv_lng_col", [128, 4])
    clb_d = din("conv_lnb_col", [128, 4])
    wco_d = din("w_conv_out", [512, D])
    qkw_d = din("qk_w_col", [128, 8, 4])
    qkb_d = din("qk_b_col", [128, 8])
    mng_d = din("mng_col", [128, 4])
    wmo_d = din("w_m_out", [512, D])
    wout_d = din("w_out", [D, D])
    wr_d = din("w_router", [D, 36])
    br_d = din("b_router_bc", [128, 36])
    weg_d = din("w_e_gate_l", [32 * 128, 8 * 512])
    weu_d = din("w_e_up_l", [32 * 128, 8 * 512])
    wed_d = din("w_e_down_l", [32 * 128, 4 * D])
    gfin_d = din("g_final_bc", [128, D])
    out_d = nc.dram_tensor("out", [T, D], F32, kind="ExternalOutput").ap()

    dbg_outs = {}

    def dbg_out(name, ap, reads):
        if name not in dbg:
            return
        shape = list(ap.shape)
        dt = ap.dtype
        d = nc.dram_tensor("dbg_" + name, shape, dt, kind="ExternalOutput").ap()
        dbg_outs[name] = d
        P.add("sp", lambda e: e.dma_start(out=d, in_=ap), reads=reads, dma=True, grp="dbgout")

    psb = [nc.alloc_psum_tensor("ps%d" % i, [128, 512], F32) for i in range(8)]
    ps_rot = list(range(8))

    def next_ps():
        i = ps_rot.pop(0)
        ps_rot.append(i)
        return psb[i], ("ps%d" % i,)

    def hold_ps():
        i = ps_rot.pop(0)
        return psb[i], ("ps%d" % i,)

    def release_ps(key):
        ps_rot.append(int(key[0][2:]))

    ident_f = A.alloc("ident_f", [128], F32)
    ident_b = A.alloc("ident_b", [128], BF16)
    ones_b = A.alloc("ones_b", [128], BF16)
    ones_f = A.alloc("ones_f", [128], F32)
    mask_ut = A.alloc("mask_ut", [128], BF16)
    tri_f = A.alloc("tri_f", [128], F32)
    K_ID = ("ident_f", 0)
    P.add("pool", lambda e: e.memset(ident_f, 0.0), writes=[("ident_f", 0)])
    P.add("pool", lambda e: e.affine_select(out=ident_f, in_=ident_f, pattern=[[-1, 128]],
                                             compare_op=ALU.not_equal, fill=1.0, base=0, channel_multiplier=1),
          reads=[("ident_f", 0)], writes=[("ident_f", 0)])
    P.add("pool", lambda e: e.tensor_copy(out=ident_b, in_=ident_f), reads=[("ident_f", 0)], writes=[("ident_b", 0)])
    P.add("pool", lambda e: e.memset(ones_b, 1.0), writes=[("ones_b", 0)])
    P.add("pool", lambda e: e.memset(ones_f, 1.0), writes=[("ones_f", 0)])
    P.add("pool", lambda e: e.memset(tri_f, 1.0), writes=[("tri_f", 0)])
    P.add("pool", lambda e: e.affine_select(out=tri_f, in_=tri_f, pattern=[[1, 128]],
                                             compare_op=ALU.is_ge, fill=0.0, base=0, channel_multiplier=-1),
          reads=[("tri_f", 0)], writes=[("tri_f", 0)])
    P.add("pool", lambda e: e.tensor_copy(out=mask_ut, in_=tri_f), reads=[("tri_f", 0)], writes=[("mask_ut", 0)])

    def load_const(name, dram, shape, dt=F32):
        t = A.alloc(name, shape, dt)
        P.add("sp", lambda e: e.dma_start(out=t, in_=dram), writes=[(name, 0)], dma=True, grp="c_" + name)
        return t

    ccol = load_const("ccol", ccol_d, [8])
    bada = load_const("bada", bada_d, [48])
    g1c = load_const("g1c", g1_d, [8])
    g2c = load_const("g2c", g2_d, [8])

    silc = A.alloc("silc", [8], F32)
    silb = A.alloc("silb", [8], BF16)
    P.add("act", lambda e: e.activation(out=silc, in_=ccol, func=AF.Silu), reads=[("ccol", 0)], writes=[("silc", 0)])
    P.add("dve", lambda e: e.tensor_copy(out=silb, in_=silc), reads=[("silc", 0)], writes=[("silb", 0)])
    modT = A.alloc("modT", [48], F32)
    wada_v = wada_d.rearrange("(c p) n -> p c n", p=128)
    NWA = 3
    wab = [A.alloc("wada%d" % i, [8, 512], BF16) for i in range(NWA)]
    a1 = A.alloc("a1", [8], F32)
    a2 = A.alloc("a2", [8], F32)

    def adaln_block(blk, ps_mod, k_mod):
        s_ = blk % NWA
        buf = wab[s_]
        nm = "wada%d" % s_
        P.add("pool", (lambda e, buf=buf, blk=blk: e.dma_start(out=buf, in_=wada_v[:, :, blk * 512:(blk + 1) * 512])),
              writes=[(nm, 0)], dma=True, grp=nm)

        def mm(e, buf=buf, blk=blk):
            ins = None
            for jj in range(4):
                j = blk * 4 + jj
                for k in range(8):
                    ins = e.matmul(ps_mod[:, j:j + 1], lhsT=buf[:, k, jj * 128:(jj + 1) * 128], rhs=silb[:, k:k + 1],
                                   start=(k == 0), stop=(k == 7))
            return ins
        P.add("pe", mm, reads=[(nm, 0), ("silb", 0)], writes=[k_mod])

    def adaln_finish(ps_mod, k_mod, c0, c1, part):
        P.add("dve", lambda e: e.tensor_tensor(out=modT[:, c0:c1], in0=ps_mod[:, c0:c1], in1=bada[:, c0:c1], op=ALU.add),
              reads=[k_mod, ("bada", 0)], writes=[("modT", part)])
        release_ps(k_mod)

    pm0, km0 = hold_ps()
    for blk in range(4):
        adaln_block(blk, pm0, km0)
    adaln_finish(pm0, km0, 0, 16, 0)
    P.add("dve", lambda e: e.scalar_tensor_tensor(out=a1, in0=modT[:, 8:16], scalar=1.0, in1=g1c, op0=ALU.add, op1=ALU.mult),
          reads=[("modT", 0), ("g1c", 0)], writes=[("a1", 0)])
    p2state = {}

    def adaln_p2_block(blk):
        if "ps" not in p2state:
            p2state["ps"] = hold_ps()
        adaln_block(blk, *p2state["ps"])

    def adaln_p2_end():
        pm1, km1 = p2state["ps"]
        adaln_finish(pm1, km1, 16, 48, 1)
        P.add("dve", lambda e: e.scalar_tensor_tensor(out=a2, in0=modT[:, 32:40], scalar=1.0, in1=g2c, op0=ALU.add, op1=ALU.mult),
              reads=[("modT", 1), ("g2c", 0)], writes=[("a2", 0)])
        dbg_out("modT", modT, [("modT", 0), ("modT", 1)])
        for i in range(NWA):
            A.free("wada%d" % i)

    hT = A.alloc("hT", [8, T], BF16)
    xin = [A.alloc("xin%d" % i, [D], F32) for i in range(4)]
    xnb = [A.alloc("xnb%d" % i, [D], BF16) for i in range(4)]
    junk = A.alloc("junk", [D], F32)
    ss1 = A.alloc("ss1", [NT], F32)
    rs1 = A.alloc("rs1", [NT], F32)
    for nb in range(NB):
        pst = [next_ps() for _ in range(4)]
        for tt in range(4):
            ti = nb * 4 + tt
            s = ti % 4
            P.add("sp", (lambda e, s=s, ti=ti: e.dma_start(out=xin[s], in_=x_d[ti * 128:(ti + 1) * 128, :])),
                  writes=[("xin%d" % s, 0)], dma=True, grp="xin%d" % s)
            P.add("act", (lambda e, s=s, ti=ti: e.activation(out=junk, in_=xin[s], func=AF.Square,
                                                             accum_out=ss1[:, ti:ti + 1])),
                  reads=[("xin%d" % s, 0)], writes=[("junk", 0), ("ss1", ti)])
            P.add("act", (lambda e, ti=ti: e.activation(out=rs1[:, ti:ti + 1], in_=ss1[:, ti:ti + 1], func=AF.Sqrt,
                                                        scale=1.0 / D, bias=1e-6)),
                  reads=[("ss1", ti)], writes=[("rs1", ti)])
            P.add("dve", (lambda e, ti=ti: e.reciprocal(out=rs1[:, ti:ti + 1], in_=rs1[:, ti:ti + 1])),
                  reads=[("rs1", ti)], writes=[("rs1", ti)])
            P.add("dve", (lambda e, s=s, ti=ti: e.tensor_scalar(out=xnb[s], in0=xin[s], scalar1=rs1[:, ti:ti + 1],
                                                                scalar2=None, op0=ALU.mult)),
                  reads=[("xin%d" % s, 0), ("rs1", ti)], writes=[("xnb%d" % s, 0)])

            def tr(e, s=s, tt=tt, pst=pst):
                ins = None
                for c in range(8):
                    pb = pst[c // 2][0].bitcast(BF16)
                    ins = e.transpose(out=pb[:, (c % 2) * 512 + tt * 128:(c % 2) * 512 + (tt + 1) * 128],
                                      in_=xnb[s][:, c * 128:(c + 1) * 128], identity=ident_b)
                return ins
            P.add("pe", tr, reads=[("xnb%d" % s, 0), ("ident_b", 0)], writes=[pst[i][1] for i in range(4)])
        for c in range(8):
            pb = pst[c // 2][0].bitcast(BF16)
            P.add("act", (lambda e, c=c, pb=pb, nb=nb: e.activation(
                out=hT[:, c, nb * 512:(nb + 1) * 512], in_=pb[:, (c % 2) * 512:(c % 2 + 1) * 512],
                func=AF.Identity, scale=a1[:, c:c + 1], bias=modT[:, c:c + 1])),
                reads=[pst[c // 2][1], ("a1", 0), ("modT", 0)],
                writes=[("hT", c, nb)])
    dbg_out("hT", hT, [("hT", c, nb) for c in range(8) for nb in range(NB)])
    for i in range(4):
        A.free("xin%d" % i); A.free("xnb%d" % i)

    def finish():
        P.emit(final_wait_groups=["dbgout"] if "dbgout" in P.dma_groups else [])
        return nc, dbg_outs

    GROWS = 256
    NG = -(-(2 * T + 32 * (GROWS - 1)) // GROWS)
    XS = nc.dram_tensor("xs_scratch", [NG * GROWS, D], BF16).ap()
    YS = nc.dram_tensor("ys_scratch", [NG * GROWS, D], BF16).ap()
    zt = A.alloc("zt", [D], BF16)
    P.add("pool", lambda e: e.memset(zt, 0.0), writes=[("zt", 0)])
    XSZ_KEYS = []
    for zi in range(NG * GROWS // 1024):
        P.add("sp", (lambda e, zi=zi: e.dma_start(out=XS[zi * 1024:(zi + 1) * 1024, :].rearrange("(n p) d -> p n d", p=128),
                                                  in_=zt.unsqueeze(1).to_broadcast([128, 8, D]))),
              reads=[("zt", 0)], writes=[("XSZ", zi)], dma=True, grp="xs_zero")
        XSZ_KEYS.append(("XSZ", zi))
    A.free("zt")
    if stage <= 1:
        return finish()

    win_v = win_d.rearrange("(c p) n -> p c n", p=128)
    NWB = 3
    wbufs = [A.alloc("wblk%d" % i, [8, 512], BF16) for i in range(NWB)]
    wb_ctr = [0]

    def load_wblock(col0, ncols=512):
        i = wb_ctr[0] % NWB
        wb_ctr[0] += 1
        buf = wbufs[i]
        nm = "wblk%d" % i
        P.add("pool", lambda e: e.dma_start(out=buf[:, :, 0:ncols], in_=win_v[:, :, col0:col0 + ncols]),
              writes=[(nm, 0)], dma=True, grp=nm)
        return buf, (nm, 0)

    def load_w4(dram_v):
        i = wb_ctr[0] % NWB
        wb_ctr[0] += 1
        nm = "wblk%d" % i
        v = wbufs[i].rearrange("p a b -> p (a b)").rearrange("p (a b) -> p a b", b=D)
        P.add("pool", lambda e: e.dma_start(out=v, in_=dram_v), writes=[(nm, 0)], dma=True, grp=nm)
        return v, (nm, 0)

    def load_cast(name, dram_ap, shape):
        t = A.alloc(name, shape, BF16)
        P.add("pool", lambda e: e.dma_start(out=t, in_=dram_ap), writes=[(name, 0)], dma=True, grp="c_" + name)
        return t

    hT_keys = lambda nb: [("hT", c, nb) for c in range(8)]

    def proj_fm(wb, wkey, mcol, nb):
        ps, pk = next_ps()

        def mm(e):
            ins = None
            for k in range(8):
                ins = e.matmul(ps[:, :], lhsT=wb[:, k, mcol * 128:(mcol + 1) * 128], rhs=hT[:, k, nb * 512:(nb + 1) * 512],
                               start=(k == 0), stop=(k == 7))
            return ins
        P.add("pe", mm, reads=[wkey] + hT_keys(nb), writes=[pk])
        return ps, pk

    cw = load_const("cw", cw_d, [4, 31])
    cb = load_const("cb", cb_d, [4])
    clg = load_const("clg", clg_d, [4])
    clb = load_const("clb", clb_d, [4])
    merged = A.alloc("merged", [8, T], BF16)
    u = A.alloc("u", [4, 32 + T], BF16)
    PADU = 32
    for m in range(4):
        P.add("pool", (lambda e, m=m: e.memset(u[:, m, 0:PADU], 0.0)), writes=[("u", m, -1)])
    dg31 = A.alloc("dg31", [4, 31, 128], BF16)
    for m in range(4):
        P.add("pool", (lambda e, m=m: e.tensor_tensor(
            out=dg31[:, m], in0=ident_b.unsqueeze(1).to_broadcast([128, 31, 128]),
            in1=cw[:, m, :].unsqueeze(2).to_broadcast([128, 31, 128]), op=ALU.mult)),
            reads=[("ident_b", 0), ("cw", 0)], writes=[("dg31", m)])
    sgt = [A.alloc("sgt%d" % i, [512], BF16) for i in range(2)]
    sg_ctr = [0]

    def next_sgt():
        i = sg_ctr[0] % 2
        sg_ctr[0] += 1
        return sgt[i], ("sgt%d" % i, 0)

    wa, wak = load_wblock(0)
    wbk, wbkk = load_wblock(512)
    for m in range(4):
        for nb in range(NB):
            psa, pka = proj_fm(wa, wak, m, nb)
            psb_, pkb = proj_fm(wbk, wbkk, m, nb)
            sg, sgk = next_sgt()
            P.add("act", (lambda e, sg=sg, p=psb_: e.activation(out=sg, in_=p[:, :], func=AF.Sigmoid)),
                  reads=[pkb], writes=[sgk])
            P.add("dve", (lambda e, sg=sg, p=psa, m=m, nb=nb: e.tensor_tensor(
                out=u[:, m, PADU + nb * 512:PADU + (nb + 1) * 512], in0=p[:, :], in1=sg, op=ALU.mult)),
                reads=[pka, sgk], writes=[("u", m, nb)])
    dbg_out("u", u, [("u", m, nb) for m in range(4) for nb in range(-1, NB)])

    wco, wcok = load_w4(wco_d.rearrange("(c p) n -> p c n", p=128))
    gA_blocks = {0: load_wblock(3080)}
    cT = A.alloc("cT", [4, T], BF16)
    sqT = A.alloc("sqT", [4, T], BF16)
    for m in range(4):
        for nb in range(NB):
            ps, pk = next_ps()

            def cmm(e, ps=ps, m=m, nb=nb):
                ins = None
                for k in range(31):
                    o = PADU - 30 + nb * 512 + k
                    ins = e.matmul(ps[:, :], lhsT=dg31[:, m, k, :], rhs=u[:, m, o:o + 512], start=(k == 0), stop=(k == 30))
                return ins
            P.add("pe", cmm, reads=[("dg31", m), ("u", m, nb), ("u", m, nb - 1)], writes=[pk])
            P.add("act", (lambda e, ps=ps, m=m, nb=nb: e.activation(
                out=cT[:, m, nb * 512:(nb + 1) * 512], in_=ps[:, :], func=AF.Identity, bias=cb[:, m:m + 1])),
                reads=[pk, ("cb", 0)], writes=[("cT", m, nb)])
            P.add("act", (lambda e, ps=ps, m=m, nb=nb: e.activation(
                out=sqT[:, m, nb * 512:(nb + 1) * 512], in_=ps[:, :], func=AF.Square, bias=cb[:, m:m + 1])),
                reads=[pk, ("cb", 0)], writes=[("sqT", m, nb)])
            gi = m * NB + nb
            if gi % 2 == 1:
                adaln_p2_block(4 + gi // 2)
    adaln_p2_end()
    dbg_out("cT", cT, [("cT", m, nb) for m in range(4) for nb in range(NB)])
    A.free("u")
    A.free("dg31")

    actT = A.alloc("actT", [4, T], BF16)
    mean_t = A.alloc("mean_t", [512], F32)
    rstd_t = A.alloc("rstd_t", [512], F32)
    msq_t = A.alloc("msq_t", [512], F32)
    nrm_t = [A.alloc("nrm_t%d" % i, [512], F32) for i in range(2)]
    for nb in range(NB):
        ps1, pk1 = next_ps()
        ps2, pk2 = next_ps()

        def smm(e, ps1=ps1, ps2=ps2, nb=nb):
            ins = None
            for m in range(4):
                ins = e.matmul(ps1[:, :], lhsT=ones_b, rhs=cT[:, m, nb * 512:(nb + 1) * 512], start=(m == 0), stop=(m == 3))
            for m in range(4):
                ins = e.matmul(ps2[:, :], lhsT=ones_b, rhs=sqT[:, m, nb * 512:(nb + 1) * 512], start=(m == 0), stop=(m == 3))
            return ins
        P.add("pe", smm, reads=[("ones_b", 0)] + [("cT", m, nb) for m in range(4)] + [("sqT", m, nb) for m in range(4)],
              writes=[pk1, pk2])
        P.add("dve", (lambda e, ps1=ps1: e.tensor_scalar(out=mean_t, in0=ps1[:, :], scalar1=1.0 / 512, scalar2=None, op0=ALU.mult)),
              reads=[pk1], writes=[("mean_t", 0)])
        P.add("dve", lambda e: e.tensor_tensor(out=msq_t, in0=mean_t, in1=mean_t, op=ALU.mult),
              reads=[("mean_t", 0)], writes=[("msq_t", 0)])
        P.add("dve", (lambda e, ps2=ps2: e.scalar_tensor_tensor(out=rstd_t, in0=ps2[:, :], scalar=1.0 / 512, in1=msq_t,
                                                                op0=ALU.mult, op1=ALU.subtract)),
              reads=[pk2, ("msq_t", 0)], writes=[("rstd_t", 0)])
        P.add("act", lambda e: e.activation(out=rstd_t, in_=rstd_t, func=AF.Sqrt, bias=1e-5),
              reads=[("rstd_t", 0)], writes=[("rstd_t", 0)])
        P.add("dve", lambda e: e.reciprocal(out=rstd_t, in_=rstd_t), reads=[("rstd_t", 0)], writes=[("rstd_t", 0)])
        for m in range(4):
            nt = nrm_t[m % 2]
            ntk = ("nrm_t%d" % (m % 2), 0)
            P.add("dve", (lambda e, nt=nt, m=m, nb=nb: e.tensor_tensor(out=nt, in0=cT[:, m, nb * 512:(nb + 1) * 512], in1=mean_t,
                                                                      op=ALU.subtract)),
                  reads=[("cT", m, nb), ("mean_t", 0)], writes=[ntk])
            P.add("dve", (lambda e, nt=nt: e.tensor_tensor(out=nt, in0=nt, in1=rstd_t, op=ALU.mult)),
                  reads=[ntk, ("rstd_t", 0)], writes=[ntk])
            P.add("act", (lambda e, nt=nt, m=m, nb=nb: e.activation(
                out=actT[:, m, nb * 512:(nb + 1) * 512], in_=nt, func=AF.Silu, scale=clg[:, m:m + 1], bias=clb[:, m:m + 1])),
                reads=[ntk, ("clg", 0), ("clb", 0)], writes=[("actT", m, nb)])
    dbg_out("actT", actT, [("actT", m, nb) for m in range(4) for nb in range(NB)])
    A.free("cT"); A.free("sqT"); A.free("mean_t"); A.free("rstd_t"); A.free("msq_t"); A.free("nrm_t0"); A.free("nrm_t1")

    for jb in range(2):
        wg_, wgk = gA_blocks[jb] if jb in gA_blocks else load_wblock(3080 + jb * 512)
        for jj in range(4):
            j = jb * 4 + jj
            for nb in range(NB):
                psy, pky = next_ps()

                def ymm(e, psy=psy, j=j, nb=nb):
                    ins = None
                    for m in range(4):
                        ins = e.matmul(psy[:, :], lhsT=wco[:, m, j * 128:(j + 1) * 128], rhs=actT[:, m, nb * 512:(nb + 1) * 512],
                                       start=(m == 0), stop=(m == 3))
                    return ins
                P.add("pe", ymm, reads=[wcok] + [("actT", m, nb) for m in range(4)], writes=[pky])
                psg, pkg = proj_fm(wg_, wgk, jj, nb)
                sg, sgk = next_sgt()
                P.add("act", (lambda e, sg=sg, p=psg: e.activation(out=sg, in_=p[:, :], func=AF.Sigmoid)),
                      reads=[pkg], writes=[sgk])
                P.add("dve", (lambda e, sg=sg, p=psy, j=j, nb=nb: e.tensor_tensor(
                    out=merged[:, j, nb * 512:(nb + 1) * 512], in0=p[:, :], in1=sg, op=ALU.mult)),
                    reads=[pky, sgk], writes=[("merged", j, nb)])
    dbg_out("mergedA", merged, [("merged", j, nb) for j in range(8) for nb in range(NB)])
    A.free("actT")
    if stage <= 2:
        return finish()

    PADQ = 4
    qkw = load_const("qkw", qkw_d, [8, 4])
    qkb = load_const("qkb", qkb_d, [8])
    bif = load_const("bif", bif_d, [8])
    mng = load_const("mng", mng_d, [4])
    qk_raw = A.alloc("qk_raw", [8, PADQ + T], BF16)
    for cc in range(8):
        P.add("pool", (lambda e, cc=cc: e.memset(qk_raw[:, cc, 0:PADQ], 0.0)), writes=[("qk_raw", cc, -1)])
    dg4 = A.alloc("dg4", [8, 4, 128], BF16)
    P.add("pool", lambda e: e.tensor_tensor(
        out=dg4.rearrange("p a b c -> p (a b) c"), in0=ident_b.unsqueeze(1).to_broadcast([128, 32, 128]),
        in1=qkw.rearrange("p a b -> p (a b)").unsqueeze(2).to_broadcast([128, 32, 128]), op=ALU.mult),
        reads=[("ident_b", 0), ("qkw", 0)], writes=[("dg4", 0)])
    for half in range(2):
        wq_, wqk = load_wblock(1024 + half * 512)
        for m in range(4):
            cc = half * 4 + m
            for nb in range(NB):
                ps, pk = proj_fm(wq_, wqk, m, nb)
                P.add("act", (lambda e, ps=ps, cc=cc, nb=nb: e.activation(
                    out=qk_raw[:, cc, PADQ + nb * 512:PADQ + (nb + 1) * 512], in_=ps[:, :], func=AF.Identity)),
                    reads=[pk], writes=[("qk_raw", cc, nb)])
    qkc = A.alloc("qkc", [8, T], BF16)
    for cc in range(8):
        for nb in range(NB):
            ps, pk = next_ps()

            def qmm(e, ps=ps, cc=cc, nb=nb):
                ins = None
                for k in range(4):
                    o = PADQ - 3 + nb * 512 + k
                    ins = e.matmul(ps[:, :], lhsT=dg4[:, cc, k, :], rhs=qk_raw[:, cc, o:o + 512], start=(k == 0), stop=(k == 3))
                return ins
            P.add("pe", qmm, reads=[("dg4", 0), ("qk_raw", cc, nb), ("qk_raw", cc, nb - 1)], writes=[pk])
            P.add("act", (lambda e, ps=ps, cc=cc, nb=nb: e.activation(
                out=qkc[:, cc, nb * 512:(nb + 1) * 512], in_=ps[:, :], func=AF.Silu, bias=qkb[:, cc:cc + 1])),
                reads=[pk, ("qkb", 0)], writes=[("qkc", cc, nb)])
    dbg_out("qkc", qkc, [("qkc", cc, nb) for cc in range(8) for nb in range(NB)])
    A.free("qk_raw"); A.free("dg4")
    if stage <= 2.2:
        return finish()

    wif = A.alloc("wif", [8, 8], BF16)
    wif_f = A.alloc("wif_f", [8, 8], F32)
    with nc.allow_non_contiguous_dma(reason="tiny gate-weight columns"):
        P.add("sp", lambda e: e.dma_start(out=wif_f, in_=win_v[:, :, 3072:3080]), writes=[("wif_f", 0)], dma=True, grp="c_wif")
    P.add("dve", lambda e: e.tensor_copy(out=wif, in_=wif_f), reads=[("wif_f", 0)], writes=[("wif", 0)])
    G = A.alloc("G", [NT, 8], F32)
    nlf = A.alloc("nlf", [NT, 4], F32)
    gtmp = A.alloc("gtmp", [NT, 4], F32)
    A_inv = A.alloc("A_inv", [NT, 4], F32)
    Bv = A.alloc("Bv", [NT, 4], F32)
    dec = A.alloc("dec", [NT, 4], F32)
    psg, pkg = hold_ps()

    def gmm(e):
        ins = None
        for ti in range(NT):
            for k in range(8):
                ins = e.matmul(psg[:, ti * 8:(ti + 1) * 8], lhsT=hT[:, k, ti * 128:(ti + 1) * 128], rhs=wif[:, k, :],
                               start=(k == 0), stop=(k == 7))
        return ins
    P.add("pe", gmm, reads=[("wif", 0)] + [("hT", c, nb) for c in range(8) for nb in range(NB)], writes=[pkg])
    P.add("dve", lambda e: e.tensor_tensor(out=G, in0=psg[:, 0:128].rearrange("p (a b) -> p a b", b=8),
                                           in1=bif.unsqueeze(1).to_broadcast([128, NT, 8]), op=ALU.add),
          reads=[pkg, ("bif", 0)], writes=[("G", 0)])
    release_ps(pkg)
    dbg_out("G", G, [("G", 0)])
    if stage <= 2.31:
        return finish()
    P.add("act", lambda e: e.activation(out=gtmp, in_=G[:, :, 4:8], func=AF.Exp, scale=-1.0),
          reads=[("G", 0)], writes=[("gtmp", 0)])
    P.add("act", lambda e: e.activation(out=nlf, in_=gtmp, func=AF.Ln, bias=1.0),
          reads=[("gtmp", 0)], writes=[("nlf", 0)])
    dbg_out("nlf", nlf, [("nlf", 0)])
    if stage <= 2.32:
        return finish()
    psc, pkc = next_ps()
    nlf2 = nlf.rearrange("p a b -> p (a b)")
    nl_hi = A.alloc("nl_hi", [64], BF16)
    nl_lo = A.alloc("nl_lo", [64], BF16)
    P.add("dve", lambda e: e.tensor_copy(out=nl_hi, in_=nlf2), reads=[("nlf", 0)], writes=[("nl_hi", 0)])
    P.add("dve", lambda e: e.tensor_tensor(out=nl_lo, in0=nlf2, in1=nl_hi, op=ALU.subtract),
          reads=[("nlf", 0), ("nl_hi", 0)], writes=[("nl_lo", 0)])

    def cmm2(e):
        e.matmul(psc[:, 0:64], lhsT=mask_ut, rhs=nl_hi, start=True, stop=False)
        e.matmul(psc[:, 0:64], lhsT=mask_ut, rhs=nl_lo, start=False, stop=True)
        e.matmul(psc[:, 64:128], lhsT=ones_b, rhs=nl_hi, start=True, stop=False)
        return e.matmul(psc[:, 64:128], lhsT=ones_b, rhs=nl_lo, start=False, stop=True)
    P.add("pe", cmm2, reads=[("mask_ut", 0), ("ones_b", 0), ("nl_hi", 0), ("nl_lo", 0)], writes=[pkc])
    if stage <= 2.33:
        P.add("dve", lambda e: e.tensor_copy(out=gtmp.rearrange("p a b -> p (a b)"), in_=psc[:, 0:64]), reads=[pkc], writes=[("gtmp", 0)])
        dbg_out("ncum", gtmp, [("gtmp", 0)])
        return finish()
    LNS = float(np.log(128.0 ** 0.5))
    cval = A.alloc("cval", [2], F32)
    P.add("pool", lambda e: e.memset(cval[:, 0:1], LNS), writes=[("cval", 0)])
    P.add("pool", lambda e: e.memset(cval[:, 1:2], -LNS), writes=[("cval", 1)])
    P.add("act", lambda e: e.activation(out=A_inv.rearrange("p a b -> p (a b)"), in_=psc[:, 0:64], func=AF.Exp, bias=cval[:, 0:1]),
          reads=[pkc, ("cval", 0)], writes=[("A_inv", 0)])
    A_ = A.alloc("A_", [NT, 4], F32)
    P.add("act", lambda e: e.activation(out=A_.rearrange("p a b -> p (a b)"), in_=psc[:, 0:64], func=AF.Exp, scale=-1.0, bias=cval[:, 1:2]),
          reads=[pkc, ("cval", 1)], writes=[("A_", 0)])
    if stage <= 2.34:
        dbg_out("A_", A_, [("A_", 0)])
        dbg_out("A_inv", A_inv, [("A_inv", 0)])
        return finish()
    P.add("dve", lambda e: e.tensor_tensor(out=gtmp, in0=psc[:, 0:64].rearrange("p (a b) -> p a b", b=4), in1=G[:, :, 0:4], op=ALU.add),
          reads=[pkc, ("G", 0), ("gtmp", 0)], writes=[("gtmp", 0)])
    P.add("act", lambda e: e.activation(out=Bv, in_=gtmp, func=AF.Exp), reads=[("gtmp", 0)], writes=[("Bv", 0)])
    if stage <= 2.36:
        dbg_out("Bv", Bv, [("Bv", 0)])
        return finish()
    P.add("act", lambda e: e.activation(out=dec.rearrange("p a b -> p (a b)"), in_=psc[:, 64:128], func=AF.Exp, scale=-1.0),
          reads=[pkc], writes=[("dec", 0)])
    dbg_out("Bv", Bv, [("Bv", 0)])
    dbg_out("A_", A_, [("A_", 0)])
    dbg_out("decay", dec, [("dec", 0)])

    if stage <= 2.4:
        return finish()
    ktok = A.alloc("ktok", [NT, 512], BF16)
    for c in range(NT):
        ps, pk = next_ps()
        pb = ps.bitcast(BF16)

        def ktr(e, pb=pb, c=c):
            ins = None
            for h in range(4):
                ins = e.transpose(out=pb[:, h * 128:(h + 1) * 128], in_=qkc[:, 4 + h, c * 128:(c + 1) * 128], identity=ident_b)
            return ins
        P.add("pe", ktr, reads=[("ident_b", 0)] + [("qkc", 4 + h, c // 4) for h in range(4)], writes=[pk])
        P.add("act", (lambda e, pb=pb, c=c: e.activation(out=ktok[:, c, :], in_=pb[:, 0:512], func=AF.Identity)),
              reads=[pk], writes=[("ktok", c)])

    vB = A.alloc("vB", [NT, 4, 129], BF16)
    wv_, wvk = load_wblock(2048)
    for c in range(NT):
        ps, pk = next_ps()

        def vmm(e, ps=ps, c=c):
            ins = None
            for k in range(8):
                ins = e.matmul(ps[:, :], lhsT=hT[:, k, c * 128:(c + 1) * 128], rhs=wv_[:, k, 0:512], start=(k == 0), stop=(k == 7))
            return ins
        P.add("pe", vmm, reads=[wvk] + hT_keys(c // 4), writes=[pk])
        P.add("dve", (lambda e, ps=ps, c=c: e.tensor_tensor(
            out=vB[:, c, :, 0:128], in0=ps[:, :].rearrange("p (a b) -> p a b", b=128),
            in1=Bv[:, c, :].unsqueeze(2).to_broadcast([128, 4, 128]), op=ALU.mult)),
            reads=[pk, ("Bv", 0)], writes=[("vB", c, 0)])
        P.add("dve", (lambda e, c=c: e.tensor_copy(out=vB[:, c, :, 128], in_=Bv[:, c, :])),
              reads=[("Bv", 0)], writes=[("vB", c, 1)])

    if stage <= 2.6:
        return finish()
    E = A.alloc("E", [4, 129], F32)
    Cb = [A.alloc("Cb%d" % i, [4, 129], BF16) for i in range(2)]
    sm = [A.alloc("sm%d" % i, [4, 128], BF16) for i in range(2)]
    hn = [A.alloc("hn%d" % i, [4, 128], BF16) for i in range(2)]
    st6 = A.alloc("st6", [4, 6], F32)
    mv = A.alloc("mv", [4, 2], F32)
    den = A.alloc("den", [4], F32)
    qq = A.alloc("qq", [4], F32)
    rstd = A.alloc("rstd", [4], F32)
    sgo = [A.alloc("sgo%d" % i, [4, 512], BF16) for i in range(2)]
    hmT = A.alloc("hmT", [4, T], BF16)
    wo_, wok = load_wblock(2560)
    CW = 256

    chs = {}

    def chunk_A(c):
        nb = c // 4
        cs = slice(c * 128, (c + 1) * 128)
        if c % 4 == 0:
            for h in range(4):
                ps, pk = proj_fm(wo_, wok, h, nb)
                P.add("act", (lambda e, ps=ps, h=h, nb=nb: e.activation(out=sgo[nb % 2][:, h, :], in_=ps[:, :], func=AF.Sigmoid)),
                      reads=[pk], writes=[("sgo%d" % (nb % 2), h)])
        pss, pks = next_ps()

        def smm2(e, pss=pss, cs=cs):
            ins = None
            for h in range(4):
                ins = e.matmul(pss[:, h * 128:(h + 1) * 128], lhsT=qkc[:, 4 + h, cs], rhs=qkc[:, h, cs], start=True, stop=True)
            return ins
        P.add("pe", smm2, reads=[("qkc", cc, nb) for cc in range(8)], writes=[pks])
        smc = sm[c % 2]
        smk = ("sm%d" % (c % 2), 0)
        P.add("dve", (lambda e, pss=pss, smc=smc: e.tensor_tensor(
            out=smc, in0=pss[:, :].rearrange("p (a b) -> p a b", b=128),
            in1=mask_ut.unsqueeze(1).to_broadcast([128, 4, 128]), op=ALU.mult)),
            reads=[pks, ("mask_ut", 0)], writes=[smk])
        pu = [hold_ps(), hold_ps()]

        def umm(e, pu=pu, c=c):
            ins = None
            for h in range(4):
                o = pu[h // 2][0][:, (h % 2) * CW:(h % 2) * CW + 129]
                ins = e.matmul(o, lhsT=ktok[:, c, h * 128:(h + 1) * 128], rhs=vB[:, c, h, :], start=True, stop=True)
            return ins
        P.add("pe", umm, reads=[("ktok", c), ("vB", c, 0), ("vB", c, 1)], writes=[pu[0][1], pu[1][1]])
        chs[c] = (smc, smk, pu)

    def chunk_B(c):
        nb = c // 4
        cs = slice(c * 128, (c + 1) * 128)
        smc, smk, pu = chs[c]
        pn = [next_ps(), next_ps()]

        def nmm(e, pn=pn, smc=smc, c=c, cs=cs):
            ins = None
            for h in range(4):
                o = pn[h // 2][0][:, (h % 2) * CW:(h % 2) * CW + 129]
                ins = e.matmul(o, lhsT=smc[:, h, :], rhs=vB[:, c, h, :], start=True, stop=(c == 0))
                if c > 0:
                    ins = e.matmul(o, lhsT=qkc[:, h, cs], rhs=Cb[(c - 1) % 2][:, h, :], start=False, stop=True)
            return ins
        rd = [smk, ("vB", c, 0), ("vB", c, 1)] + [("qkc", h, nb) for h in range(4)]
        if c > 0:
            rd += [("Cb%d" % ((c - 1) % 2), h) for h in range(4)]
        P.add("pe", nmm, reads=rd, writes=[pn[0][1], pn[1][1]])
        for h in range(4):
            src = pu[h // 2][0][:, (h % 2) * CW:(h % 2) * CW + 129]
            if c == 0:
                P.add("dve", (lambda e, src=src, h=h: e.tensor_copy(out=E[:, h, :], in_=src)),
                      reads=[pu[h // 2][1]], writes=[("E", h)])
            else:
                P.add("dve", (lambda e, src=src, h=h, c=c: e.scalar_tensor_tensor(
                    out=E[:, h, :], in0=E[:, h, :], scalar=dec[:, c - 1, h:h + 1], in1=src, op0=ALU.mult, op1=ALU.add)),
                    reads=[pu[h // 2][1], ("E", h), ("dec", 0)], writes=[("E", h)])
            if c < NT - 1:
                P.add("act", (lambda e, h=h, c=c: e.activation(out=Cb[c % 2][:, h, :], in_=E[:, h, :], func=AF.Identity,
                                                               scale=dec[:, c, h:h + 1])),
                      reads=[("E", h), ("dec", 0)], writes=[("Cb%d" % (c % 2), h)])
        release_ps(pu[0][1]); release_ps(pu[1][1])
        chs[c] = pn

    def chunk_C(c):
        nb = c // 4
        cs = slice(c * 128, (c + 1) * 128)
        pn = chs[c]
        for h in range(4):
            src = pn[h // 2][0][:, (h % 2) * CW:(h % 2) * CW + 128]
            P.add("dve", (lambda e, src=src, h=h: e.bn_stats(out=st6[:, h, :], in_=src)),
                  reads=[pn[h // 2][1]], writes=[("st6", h)])
            P.add("dve", (lambda e, h=h: e.bn_aggr(out=mv[:, h, :], in_=st6[:, h, :])),
                  reads=[("st6", h)], writes=[("mv", h)])
        for b2 in range(2):
            dsrc = pn[b2][0][:, 0:512].rearrange("p (a b) -> p a b", b=CW)[:, :, 128]
            P.add("dve", (lambda e, dsrc=dsrc, b2=b2, c=c: e.tensor_tensor(
                out=den[:, 2 * b2:2 * b2 + 2], in0=dsrc, in1=A_[:, c, 2 * b2:2 * b2 + 2], op=ALU.mult)),
                reads=[pn[b2][1], ("A_", 0)], writes=[("den", b2)])
        P.add("dve", lambda e: e.scalar_tensor_tensor(out=den, in0=den, scalar=-1.0, in1=den, op0=ALU.mult, op1=ALU.max),
              reads=[("den", 0), ("den", 1)], writes=[("den", 0), ("den", 1)])
        P.add("dve", lambda e: e.tensor_scalar(out=den, in0=den, scalar1=1.0, scalar2=None, op0=ALU.max),
              reads=[("den", 0), ("den", 1)], writes=[("den", 0), ("den", 1)])
        P.add("dve", (lambda e, c=c: e.tensor_tensor(out=qq, in0=den, in1=A_inv[:, c, :], op=ALU.mult)),
              reads=[("den", 0), ("den", 1), ("A_inv", 0)], writes=[("qq", 0)])
        P.add("dve", lambda e: e.tensor_tensor(out=qq, in0=qq, in1=qq, op=ALU.mult), reads=[("qq", 0)], writes=[("qq", 0)])
        P.add("dve", lambda e: e.scalar_tensor_tensor(out=rstd, in0=qq, scalar=1e-5, in1=mv[:, :, 1], op0=ALU.mult, op1=ALU.add),
              reads=[("qq", 0)] + [("mv", h) for h in range(4)], writes=[("rstd", 0)])
        P.add("act", lambda e: e.activation(out=rstd, in_=rstd, func=AF.Sqrt), reads=[("rstd", 0)], writes=[("rstd", 0)])
        P.add("dve", lambda e: e.reciprocal(out=rstd, in_=rstd), reads=[("rstd", 0)], writes=[("rstd", 0)])
        hnc = hn[c % 2]
        hnk = "hn%d" % (c % 2)
        for h in range(4):
            src = pn[h // 2][0][:, (h % 2) * CW:(h % 2) * CW + 128]
            P.add("dve", (lambda e, src=src, h=h, hnc=hnc: e.tensor_scalar(
                out=hnc[:, h, :], in0=src, scalar1=mv[:, h, 0:1], scalar2=rstd[:, h:h + 1], op0=ALU.subtract, op1=ALU.mult)),
                reads=[pn[h // 2][1], ("mv", h), ("rstd", 0)], writes=[(hnk, h)])
        pt, pkt = next_ps()
        ptb = pt.bitcast(BF16)

        def htr(e, ptb=ptb, hnc=hnc):
            ins = None
            for h in range(4):
                ins = e.transpose(out=ptb[:, h * 128:(h + 1) * 128], in_=hnc[:, h, :], identity=ident_b)
            return ins
        P.add("pe", htr, reads=[("ident_b", 0)] + [(hnk, h) for h in range(4)], writes=[pkt])
        P.add("dve", (lambda e, ptb=ptb, c=c, nb=nb, cs=cs: e.tensor_tensor(
            out=hmT[:, :, cs], in0=ptb[:, 0:512].rearrange("p (a b) -> p a b", b=128),
            in1=sgo[nb % 2][:, :, (c % 4) * 128:(c % 4 + 1) * 128], op=ALU.mult)),
            reads=[pkt] + [("sgo%d" % (nb % 2), h) for h in range(4)], writes=[("hmT", c)])

    chunk_A(0)
    for c in range(NT):
        if c + 1 < NT:
            chunk_A(c + 1)
        chunk_B(c)
        chunk_C(c)
    dbg_out("hmT", hmT, [("hmT", c) for c in range(NT)])
    for nm in ("qkc", "wif", "wif_f", "nl_hi", "nl_lo", "G", "nlf", "gtmp", "A_inv", "Bv", "dec", "A_", "ktok", "vB", "E", "Cb0", "Cb1", "sm0", "sm1",
               "hn0", "hn1", "st6", "mv", "den", "qq", "rstd", "sgo0", "sgo1"):
        A.free(nm)

    wmo = load_cast("wmo", wmo_d.rearrange("(c p) n -> p c n", p=128), [4, D])
    for h in range(4):
        P.add("dve", (lambda e, h=h: e.tensor_scalar(out=wmo[:, h, :], in0=wmo[:, h, :], scalar1=mng[:, h:h + 1], scalar2=None,
                                                     op0=ALU.mult)),
              reads=[("wmo", 0), ("wmo", 1 + h), ("mng", 0)], writes=[("wmo", 1 + h)])
    mtmp = [A.alloc("mtmp%d" % i, [512], BF16) for i in range(2)]
    for jb in range(2):
        wg_, wgk = load_wblock(4104 + jb * 512)
        for jj in range(4):
            j = jb * 4 + jj
            for nb in range(NB):
                psy, pky = next_ps()

                def ymm2(e, psy=psy, j=j, nb=nb):
                    ins = None
                    for h in range(4):
                        ins = e.matmul(psy[:, :], lhsT=wmo[:, h, j * 128:(j + 1) * 128], rhs=hmT[:, h, nb * 512:(nb + 1) * 512],
                                       start=(h == 0), stop=(h == 3))
                    return ins
                P.add("pe", ymm2, reads=[("wmo", 1 + h) for h in range(4)] + [("hmT", c) for c in range(nb * 4, nb * 4 + 4)],
                      writes=[pky])
                psg2, pkg2 = proj_fm(wg_, wgk, jj, nb)
                sg, sgk = next_sgt()
                P.add("act", (lambda e, sg=sg, p=psg2: e.activation(out=sg, in_=p[:, :], func=AF.Sigmoid)),
                      reads=[pkg2], writes=[sgk])
                mt = mtmp[(j * NB + nb) % 2]
                mtk = ("mtmp%d" % ((j * NB + nb) % 2), 0)
                P.add("dve", (lambda e, sg=sg, p=psy, mt=mt: e.tensor_tensor(out=mt, in0=p[:, :], in1=sg, op=ALU.mult)),
                      reads=[pky, sgk], writes=[mtk])
                P.add("dve", (lambda e, mt=mt, j=j, nb=nb: e.tensor_tensor(
                    out=merged[:, j, nb * 512:(nb + 1) * 512], in0=merged[:, j, nb * 512:(nb + 1) * 512], in1=mt, op=ALU.add)),
                    reads=[mtk, ("merged", j, nb)], writes=[("merged", j, nb)])
    dbg_out("merged", merged, [("merged", j, nb) for j in range(8) for nb in range(NB)])
    for nm in ("hmT", "wmo", "mtmp0", "mtmp1", "sgt0", "sgt1", "hT", "wblk0", "wblk1", "wblk2"):
        A.free(nm)
    if stage <= 3:
        return finish()

    dgf = A.alloc("dgf", [128], F32)
    dgh = A.alloc("dgh", [128], BF16)
    dgl = A.alloc("dgl", [128], BF16)

    def row_bcast(name, col0, src=None, srckey=("modT", 1)):
        src = modT if src is None else src
        row = A.alloc(name, [D], F32)
        banks = [next_ps(), next_ps()]
        for j in range(8):
            P.add("dve", (lambda e, j=j: e.tensor_scalar(out=dgf, in0=ident_f, scalar1=src[:, col0 + j:col0 + j + 1], scalar2=None,
                                                         op0=ALU.mult)),
                  reads=[("ident_f", 0), srckey], writes=[("dgf", 0)])
            P.add("dve", lambda e: e.tensor_copy(out=dgh, in_=dgf), reads=[("dgf", 0)], writes=[("dgh", 0)])
            P.add("dve", lambda e: e.tensor_tensor(out=dgl, in0=dgf, in1=dgh, op=ALU.subtract),
                  reads=[("dgf", 0), ("dgh", 0)], writes=[("dgl", 0)])
            bk, bkk = banks[j // 4]

            def bmm(e, bk=bk, j=j):
                o = bk[:, (j % 4) * 128:(j % 4 + 1) * 128]
                e.matmul(o, lhsT=ones_b, rhs=dgh, start=True, stop=False)
                return e.matmul(o, lhsT=ones_b, rhs=dgl, start=False, stop=True)
            P.add("pe", bmm, reads=[("ones_b", 0), ("dgh", 0), ("dgl", 0)], writes=[bkk])
        for b2 in range(2):
            bk, bkk = banks[b2]
            P.add("act", (lambda e, bk=bk, b2=b2: e.activation(out=row[:, b2 * 512:(b2 + 1) * 512], in_=bk[:, :], func=AF.Identity)),
                  reads=[bkk], writes=[(name, b2)])
        return row

    gt1row = row_bcast("gt1row", 16)
    a2row = row_bcast("a2row", 0, src=a2, srckey=("a2", 0))
    sh2row = row_bcast("sh2row", 24)
    gt2row = row_bcast("gt2row", 40)
    wout = load_cast("wout", wout_d.rearrange("(c p) n -> p c n", p=128), [8, D])
    for k in range(8):
        P.add("dve", (lambda e, k=k: e.tensor_tensor(out=wout[:, k, :], in0=wout[:, k, :], in1=gt1row, op=ALU.mult)),
              reads=[("wout", 0), ("wout", 1 + k), ("gt1row", 0), ("gt1row", 1)], writes=[("wout", 1 + k)])
    x1 = A.alloc("x1", [NT, D], F32)
    for ti in range(NT):
        P.add("sp", (lambda e, ti=ti: e.dma_start(out=x1[:, ti, :], in_=x_d[ti * 128:(ti + 1) * 128, :])),
              writes=[("x1", ti)], dma=True, grp="x1ld%d" % ti)
    wr_f = A.alloc("wr_f", [8, 36], F32)
    wr_b = A.alloc("wr_b", [8, 36], BF16)
    with nc.allow_non_contiguous_dma(reason="small router weight rows"):
        P.add("sp", lambda e: e.dma_start(out=wr_f, in_=wr_d.rearrange("(c p) n -> p c n", p=128)), writes=[("wr_f", 0)],
              dma=True, grp="c_wr")
    P.add("dve", lambda e: e.tensor_copy(out=wr_b, in_=wr_f), reads=[("wr_f", 0)], writes=[("wr_b", 0)])
    brt = load_const("brt", br_d, [36])
    h2tok = A.alloc("h2tok", [NT, D], BF16)
    xn2 = [A.alloc("xn2_%d" % i, [D], F32) for i in range(2)]
    h2T = [A.alloc("h2T%d" % i, [8, 128], BF16) for i in range(2)]
    ss2 = A.alloc("ss2", [NT], F32)
    rs2 = A.alloc("rs2", [NT], F32)
    psr = [hold_ps(), hold_ps()]
    def emit_p5(ti):
        s2 = ti % 2
        P.add("act", (lambda e, ti=ti: e.activation(out=junk, in_=x1[:, ti, :], func=AF.Square, accum_out=ss2[:, ti:ti + 1])),
              reads=[("x1", ti)], writes=[("junk", 0), ("ss2", ti)])
        P.add("act", (lambda e, ti=ti: e.activation(out=rs2[:, ti:ti + 1], in_=ss2[:, ti:ti + 1], func=AF.Sqrt, scale=1.0 / D, bias=1e-6)),
              reads=[("ss2", ti)], writes=[("rs2", ti)])
        P.add("dve", (lambda e, ti=ti: e.reciprocal(out=rs2[:, ti:ti + 1], in_=rs2[:, ti:ti + 1])),
              reads=[("rs2", ti)], writes=[("rs2", ti)])
        P.add("act", (lambda e, ti=ti, s2=s2: e.activation(out=xn2[s2], in_=x1[:, ti, :], func=AF.Identity, scale=rs2[:, ti:ti + 1])),
              reads=[("x1", ti), ("rs2", ti)], writes=[("xn2_%d" % s2, 0)])
        P.add("dve", (lambda e, s2=s2: e.tensor_tensor(out=xn2[s2], in0=xn2[s2], in1=a2row, op=ALU.mult)),
              reads=[("xn2_%d" % s2, 0), ("a2row", 0), ("a2row", 1)], writes=[("xn2_%d" % s2, 0)])
        P.add("dve", (lambda e, s2=s2, ti=ti: e.tensor_tensor(out=h2tok[:, ti, :], in0=xn2[s2], in1=sh2row, op=ALU.add)),
              reads=[("xn2_%d" % s2, 0), ("sh2row", 0), ("sh2row", 1)], writes=[("h2tok", ti)])
        pt, pkt = next_ps()
        ptb = pt.bitcast(BF16)

        def h2tr(e, ptb=ptb, ti=ti):
            ins = None
            for c in range(8):
                ins = e.transpose(out=ptb[:, c * 128:(c + 1) * 128], in_=h2tok[:, ti, c * 128:(c + 1) * 128], identity=ident_b)
            return ins
        P.add("pe", h2tr, reads=[("ident_b", 0), ("h2tok", ti)], writes=[pkt])
        P.add("act", (lambda e, ptb=ptb, s2=s2: e.activation(out=h2T[s2].rearrange("p a b -> p (a b)"), in_=ptb[:, 0:1024], func=AF.Identity)),
              reads=[pkt], writes=[("h2T%d" % s2, 0)])
        bk, bkk = psr[ti // 8]

        def rmm(e, bk=bk, ti=ti, s2=s2):
            ins = None
            o = bk[:, (ti % 8) * 36:(ti % 8 + 1) * 36]
            for k in range(8):
                ins = e.matmul(o, lhsT=h2T[s2][:, k, :], rhs=wr_b[:, k, :], start=(k == 0), stop=(k == 7))
            return ins
        P.add("pe", rmm, reads=[("h2T%d" % s2, 0), ("wr_b", 0)], writes=[bkk])

    def emit_p4(ti):
        for half in range(2):
            ps, pk = next_ps()

            def omm(e, ps=ps, ti=ti, half=half):
                ins = None
                for k in range(8):
                    ins = e.matmul(ps[:, :], lhsT=merged[:, k, ti * 128:(ti + 1) * 128], rhs=wout[:, k, half * 512:(half + 1) * 512],
                                   start=(k == 0), stop=(k == 7))
                return ins
            P.add("pe", omm, reads=[("wout", 1 + k) for k in range(8)] + [("merged", k, ti // 4) for k in range(8)], writes=[pk])
            P.add("dve", (lambda e, ps=ps, ti=ti, half=half: e.tensor_tensor(
                out=x1[:, ti, half * 512:(half + 1) * 512], in0=x1[:, ti, half * 512:(half + 1) * 512], in1=ps[:, :], op=ALU.add)),
                reads=[pk, ("x1", ti)], writes=[("x1", ti)])

    emit_p4(0)
    for ti in range(NT):
        if ti + 1 < NT:
            emit_p4(ti + 1)
        emit_p5(ti)
    dbg_out("x1", x1, [("x1", ti) for ti in range(NT)])
    A.free("merged"); A.free("wout"); A.free("gt1row")

    Lg = A.alloc("Lg", [NT, 36], F32)
    for b2 in range(2):
        bk, bkk = psr[b2]
        P.add("dve", (lambda e, bk=bk, b2=b2: e.tensor_tensor(
            out=Lg[:, b2 * 8:(b2 + 1) * 8, :], in0=bk[:, 0:288].rearrange("p (a b) -> p a b", b=36),
            in1=brt.unsqueeze(1).to_broadcast([128, 8, 36]), op=ALU.add)),
            reads=[bkk, ("brt", 0)], writes=[("Lg", b2)])
    release_ps(psr[0][1]); release_ps(psr[1][1])
    dbg_out("Lg", Lg, [("Lg", 0), ("Lg", 1)])
    dbg_out("h2tok", h2tok, [("h2tok", ti) for ti in range(NT)])
    for nm in ("xn2_0", "xn2_1", "h2T0", "h2T1", "a2row", "sh2row", "wr_f", "wr_b"):
        A.free(nm)
    if stage <= 5:
        return finish()

    NCHK0 = NG - 15
    def T_(name, shape, dt=F32):
        return A.alloc(name, shape, dt)
    LK = [("Lg", 0), ("Lg", 1)]
    lg = Lg[:, :, 0:4]
    le = Lg[:, :, 4:36]
    gmax = T_("gmax", [NT])
    G1h = T_("G1h", [NT, 4])
    egs = T_("egs", [NT, 4])
    p_g = T_("p_g", [NT])
    P.add("dve", lambda e: e.tensor_reduce(out=gmax, in_=lg, axis=AX.X, op=ALU.max), reads=LK, writes=[("gmax", 0)])
    gmb = gmax.unsqueeze(2).to_broadcast([128, NT, 4])
    P.add("dve", lambda e: e.tensor_tensor(out=G1h, in0=lg, in1=gmb, op=ALU.is_equal), reads=LK + [("gmax", 0)], writes=[("G1h", 0)])
    P.add("dve", lambda e: e.tensor_tensor(out=egs, in0=lg, in1=gmb, op=ALU.subtract), reads=LK + [("gmax", 0)], writes=[("egs", 0)])
    P.add("act", lambda e: e.activation(out=egs, in_=egs, func=AF.Exp), reads=[("egs", 0)], writes=[("egs", 0)])
    P.add("dve", lambda e: e.tensor_reduce(out=p_g, in_=egs, axis=AX.X, op=ALU.add), reads=[("egs", 0)], writes=[("p_g", 0)])
    P.add("dve", lambda e: e.reciprocal(out=p_g, in_=p_g), reads=[("p_g", 0)], writes=[("p_g", 0)])
    tmp32 = T_("tmp32", [NT, 32])
    lsel = T_("lsel", [NT, 8])
    P.add("dve", lambda e: e.tensor_tensor(
        out=tmp32.rearrange("p t (g j) -> p t g j", j=8), in0=le.rearrange("p t (g j) -> p t g j", j=8),
        in1=G1h.unsqueeze(3).to_broadcast([128, NT, 4, 8]), op=ALU.mult),
        reads=LK + [("G1h", 0)], writes=[("tmp32", 0)])
    P.add("dve", lambda e: e.tensor_reduce(out=lsel, in_=tmp32.rearrange("p t (g j) -> p t j g", j=8), axis=AX.X, op=ALU.add),
          reads=[("tmp32", 0)], writes=[("lsel", 0)])
    m1 = T_("m1", [NT])
    m2 = T_("m2", [NT])
    E1 = T_("E1", [NT, 8])
    E2 = T_("E2", [NT, 8])
    ls2 = T_("ls2", [NT, 8])
    P.add("dve", lambda e: e.tensor_reduce(out=m1, in_=lsel, axis=AX.X, op=ALU.max), reads=[("lsel", 0)], writes=[("m1", 0)])
    P.add("dve", lambda e: e.tensor_tensor(out=E1, in0=lsel, in1=m1.unsqueeze(2).to_broadcast([128, NT, 8]), op=ALU.is_equal),
          reads=[("lsel", 0), ("m1", 0)], writes=[("E1", 0)])
    P.add("dve", lambda e: e.scalar_tensor_tensor(out=ls2.rearrange("p a b -> p (a b)"), in0=E1.rearrange("p a b -> p (a b)"),
                                                  scalar=-1e30, in1=lsel.rearrange("p a b -> p (a b)"), op0=ALU.mult, op1=ALU.add),
          reads=[("E1", 0), ("lsel", 0)], writes=[("ls2", 0)])
    P.add("dve", lambda e: e.tensor_reduce(out=m2, in_=ls2, axis=AX.X, op=ALU.max), reads=[("ls2", 0)], writes=[("m2", 0)])
    P.add("dve", lambda e: e.tensor_tensor(out=E2, in0=ls2, in1=m2.unsqueeze(2).to_broadcast([128, NT, 8]), op=ALU.is_equal),
          reads=[("ls2", 0), ("m2", 0)], writes=[("E2", 0)])
    w1 = T_("w1", [NT])
    w2 = T_("w2", [NT])
    P.add("dve", lambda e: e.tensor_tensor(out=w2, in0=m1, in1=m2, op=ALU.subtract), reads=[("m1", 0), ("m2", 0)], writes=[("w2", 0)])
    P.add("act", lambda e: e.activation(out=w1, in_=w2, func=AF.Sigmoid), reads=[("w2", 0)], writes=[("w1", 0)])
    P.add("dve", lambda e: e.tensor_tensor(out=w1, in0=w1, in1=p_g, op=ALU.mult), reads=[("w1", 0), ("p_g", 0)], writes=[("w1", 0)])
    P.add("dve", lambda e: e.tensor_tensor(out=w2, in0=p_g, in1=w1, op=ALU.subtract), reads=[("w1", 0), ("p_g", 0), ("w2", 0)], writes=[("w2", 0)])
    A1 = T_("A1", [NT, 32])
    A2 = T_("A2", [NT, 32])
    A12b = T_("A12b", [NT, 32], BF16)
    for g in range(4):
        gb = G1h[:, :, g].unsqueeze(2).to_broadcast([128, NT, 8])
        P.add("dve", (lambda e, g=g, gb=gb: e.tensor_tensor(out=A1[:, :, g * 8:(g + 1) * 8], in0=E1, in1=gb, op=ALU.mult)),
              reads=[("E1", 0), ("G1h", 0)], writes=[("A1", g)])
        P.add("dve", (lambda e, g=g, gb=gb: e.tensor_tensor(out=A2[:, :, g * 8:(g + 1) * 8], in0=E2, in1=gb, op=ALU.mult)),
              reads=[("E2", 0), ("G1h", 0)], writes=[("A2", g)])
    AK = [("A1", g) for g in range(4)] + [("A2", g) for g in range(4)]
    P.add("dve", lambda e: e.tensor_tensor(out=A12b, in0=A1, in1=A2, op=ALU.add), reads=AK, writes=[("A12b", 0)])
    lstrict = T_("lstrict", [128], BF16)
    lsf = T_("lsf", [128], F32)
    P.add("pool", lambda e: e.memset(lsf, 1.0), writes=[("lsf", 0)])
    P.add("pool", lambda e: e.affine_select(out=lsf, in_=lsf, pattern=[[1, 128]], compare_op=ALU.is_ge, fill=0.0, base=-1,
                                             channel_multiplier=-1), reads=[("lsf", 0)], writes=[("lsf", 0)])
    P.add("pool", lambda e: e.tensor_copy(out=lstrict, in_=lsf), reads=[("lsf", 0)], writes=[("lstrict", 0)])
    psw, pkw = next_ps()
    pst_, pkt_ = next_ps()
    A12f = A12b.rearrange("p a b -> p (a b)")
    P.add("pe", lambda e: e.matmul(psw[:, :], lhsT=lstrict, rhs=A12f, start=True, stop=True),
          reads=[("lstrict", 0), ("A12b", 0)], writes=[pkw])
    P.add("pe", lambda e: e.matmul(pst_[:, :], lhsT=ones_b, rhs=A12f, start=True, stop=True),
          reads=[("ones_b", 0), ("A12b", 0)], writes=[pkt_])
    carry = T_("carry", [NT + 1, 32])
    P.add("dve", lambda e: e.memset(carry[:, 0, :], 0.0), writes=[("carry", 0)])
    for ti in range(NT):
        P.add("dve", (lambda e, ti=ti: e.tensor_tensor(out=carry[:, ti + 1, :], in0=carry[:, ti, :], in1=pst_[:, ti * 32:(ti + 1) * 32],
                                                      op=ALU.add)),
              reads=[("carry", ti), pkt_], writes=[("carry", ti + 1)])
    counts = carry[:, NT, :]
    CK = [("carry", ti) for ti in range(NT + 1)]
    thr = T_("thr", [64])
    thr_i = T_("thr_i", [64], I32)
    P.add("pool", lambda e: e.iota(thr_i, pattern=[[1, 64]], base=0, channel_multiplier=0), writes=[("thr_i", 0)])
    P.add("pool", lambda e: e.tensor_copy(out=thr, in_=thr_i), reads=[("thr_i", 0)], writes=[("thr", 0)])
    thr128 = T_("thr128", [8])
    P.add("pool", lambda e: e.tensor_scalar(out=thr128, in0=thr[:, 0:8], scalar1=float(GROWS), scalar2=None, op0=ALU.mult),
          reads=[("thr", 0)], writes=[("thr128", 0)])
    cmp1 = T_("cmp1", [32, 8])
    ngrp = T_("ngrp", [32])
    P.add("dve", lambda e: e.tensor_tensor(out=cmp1, in0=counts.unsqueeze(2).to_broadcast([128, 32, 8]),
                                           in1=thr128.unsqueeze(1).to_broadcast([128, 32, 8]), op=ALU.is_gt),
          reads=CK + [("thr128", 0)], writes=[("cmp1", 0)])
    P.add("dve", lambda e: e.tensor_reduce(out=ngrp, in_=cmp1, axis=AX.X, op=ALU.add), reads=[("cmp1", 0)], writes=[("ngrp", 0)])
    cs = [T_("cs0", [32]), T_("cs1", [32])]
    src, srck = ngrp, ("ngrp", 0)
    for si, sh in enumerate((1, 2, 4, 8, 16)):
        dst = cs[si % 2]
        dk = ("cs%d" % (si % 2),)
        P.add("dve", (lambda e, dst=dst, src=src, sh=sh: e.tensor_copy(out=dst[:, 0:sh], in_=src[:, 0:sh])),
              reads=[srck], writes=[dk + (0,)])
        P.add("dve", (lambda e, dst=dst, src=src, sh=sh: e.tensor_tensor(out=dst[:, sh:32], in0=src[:, sh:32], in1=src[:, 0:32 - sh],
                                                                        op=ALU.add)),
              reads=[srck], writes=[dk + (1,)])
        src, srck = dst, dk + (1,)
        if si > 0:
            pass
    pend = src
    PK = [("cs0", 0), ("cs0", 1), ("cs1", 0), ("cs1", 1)]
    pstart = T_("pstart", [32])
    P.add("dve", lambda e: e.tensor_tensor(out=pstart, in0=pend, in1=ngrp, op=ALU.subtract), reads=PK + [("ngrp", 0)],
          writes=[("pstart", 0)])
    P.add("dve", lambda e: e.tensor_scalar(out=pstart, in0=pstart, scalar1=float(GROWS), scalar2=None, op0=ALU.mult),
          reads=[("pstart", 0)], writes=[("pstart", 0)])
    cmp2 = T_("cmp2", [NG, 32])
    grpf = T_("grpf", [NG])
    grpi = T_("grpi", [NG], I32)
    P.add("dve", lambda e: e.tensor_tensor(out=cmp2, in0=pend.unsqueeze(1).to_broadcast([128, NG, 32]),
                                           in1=thr[:, 0:NG].unsqueeze(2).to_broadcast([128, NG, 32]), op=ALU.is_le),
          reads=PK + [("thr", 0)], writes=[("cmp2", 0)])
    P.add("dve", lambda e: e.tensor_reduce(out=grpf, in_=cmp2, axis=AX.X, op=ALU.add), reads=[("cmp2", 0)], writes=[("grpf", 0)])
    P.add("dve", lambda e: e.tensor_scalar(out=grpf, in0=grpf, scalar1=31.0, scalar2=None, op0=ALU.min),
          reads=[("grpf", 0)], writes=[("grpf", 0)])
    P.add("dve", lambda e: e.tensor_copy(out=grpi, in_=grpf), reads=[("grpf", 0)], writes=[("grpi", 0)])
    pidx_i = T_("pidx_i", [1], I32)
    pidx = T_("pidx", [1])
    idxf = T_("idxf", [NG])
    inval = T_("inval", [NG])
    idxw = T_("idxw", [NG], I32)
    idxs = T_("idxs", [NG], I32)
    P.add("pool", lambda e: e.iota(pidx_i, pattern=[[0, 1]], base=0, channel_multiplier=1), writes=[("pidx_i", 0)])
    P.add("pool", lambda e: e.tensor_copy(out=pidx, in_=pidx_i), reads=[("pidx_i", 0)], writes=[("pidx", 0)])
    P.add("dve", lambda e: e.tensor_scalar(out=idxf, in0=grpf, scalar1=128.0, scalar2=pidx[:, 0:1], op0=ALU.mult, op1=ALU.add),
          reads=[("grpf", 0), ("pidx", 0)], writes=[("idxf", 0)])
    P.add("dve", lambda e: e.tensor_scalar(out=inval, in0=thr[:, 0:NG], scalar1=pend[:, 31:32], scalar2=None, op0=ALU.is_ge),
          reads=PK + [("thr", 0)], writes=[("inval", 0)])
    P.add("dve", lambda e: e.tensor_copy(out=idxw, in_=idxf), reads=[("idxf", 0)], writes=[("idxw", 0)])
    P.add("dve", lambda e: e.scalar_tensor_tensor(out=idxf, in0=inval, scalar=1.0e6, in1=idxf, op0=ALU.mult, op1=ALU.add),
          reads=[("inval", 0), ("idxf", 0), ("idxw", 0)], writes=[("idxf", 0)])
    P.add("dve", lambda e: e.tensor_copy(out=idxs, in_=idxf), reads=[("idxf", 0)], writes=[("idxs", 0)])
    stg = {wn: A.alloc("stg_" + wn, [4096], F32) for wn in ("wg", "wu", "wd")}
    for (wsrc_, wn_) in ((weg_d, "wg"), (weu_d, "wu"), (wed_d, "wd")):
        P.add("pool", (lambda e, wsrc_=wsrc_, wn_=wn_: e.indirect_dma_start(
            out=stg[wn_], out_offset=None, in_=wsrc_, in_offset=bass.IndirectOffsetOnAxis(ap=idxw[:, 0:1], axis=0))),
            reads=[("idxw", 0)], writes=[("stg_" + wn_, 0)], dma=True, grp="stg_" + wn_)
    slot = T_("slot", [NT, 32])
    P.add("dve", lambda e: e.tensor_tensor(out=slot, in0=psw[:, :].rearrange("p (a b) -> p a b", b=32), in1=carry[:, 0:NT, :], op=ALU.add),
          reads=[pkw] + CK, writes=[("slot", 0)])
    P.add("dve", lambda e: e.tensor_tensor(out=slot, in0=slot, in1=pstart.unsqueeze(1).to_broadcast([128, NT, 32]), op=ALU.add),
          reads=[("slot", 0), ("pstart", 0)], writes=[("slot", 0)])
    dstf = T_("dstf", [2, NT])
    dsti = T_("dsti", [2, NT], I32)
    for q, (Aq, qk) in enumerate(((A1, "A1"), (A2, "A2"))):
        P.add("dve", (lambda e, Aq=Aq: e.tensor_tensor(out=tmp32, in0=Aq, in1=slot, op=ALU.mult)),
              reads=[(qk, g) for g in range(4)] + [("slot", 0), ("tmp32", 0)], writes=[("tmp32", 0)])
        P.add("dve", (lambda e, q=q: e.tensor_reduce(out=dstf[:, q, :], in_=tmp32, axis=AX.X, op=ALU.add)),
              reads=[("tmp32", 0)], writes=[("dstf", q)])
    P.add("dve", lambda e: e.tensor_copy(out=dsti, in_=dstf), reads=[("dstf", 0), ("dstf", 1)], writes=[("dsti", 0)])
    dbg_out("dstf", dstf, [("dstf", 0), ("dstf", 1)])
    dbg_out("grpf", grpf, [("grpf", 0)])
    dbg_out("w1", w1, [("w1", 0)])
    dbg_out("w2", w2, [("w2", 0)])
    for nm in ("gmax", "G1h", "egs", "p_g", "tmp32", "lsel", "m1", "m2", "E1", "E2", "ls2", "A1", "A2", "A12b", "lstrict", "lsf",
               "carry", "thr", "thr_i", "thr128", "cmp1", "ngrp", "cs0", "cs1", "pstart", "slot", "cmp2", "Lg"):
        A.free(nm)
    if stage <= 5.5:
        return finish()

    for ti in range(NT):
        for q in range(2):
            P.add("pool", (lambda e, ti=ti, q=q: e.indirect_dma_start(
                out=XS, out_offset=bass.IndirectOffsetOnAxis(ap=dsti[:, q, ti:ti + 1], axis=0),
                in_=h2tok[:, ti, :], in_offset=None)),
                reads=[("h2tok", ti), ("dsti", 0)] + XSZ_KEYS, writes=[("XS", ti, q)], dma=True, grp="xs_sc")
    XS_KEYS = [("XS", ti, q) for ti in range(NT) for q in range(2)]
    A.free("h2tok")
    NS = 2
    wgs = [A.alloc("wg%d" % i, [8, 512], BF16) for i in range(NS)]
    wus = [A.alloc("wu%d" % i, [8, 512], BF16) for i in range(NS)]
    wds = [A.alloc("wd%d" % i, [4, D], BF16) for i in range(NS)]
    xgt = [A.alloc("xgt%d" % i, [D], BF16) for i in range(2)]
    xgT = [A.alloc("xgT%d" % i, [8, 256], BF16) for i in range(2)]
    sgl = [A.alloc("sgl%d" % i, [4, 256], BF16) for i in range(1)]
    aT = [A.alloc("aT%d" % i, [4, 256], BF16) for i in range(2)]
    ysb = [A.alloc("ysb%d" % i, [D], BF16) for i in range(2)]
    def emit_load(g, part="both"):
        sl = g % NS
        s2 = g % 2
        for (wt, wsrc, wn, ceng) in ((wgs, weg_d, "wg", "act"), (wus, weu_d, "wu", "dve"), (wds, wed_d, "wd", "pool")):
            st_ = stg[wn]
            if part in ("both", "dma") and g >= NCHK0:
                P.add("pool", (lambda e, g=g, st_=st_, wsrc=wsrc: e.indirect_dma_start(
                    out=st_, out_offset=None, in_=wsrc,
                    in_offset=bass.IndirectOffsetOnAxis(ap=idxs[:, g:g + 1], axis=0), bounds_check=32 * 128 - 1, oob_is_err=False)),
                    reads=[("idxs", 0)], writes=[("stg_" + wn, 0)], dma=True, grp="stg_" + wn)
            elif part in ("both", "dma"):
                P.add("pool", (lambda e, g=g, st_=st_, wsrc=wsrc: e.indirect_dma_start(
                    out=st_, out_offset=None, in_=wsrc,
                    in_offset=bass.IndirectOffsetOnAxis(ap=idxw[:, g:g + 1], axis=0))),
                    reads=[("idxw", 0)], writes=[("stg_" + wn, 0)], dma=True, grp="stg_" + wn)
            if part == "dma":
                continue
            dstv = wt[sl].rearrange("p a b -> p (a b)")
            if ceng == "act":
                P.add("act", (lambda e, dstv=dstv, st_=st_: e.activation(out=dstv, in_=st_, func=AF.Identity)),
                      reads=[("stg_" + wn, 0)], writes=[("%s%d" % (wn, sl), 0)])
            elif ceng == "dve":
                P.add("dve", (lambda e, dstv=dstv, st_=st_: e.tensor_copy(out=dstv, in_=st_)),
                      reads=[("stg_" + wn, 0)], writes=[("%s%d" % (wn, sl), 0)])
            else:
                P.add("act", (lambda e, dstv=dstv, st_=st_: e.activation(out=dstv[:, 0:2048], in_=st_[:, 0:2048], func=AF.Identity)),
                      reads=[("stg_" + wn, 0)], writes=[("%s%d" % (wn, sl), 0)])
                P.add("dve", (lambda e, dstv=dstv, st_=st_: e.tensor_copy(out=dstv[:, 2048:4096], in_=st_[:, 2048:4096])),
                      reads=[("stg_" + wn, 0)], writes=[("%s%d" % (wn, sl), 1)])

    def emit_compute_a(g):
        s2 = g % 2
        for hf in range(2):
            xi = (2 * g + hf) % 2
            r0 = g * GROWS + hf * 128
            P.add("sp", (lambda e, r0=r0, xi=xi: e.dma_start(out=xgt[xi], in_=XS[r0:r0 + 128, :])),
                  reads=XS_KEYS, writes=[("xgt%d" % xi, 0)], dma=True, grp="xgt%d" % xi)
            pt, pkt = next_ps()
            ptb = pt.bitcast(BF16)

            def xtr(e, ptb=ptb, xi=xi):
                ins = None
                for c in range(8):
                    ins = e.transpose(out=ptb[:, c * 128:(c + 1) * 128], in_=xgt[xi][:, c * 128:(c + 1) * 128], identity=ident_b)
                return ins
            P.add("pe", xtr, reads=[("ident_b", 0), ("xgt%d" % xi, 0)], writes=[pkt])
            P.add("act", (lambda e, ptb=ptb, s2=s2, hf=hf: e.activation(
                out=xgT[s2][:, :, hf * 128:(hf + 1) * 128], in_=ptb[:, 0:1024].rearrange("p (a b) -> p a b", b=128), func=AF.Identity)),
                reads=[pkt], writes=[("xgT%d" % s2, hf)])

    def emit_compute_b1(g):
        sl = g % NS
        s2 = g % 2
        pg_ = [next_ps(), next_ps()]
        pu_ = [next_ps(), next_ps()]

        def gumm(e, pg_=pg_, pu_=pu_, sl=sl, s2=s2):
            ins = None
            for (pp, ww) in ((pg_, wgs), (pu_, wus)):
                for fc in range(4):
                    o = pp[fc // 2][0][:, (fc % 2) * 256:(fc % 2 + 1) * 256]
                    for k in range(8):
                        ins = e.matmul(o, lhsT=ww[sl][:, k, fc * 128:(fc + 1) * 128], rhs=xgT[s2][:, k, :], start=(k == 0), stop=(k == 7))
            return ins
        P.add("pe", gumm, reads=[("wg%d" % sl, 0), ("wu%d" % sl, 0), ("xgT%d" % s2, 0), ("xgT%d" % s2, 1)],
              writes=[pg_[0][1], pg_[1][1], pu_[0][1], pu_[1][1]])
        for b2 in range(2):
            P.add("act", (lambda e, pg_=pg_, s2=s2, b2=b2: e.activation(
                out=sgl[0][:, 2 * b2:2 * b2 + 2, :].rearrange("p a b -> p (a b)"), in_=pg_[b2][0][:, :], func=AF.Silu)),
                reads=[pg_[b2][1]], writes=[("sgl0", b2)])
            P.add("dve", (lambda e, pu_=pu_, s2=s2, b2=b2: e.tensor_tensor(
                out=aT[s2][:, 2 * b2:2 * b2 + 2, :].rearrange("p a b -> p (a b)"), in0=pu_[b2][0][:, :],
                in1=sgl[0][:, 2 * b2:2 * b2 + 2, :].rearrange("p a b -> p (a b)"), op=ALU.mult)),
                reads=[pu_[b2][1], ("sgl0", b2)], writes=[("aT%d" % s2, b2)])

    def emit_compute_b2(g):
        sl = g % NS
        s2 = g % 2
        for hf in range(2):
            yi = (2 * g + hf) % 2
            py = [next_ps(), next_ps()]

            def dmm(e, py=py, sl=sl, s2=s2, hf=hf):
                ins = None
                for half in range(2):
                    for fc in range(4):
                        ins = e.matmul(py[half][0][:, :], lhsT=aT[s2][:, fc, hf * 128:(hf + 1) * 128],
                                       rhs=wds[sl][:, fc, half * 512:(half + 1) * 512], start=(fc == 0), stop=(fc == 3))
                return ins
            P.add("pe", dmm, reads=[("wd%d" % sl, 0), ("wd%d" % sl, 1), ("aT%d" % s2, 0), ("aT%d" % s2, 1)], writes=[py[0][1], py[1][1]])
            for half in range(2):
                P.add("dve", (lambda e, py=py, yi=yi, half=half: e.tensor_tensor(
                    out=ysb[yi][:, half * 512:(half + 1) * 512], in0=py[half][0][:, :], in1=gt2row[:, half * 512:(half + 1) * 512],
                    op=ALU.mult)),
                    reads=[py[half][1], ("gt2row", half)], writes=[("ysb%d" % yi, half)])
            r0 = g * GROWS + hf * 128
            P.add("sp", (lambda e, r0=r0, yi=yi: e.dma_start(out=YS[r0:r0 + 128, :], in_=ysb[yi])),
                  reads=[("ysb%d" % yi, 0), ("ysb%d" % yi, 1)], writes=[("YS", g, hf)], dma=True, grp="ys_st")

    emit_load(0, "cast")
    emit_compute_a(0)
    for g in range(NG):
        emit_compute_b1(g)
        if g + 1 < NG:
            emit_load(g + 1)
            emit_compute_a(g + 1)
        emit_compute_b2(g)
    YS_KEYS = [("YS", g, hf) for g in range(NG) for hf in range(2)]
    for i in range(NS):
        A.free("wg%d" % i); A.free("wu%d" % i); A.free("wd%d" % i)
    for nm in ("stg_wg", "stg_wu", "stg_wd", "xgt0", "xgt1", "xgT0", "xgT1", "sgl0", "aT0", "aT1", "ysb0", "ysb1"):
        A.free(nm)

    gfin = load_const("gfin", gfin_d, [D])
    NYG = 4
    yg = [[A.alloc("yg%d_%d" % (q, i), [D], BF16) for i in range(NYG)] for q in range(2)]
    acc = [A.alloc("acc%d" % i, [D], F32) for i in range(2)]
    outt = [A.alloc("outt%d" % i, [D], F32) for i in range(2)]
    ssf = A.alloc("ssf", [NT], F32)
    rsf = A.alloc("rsf", [NT], F32)

    def emit_g(ti):
        s4 = ti % NYG
        for q in range(2):
            P.add("pool", (lambda e, ti=ti, q=q, s4=s4: e.indirect_dma_start(
                out=yg[q][s4], out_offset=None, in_=YS,
                in_offset=bass.IndirectOffsetOnAxis(ap=dsti[:, q, ti:ti + 1], axis=0))),
                reads=YS_KEYS + [("dsti", 0)], writes=[("yg%d_%d" % (q, s4), 0)], dma=True, grp="yg%d_%d" % (q, s4))

    def emit_c1(ti):
        s4 = ti % NYG
        s2 = ti % 2
        y1, y2 = yg[0][s4], yg[1][s4]
        k1, k2 = ("yg0_%d" % s4, 0), ("yg1_%d" % s4, 0)
        ac = acc[s2]
        ak = ("acc%d" % s2, 0)
        P.add("act", (lambda e, y1=y1, ac=ac, ti=ti: e.activation(out=ac, in_=y1, func=AF.Identity, scale=w1[:, ti:ti + 1])),
              reads=[k1, ("w1", 0)], writes=[ak])
        P.add("dve", (lambda e, ac=ac, y2=y2, ti=ti: e.scalar_tensor_tensor(out=ac, in0=y2, scalar=w2[:, ti:ti + 1], in1=ac,
                                                                          op0=ALU.mult, op1=ALU.add)),
              reads=[ak, k2, ("w2", 0)], writes=[ak])
        P.add("dve", (lambda e, ac=ac, ti=ti: e.tensor_tensor(out=x1[:, ti, :], in0=x1[:, ti, :], in1=ac, op=ALU.add)),
              reads=[ak, ("x1", ti)], writes=[("x1", ti)])

    def emit_c2(ti):
        s2 = ti % 2
        P.add("act", (lambda e, ti=ti: e.activation(out=junk, in_=x1[:, ti, :], func=AF.Square, accum_out=ssf[:, ti:ti + 1])),
              reads=[("x1", ti)], writes=[("junk", 0), ("ssf", ti)])
        P.add("act", (lambda e, ti=ti: e.activation(out=rsf[:, ti:ti + 1], in_=ssf[:, ti:ti + 1], func=AF.Sqrt, scale=1.0 / D, bias=1e-6)),
              reads=[("ssf", ti)], writes=[("rsf", ti)])
        P.add("dve", (lambda e, ti=ti: e.reciprocal(out=rsf[:, ti:ti + 1], in_=rsf[:, ti:ti + 1])),
              reads=[("rsf", ti)], writes=[("rsf", ti)])
        ot = outt[s2]
        ok = ("outt%d" % s2, 0)
        P.add("act", (lambda e, ot=ot, ti=ti: e.activation(out=ot, in_=x1[:, ti, :], func=AF.Identity, scale=rsf[:, ti:ti + 1])),
              reads=[("x1", ti), ("rsf", ti)], writes=[ok])
        P.add("dve", (lambda e, ot=ot: e.tensor_tensor(out=ot, in0=ot, in1=gfin, op=ALU.mult)),
              reads=[ok, ("gfin", 0)], writes=[ok])
        P.add("sp", (lambda e, ot=ot, ti=ti: e.dma_start(out=out_d[ti * 128:(ti + 1) * 128, :], in_=ot)),
              reads=[ok], dma=True, grp="out")

    for ti in range(min(3, NT)):
        emit_g(ti)
    for ti in range(NT):
        if ti + 3 < NT:
            emit_g(ti + 3)
        emit_c1(ti)
        if ti > 0:
            emit_c2(ti - 1)
    emit_c2(NT - 1)
    P.emit(final_wait_groups=["out"] + (["dbgout"] if "dbgout" in P.dma_groups else []))
    build.stats = dict(peak_kb=A.peak * 4 / 1024.0, n_ops=len(P.all_ops), n_groups=len(P.dma_groups))
    return nc, dbg_outs


def host_layout(inp, b):
    f = lambda a: np.ascontiguousarray(a, dtype=np.float32)
    col = lambda v, n: f(np.asarray(v).reshape(n, 128).T)
    m = {}
    m["x"] = f(inp["x"][b])
    m["c_col"] = col(inp["c"][b], 8)
    m["w_ada"] = f(inp["w_ada"][0])
    m["b_ada_col"] = col(inp["b_ada"][0], 48)
    m["g1_col"] = col(inp["g_norm1"][0], 8)
    m["g2_col"] = col(inp["g_norm2"][0], 8)
    m["w_in"] = f(inp["w_in"][0])
    m["b_if_bc"] = f(np.broadcast_to(inp["b_if"][0][None, :], (128, 8)))
    m["conv_w_col"] = f(inp["conv_dw_w"][0].reshape(31, 4, 128).transpose(2, 1, 0))
    m["conv_b_col"] = col(inp["conv_dw_b"][0], 4)
    m["conv_lng_col"] = col(inp["conv_ln_g"][0], 4)
    m["conv_lnb_col"] = col(inp["conv_ln_b"][0], 4)
    m["w_conv_out"] = f(inp["w_conv_out"][0])
    m["qk_w_col"] = f(inp["qk_conv_w"][0].reshape(4, 8, 128).transpose(2, 1, 0))
    m["qk_b_col"] = col(inp["qk_conv_b"][0], 8)
    m["mng_col"] = col(inp["m_norm_g"][0], 4)
    m["w_m_out"] = f(inp["w_m_out"][0])
    m["w_out"] = f(inp["w_out"][0])
    m["w_router"] = f(np.concatenate([inp["w_rg"][0], inp["w_re"][0]], axis=1))
    m["b_router_bc"] = f(np.broadcast_to(np.concatenate([inp["b_rg"][0], inp["b_re"][0]])[None, :], (128, 36)))
    m["w_e_gate_l"] = f(inp["w_e_gate"][0].reshape(32, 8, 128, 512).transpose(0, 2, 1, 3).reshape(32 * 128, 8 * 512))
    m["w_e_up_l"] = f(inp["w_e_up"][0].reshape(32, 8, 128, 512).transpose(0, 2, 1, 3).reshape(32 * 128, 8 * 512))
    m["w_e_down_l"] = f(inp["w_e_down"][0].reshape(32, 4, 128, D).transpose(0, 2, 1, 3).reshape(32 * 128, 4 * D))
    m["g_final_bc"] = f(np.broadcast_to(np.asarray(inp["g_final"])[None, :], (128, D)))
    return m


def kernel(**inputs):
    nc, _ = build()
    shared = host_layout(inputs, 0)
    in_maps = []
    for b in range(8):
        m = dict(shared)
        m["x"] = np.ascontiguousarray(inputs["x"][b], dtype=np.float32)
        m["c_col"] = np.ascontiguousarray(np.asarray(inputs["c"][b]).reshape(8, 128).T, dtype=np.float32)
        in_maps.append(m)
    res = run_bass_kernel_spmd(nc, in_maps, core_ids=list(range(8)))
    return np.stack([np.asarray(r["out"]) for r in res.results], axis=0).astype(np.float32)
```

```python
import contextlib
import numpy as np
import concourse.bass as bass
import concourse.mybir as mybir
from concourse.bass_utils import run_bass_kernel_spmd

F32 = mybir.dt.float32
BF16 = mybir.dt.bfloat16
I32 = mybir.dt.int32
AF = mybir.ActivationFunctionType
ALU = mybir.AluOpType
AX = mybir.AxisListType

T = 2048
D = 1024
NT = 16
NB = 4
DIN = 5128
ENG_NAMES = ("pe", "act", "dve", "pool", "sp")


class Op:
    __slots__ = ("eng", "fn", "is_dma", "grp", "signal", "val", "idx", "deps")

    def __init__(self, eng, fn, is_dma, grp):
        self.eng = eng
        self.fn = fn
        self.is_dma = is_dma
        self.grp = grp
        self.signal = False
        self.val = None
        self.idx = None
        self.deps = []


def _reduce_ops(ops):
    latest = {}
    dm = {}
    for o in ops:
        if o.is_dma:
            if o.grp not in dm or dm[o.grp].idx < o.idx:
                dm[o.grp] = o
        else:
            if o.eng not in latest or latest[o.eng].idx < o.idx:
                latest[o.eng] = o
    return list(latest.values()) + list(dm.values())


class Prog:
    def __init__(self, nc):
        self.nc = nc
        self.ops = {e: [] for e in ENG_NAMES}
        self.all_ops = []
        self.last_writer = {}
        self.readers = {}
        self.dma_groups = {}
        self.buf_pred = {}
        self.keys_by_buf = {}
        self.wait_all_groups = set()

    def _touch(self, k):
        if k not in self.readers:
            self.readers[k] = list(self.buf_pred.get(k[0], ()))
            self.last_writer[k] = None
            self.keys_by_buf.setdefault(k[0], set()).add(k)

    def ops_touching(self, bufname):
        s = list(self.buf_pred.get(bufname, ()))
        for k in self.keys_by_buf.get(bufname, ()):
            w = self.last_writer.get(k)
            if w is not None:
                s.append(w)
            s.extend(self.readers.get(k, ()))
        return _reduce_ops(s)

    def add(self, eng, fn, reads=(), writes=(), dma=False, grp=None):
        op = Op(eng, fn, dma, grp)
        op.idx = len(self.all_ops)
        self.all_ops.append(op)
        self.ops[eng].append(op)
        if dma:
            assert grp is not None
            self.dma_groups.setdefault(grp, []).append(op)
        deps = []
        for k in reads:
            self._touch(k)
            w = self.last_writer[k]
            if w is not None:
                deps.append((w, "raw"))
            elif self.readers[k] and k[0] in self.buf_pred:
                pass
        for k in writes:
            self._touch(k)
            w = self.last_writer[k]
            if w is not None:
                deps.append((w, "waw"))
            for r in self.readers[k]:
                deps.append((r, "war"))
        for d, kind in deps:
            if d is op:
                continue
            if (not d.is_dma) and (not dma) and d.eng == eng:
                if eng == "pe":
                    continue
            op.deps.append(d)
        for k in reads:
            self.readers[k].append(op)
        for k in writes:
            self.last_writer[k] = op
            self.readers[k] = []
        return op

    def emit(self, final_wait_groups=()):
        nc = self.nc
        for op in self.all_ops:
            op.deps = _reduce_ops(op.deps)
            for d in op.deps:
                d.signal = True
        for e in ENG_NAMES:
            c = 0
            for op in self.ops[e]:
                if (not op.is_dma) and op.signal:
                    c += 1
                    op.val = c
        gtotal = {}
        for g, lst in self.dma_groups.items():
            c = 0
            for op in lst:
                c += 16
                op.val = c
            gtotal[g] = c
        with contextlib.ExitStack() as st:
            esem = {e: st.enter_context(nc.semaphore("s_" + e)) for e in ENG_NAMES}
            gsem = {g: st.enter_context(nc.semaphore("d_%d" % i))
                    for i, g in enumerate(self.dma_groups)}
            block = st.enter_context(nc.Block())

            def run(e, engobj):
                seen = {}
                for op in self.ops[e]:
                    for d in op.deps:
                        if d.is_dma:
                            key = ("g", d.grp)
                            sem = gsem[d.grp]
                            v = gtotal[d.grp] if d.grp in self.wait_all_groups else d.val
                        else:
                            key = ("e", d.eng)
                            sem = esem[d.eng]
                            v = d.val
                        if seen.get(key, 0) >= v:
                            continue
                        seen[key] = v
                        engobj.wait_ge(sem, v)
                    ins = op.fn(engobj)
                    if op.is_dma:
                        ins.then_inc(gsem[op.grp], 16)
                    elif op.signal:
                        ins.then_inc(esem[e], 1)
                if e == "sp":
                    for g in final_wait_groups:
                        engobj.wait_ge(gsem[g], gtotal[g])

            block.tensor(lambda eng: run("pe", eng))
            block.scalar(lambda eng: run("act", eng))
            block.vector(lambda eng: run("dve", eng))
            block.gpsimd(lambda eng: run("pool", eng))
            block.sync(lambda eng: run("sp", eng))


class Arena:
    def __init__(self, nc, prog, words):
        self.t = nc.alloc_sbuf_tensor("arena", [128, words], F32)
        self.P = prog
        self.free_list = [(0, words)]
        self.live = {}
        self.dead = []
        self.peak = 0

    def alloc(self, name, shape, dt, parts=128):
        n = int(np.prod(shape))
        esz = 2 if dt == BF16 else 4
        words = (n * esz + 31) // 32 * 8
        small = words <= 1100
        order = range(len(self.free_list) - 1, -1, -1) if small else range(len(self.free_list))
        for i in order:
            o, w = self.free_list[i]
            if w >= words:
                if w == words:
                    off = o
                    self.free_list.pop(i)
                elif small:
                    off = o + w - words
                    self.free_list[i] = (o, w - words)
                else:
                    off = o
                    self.free_list[i] = (o + words, w - words)
                break
        else:
            raise RuntimeError("SBUF arena full allocating %s (%d words); live=%s" % (
                name, words, {k: v[1] for k, v in self.live.items()}))
        self.live[name] = (off, words)
        self.peak = max(self.peak, off + words)
        preds = []
        for (o, w, nm) in self.dead:
            if o < off + words and off < o + w:
                preds.extend(self.P.ops_touching(nm))
        assert name not in self.P.keys_by_buf, name
        self.P.buf_pred[name] = _reduce_ops(preds)
        v = self.t[0:parts, off:off + words]
        if dt != F32:
            v = v.bitcast(dt)
        v = v[:, 0:n]
        if len(shape) == 2:
            v = v.rearrange("p (a b) -> p a b", b=shape[1])
        elif len(shape) == 3:
            v = v.rearrange("p (a b c) -> p a b c", b=shape[1], c=shape[2])
        return v

    def free(self, name):
        off, words = self.live.pop(name)
        self.dead.append((off, words, name))
        fl = self.free_list + [(off, words)]
        fl.sort()
        merged = []
        for o, w in fl:
            if merged and merged[-1][0] + merged[-1][1] == o:
                merged[-1] = (merged[-1][0], merged[-1][1] + w)
            else:
                merged.append((o, w))
        self.free_list = merged


def build(stage=99, dbg=()):
    nc = bass.Bass("TRN2", target_bir_lowering=False)
    P = Prog(nc)
    A = Arena(nc, P, 52992)

    def din(name, shape, dt=F32):
        return nc.dram_tensor(name, list(shape), dt, kind="ExternalInput").ap()

    x_d = din("x", [T, D])
    ccol_d = din("c_col", [128, 8])
    wada_d = din("w_ada", [D, 6 * D])
    bada_d = din("b_ada_col", [128, 48])
    g1_d = din("g1_col", [128, 8])
    g2_d = din("g2_col", [128, 8])
    win_d = din("w_in", [D, DIN])
    bif_d = din("b_if_bc", [128, 8])
    cw_d = din("conv_w_col", [128, 4, 31])
    cb_d = din("conv_b_col", [128, 4])
    clg_d = din("conv_lng_col", [128, 4])
    clb_d = din("conv_lnb_col", [128, 4])
    wco_d = din("w_conv_out", [512, D])
    qkw_d = din("qk_w_col", [128, 8, 4])
    qkb_d = din("qk_b_col", [128, 8])
    mng_d = din("mng_col", [128, 4])
    wmo_d = din("w_m_out", [512, D])
    wout_d = din("w_out", [D, D])
    wr_d = din("w_router", [D, 36])
    br_d = din("b_router_bc", [128, 36])
    weg_d = din("w_e_gate_l", [32 * 128, 8 * 512])
    weu_d = din("w_e_up_l", [32 * 128, 8 * 512])
    wed_d = din("w_e_down_l", [32 * 128, 4 * D])
    gfin_d = din("g_final_bc", [128, D])
    out_d = nc.dram_tensor("out", [T, D], F32, kind="ExternalOutput").ap()

    dbg_outs = {}

    def dbg_out(name, ap, reads):
        if name not in dbg:
            return
        shape = list(ap.shape)
        dt = ap.dtype
        d = nc.dram_tensor("dbg_" + name, shape, dt, kind="ExternalOutput").ap()
        dbg_outs[name] = d
        P.add("sp", lambda e: e.dma_start(out=d, in_=ap), reads=reads, dma=True, grp="dbgout")

    psb = [nc.alloc_psum_tensor("ps%d" % i, [128, 512], F32) for i in range(8)]
    ps_rot = list(range(8))

    def next_ps():
        i = ps_rot.pop(0)
        ps_rot.append(i)
        return psb[i], ("ps%d" % i,)

    def hold_ps():
        i = ps_rot.pop(0)
        return psb[i], ("ps%d" % i,)

    def release_ps(key):
        ps_rot.append(int(key[0][2:]))

    ident_f = A.alloc("ident_f", [128], F32)
    ident_b = A.alloc("ident_b", [128], BF16)
    ones_b = A.alloc("ones_b", [128], BF16)
    ones_f = A.alloc("ones_f", [128], F32)
    mask_ut = A.alloc("mask_ut", [128], BF16)
    tri_f = A.alloc("tri_f", [128], F32)
    K_ID = ("ident_f", 0)
    P.add("pool", lambda e: e.memset(ident_f, 0.0), writes=[("ident_f", 0)])
    P.add("pool", lambda e: e.affine_select(out=ident_f, in_=ident_f, pattern=[[-1, 128]],
                                             compare_op=ALU.not_equal, fill=1.0, base=0, channel_multiplier=1),
          reads=[("ident_f", 0)], writes=[("ident_f", 0)])
    P.add("pool", lambda e: e.tensor_copy(out=ident_b, in_=ident_f), reads=[("ident_f", 0)], writes=[("ident_b", 0)])
    P.add("pool", lambda e: e.memset(ones_b, 1.0), writes=[("ones_b", 0)])
    P.add("pool", lambda e: e.memset(ones_f, 1.0), writes=[("ones_f", 0)])
    P.add("pool", lambda e: e.memset(tri_f, 1.0), writes=[("tri_f", 0)])
    P.add("pool", lambda e: e.affine_select(out=tri_f, in_=tri_f, pattern=[[1, 128]],
                                             compare_op=ALU.is_ge, fill=0.0, base=0, channel_multiplier=-1),
          reads=[("tri_f", 0)], writes=[("tri_f", 0)])
    P.add("pool", lambda e: e.tensor_copy(out=mask_ut, in_=tri_f), reads=[("tri_f", 0)], writes=[("mask_ut", 0)])

    def load_const(name, dram, shape, dt=F32):
        t = A.alloc(name, shape, dt)
        P.add("sp", lambda e: e.dma_start(out=t, in_=dram), writes=[(name, 0)], dma=True, grp="c_" + name)
        return t

    ccol = load_const("ccol", ccol_d, [8])
    bada = load_const("bada", bada_d, [48])
    g1c = load_const("g1c", g1_d, [8])
    g2c = load_const("g2c", g2_d, [8])

    silc = A.alloc("silc", [8], F32)
    silb = A.alloc("silb", [8], BF16)
    P.add("act", lambda e: e.activation(out=silc, in_=ccol, func=AF.Silu), reads=[("ccol", 0)], writes=[("silc", 0)])
    P.add("dve", lambda e: e.tensor_copy(out=silb, in_=silc), reads=[("silc", 0)], writes=[("silb", 0)])
    modT = A.alloc("modT", [48], F32)
    wada_v = wada_d.rearrange("(c p) n -> p c n", p=128)
    NWA = 2
    wab = [A.alloc("wada%d" % i, [8, 512], BF16) for i in range(NWA)]
    a1 = A.alloc("a1", [8], F32)
    a2 = A.alloc("a2", [8], F32)

    def adaln_block(blk, ps_mod, k_mod):
        s_ = blk % NWA
        buf = wab[s_]
        nm = "wada%d" % s_
        P.add("pool", (lambda e, buf=buf, blk=blk: e.dma_start(out=buf, in_=wada_v[:, :, blk * 512:(blk + 1) * 512])),
              writes=[(nm, 0)], dma=True, grp=nm)

        def mm(e, buf=buf, blk=blk):
            ins = None
            for jj in range(4):
                j = blk * 4 + jj
                for k in range(8):
                    ins = e.matmul(ps_mod[:, j:j + 1], lhsT=buf[:, k, jj * 128:(jj + 1) * 128], rhs=silb[:, k:k + 1],
                                   start=(k == 0), stop=(k == 7))
            return ins
        P.add("pe", mm, reads=[(nm, 0), ("silb", 0)], writes=[k_mod])

    def adaln_finish(ps_mod, k_mod, c0, c1, part):
        P.add("dve", lambda e: e.tensor_tensor(out=modT[:, c0:c1], in0=ps_mod[:, c0:c1], in1=bada[:, c0:c1], op=ALU.add),
              reads=[k_mod, ("bada", 0)], writes=[("modT", part)])
        release_ps(k_mod)

    pm0, km0 = hold_ps()
    for blk in range(4):
        adaln_block(blk, pm0, km0)
    adaln_finish(pm0, km0, 0, 16, 0)
    P.add("dve", lambda e: e.scalar_tensor_tensor(out=a1, in0=modT[:, 8:16], scalar=1.0, in1=g1c, op0=ALU.add, op1=ALU.mult),
          reads=[("modT", 0), ("g1c", 0)], writes=[("a1", 0)])
    p2state = {}

    def adaln_p2_block(blk):
        if "ps" not in p2state:
            p2state["ps"] = hold_ps()
        adaln_block(blk, *p2state["ps"])

    def adaln_p2_end():
        pm1, km1 = p2state["ps"]
        adaln_finish(pm1, km1, 16, 48, 1)
        P.add("dve", lambda e: e.scalar_tensor_tensor(out=a2, in0=modT[:, 32:40], scalar=1.0, in1=g2c, op0=ALU.add, op1=ALU.mult),
              reads=[("modT", 1), ("g2c", 0)], writes=[("a2", 0)])
        dbg_out("modT", modT, [("modT", 0), ("modT", 1)])
        for i in range(NWA):
            A.free("wada%d" % i)

    hT = A.alloc("hT", [8, T], BF16)
    merged = A.alloc("merged", [8, T], BF16)
    NWB = 3
    wbufs = [A.alloc("wblk%d" % i, [8, 512], BF16) for i in range(NWB)]
    NXB = 8
    xin = [A.alloc("xin%d" % i, [D], F32) for i in range(NXB)]
    xnb = [A.alloc("xnb%d" % i, [D], BF16) for i in range(NXB)]
    junk = A.alloc("junk", [D], F32)
    ss1 = A.alloc("ss1", [NT], F32)
    rs1 = A.alloc("rs1", [NT], F32)

    def p2_stats(nb):
        for tt in range(4):
            ti = nb * 4 + tt
            s = ti % NXB
            P.add("sp", (lambda e, s=s, ti=ti: e.dma_start(out=xin[s], in_=x_d[ti * 128:(ti + 1) * 128, :])),
                  writes=[("xin%d" % s, 0)], dma=True, grp="xin%d" % s)
            P.add("act", (lambda e, s=s, ti=ti: e.activation(out=junk, in_=xin[s], func=AF.Square,
                                                             accum_out=ss1[:, ti:ti + 1])),
                  reads=[("xin%d" % s, 0)], writes=[("junk", 0), ("ss1", ti)])
            P.add("act", (lambda e, ti=ti: e.activation(out=rs1[:, ti:ti + 1], in_=ss1[:, ti:ti + 1], func=AF.Sqrt,
                                                        scale=1.0 / D, bias=1e-6)),
                  reads=[("ss1", ti)], writes=[("rs1", ti)])
            P.add("dve", (lambda e, ti=ti: e.reciprocal(out=rs1[:, ti:ti + 1], in_=rs1[:, ti:ti + 1])),
                  reads=[("rs1", ti)], writes=[("rs1", ti)])
            P.add("dve", (lambda e, s=s, ti=ti: e.tensor_scalar(out=xnb[s], in0=xin[s], scalar1=rs1[:, ti:ti + 1],
                                                                scalar2=None, op0=ALU.mult)),
                  reads=[("xin%d" % s, 0), ("rs1", ti)], writes=[("xnb%d" % s, 0)])

    def p2_tr(nb):
        pst = [next_ps() for _ in range(4)]
        for tt in range(4):
            ti = nb * 4 + tt
            s = ti % NXB

            def tr(e, s=s, tt=tt, pst=pst):
                ins = None
                for c in range(8):
                    pb = pst[c // 2][0].bitcast(BF16)
                    ins = e.transpose(out=pb[:, (c % 2) * 512 + tt * 128:(c % 2) * 512 + (tt + 1) * 128],
                                      in_=xnb[s][:, c * 128:(c + 1) * 128], identity=ident_b)
                return ins
            P.add("pe", tr, reads=[("xnb%d" % s, 0), ("ident_b", 0)], writes=[pst[i][1] for i in range(4)])
        for c in range(8):
            pb = pst[c // 2][0].bitcast(BF16)
            P.add("act", (lambda e, c=c, pb=pb, nb=nb: e.activation(
                out=hT[:, c, nb * 512:(nb + 1) * 512], in_=pb[:, (c % 2) * 512:(c % 2 + 1) * 512],
                func=AF.Identity, scale=a1[:, c:c + 1], bias=modT[:, c:c + 1])),
                reads=[pst[c // 2][1], ("a1", 0), ("modT", 0)],
                writes=[("hT", c, nb)])

    p2_stats(0)
    for nb in range(NB):
        if nb + 1 < NB:
            p2_stats(nb + 1)
        p2_tr(nb)
    dbg_out("hT", hT, [("hT", c, nb) for c in range(8) for nb in range(NB)])
    for i in range(NXB):
        A.free("xin%d" % i); A.free("xnb%d" % i)

    def finish():
        P.emit(final_wait_groups=["dbgout"] if "dbgout" in P.dma_groups else [])
        return nc, dbg_outs

    GROWS = 256
    NG = -(-(2 * T + 32 * (GROWS - 1)) // GROWS)
    XS = nc.dram_tensor("xs_scratch", [NG * GROWS, D], BF16).ap()
    YS = nc.dram_tensor("ys_scratch", [NG * GROWS, D], BF16).ap()
    zt = A.alloc("zt", [D], BF16)
    P.add("pool", lambda e: e.memset(zt, 0.0), writes=[("zt", 0)])
    XSZ_KEYS = []
    for zi in range(NG * GROWS // 1024):
        P.add("sp", (lambda e, zi=zi: e.dma_start(out=XS[zi * 1024:(zi + 1) * 1024, :].rearrange("(n p) d -> p n d", p=128),
                                                  in_=zt.unsqueeze(1).to_broadcast([128, 8, D]))),
              reads=[("zt", 0)], writes=[("XSZ", zi)], dma=True, grp="xs_zero")
        XSZ_KEYS.append(("XSZ", zi))
    A.free("zt")
    if stage <= 1:
        return finish()

    win_v = win_d.rearrange("(c p) n -> p c n", p=128)
    NWB = 3
    wb_ctr = [0]

    def load_wblock(col0, ncols=512):
        i = wb_ctr[0] % NWB
        wb_ctr[0] += 1
        buf = wbufs[i]
        nm = "wblk%d" % i
        P.add("pool", lambda e: e.dma_start(out=buf[:, :, 0:ncols], in_=win_v[:, :, col0:col0 + ncols]),
              writes=[(nm, 0)], dma=True, grp=nm)
        return buf, (nm, 0)

    def load_w4(dram_v):
        i = wb_ctr[0] % NWB
        wb_ctr[0] += 1
        nm = "wblk%d" % i
        v = wbufs[i].rearrange("p a b -> p (a b)").rearrange("p (a b) -> p a b", b=D)
        P.add("pool", lambda e: e.dma_start(out=v, in_=dram_v), writes=[(nm, 0)], dma=True, grp=nm)
        return v, (nm, 0)

    def load_cast(name, dram_ap, shape):
        t = A.alloc(name, shape, BF16)
        P.add("pool", lambda e: e.dma_start(out=t, in_=dram_ap), writes=[(name, 0)], dma=True, grp="c_" + name)
        return t

    hT_keys = lambda nb: [("hT", c, nb) for c in range(8)]

    def proj_fm(wb, wkey, mcol, nb):
        ps, pk = next_ps()

        def mm(e):
            ins = None
            for k in range(8):
                ins = e.matmul(ps[:, :], lhsT=wb[:, k, mcol * 128:(mcol + 1) * 128], rhs=hT[:, k, nb * 512:(nb + 1) * 512],
                               start=(k == 0), stop=(k == 7))
            return ins
        P.add("pe", mm, reads=[wkey] + hT_keys(nb), writes=[pk])
        return ps, pk

    cw = load_const("cw", cw_d, [4, 31])
    cb = load_const("cb", cb_d, [4])
    clg = load_const("clg", clg_d, [4])
    clb = load_const("clb", clb_d, [4])
    u = A.alloc("u", [4, 32 + T], BF16)
    PADU = 32
    for m in range(4):
        P.add("pool", (lambda e, m=m: e.memset(u[:, m, 0:PADU], 0.0)), writes=[("u", m, -1)])
    dg31 = A.alloc("dg31", [4, 31, 128], BF16)
    for m in range(4):
        P.add("pool", (lambda e, m=m: e.tensor_tensor(
            out=dg31[:, m], in0=ident_b.unsqueeze(1).to_broadcast([128, 31, 128]),
            in1=cw[:, m, :].unsqueeze(2).to_broadcast([128, 31, 128]), op=ALU.mult)),
            reads=[("ident_b", 0), ("cw", 0)], writes=[("dg31", m)])
    sgt = [A.alloc("sgt%d" % i, [512], BF16) for i in range(2)]
    sg_ctr = [0]

    def next_sgt():
        i = sg_ctr[0] % 2
        sg_ctr[0] += 1
        return sgt[i], ("sgt%d" % i, 0)

    wa, wak = load_wblock(0)
    wbk, wbkk = load_wblock(512)
    for m in range(4):
        for nb in range(NB):
            psa, pka = proj_fm(wa, wak, m, nb)
            psb_, pkb = proj_fm(wbk, wbkk, m, nb)
            sg, sgk = next_sgt()
            P.add("act", (lambda e, sg=sg, p=psb_: e.activation(out=sg, in_=p[:, :], func=AF.Sigmoid)),
                  reads=[pkb], writes=[sgk])
            P.add("dve", (lambda e, sg=sg, p=psa, m=m, nb=nb: e.tensor_tensor(
                out=u[:, m, PADU + nb * 512:PADU + (nb + 1) * 512], in0=p[:, :], in1=sg, op=ALU.mult)),
                reads=[pka, sgk], writes=[("u", m, nb)])
    dbg_out("u", u, [("u", m, nb) for m in range(4) for nb in range(-1, NB)])

    wco, wcok = load_w4(wco_d.rearrange("(c p) n -> p c n", p=128))
    gA_blocks = {0: load_wblock(3080)}
    cT = A.alloc("cT", [4, T], BF16)
    sqT = A.alloc("sqT", [4, T], BF16)
    for m in range(4):
        for nb in range(NB):
            ps, pk = next_ps()

            def cmm(e, ps=ps, m=m, nb=nb):
                ins = None
                for k in range(31):
                    o = PADU - 30 + nb * 512 + k
                    ins = e.matmul(ps[:, :], lhsT=dg31[:, m, k, :], rhs=u[:, m, o:o + 512], start=(k == 0), stop=(k == 30))
                return ins
            P.add("pe", cmm, reads=[("dg31", m), ("u", m, nb), ("u", m, nb - 1)], writes=[pk])
            P.add("act", (lambda e, ps=ps, m=m, nb=nb: e.activation(
                out=cT[:, m, nb * 512:(nb + 1) * 512], in_=ps[:, :], func=AF.Identity, bias=cb[:, m:m + 1])),
                reads=[pk, ("cb", 0)], writes=[("cT", m, nb)])
            P.add("act", (lambda e, ps=ps, m=m, nb=nb: e.activation(
                out=sqT[:, m, nb * 512:(nb + 1) * 512], in_=ps[:, :], func=AF.Square, bias=cb[:, m:m + 1])),
                reads=[pk, ("cb", 0)], writes=[("sqT", m, nb)])
            gi = m * NB + nb
            if gi % 2 == 1:
                adaln_p2_block(4 + gi // 2)
    adaln_p2_end()
    dbg_out("cT", cT, [("cT", m, nb) for m in range(4) for nb in range(NB)])
    A.free("u")
    A.free("dg31")

    actT = A.alloc("actT", [4, T], BF16)
    mean_t = A.alloc("mean_t", [512], F32)
    rstd_t = A.alloc("rstd_t", [512], F32)
    msq_t = A.alloc("msq_t", [512], F32)
    nrm_t = [A.alloc("nrm_t%d" % i, [512], F32) for i in range(2)]
    for nb in range(NB):
        ps1, pk1 = next_ps()
        ps2, pk2 = next_ps()

        def smm(e, ps1=ps1, ps2=ps2, nb=nb):
            ins = None
            for m in range(4):
                ins = e.matmul(ps1[:, :], lhsT=ones_b, rhs=cT[:, m, nb * 512:(nb + 1) * 512], start=(m == 0), stop=(m == 3))
            for m in range(4):
                ins = e.matmul(ps2[:, :], lhsT=ones_b, rhs=sqT[:, m, nb * 512:(nb + 1) * 512], start=(m == 0), stop=(m == 3))
            return ins
        P.add("pe", smm, reads=[("ones_b", 0)] + [("cT", m, nb) for m in range(4)] + [("sqT", m, nb) for m in range(4)],
              writes=[pk1, pk2])
        P.add("dve", (lambda e, ps1=ps1: e.tensor_scalar(out=mean_t, in0=ps1[:, :], scalar1=1.0 / 512, scalar2=None, op0=ALU.mult)),
              reads=[pk1], writes=[("mean_t", 0)])
        P.add("dve", lambda e: e.tensor_tensor(out=msq_t, in0=mean_t, in1=mean_t, op=ALU.mult),
              reads=[("mean_t", 0)], writes=[("msq_t", 0)])
        P.add("dve", (lambda e, ps2=ps2: e.scalar_tensor_tensor(out=rstd_t, in0=ps2[:, :], scalar=1.0 / 512, in1=msq_t,
                                                                op0=ALU.mult, op1=ALU.subtract)),
              reads=[pk2, ("msq_t", 0)], writes=[("rstd_t", 0)])
        P.add("act", lambda e: e.activation(out=rstd_t, in_=rstd_t, func=AF.Sqrt, bias=1e-5),
              reads=[("rstd_t", 0)], writes=[("rstd_t", 0)])
        P.add("dve", lambda e: e.reciprocal(out=rstd_t, in_=rstd_t), reads=[("rstd_t", 0)], writes=[("rstd_t", 0)])
        for m in range(4):
            nt = nrm_t[m % 2]
            ntk = ("nrm_t%d" % (m % 2), 0)
            P.add("dve", (lambda e, nt=nt, m=m, nb=nb: e.tensor_tensor(out=nt, in0=cT[:, m, nb * 512:(nb + 1) * 512], in1=mean_t,
                                                                      op=ALU.subtract)),
                  reads=[("cT", m, nb), ("mean_t", 0)], writes=[ntk])
            P.add("dve", (lambda e, nt=nt: e.tensor_tensor(out=nt, in0=nt, in1=rstd_t, op=ALU.mult)),
                  reads=[ntk, ("rstd_t", 0)], writes=[ntk])
            P.add("act", (lambda e, nt=nt, m=m, nb=nb: e.activation(
                out=actT[:, m, nb * 512:(nb + 1) * 512], in_=nt, func=AF.Silu, scale=clg[:, m:m + 1], bias=clb[:, m:m + 1])),
                reads=[ntk, ("clg", 0), ("clb", 0)], writes=[("actT", m, nb)])
    dbg_out("actT", actT, [("actT", m, nb) for m in range(4) for nb in range(NB)])
    A.free("cT"); A.free("sqT"); A.free("mean_t"); A.free("rstd_t"); A.free("msq_t"); A.free("nrm_t0"); A.free("nrm_t1")

    for jb in range(2):
        wg_, wgk = gA_blocks[jb] if jb in gA_blocks else load_wblock(3080 + jb * 512)
        for jj in range(4):
            j = jb * 4 + jj
            for nb in range(NB):
                psy, pky = next_ps()

                def ymm(e, psy=psy, j=j, nb=nb):
                    ins = None
                    for m in range(4):
                        ins = e.matmul(psy[:, :], lhsT=wco[:, m, j * 128:(j + 1) * 128], rhs=actT[:, m, nb * 512:(nb + 1) * 512],
                                       start=(m == 0), stop=(m == 3))
                    return ins
                P.add("pe", ymm, reads=[wcok] + [("actT", m, nb) for m in range(4)], writes=[pky])
                psg, pkg = proj_fm(wg_, wgk, jj, nb)
                sg, sgk = next_sgt()
                P.add("act", (lambda e, sg=sg, p=psg: e.activation(out=sg, in_=p[:, :], func=AF.Sigmoid)),
                      reads=[pkg], writes=[sgk])
                P.add("dve", (lambda e, sg=sg, p=psy, j=j, nb=nb: e.tensor_tensor(
                    out=merged[:, j, nb * 512:(nb + 1) * 512], in0=p[:, :], in1=sg, op=ALU.mult)),
                    reads=[pky, sgk], writes=[("merged", j, nb)])
    dbg_out("mergedA", merged, [("merged", j, nb) for j in range(8) for nb in range(NB)])
    A.free("actT")
    if stage <= 2:
        return finish()

    PADQ = 4
    qkw = load_const("qkw", qkw_d, [8, 4])
    qkb = load_const("qkb", qkb_d, [8])
    bif = load_const("bif", bif_d, [8])
    mng = load_const("mng", mng_d, [4])
    qk_raw = A.alloc("qk_raw", [8, PADQ + T], BF16)
    for cc in range(8):
        P.add("pool", (lambda e, cc=cc: e.memset(qk_raw[:, cc, 0:PADQ], 0.0)), writes=[("qk_raw", cc, -1)])
    dg4 = A.alloc("dg4", [8, 4, 128], BF16)
    P.add("pool", lambda e: e.tensor_tensor(
        out=dg4.rearrange("p a b c -> p (a b) c"), in0=ident_b.unsqueeze(1).to_broadcast([128, 32, 128]),
        in1=qkw.rearrange("p a b -> p (a b)").unsqueeze(2).to_broadcast([128, 32, 128]), op=ALU.mult),
        reads=[("ident_b", 0), ("qkw", 0)], writes=[("dg4", 0)])
    for half in range(2):
        wq_, wqk = load_wblock(1024 + half * 512)
        for m in range(4):
            cc = half * 4 + m
            for nb in range(NB):
                ps, pk = proj_fm(wq_, wqk, m, nb)
                P.add("act", (lambda e, ps=ps, cc=cc, nb=nb: e.activation(
                    out=qk_raw[:, cc, PADQ + nb * 512:PADQ + (nb + 1) * 512], in_=ps[:, :], func=AF.Identity)),
                    reads=[pk], writes=[("qk_raw", cc, nb)])
    qkc = A.alloc("qkc", [8, T], BF16)
    for cc in range(8):
        for nb in range(NB):
            ps, pk = next_ps()

            def qmm(e, ps=ps, cc=cc, nb=nb):
                ins = None
                for k in range(4):
                    o = PADQ - 3 + nb * 512 + k
                    ins = e.matmul(ps[:, :], lhsT=dg4[:, cc, k, :], rhs=qk_raw[:, cc, o:o + 512], start=(k == 0), stop=(k == 3))
                return ins
            P.add("pe", qmm, reads=[("dg4", 0), ("qk_raw", cc, nb), ("qk_raw", cc, nb - 1)], writes=[pk])
            P.add("act", (lambda e, ps=ps, cc=cc, nb=nb: e.activation(
                out=qkc[:, cc, nb * 512:(nb + 1) * 512], in_=ps[:, :], func=AF.Silu, bias=qkb[:, cc:cc + 1])),
                reads=[pk, ("qkb", 0)], writes=[("qkc", cc, nb)])
    dbg_out("qkc", qkc, [("qkc", cc, nb) for cc in range(8) for nb in range(NB)])
    A.free("qk_raw"); A.free("dg4")
    if stage <= 2.2:
        return finish()

    wif = A.alloc("wif", [8, 8], BF16)
    wif_f = A.alloc("wif_f", [8, 8], F32)
    with nc.allow_non_contiguous_dma(reason="tiny gate-weight columns"):
        P.add("sp", lambda e: e.dma_start(out=wif_f, in_=win_v[:, :, 3072:3080]), writes=[("wif_f", 0)], dma=True, grp="c_wif")
    P.add("dve", lambda e: e.tensor_copy(out=wif, in_=wif_f), reads=[("wif_f", 0)], writes=[("wif", 0)])
    G = A.alloc("G", [NT, 8], F32)
    nlf = A.alloc("nlf", [NT, 4], F32)
    gtmp = A.alloc("gtmp", [NT, 4], F32)
    A_inv = A.alloc("A_inv", [NT, 4], F32)
    Bv = A.alloc("Bv", [NT, 4], F32)
    dec = A.alloc("dec", [NT, 4], F32)
    psg, pkg = hold_ps()

    def gmm(e):
        ins = None
        for ti in range(NT):
            for k in range(8):
                ins = e.matmul(psg[:, ti * 8:(ti + 1) * 8], lhsT=hT[:, k, ti * 128:(ti + 1) * 128], rhs=wif[:, k, :],
                               start=(k == 0), stop=(k == 7))
        return ins
    P.add("pe", gmm, reads=[("wif", 0)] + [("hT", c, nb) for c in range(8) for nb in range(NB)], writes=[pkg])
    P.add("dve", lambda e: e.tensor_tensor(out=G, in0=psg[:, 0:128].rearrange("p (a b) -> p a b", b=8),
                                           in1=bif.unsqueeze(1).to_broadcast([128, NT, 8]), op=ALU.add),
          reads=[pkg, ("bif", 0)], writes=[("G", 0)])
    release_ps(pkg)
    dbg_out("G", G, [("G", 0)])
    if stage <= 2.31:
        return finish()
    P.add("act", lambda e: e.activation(out=gtmp, in_=G[:, :, 4:8], func=AF.Exp, scale=-1.0),
          reads=[("G", 0)], writes=[("gtmp", 0)])
    P.add("act", lambda e: e.activation(out=nlf, in_=gtmp, func=AF.Ln, bias=1.0),
          reads=[("gtmp", 0)], writes=[("nlf", 0)])
    dbg_out("nlf", nlf, [("nlf", 0)])
    if stage <= 2.32:
        return finish()
    psc, pkc = next_ps()
    nlf2 = nlf.rearrange("p a b -> p (a b)")
    nl_hi = A.alloc("nl_hi", [64], BF16)
    nl_lo = A.alloc("nl_lo", [64], BF16)
    P.add("dve", lambda e: e.tensor_copy(out=nl_hi, in_=nlf2), reads=[("nlf", 0)], writes=[("nl_hi", 0)])
    P.add("dve", lambda e: e.tensor_tensor(out=nl_lo, in0=nlf2, in1=nl_hi, op=ALU.subtract),
          reads=[("nlf", 0), ("nl_hi", 0)], writes=[("nl_lo", 0)])

    def cmm2(e):
        e.matmul(psc[:, 0:64], lhsT=mask_ut, rhs=nl_hi, start=True, stop=False)
        e.matmul(psc[:, 0:64], lhsT=mask_ut, rhs=nl_lo, start=False, stop=True)
        e.matmul(psc[:, 64:128], lhsT=ones_b, rhs=nl_hi, start=True, stop=False)
        return e.matmul(psc[:, 64:128], lhsT=ones_b, rhs=nl_lo, start=False, stop=True)
    P.add("pe", cmm2, reads=[("mask_ut", 0), ("ones_b", 0), ("nl_hi", 0), ("nl_lo", 0)], writes=[pkc])
    if stage <= 2.33:
        P.add("dve", lambda e: e.tensor_copy(out=gtmp.rearrange("p a b -> p (a b)"), in_=psc[:, 0:64]), reads=[pkc], writes=[("gtmp", 0)])
        dbg_out("ncum", gtmp, [("gtmp", 0)])
        return finish()
    LNS = float(np.log(128.0 ** 0.5))
    cval = A.alloc("cval", [2], F32)
    P.add("pool", lambda e: e.memset(cval[:, 0:1], LNS), writes=[("cval", 0)])
    P.add("pool", lambda e: e.memset(cval[:, 1:2], -LNS), writes=[("cval", 1)])
    P.add("act", lambda e: e.activation(out=A_inv.rearrange("p a b -> p (a b)"), in_=psc[:, 0:64], func=AF.Exp, bias=cval[:, 0:1]),
          reads=[pkc, ("cval", 0)], writes=[("A_inv", 0)])
    A_ = A.alloc("A_", [NT, 4], F32)
    P.add("act", lambda e: e.activation(out=A_.rearrange("p a b -> p (a b)"), in_=psc[:, 0:64], func=AF.Exp, scale=-1.0, bias=cval[:, 1:2]),
          reads=[pkc, ("cval", 1)], writes=[("A_", 0)])
    if stage <= 2.34:
        dbg_out("A_", A_, [("A_", 0)])
        dbg_out("A_inv", A_inv, [("A_inv", 0)])
        return finish()
    P.add("dve", lambda e: e.tensor_tensor(out=gtmp, in0=psc[:, 0:64].rearrange("p (a b) -> p a b", b=4), in1=G[:, :, 0:4], op=ALU.add),
          reads=[pkc, ("G", 0), ("gtmp", 0)], writes=[("gtmp", 0)])
    P.add("act", lambda e: e.activation(out=Bv, in_=gtmp, func=AF.Exp), reads=[("gtmp", 0)], writes=[("Bv", 0)])
    if stage <= 2.36:
        dbg_out("Bv", Bv, [("Bv", 0)])
        return finish()
    P.add("act", lambda e: e.activation(out=dec.rearrange("p a b -> p (a b)"), in_=psc[:, 64:128], func=AF.Exp, scale=-1.0),
          reads=[pkc], writes=[("dec", 0)])
    dbg_out("Bv", Bv, [("Bv", 0)])
    dbg_out("A_", A_, [("A_", 0)])
    dbg_out("decay", dec, [("dec", 0)])

    if stage <= 2.4:
        return finish()
    ktok = A.alloc("ktok", [NT, 512], BF16)
    for c in range(NT):
        ps, pk = next_ps()
        pb = ps.bitcast(BF16)

        def ktr(e, pb=pb, c=c):
            ins = None
            for h in range(4):
                ins = e.transpose(out=pb[:, h * 128:(h + 1) * 128], in_=qkc[:, 4 + h, c * 128:(c + 1) * 128], identity=ident_b)
            return ins
        P.add("pe", ktr, reads=[("ident_b", 0)] + [("qkc", 4 + h, c // 4) for h in range(4)], writes=[pk])
        P.add("act", (lambda e, pb=pb, c=c: e.activation(out=ktok[:, c, :], in_=pb[:, 0:512], func=AF.Identity)),
              reads=[pk], writes=[("ktok", c)])

    vB = A.alloc("vB", [NT, 4, 129], BF16)
    wv_, wvk = load_wblock(2048)
    for c in range(NT):
        ps, pk = next_ps()

        def vmm(e, ps=ps, c=c):
            ins = None
            for k in range(8):
                ins = e.matmul(ps[:, :], lhsT=hT[:, k, c * 128:(c + 1) * 128], rhs=wv_[:, k, 0:512], start=(k == 0), stop=(k == 7))
            return ins
        P.add("pe", vmm, reads=[wvk] + hT_keys(c // 4), writes=[pk])
        P.add("dve", (lambda e, ps=ps, c=c: e.tensor_tensor(
            out=vB[:, c, :, 0:128], in0=ps[:, :].rearrange("p (a b) -> p a b", b=128),
            in1=Bv[:, c, :].unsqueeze(2).to_broadcast([128, 4, 128]), op=ALU.mult)),
            reads=[pk, ("Bv", 0)], writes=[("vB", c, 0)])
        P.add("dve", (lambda e, c=c: e.tensor_copy(out=vB[:, c, :, 128], in_=Bv[:, c, :])),
              reads=[("Bv", 0)], writes=[("vB", c, 1)])

    if stage <= 2.6:
        return finish()
    E = A.alloc("E", [4, 129], F32)
    Cb = [A.alloc("Cb%d" % i, [4, 129], BF16) for i in range(2)]
    sm = [A.alloc("sm%d" % i, [4, 128], BF16) for i in range(2)]
    hn = [A.alloc("hn%d" % i, [4, 128], BF16) for i in range(2)]
    st6 = A.alloc("st6", [4, 6], F32)
    mv = A.alloc("mv", [4, 2], F32)
    den = A.alloc("den", [4], F32)
    qq = A.alloc("qq", [4], F32)
    rstd = A.alloc("rstd", [4], F32)
    sgo = [A.alloc("sgo%d" % i, [4, 512], BF16) for i in range(2)]
    hmT = A.alloc("hmT", [4, T], BF16)
    wo_, wok = load_wblock(2560)
    CW = 256

    chs = {}

    def chunk_A(c):
        nb = c // 4
        cs = slice(c * 128, (c + 1) * 128)
        if c % 4 == 0:
            for h in range(4):
                ps, pk = proj_fm(wo_, wok, h, nb)
                P.add("act", (lambda e, ps=ps, h=h, nb=nb: e.activation(out=sgo[nb % 2][:, h, :], in_=ps[:, :], func=AF.Sigmoid)),
                      reads=[pk], writes=[("sgo%d" % (nb % 2), h)])
        pss, pks = next_ps()

        def smm2(e, pss=pss, cs=cs):
            ins = None
            for h in range(4):
                ins = e.matmul(pss[:, h * 128:(h + 1) * 128], lhsT=qkc[:, 4 + h, cs], rhs=qkc[:, h, cs], start=True, stop=True)
            return ins
        P.add("pe", smm2, reads=[("qkc", cc, nb) for cc in range(8)], writes=[pks])
        smc = sm[c % 2]
        smk = ("sm%d" % (c % 2), 0)
        P.add("dve", (lambda e, pss=pss, smc=smc: e.tensor_tensor(
            out=smc, in0=pss[:, :].rearrange("p (a b) -> p a b", b=128),
            in1=mask_ut.unsqueeze(1).to_broadcast([128, 4, 128]), op=ALU.mult)),
            reads=[pks, ("mask_ut", 0)], writes=[smk])
        pu = [hold_ps(), hold_ps()]

        def umm(e, pu=pu, c=c):
            ins = None
            for h in range(4):
                o = pu[h // 2][0][:, (h % 2) * CW:(h % 2) * CW + 129]
                ins = e.matmul(o, lhsT=ktok[:, c, h * 128:(h + 1) * 128], rhs=vB[:, c, h, :], start=True, stop=True)
            return ins
        P.add("pe", umm, reads=[("ktok", c), ("vB", c, 0), ("vB", c, 1)], writes=[pu[0][1], pu[1][1]])
        chs[c] = (smc, smk, pu)

    def chunk_B(c):
        nb = c // 4
        cs = slice(c * 128, (c + 1) * 128)
        smc, smk, pu = chs[c]
        pn = [next_ps(), next_ps()]

        def nmm(e, pn=pn, smc=smc, c=c, cs=cs):
            ins = None
            for h in range(4):
                o = pn[h // 2][0][:, (h % 2) * CW:(h % 2) * CW + 129]
                ins = e.matmul(o, lhsT=smc[:, h, :], rhs=vB[:, c, h, :], start=True, stop=(c == 0))
                if c > 0:
                    ins = e.matmul(o, lhsT=qkc[:, h, cs], rhs=Cb[(c - 1) % 2][:, h, :], start=False, stop=True)
            return ins
        rd = [smk, ("vB", c, 0), ("vB", c, 1)] + [("qkc", h, nb) for h in range(4)]
        if c > 0:
            rd += [("Cb%d" % ((c - 1) % 2), h) for h in range(4)]
        P.add("pe", nmm, reads=rd, writes=[pn[0][1], pn[1][1]])
        for h in range(4):
            src = pu[h // 2][0][:, (h % 2) * CW:(h % 2) * CW + 129]
            if c == 0:
                P.add("dve", (lambda e, src=src, h=h: e.tensor_copy(out=E[:, h, :], in_=src)),
                      reads=[pu[h // 2][1]], writes=[("E", h)])
            else:
                P.add("dve", (lambda e, src=src, h=h, c=c: e.scalar_tensor_tensor(
                    out=E[:, h, :], in0=E[:, h, :], scalar=dec[:, c - 1, h:h + 1], in1=src, op0=ALU.mult, op1=ALU.add)),
                    reads=[pu[h // 2][1], ("E", h), ("dec", 0)], writes=[("E", h)])
            if c < NT - 1:
                P.add("act", (lambda e, h=h, c=c: e.activation(out=Cb[c % 2][:, h, :], in_=E[:, h, :], func=AF.Identity,
                                                               scale=dec[:, c, h:h + 1])),
                      reads=[("E", h), ("dec", 0)], writes=[("Cb%d" % (c % 2), h)])
        release_ps(pu[0][1]); release_ps(pu[1][1])
        chs[c] = pn

    def chunk_C(c):
        nb = c // 4
        cs = slice(c * 128, (c + 1) * 128)
        pn = chs[c]
        for h in range(4):
            src = pn[h // 2][0][:, (h % 2) * CW:(h % 2) * CW + 128]
            P.add("dve", (lambda e, src=src, h=h: e.bn_stats(out=st6[:, h, :], in_=src)),
                  reads=[pn[h // 2][1]], writes=[("st6", h)])
            P.add("dve", (lambda e, h=h: e.bn_aggr(out=mv[:, h, :], in_=st6[:, h, :])),
                  reads=[("st6", h)], writes=[("mv", h)])
        for b2 in range(2):
            dsrc = pn[b2][0][:, 0:512].rearrange("p (a b) -> p a b", b=CW)[:, :, 128]
            P.add("dve", (lambda e, dsrc=dsrc, b2=b2, c=c: e.tensor_tensor(
                out=den[:, 2 * b2:2 * b2 + 2], in0=dsrc, in1=A_[:, c, 2 * b2:2 * b2 + 2], op=ALU.mult)),
                reads=[pn[b2][1], ("A_", 0)], writes=[("den", b2)])
        P.add("dve", lambda e: e.scalar_tensor_tensor(out=den, in0=den, scalar=-1.0, in1=den, op0=ALU.mult, op1=ALU.max),
              reads=[("den", 0), ("den", 1)], writes=[("den", 0), ("den", 1)])
        P.add("dve", lambda e: e.tensor_scalar(out=den, in0=den, scalar1=1.0, scalar2=None, op0=ALU.max),
              reads=[("den", 0), ("den", 1)], writes=[("den", 0), ("den", 1)])
        P.add("dve", (lambda e, c=c: e.tensor_tensor(out=qq, in0=den, in1=A_inv[:, c, :], op=ALU.mult)),
              reads=[("den", 0), ("den", 1), ("A_inv", 0)], writes=[("qq", 0)])
        P.add("dve", lambda e: e.tensor_tensor(out=qq, in0=qq, in1=qq, op=ALU.mult), reads=[("qq", 0)], writes=[("qq", 0)])
        P.add("dve", lambda e: e.scalar_tensor_tensor(out=rstd, in0=qq, scalar=1e-5, in1=mv[:, :, 1], op0=ALU.mult, op1=ALU.add),
              reads=[("qq", 0)] + [("mv", h) for h in range(4)], writes=[("rstd", 0)])
        P.add("act", lambda e: e.activation(out=rstd, in_=rstd, func=AF.Sqrt), reads=[("rstd", 0)], writes=[("rstd", 0)])
        P.add("dve", lambda e: e.reciprocal(out=rstd, in_=rstd), reads=[("rstd", 0)], writes=[("rstd", 0)])
        hnc = hn[c % 2]
        hnk = "hn%d" % (c % 2)
        for h in range(4):
            src = pn[h // 2][0][:, (h % 2) * CW:(h % 2) * CW + 128]
            P.add("dve", (lambda e, src=src, h=h, hnc=hnc: e.tensor_scalar(
                out=hnc[:, h, :], in0=src, scalar1=mv[:, h, 0:1], scalar2=rstd[:, h:h + 1], op0=ALU.subtract, op1=ALU.mult)),
                reads=[pn[h // 2][1], ("mv", h), ("rstd", 0)], writes=[(hnk, h)])
        pt, pkt = next_ps()
        ptb = pt.bitcast(BF16)

        def htr(e, ptb=ptb, hnc=hnc):
            ins = None
            for h in range(4):
                ins = e.transpose(out=ptb[:, h * 128:(h + 1) * 128], in_=hnc[:, h, :], identity=ident_b)
            return ins
        P.add("pe", htr, reads=[("ident_b", 0)] + [(hnk, h) for h in range(4)], writes=[pkt])
        P.add("dve", (lambda e, ptb=ptb, c=c, nb=nb, cs=cs: e.tensor_tensor(
            out=hmT[:, :, cs], in0=ptb[:, 0:512].rearrange("p (a b) -> p a b", b=128),
            in1=sgo[nb % 2][:, :, (c % 4) * 128:(c % 4 + 1) * 128], op=ALU.mult)),
            reads=[pkt] + [("sgo%d" % (nb % 2), h) for h in range(4)], writes=[("hmT", c)])

    chunk_A(0)
    for c in range(NT):
        if c + 1 < NT:
            chunk_A(c + 1)
        chunk_B(c)
        chunk_C(c)
    dbg_out("hmT", hmT, [("hmT", c) for c in range(NT)])
    for nm in ("qkc", "wif", "wif_f", "nl_hi", "nl_lo", "G", "nlf", "gtmp", "A_inv", "Bv", "dec", "A_", "ktok", "vB", "E", "Cb0", "Cb1", "sm0", "sm1",
               "hn0", "hn1", "st6", "mv", "den", "qq", "rstd", "sgo0", "sgo1"):
        A.free(nm)

    wmo = load_cast("wmo", wmo_d.rearrange("(c p) n -> p c n", p=128), [4, D])
    for h in range(4):
        P.add("dve", (lambda e, h=h: e.tensor_scalar(out=wmo[:, h, :], in0=wmo[:, h, :], scalar1=mng[:, h:h + 1], scalar2=None,
                                                     op0=ALU.mult)),
              reads=[("wmo", 0), ("wmo", 1 + h), ("mng", 0)], writes=[("wmo", 1 + h)])
    mtmp = [A.alloc("mtmp%d" % i, [512], BF16) for i in range(2)]
    for jb in range(2):
        wg_, wgk = load_wblock(4104 + jb * 512)
        for jj in range(4):
            j = jb * 4 + jj
            for nb in range(NB):
                psy, pky = next_ps()

                def ymm2(e, psy=psy, j=j, nb=nb):
                    ins = None
                    for h in range(4):
                        ins = e.matmul(psy[:, :], lhsT=wmo[:, h, j * 128:(j + 1) * 128], rhs=hmT[:, h, nb * 512:(nb + 1) * 512],
                                       start=(h == 0), stop=(h == 3))
                    return ins
                P.add("pe", ymm2, reads=[("wmo", 1 + h) for h in range(4)] + [("hmT", c) for c in range(nb * 4, nb * 4 + 4)],
                      writes=[pky])
                psg2, pkg2 = proj_fm(wg_, wgk, jj, nb)
                sg, sgk = next_sgt()
                P.add("act", (lambda e, sg=sg, p=psg2: e.activation(out=sg, in_=p[:, :], func=AF.Sigmoid)),
                      reads=[pkg2], writes=[sgk])
                mt = mtmp[(j * NB + nb) % 2]
                mtk = ("mtmp%d" % ((j * NB + nb) % 2), 0)
                P.add("dve", (lambda e, sg=sg, p=psy, mt=mt: e.tensor_tensor(out=mt, in0=p[:, :], in1=sg, op=ALU.mult)),
                      reads=[pky, sgk], writes=[mtk])
                P.add("dve", (lambda e, mt=mt, j=j, nb=nb: e.tensor_tensor(
                    out=merged[:, j, nb * 512:(nb + 1) * 512], in0=merged[:, j, nb * 512:(nb + 1) * 512], in1=mt, op=ALU.add)),
                    reads=[mtk, ("merged", j, nb)], writes=[("merged", j, nb)])
    dbg_out("merged", merged, [("merged", j, nb) for j in range(8) for nb in range(NB)])
    for nm in ("hmT", "wmo", "mtmp0", "mtmp1", "sgt0", "sgt1", "hT", "wblk0", "wblk1", "wblk2"):
        A.free(nm)
    if stage <= 3:
        return finish()

    dgf = A.alloc("dgf", [128], F32)
    dgh = A.alloc("dgh", [128], BF16)
    dgl = A.alloc("dgl", [128], BF16)

    def row_bcast(name, col0, src=None, srckey=("modT", 1)):
        src = modT if src is None else src
        row = A.alloc(name, [D], F32)
        banks = [next_ps(), next_ps()]
        for j in range(8):
            P.add("dve", (lambda e, j=j: e.tensor_scalar(out=dgf, in0=ident_f, scalar1=src[:, col0 + j:col0 + j + 1], scalar2=None,
                                                         op0=ALU.mult)),
                  reads=[("ident_f", 0), srckey], writes=[("dgf", 0)])
            P.add("dve", lambda e: e.tensor_copy(out=dgh, in_=dgf), reads=[("dgf", 0)], writes=[("dgh", 0)])
            P.add("dve", lambda e: e.tensor_tensor(out=dgl, in0=dgf, in1=dgh, op=ALU.subtract),
                  reads=[("dgf", 0), ("dgh", 0)], writes=[("dgl", 0)])
            bk, bkk = banks[j // 4]

            def bmm(e, bk=bk, j=j):
                o = bk[:, (j % 4) * 128:(j % 4 + 1) * 128]
                e.matmul(o, lhsT=ones_b, rhs=dgh, start=True, stop=False)
                return e.matmul(o, lhsT=ones_b, rhs=dgl, start=False, stop=True)
            P.add("pe", bmm, reads=[("ones_b", 0), ("dgh", 0), ("dgl", 0)], writes=[bkk])
        for b2 in range(2):
            bk, bkk = banks[b2]
            P.add("act", (lambda e, bk=bk, b2=b2: e.activation(out=row[:, b2 * 512:(b2 + 1) * 512], in_=bk[:, :], func=AF.Identity)),
                  reads=[bkk], writes=[(name, b2)])
        return row

    gt1row = row_bcast("gt1row", 16)
    a2row = row_bcast("a2row", 0, src=a2, srckey=("a2", 0))
    sh2row = row_bcast("sh2row", 24)
    gt2row = row_bcast("gt2row", 40)
    wout = load_cast("wout", wout_d.rearrange("(c p) n -> p c n", p=128), [8, D])
    for k in range(8):
        P.add("dve", (lambda e, k=k: e.tensor_tensor(out=wout[:, k, :], in0=wout[:, k, :], in1=gt1row, op=ALU.mult)),
              reads=[("wout", 0), ("wout", 1 + k), ("gt1row", 0), ("gt1row", 1)], writes=[("wout", 1 + k)])
    x1 = A.alloc("x1", [NT, D], F32)
    for ti in range(NT):
        P.add("sp", (lambda e, ti=ti: e.dma_start(out=x1[:, ti, :], in_=x_d[ti * 128:(ti + 1) * 128, :])),
              writes=[("x1", ti)], dma=True, grp="x1ld%d" % ti)
    wr_f = A.alloc("wr_f", [8, 36], F32)
    wr_b = A.alloc("wr_b", [8, 36], BF16)
    with nc.allow_non_contiguous_dma(reason="small router weight rows"):
        P.add("sp", lambda e: e.dma_start(out=wr_f, in_=wr_d.rearrange("(c p) n -> p c n", p=128)), writes=[("wr_f", 0)],
              dma=True, grp="c_wr")
    P.add("dve", lambda e: e.tensor_copy(out=wr_b, in_=wr_f), reads=[("wr_f", 0)], writes=[("wr_b", 0)])
    brt = load_const("brt", br_d, [36])
    h2tok = A.alloc("h2tok", [NT, D], BF16)
    xn2 = [A.alloc("xn2_%d" % i, [D], F32) for i in range(2)]
    h2T = [A.alloc("h2T%d" % i, [8, 128], BF16) for i in range(2)]
    ss2 = A.alloc("ss2", [NT], F32)
    rs2 = A.alloc("rs2", [NT], F32)
    psr = [hold_ps(), hold_ps()]
    def emit_p5(ti):
        s2 = ti % 2
        P.add("act", (lambda e, ti=ti: e.activation(out=junk, in_=x1[:, ti, :], func=AF.Square, accum_out=ss2[:, ti:ti + 1])),
              reads=[("x1", ti)], writes=[("junk", 0), ("ss2", ti)])
        P.add("act", (lambda e, ti=ti: e.activation(out=rs2[:, ti:ti + 1], in_=ss2[:, ti:ti + 1], func=AF.Sqrt, scale=1.0 / D, bias=1e-6)),
              reads=[("ss2", ti)], writes=[("rs2", ti)])
        P.add("dve", (lambda e, ti=ti: e.reciprocal(out=rs2[:, ti:ti + 1], in_=rs2[:, ti:ti + 1])),
              reads=[("rs2", ti)], writes=[("rs2", ti)])
        P.add("act", (lambda e, ti=ti, s2=s2: e.activation(out=xn2[s2], in_=x1[:, ti, :], func=AF.Identity, scale=rs2[:, ti:ti + 1])),
              reads=[("x1", ti), ("rs2", ti)], writes=[("xn2_%d" % s2, 0)])
        P.add("dve", (lambda e, s2=s2: e.tensor_tensor(out=xn2[s2], in0=xn2[s2], in1=a2row, op=ALU.mult)),
              reads=[("xn2_%d" % s2, 0), ("a2row", 0), ("a2row", 1)], writes=[("xn2_%d" % s2, 0)])
        P.add("dve", (lambda e, s2=s2, ti=ti: e.tensor_tensor(out=h2tok[:, ti, :], in0=xn2[s2], in1=sh2row, op=ALU.add)),
              reads=[("xn2_%d" % s2, 0), ("sh2row", 0), ("sh2row", 1)], writes=[("h2tok", ti)])
        pt, pkt = next_ps()
        ptb = pt.bitcast(BF16)

        def h2tr(e, ptb=ptb, ti=ti):
            ins = None
            for c in range(8):
                ins = e.transpose(out=ptb[:, c * 128:(c + 1) * 128], in_=h2tok[:, ti, c * 128:(c + 1) * 128], identity=ident_b)
            return ins
        P.add("pe", h2tr, reads=[("ident_b", 0), ("h2tok", ti)], writes=[pkt])
        P.add("act", (lambda e, ptb=ptb, s2=s2: e.activation(out=h2T[s2].rearrange("p a b -> p (a b)"), in_=ptb[:, 0:1024], func=AF.Identity)),
              reads=[pkt], writes=[("h2T%d" % s2, 0)])
        bk, bkk = psr[ti // 8]

        def rmm(e, bk=bk, ti=ti, s2=s2):
            ins = None
            o = bk[:, (ti % 8) * 36:(ti % 8 + 1) * 36]
            for k in range(8):
                ins = e.matmul(o, lhsT=h2T[s2][:, k, :], rhs=wr_b[:, k, :], start=(k == 0), stop=(k == 7))
            return ins
        P.add("pe", rmm, reads=[("h2T%d" % s2, 0), ("wr_b", 0)], writes=[bkk])

    def emit_p4(ti):
        for half in range(2):
            ps, pk = next_ps()

            def omm(e, ps=ps, ti=ti, half=half):
                ins = None
                for k in range(8):
                    ins = e.matmul(ps[:, :], lhsT=merged[:, k, ti * 128:(ti + 1) * 128], rhs=wout[:, k, half * 512:(half + 1) * 512],
                                   start=(k == 0), stop=(k == 7))
                return ins
            P.add("pe", omm, reads=[("wout", 1 + k) for k in range(8)] + [("merged", k, ti // 4) for k in range(8)], writes=[pk])
            P.add("dve", (lambda e, ps=ps, ti=ti, half=half: e.tensor_tensor(
                out=x1[:, ti, half * 512:(half + 1) * 512], in0=x1[:, ti, half * 512:(half + 1) * 512], in1=ps[:, :], op=ALU.add)),
                reads=[pk, ("x1", ti)], writes=[("x1", ti)])

    emit_p4(0)
    for ti in range(NT):
        if ti + 1 < NT:
            emit_p4(ti + 1)
        emit_p5(ti)
    dbg_out("x1", x1, [("x1", ti) for ti in range(NT)])
    A.free("merged"); A.free("wout"); A.free("gt1row")

    Lg = A.alloc("Lg", [NT, 36], F32)
    for b2 in range(2):
        bk, bkk = psr[b2]
        P.add("dve", (lambda e, bk=bk, b2=b2: e.tensor_tensor(
            out=Lg[:, b2 * 8:(b2 + 1) * 8, :], in0=bk[:, 0:288].rearrange("p (a b) -> p a b", b=36),
            in1=brt.unsqueeze(1).to_broadcast([128, 8, 36]), op=ALU.add)),
            reads=[bkk, ("brt", 0)], writes=[("Lg", b2)])
    release_ps(psr[0][1]); release_ps(psr[1][1])
    dbg_out("Lg", Lg, [("Lg", 0), ("Lg", 1)])
    dbg_out("h2tok", h2tok, [("h2tok", ti) for ti in range(NT)])
    for nm in ("xn2_0", "xn2_1", "h2T0", "h2T1", "a2row", "sh2row", "wr_f", "wr_b"):
        A.free(nm)
    if stage <= 5:
        return finish()

    NCHK0 = NG - 15
    def T_(name, shape, dt=F32):
        return A.alloc(name, shape, dt)
    LK = [("Lg", 0), ("Lg", 1)]
    lg = Lg[:, :, 0:4]
    le = Lg[:, :, 4:36]
    gmax = T_("gmax", [NT])
    G1h = T_("G1h", [NT, 4])
    egs = T_("egs", [NT, 4])
    p_g = T_("p_g", [NT])
    P.add("dve", lambda e: e.tensor_reduce(out=gmax, in_=lg, axis=AX.X, op=ALU.max), reads=LK, writes=[("gmax", 0)])
    gmb = gmax.unsqueeze(2).to_broadcast([128, NT, 4])
    P.add("dve", lambda e: e.tensor_tensor(out=G1h, in0=lg, in1=gmb, op=ALU.is_equal), reads=LK + [("gmax", 0)], writes=[("G1h", 0)])
    P.add("dve", lambda e: e.tensor_tensor(out=egs, in0=lg, in1=gmb, op=ALU.subtract), reads=LK + [("gmax", 0)], writes=[("egs", 0)])
    P.add("act", lambda e: e.activation(out=egs, in_=egs, func=AF.Exp), reads=[("egs", 0)], writes=[("egs", 0)])
    P.add("dve", lambda e: e.tensor_reduce(out=p_g, in_=egs, axis=AX.X, op=ALU.add), reads=[("egs", 0)], writes=[("p_g", 0)])
    P.add("dve", lambda e: e.reciprocal(out=p_g, in_=p_g), reads=[("p_g", 0)], writes=[("p_g", 0)])
    tmp32 = T_("tmp32", [NT, 32])
    lsel = T_("lsel", [NT, 8])
    P.add("dve", lambda e: e.tensor_tensor(
        out=tmp32.rearrange("p t (g j) -> p t g j", j=8), in0=le.rearrange("p t (g j) -> p t g j", j=8),
        in1=G1h.unsqueeze(3).to_broadcast([128, NT, 4, 8]), op=ALU.mult),
        reads=LK + [("G1h", 0)], writes=[("tmp32", 0)])
    P.add("dve", lambda e: e.tensor_reduce(out=lsel, in_=tmp32.rearrange("p t (g j) -> p t j g", j=8), axis=AX.X, op=ALU.add),
          reads=[("tmp32", 0)], writes=[("lsel", 0)])
    m1 = T_("m1", [NT])
    m2 = T_("m2", [NT])
    E1 = T_("E1", [NT, 8])
    E2 = T_("E2", [NT, 8])
    ls2 = T_("ls2", [NT, 8])
    P.add("dve", lambda e: e.tensor_reduce(out=m1, in_=lsel, axis=AX.X, op=ALU.max), reads=[("lsel", 0)], writes=[("m1", 0)])
    P.add("dve", lambda e: e.tensor_tensor(out=E1, in0=lsel, in1=m1.unsqueeze(2).to_broadcast([128, NT, 8]), op=ALU.is_equal),
          reads=[("lsel", 0), ("m1", 0)], writes=[("E1", 0)])
    P.add("dve", lambda e: e.scalar_tensor_tensor(out=ls2.rearrange("p a b -> p (a b)"), in0=E1.rearrange("p a b -> p (a b)"),
                                                  scalar=-1e30, in1=lsel.rearrange("p a b -> p (a b)"), op0=ALU.mult, op1=ALU.add),
          reads=[("E1", 0), ("lsel", 0)], writes=[("ls2", 0)])
    P.add("dve", lambda e: e.tensor_reduce(out=m2, in_=ls2, axis=AX.X, op=ALU.max), reads=[("ls2", 0)], writes=[("m2", 0)])
    P.add("dve", lambda e: e.tensor_tensor(out=E2, in0=ls2, in1=m2.unsqueeze(2).to_broadcast([128, NT, 8]), op=ALU.is_equal),
          reads=[("ls2", 0), ("m2", 0)], writes=[("E2", 0)])
    w1 = T_("w1", [NT])
    w2 = T_("w2", [NT])
    P.add("dve", lambda e: e.tensor_tensor(out=w2, in0=m1, in1=m2, op=ALU.subtract), reads=[("m1", 0), ("m2", 0)], writes=[("w2", 0)])
    P.add("act", lambda e: e.activation(out=w1, in_=w2, func=AF.Sigmoid), reads=[("w2", 0)], writes=[("w1", 0)])
    P.add("dve", lambda e: e.tensor_tensor(out=w1, in0=w1, in1=p_g, op=ALU.mult), reads=[("w1", 0), ("p_g", 0)], writes=[("w1", 0)])
    P.add("dve", lambda e: e.tensor_tensor(out=w2, in0=p_g, in1=w1, op=ALU.subtract), reads=[("w1", 0), ("p_g", 0), ("w2", 0)], writes=[("w2", 0)])
    A1 = T_("A1", [NT, 32])
    A2 = T_("A2", [NT, 32])
    A12b = T_("A12b", [NT, 32], BF16)
    for g in range(4):
        gb = G1h[:, :, g].unsqueeze(2).to_broadcast([128, NT, 8])
        P.add("dve", (lambda e, g=g, gb=gb: e.tensor_tensor(out=A1[:, :, g * 8:(g + 1) * 8], in0=E1, in1=gb, op=ALU.mult)),
              reads=[("E1", 0), ("G1h", 0)], writes=[("A1", g)])
        P.add("dve", (lambda e, g=g, gb=gb: e.tensor_tensor(out=A2[:, :, g * 8:(g + 1) * 8], in0=E2, in1=gb, op=ALU.mult)),
              reads=[("E2", 0), ("G1h", 0)], writes=[("A2", g)])
    AK = [("A1", g) for g in range(4)] + [("A2", g) for g in range(4)]
    P.add("dve", lambda e: e.tensor_tensor(out=A12b, in0=A1, in1=A2, op=ALU.add), reads=AK, writes=[("A12b", 0)])
    lstrict = T_("lstrict", [128], BF16)
    lsf = T_("lsf", [128], F32)
    P.add("pool", lambda e: e.memset(lsf, 1.0), writes=[("lsf", 0)])
    P.add("pool", lambda e: e.affine_select(out=lsf, in_=lsf, pattern=[[1, 128]], compare_op=ALU.is_ge, fill=0.0, base=-1,
                                             channel_multiplier=-1), reads=[("lsf", 0)], writes=[("lsf", 0)])
    P.add("pool", lambda e: e.tensor_copy(out=lstrict, in_=lsf), reads=[("lsf", 0)], writes=[("lstrict", 0)])
    psw, pkw = next_ps()
    pst_, pkt_ = next_ps()
    A12f = A12b.rearrange("p a b -> p (a b)")
    P.add("pe", lambda e: e.matmul(psw[:, :], lhsT=lstrict, rhs=A12f, start=True, stop=True),
          reads=[("lstrict", 0), ("A12b", 0)], writes=[pkw])
    P.add("pe", lambda e: e.matmul(pst_[:, :], lhsT=ones_b, rhs=A12f, start=True, stop=True),
          reads=[("ones_b", 0), ("A12b", 0)], writes=[pkt_])
    carry = T_("carry", [NT + 1, 32])
    P.add("dve", lambda e: e.memset(carry[:, 0, :], 0.0), writes=[("carry", 0)])
    for ti in range(NT):
        P.add("dve", (lambda e, ti=ti: e.tensor_tensor(out=carry[:, ti + 1, :], in0=carry[:, ti, :], in1=pst_[:, ti * 32:(ti + 1) * 32],
                                                      op=ALU.add)),
              reads=[("carry", ti), pkt_], writes=[("carry", ti + 1)])
    counts = carry[:, NT, :]
    CK = [("carry", ti) for ti in range(NT + 1)]
    thr = T_("thr", [64])
    thr_i = T_("thr_i", [64], I32)
    P.add("pool", lambda e: e.iota(thr_i, pattern=[[1, 64]], base=0, channel_multiplier=0), writes=[("thr_i", 0)])
    P.add("pool", lambda e: e.tensor_copy(out=thr, in_=thr_i), reads=[("thr_i", 0)], writes=[("thr", 0)])
    thr128 = T_("thr128", [8])
    P.add("pool", lambda e: e.tensor_scalar(out=thr128, in0=thr[:, 0:8], scalar1=float(GROWS), scalar2=None, op0=ALU.mult),
          reads=[("thr", 0)], writes=[("thr128", 0)])
    cmp1 = T_("cmp1", [32, 8])
    ngrp = T_("ngrp", [32])
    P.add("dve", lambda e: e.tensor_tensor(out=cmp1, in0=counts.unsqueeze(2).to_broadcast([128, 32, 8]),
                                           in1=thr128.unsqueeze(1).to_broadcast([128, 32, 8]), op=ALU.is_gt),
          reads=CK + [("thr128", 0)], writes=[("cmp1", 0)])
    P.add("dve", lambda e: e.tensor_reduce(out=ngrp, in_=cmp1, axis=AX.X, op=ALU.add), reads=[("cmp1", 0)], writes=[("ngrp", 0)])
    cs = [T_("cs0", [32]), T_("cs1", [32])]
    src, srck = ngrp, ("ngrp", 0)
    for si, sh in enumerate((1, 2, 4, 8, 16)):
        dst = cs[si % 2]
        dk = ("cs%d" % (si % 2),)
        P.add("dve", (lambda e, dst=dst, src=src, sh=sh: e.tensor_copy(out=dst[:, 0:sh], in_=src[:, 0:sh])),
              reads=[srck], writes=[dk + (0,)])
        P.add("dve", (lambda e, dst=dst, src=src, sh=sh: e.tensor_tensor(out=dst[:, sh:32], in0=src[:, sh:32], in1=src[:, 0:32 - sh],
                                                                        op=ALU.add)),
              reads=[srck], writes=[dk + (1,)])
        src, srck = dst, dk + (1,)
        if si > 0:
            pass
    pend = src
    PK = [("cs0", 0), ("cs0", 1), ("cs1", 0), ("cs1", 1)]
    pstart = T_("pstart", [32])
    P.add("dve", lambda e: e.tensor_tensor(out=pstart, in0=pend, in1=ngrp, op=ALU.subtract), reads=PK + [("ngrp", 0)],
          writes=[("pstart", 0)])
    P.add("dve", lambda e: e.tensor_scalar(out=pstart, in0=pstart, scalar1=float(GROWS), scalar2=None, op0=ALU.mult),
          reads=[("pstart", 0)], writes=[("pstart", 0)])
    cmp2 = T_("cmp2", [NG, 32])
    grpf = T_("grpf", [NG])
    grpi = T_("grpi", [NG], I32)
    P.add("dve", lambda e: e.tensor_tensor(out=cmp2, in0=pend.unsqueeze(1).to_broadcast([128, NG, 32]),
                                           in1=thr[:, 0:NG].unsqueeze(2).to_broadcast([128, NG, 32]), op=ALU.is_le),
          reads=PK + [("thr", 0)], writes=[("cmp2", 0)])
    P.add("dve", lambda e: e.tensor_reduce(out=grpf, in_=cmp2, axis=AX.X, op=ALU.add), reads=[("cmp2", 0)], writes=[("grpf", 0)])
    P.add("dve", lambda e: e.tensor_scalar(out=grpf, in0=grpf, scalar1=31.0, scalar2=None, op0=ALU.min),
          reads=[("grpf", 0)], writes=[("grpf", 0)])
    P.add("dve", lambda e: e.tensor_copy(out=grpi, in_=grpf), reads=[("grpf", 0)], writes=[("grpi", 0)])
    pidx_i = T_("pidx_i", [1], I32)
    pidx = T_("pidx", [1])
    idxf = T_("idxf", [NG])
    inval = T_("inval", [NG])
    idxw = T_("idxw", [NG], I32)
    idxs = T_("idxs", [NG], I32)
    P.add("pool", lambda e: e.iota(pidx_i, pattern=[[0, 1]], base=0, channel_multiplier=1), writes=[("pidx_i", 0)])
    P.add("pool", lambda e: e.tensor_copy(out=pidx, in_=pidx_i), reads=[("pidx_i", 0)], writes=[("pidx", 0)])
    P.add("dve", lambda e: e.tensor_scalar(out=idxf, in0=grpf, scalar1=128.0, scalar2=pidx[:, 0:1], op0=ALU.mult, op1=ALU.add),
          reads=[("grpf", 0), ("pidx", 0)], writes=[("idxf", 0)])
    P.add("dve", lambda e: e.tensor_scalar(out=inval, in0=thr[:, 0:NG], scalar1=pend[:, 31:32], scalar2=None, op0=ALU.is_ge),
          reads=PK + [("thr", 0)], writes=[("inval", 0)])
    P.add("dve", lambda e: e.tensor_copy(out=idxw, in_=idxf), reads=[("idxf", 0)], writes=[("idxw", 0)])
    P.add("dve", lambda e: e.scalar_tensor_tensor(out=idxf, in0=inval, scalar=1.0e6, in1=idxf, op0=ALU.mult, op1=ALU.add),
          reads=[("inval", 0), ("idxf", 0), ("idxw", 0)], writes=[("idxf", 0)])
    P.add("dve", lambda e: e.tensor_copy(out=idxs, in_=idxf), reads=[("idxf", 0)], writes=[("idxs", 0)])
    stg = {wn: A.alloc("stg_" + wn, [4096], F32) for wn in ("wg", "wu", "wd")}
    for (wsrc_, wn_) in ((weg_d, "wg"), (weu_d, "wu"), (wed_d, "wd")):
        P.add("pool", (lambda e, wsrc_=wsrc_, wn_=wn_: e.indirect_dma_start(
            out=stg[wn_], out_offset=None, in_=wsrc_, in_offset=bass.IndirectOffsetOnAxis(ap=idxw[:, 0:1], axis=0))),
            reads=[("idxw", 0)], writes=[("stg_" + wn_, 0)], dma=True, grp="stg_" + wn_)
    slot = T_("slot", [NT, 32])
    P.add("dve", lambda e: e.tensor_tensor(out=slot, in0=psw[:, :].rearrange("p (a b) -> p a b", b=32), in1=carry[:, 0:NT, :], op=ALU.add),
          reads=[pkw] + CK, writes=[("slot", 0)])
    P.add("dve", lambda e: e.tensor_tensor(out=slot, in0=slot, in1=pstart.unsqueeze(1).to_broadcast([128, NT, 32]), op=ALU.add),
          reads=[("slot", 0), ("pstart", 0)], writes=[("slot", 0)])
    dstf = T_("dstf", [2, NT])
    dsti = T_("dsti", [2, NT], I32)
    for q, (Aq, qk) in enumerate(((A1, "A1"), (A2, "A2"))):
        P.add("dve", (lambda e, Aq=Aq: e.tensor_tensor(out=tmp32, in0=Aq, in1=slot, op=ALU.mult)),
              reads=[(qk, g) for g in range(4)] + [("slot", 0), ("tmp32", 0)], writes=[("tmp32", 0)])
        P.add("dve", (lambda e, q=q: e.tensor_reduce(out=dstf[:, q, :], in_=tmp32, axis=AX.X, op=ALU.add)),
              reads=[("tmp32", 0)], writes=[("dstf", q)])
    P.add("dve", lambda e: e.tensor_copy(out=dsti, in_=dstf), reads=[("dstf", 0), ("dstf", 1)], writes=[("dsti", 0)])
    dbg_out("dstf", dstf, [("dstf", 0), ("dstf", 1)])
    dbg_out("grpf", grpf, [("grpf", 0)])
    dbg_out("w1", w1, [("w1", 0)])
    dbg_out("w2", w2, [("w2", 0)])
    for nm in ("gmax", "G1h", "egs", "p_g", "tmp32", "lsel", "m1", "m2", "E1", "E2", "ls2", "A1", "A2", "A12b", "lstrict", "lsf",
               "carry", "thr", "thr_i", "thr128", "cmp1", "ngrp", "cs0", "cs1", "pstart", "slot", "cmp2", "Lg"):
        A.free(nm)
    if stage <= 5.5:
        return finish()

    for ti in range(NT):
        for q in range(2):
            P.add("pool", (lambda e, ti=ti, q=q: e.indirect_dma_start(
                out=XS, out_offset=bass.IndirectOffsetOnAxis(ap=dsti[:, q, ti:ti + 1], axis=0),
                in_=h2tok[:, ti, :], in_offset=None)),
                reads=[("h2tok", ti), ("dsti", 0)] + XSZ_KEYS, writes=[("XS", ti, q)], dma=True, grp="xs_sc")
    XS_KEYS = [("XS", ti, q) for ti in range(NT) for q in range(2)]
    A.free("h2tok")
    NS = 2
    wgs = [A.alloc("wg%d" % i, [8, 512], BF16) for i in range(NS)]
    wus = [A.alloc("wu%d" % i, [8, 512], BF16) for i in range(NS)]
    wds = [A.alloc("wd%d" % i, [4, D], BF16) for i in range(NS)]
    xgt = [A.alloc("xgt%d" % i, [D], BF16) for i in range(2)]
    xgT = [A.alloc("xgT%d" % i, [8, 256], BF16) for i in range(2)]
    sgl = [A.alloc("sgl%d" % i, [4, 256], BF16) for i in range(1)]
    aT = [A.alloc("aT%d" % i, [4, 256], BF16) for i in range(2)]
    ysb = [A.alloc("ysb%d" % i, [D], BF16) for i in range(2)]
    def emit_load(g, part="both"):
        sl = g % NS
        s2 = g % 2
        for (wt, wsrc, wn, ceng) in ((wgs, weg_d, "wg", "act"), (wus, weu_d, "wu", "dve"), (wds, wed_d, "wd", "pool")):
            st_ = stg[wn]
            if part in ("both", "dma") and g >= NCHK0:
                P.add("pool", (lambda e, g=g, st_=st_, wsrc=wsrc: e.indirect_dma_start(
                    out=st_, out_offset=None, in_=wsrc,
                    in_offset=bass.IndirectOffsetOnAxis(ap=idxs[:, g:g + 1], axis=0), bounds_check=32 * 128 - 1, oob_is_err=False)),
                    reads=[("idxs", 0)], writes=[("stg_" + wn, 0)], dma=True, grp="stg_" + wn)
            elif part in ("both", "dma"):
                P.add("pool", (lambda e, g=g, st_=st_, wsrc=wsrc: e.indirect_dma_start(
                    out=st_, out_offset=None, in_=wsrc,
                    in_offset=bass.IndirectOffsetOnAxis(ap=idxw[:, g:g + 1], axis=0))),
                    reads=[("idxw", 0)], writes=[("stg_" + wn, 0)], dma=True, grp="stg_" + wn)
            if part == "dma":
                continue
            dstv = wt[sl].rearrange("p a b -> p (a b)")
            if ceng == "act":
                P.add("act", (lambda e, dstv=dstv, st_=st_: e.activation(out=dstv, in_=st_, func=AF.Identity)),
                      reads=[("stg_" + wn, 0)], writes=[("%s%d" % (wn, sl), 0)])
            elif ceng == "dve":
                P.add("dve", (lambda e, dstv=dstv, st_=st_: e.tensor_copy(out=dstv, in_=st_)),
                      reads=[("stg_" + wn, 0)], writes=[("%s%d" % (wn, sl), 0)])
            else:
                P.add("act", (lambda e, dstv=dstv, st_=st_: e.activation(out=dstv[:, 0:2048], in_=st_[:, 0:2048], func=AF.Identity)),
                      reads=[("stg_" + wn, 0)], writes=[("%s%d" % (wn, sl), 0)])
                P.add("dve", (lambda e, dstv=dstv, st_=st_: e.tensor_copy(out=dstv[:, 2048:4096], in_=st_[:, 2048:4096])),
                      reads=[("stg_" + wn, 0)], writes=[("%s%d" % (wn, sl), 1)])

    def emit_compute_a(g):
        s2 = g % 2
        for hf in range(2):
            xi = (2 * g + hf) % 2
            r0 = g * GROWS + hf * 128
            P.add("sp", (lambda e, r0=r0, xi=xi: e.dma_start(out=xgt[xi], in_=XS[r0:r0 + 128, :])),
                  reads=XS_KEYS, writes=[("xgt%d" % xi, 0)], dma=True, grp="xgt%d" % xi)
            pt, pkt = next_ps()
            ptb = pt.bitcast(BF16)

            def xtr(e, ptb=ptb, xi=xi):
                ins = None
                for c in range(8):
                    ins = e.transpose(out=ptb[:, c * 128:(c + 1) * 128], in_=xgt[xi][:, c * 128:(c + 1) * 128], identity=ident_b)
                return ins
            P.add("pe", xtr, reads=[("ident_b", 0), ("xgt%d" % xi, 0)], writes=[pkt])
            P.add("act", (lambda e, ptb=ptb, s2=s2, hf=hf: e.activation(
                out=xgT[s2][:, :, hf * 128:(hf + 1) * 128], in_=ptb[:, 0:1024].rearrange("p (a b) -> p a b", b=128), func=AF.Identity)),
                reads=[pkt], writes=[("xgT%d" % s2, hf)])

    def emit_compute_b1(g):
        sl = g % NS
        s2 = g % 2
        pg_ = [next_ps(), next_ps()]
        pu_ = [next_ps(), next_ps()]

        def gumm(e, pg_=pg_, pu_=pu_, sl=sl, s2=s2):
            ins = None
            for (pp, ww) in ((pg_, wgs), (pu_, wus)):
                for fc in range(4):
                    o = pp[fc // 2][0][:, (fc % 2) * 256:(fc % 2 + 1) * 256]
                    for k in range(8):
                        ins = e.matmul(o, lhsT=ww[sl][:, k, fc * 128:(fc + 1) * 128], rhs=xgT[s2][:, k, :], start=(k == 0), stop=(k == 7))
            return ins
        P.add("pe", gumm, reads=[("wg%d" % sl, 0), ("wu%d" % sl, 0), ("xgT%d" % s2, 0), ("xgT%d" % s2, 1)],
              writes=[pg_[0][1], pg_[1][1], pu_[0][1], pu_[1][1]])
        for b2 in range(2):
            P.add("act", (lambda e, pg_=pg_, s2=s2, b2=b2: e.activation(
                out=sgl[0][:, 2 * b2:2 * b2 + 2, :].rearrange("p a b -> p (a b)"), in_=pg_[b2][0][:, :], func=AF.Silu)),
                reads=[pg_[b2][1]], writes=[("sgl0", b2)])
            P.add("dve", (lambda e, pu_=pu_, s2=s2, b2=b2: e.tensor_tensor(
                out=aT[s2][:, 2 * b2:2 * b2 + 2, :].rearrange("p a b -> p (a b)"), in0=pu_[b2][0][:, :],
                in1=sgl[0][:, 2 * b2:2 * b2 + 2, :].rearrange("p a b -> p (a b)"), op=ALU.mult)),
                reads=[pu_[b2][1], ("sgl0", b2)], writes=[("aT%d" % s2, b2)])

    def emit_compute_b2(g):
        sl = g % NS
        s2 = g % 2
        for hf in range(2):
            yi = (2 * g + hf) % 2
            py = [next_ps(), next_ps()]

            def dmm(e, py=py, sl=sl, s2=s2, hf=hf):
                ins = None
                for half in range(2):
                    for fc in range(4):
                        ins = e.matmul(py[half][0][:, :], lhsT=aT[s2][:, fc, hf * 128:(hf + 1) * 128],
                                       rhs=wds[sl][:, fc, half * 512:(half + 1) * 512], start=(fc == 0), stop=(fc == 3))
                return ins
            P.add("pe", dmm, reads=[("wd%d" % sl, 0), ("wd%d" % sl, 1), ("aT%d" % s2, 0), ("aT%d" % s2, 1)], writes=[py[0][1], py[1][1]])
            for half in range(2):
                P.add("dve", (lambda e, py=py, yi=yi, half=half: e.tensor_tensor(
                    out=ysb[yi][:, half * 512:(half + 1) * 512], in0=py[half][0][:, :], in1=gt2row[:, half * 512:(half + 1) * 512],
                    op=ALU.mult)),
                    reads=[py[half][1], ("gt2row", half)], writes=[("ysb%d" % yi, half)])
            r0 = g * GROWS + hf * 128
            P.add("sp", (lambda e, r0=r0, yi=yi: e.dma_start(out=YS[r0:r0 + 128, :], in_=ysb[yi])),
                  reads=[("ysb%d" % yi, 0), ("ysb%d" % yi, 1)], writes=[("YS", g, hf)], dma=True, grp="ys_st")

    emit_load(0, "cast")
    emit_compute_a(0)
    for g in range(NG):
        emit_compute_b1(g)
        if g + 1 < NG:
            emit_load(g + 1)
            emit_compute_a(g + 1)
        emit_compute_b2(g)
    YS_KEYS = [("YS", g, hf) for g in range(NG) for hf in range(2)]
    for i in range(NS):
        A.free("wg%d" % i); A.free("wu%d" % i); A.free("wd%d" % i)
    for nm in ("stg_wg", "stg_wu", "stg_wd", "xgt0", "xgt1", "xgT0", "xgT1", "sgl0", "aT0", "aT1", "ysb0", "ysb1"):
        A.free(nm)

    gfin = load_const("gfin", gfin_d, [D])
    NYG = 4
    yg = [[A.alloc("yg%d_%d" % (q, i), [D], BF16) for i in range(NYG)] for q in range(2)]
    acc = [A.alloc("acc%d" % i, [D], F32) for i in range(2)]
    outt = [A.alloc("outt%d" % i, [D], F32) for i in range(2)]
    ssf = A.alloc("ssf", [NT], F32)
    rsf = A.alloc("rsf", [NT], F32)

    def emit_g(ti):
        s4 = ti % NYG
        for q in range(2):
            P.add("pool", (lambda e, ti=ti, q=q, s4=s4: e.indirect_dma_start(
                out=yg[q][s4], out_offset=None, in_=YS,
                in_offset=bass.IndirectOffsetOnAxis(ap=dsti[:, q, ti:ti + 1], axis=0))),
                reads=YS_KEYS + [("dsti", 0)], writes=[("yg%d_%d" % (q, s4), 0)], dma=True, grp="yg%d_%d" % (q, s4))

    def emit_c1(ti):
        s4 = ti % NYG
        s2 = ti % 2
        y1, y2 = yg[0][s4], yg[1][s4]
        k1, k2 = ("yg0_%d" % s4, 0), ("yg1_%d" % s4, 0)
        ac = acc[s2]
        ak = ("acc%d" % s2, 0)
        P.add("act", (lambda e, y1=y1, ac=ac, ti=ti: e.activation(out=ac, in_=y1, func=AF.Identity, scale=w1[:, ti:ti + 1])),
              reads=[k1, ("w1", 0)], writes=[ak])
        P.add("dve", (lambda e, ac=ac, y2=y2, ti=ti: e.scalar_tensor_tensor(out=ac, in0=y2, scalar=w2[:, ti:ti + 1], in1=ac,
                                                                          op0=ALU.mult, op1=ALU.add)),
              reads=[ak, k2, ("w2", 0)], writes=[ak])
        P.add("dve", (lambda e, ac=ac, ti=ti: e.tensor_tensor(out=x1[:, ti, :], in0=x1[:, ti, :], in1=ac, op=ALU.add)),
              reads=[ak, ("x1", ti)], writes=[("x1", ti)])

    def emit_c2(ti):
        s2 = ti % 2
        P.add("act", (lambda e, ti=ti: e.activation(out=junk, in_=x1[:, ti, :], func=AF.Square, accum_out=ssf[:, ti:ti + 1])),
              reads=[("x1", ti)], writes=[("junk", 0), ("ssf", ti)])
        P.add("act", (lambda e, ti=ti: e.activation(out=rsf[:, ti:ti + 1], in_=ssf[:, ti:ti + 1], func=AF.Sqrt, scale=1.0 / D, bias=1e-6)),
              reads=[("ssf", ti)], writes=[("rsf", ti)])
        P.add("dve", (lambda e, ti=ti: e.reciprocal(out=rsf[:, ti:ti + 1], in_=rsf[:, ti:ti + 1])),
              reads=[("rsf", ti)], writes=[("rsf", ti)])
        ot = outt[s2]
        ok = ("outt%d" % s2, 0)
        P.add("act", (lambda e, ot=ot, ti=ti: e.activation(out=ot, in_=x1[:, ti, :], func=AF.Identity, scale=rsf[:, ti:ti + 1])),
              reads=[("x1", ti), ("rsf", ti)], writes=[ok])
        P.add("dve", (lambda e, ot=ot: e.tensor_tensor(out=ot, in0=ot, in1=gfin, op=ALU.mult)),
              reads=[ok, ("gfin", 0)], writes=[ok])
        P.add("sp", (lambda e, ot=ot, ti=ti: e.dma_start(out=out_d[ti * 128:(ti + 1) * 128, :], in_=ot)),
              reads=[ok], dma=True, grp="out")

    for ti in range(min(3, NT)):
        emit_g(ti)
    for ti in range(NT):
        if ti + 3 < NT:
            emit_g(ti + 3)
        emit_c1(ti)
        if ti > 0:
            emit_c2(ti - 1)
    emit_c2(NT - 1)
    P.emit(final_wait_groups=["out"] + (["dbgout"] if "dbgout" in P.dma_groups else []))
    build.stats = dict(peak_kb=A.peak * 4 / 1024.0, n_ops=len(P.all_ops), n_groups=len(P.dma_groups))
    return nc, dbg_outs


def host_layout(inp, b):
    f = lambda a: np.ascontiguousarray(a, dtype=np.float32)
    col = lambda v, n: f(np.asarray(v).reshape(n, 128).T)
    m = {}
    m["x"] = f(inp["x"][b])
    m["c_col"] = col(inp["c"][b], 8)
    m["w_ada"] = f(inp["w_ada"][0])
    m["b_ada_col"] = col(inp["b_ada"][0], 48)
    m["g1_col"] = col(inp["g_norm1"][0], 8)
    m["g2_col"] = col(inp["g_norm2"][0], 8)
    m["w_in"] = f(inp["w_in"][0])
    m["b_if_bc"] = f(np.broadcast_to(inp["b_if"][0][None, :], (128, 8)))
    m["conv_w_col"] = f(inp["conv_dw_w"][0].reshape(31, 4, 128).transpose(2, 1, 0))
    m["conv_b_col"] = col(inp["conv_dw_b"][0], 4)
    m["conv_lng_col"] = col(inp["conv_ln_g"][0], 4)
    m["conv_lnb_col"] = col(inp["conv_ln_b"][0], 4)
    m["w_conv_out"] = f(inp["w_conv_out"][0])
    m["qk_w_col"] = f(inp["qk_conv_w"][0].reshape(4, 8, 128).transpose(2, 1, 0))
    m["qk_b_col"] = col(inp["qk_conv_b"][0], 8)
    m["mng_col"] = col(inp["m_norm_g"][0], 4)
    m["w_m_out"] = f(inp["w_m_out"][0])
    m["w_out"] = f(inp["w_out"][0])
    m["w_router"] = f(np.concatenate([inp["w_rg"][0], inp["w_re"][0]], axis=1))
    m["b_router_bc"] = f(np.broadcast_to(np.concatenate([inp["b_rg"][0], inp["b_re"][0]])[None, :], (128, 36)))
    m["w_e_gate_l"] = f(inp["w_e_gate"][0].reshape(32, 8, 128, 512).transpose(0, 2, 1, 3).reshape(32 * 128, 8 * 512))
    m["w_e_up_l"] = f(inp["w_e_up"][0].reshape(32, 8, 128, 512).transpose(0, 2, 1, 3).reshape(32 * 128, 8 * 512))
    m["w_e_down_l"] = f(inp["w_e_down"][0].reshape(32, 4, 128, D).transpose(0, 2, 1, 3).reshape(32 * 128, 4 * D))
    m["g_final_bc"] = f(np.broadcast_to(np.asarray(inp["g_final"])[None, :], (128, D)))
    return m


def kernel(**inputs):
    nc, _ = build()
    shared = host_layout(inputs, 0)
    in_maps = []
    for b in range(8):
        m = dict(shared)
        m["x"] = np.ascontiguousarray(inputs["x"][b], dtype=np.float32)
        m["c_col"] = np.ascontiguousarray(np.asarray(inputs["c"][b]).reshape(8, 128).T, dtype=np.float32)
        in_maps.append(m)
    res = run_bass_kernel_spmd(nc, in_maps, core_ids=list(range(8)))
    return np.stack([np.asarray(r["out"]) for r in res.results], axis=0).astype(np.float32)
```

```python
import contextlib
import numpy as np
import concourse.bass as bass
import concourse.mybir as mybir
from concourse.bass_utils import run_bass_kernel_spmd

F32 = mybir.dt.float32
BF16 = mybir.dt.bfloat16
I32 = mybir.dt.int32
AF = mybir.ActivationFunctionType
ALU = mybir.AluOpType
AX = mybir.AxisListType

T = 2048
D = 1024
NT = 16
NB = 4
DIN = 5128
ENG_NAMES = ("pe", "act", "dve", "pool", "sp")


class Op:
    __slots__ = ("eng", "fn", "is_dma", "grp", "signal", "val", "idx", "deps")

    def __init__(self, eng, fn, is_dma, grp):
        self.eng = eng
        self.fn = fn
        self.is_dma = is_dma
        self.grp = grp
        self.signal = False
        self.val = None
        self.idx = None
        self.deps = []


def _reduce_ops(ops):
    latest = {}
    dm = {}
    for o in ops:
        if o.is_dma:
            if o.grp not in dm or dm[o.grp].idx < o.idx:
                dm[o.grp] = o
        else:
            if o.eng not in latest or latest[o.eng].idx < o.idx:
                latest[o.eng] = o
    return list(latest.values()) + list(dm.values())


class Prog:
    def __init__(self, nc):
        self.nc = nc
        self.ops = {e: [] for e in ENG_NAMES}
        self.all_ops = []
        self.last_writer = {}
        self.readers = {}
        self.dma_groups = {}
        self.buf_pred = {}
        self.keys_by_buf = {}
        self.wait_all_groups = set()

    def _touch(self, k):
        if k not in self.readers:
            self.readers[k] = list(self.buf_pred.get(k[0], ()))
            self.last_writer[k] = None
            self.keys_by_buf.setdefault(k[0], set()).add(k)

    def ops_touching(self, bufname):
        s = list(self.buf_pred.get(bufname, ()))
        for k in self.keys_by_buf.get(bufname, ()):
            w = self.last_writer.get(k)
            if w is not None:
                s.append(w)
            s.extend(self.readers.get(k, ()))
        return _reduce_ops(s)

    def add(self, eng, fn, reads=(), writes=(), dma=False, grp=None):
        op = Op(eng, fn, dma, grp)
        op.idx = len(self.all_ops)
        self.all_ops.append(op)
        self.ops[eng].append(op)
        if dma:
            assert grp is not None
            self.dma_groups.setdefault(grp, []).append(op)
        deps = []
        for k in reads:
            self._touch(k)
            w = self.last_writer[k]
            if w is not None:
                deps.append((w, "raw"))
            elif self.readers[k] and k[0] in self.buf_pred:
                pass
        for k in writes:
            self._touch(k)
            w = self.last_writer[k]
            if w is not None:
                deps.append((w, "waw"))
            for r in self.readers[k]:
                deps.append((r, "war"))
        for d, kind in deps:
            if d is op:
                continue
            if (not d.is_dma) and (not dma) and d.eng == eng:
                if eng == "pe":
                    continue
            op.deps.append(d)
        for k in reads:
            self.readers[k].append(op)
        for k in writes:
            self.last_writer[k] = op
            self.readers[k] = []
        return op

    def emit(self, final_wait_groups=()):
        nc = self.nc
        for op in self.all_ops:
            op.deps = _reduce_ops(op.deps)
            for d in op.deps:
                d.signal = True
        for e in ENG_NAMES:
            c = 0
            for op in self.ops[e]:
                if (not op.is_dma) and op.signal:
                    c += 1
                    op.val = c
        gtotal = {}
        for g, lst in self.dma_groups.items():
            c = 0
            for op in lst:
                c += 16
                op.val = c
            gtotal[g] = c
        with contextlib.ExitStack() as st:
            esem = {e: st.enter_context(nc.semaphore("s_" + e)) for e in ENG_NAMES}
            gsem = {g: st.enter_context(nc.semaphore("d_%d" % i))
                    for i, g in enumerate(self.dma_groups)}
            block = st.enter_context(nc.Block())

            def run(e, engobj):
                seen = {}
                for op in self.ops[e]:
                    for d in op.deps:
                        if d.is_dma:
                            key = ("g", d.grp)
                            sem = gsem[d.grp]
                            v = gtotal[d.grp] if d.grp in self.wait_all_groups else d.val
                        else:
                            key = ("e", d.eng)
                            sem = esem[d.eng]
                            v = d.val
                        if seen.get(key, 0) >= v:
                            continue
                        seen[key] = v
                        engobj.wait_ge(sem, v)
                    ins = op.fn(engobj)
                    if op.is_dma:
                        ins.then_inc(gsem[op.grp], 16)
                    elif op.signal:
                        ins.then_inc(esem[e], 1)
                if e == "sp":
                    for g in final_wait_groups:
                        engobj.wait_ge(gsem[g], gtotal[g])

            block.tensor(lambda eng: run("pe", eng))
            block.scalar(lambda eng: run("act", eng))
            block.vector(lambda eng: run("dve", eng))
            block.gpsimd(lambda eng: run("pool", eng))
            block.sync(lambda eng: run("sp", eng))


class Arena:
    def __init__(self, nc, prog, words):
        self.t = nc.alloc_sbuf_tensor("arena", [128, words], F32)
        self.P = prog
        self.free_list = [(0, words)]
        self.live = {}
        self.dead = []
        self.peak = 0

    def alloc(self, name, shape, dt, parts=128):
        n = int(np.prod(shape))
        esz = 2 if dt == BF16 else 4
        words = (n * esz + 31) // 32 * 8
        small = words <= 1100
        order = range(len(self.free_list) - 1, -1, -1) if small else range(len(self.free_list))
        for i in order:
            o, w = self.free_list[i]
            if w >= words:
                if w == words:
                    off = o
                    self.free_list.pop(i)
                elif small:
                    off = o + w - words
                    self.free_list[i] = (o, w - words)
                else:
                    off = o
                    self.free_list[i] = (o + words, w - words)
                break
        else:
            raise RuntimeError("SBUF arena full allocating %s (%d words); live=%s" % (
                name, words, {k: v[1] for k, v in self.live.items()}))
        self.live[name] = (off, words)
        self.peak = max(self.peak, off + words)
        preds = []
        for (o, w, nm) in self.dead:
            if o < off + words and off < o + w:
                preds.extend(self.P.ops_touching(nm))
        assert name not in self.P.keys_by_buf, name
        self.P.buf_pred[name] = _reduce_ops(preds)
        v = self.t[0:parts, off:off + words]
        if dt != F32:
            v = v.bitcast(dt)
        v = v[:, 0:n]
        if len(shape) == 2:
            v = v.rearrange("p (a b) -> p a b", b=shape[1])
        elif len(shape) == 3:
            v = v.rearrange("p (a b c) -> p a b c", b=shape[1], c=shape[2])
        return v

    def free(self, name):
        off, words = self.live.pop(name)
        self.dead.append((off, words, name))
        fl = self.free_list + [(off, words)]
        fl.sort()
        merged = []
        for o, w in fl:
            if merged and merged[-1][0] + merged[-1][1] == o:
                merged[-1] = (merged[-1][0], merged[-1][1] + w)
            else:
                merged.append((o, w))
        self.free_list = merged


def build(stage=99, dbg=()):
    nc = bass.Bass("TRN2", target_bir_lowering=False)
    P = Prog(nc)
    A = Arena(nc, P, 52992)

    def din(name, shape, dt=F32):
        return nc.dram_tensor(name, list(shape), dt, kind="ExternalInput").ap()

    x_d = din("x", [T, D])
    ccol_d = din("c_col", [128, 8])
    wada_d = din("w_ada", [D, 6 * D])
    bada_d = din("b_ada_col", [128, 48])
    g1_d = din("g1_col", [128, 8])
    g2_d = din("g2_col", [128, 8])
    win_d = din("w_in", [D, DIN])
    bif_d = din("b_if_bc", [128, 8])
    cw_d = din("conv_w_col", [128, 4, 31])
    cb_d = din("conv_b_col", [128, 4])
    clg_d = din("conv_lng_col", [128, 4])
    clb_d = din("conv_lnb_col", [128, 4])
    wco_d = din("w_conv_out", [512, D])
    qkw_d = din("qk_w_col", [128, 8, 4])
    qkb_d = din("qk_b_col", [128, 8])
    mng_d = din("mng_col", [128, 4])
    wmo_d = din("w_m_out", [512, D])
    wout_d = din("w_out", [D, D])
    wr_d = din("w_router", [D, 36])
    br_d = din("b_router_bc", [128, 36])
    weg_d = din("w_e_gate_l", [32 * 128, 8 * 512])
    weu_d = din("w_e_up_l", [32 * 128, 8 * 512])
    wed_d = din("w_e_down_l", [32 * 128, 4 * D])
    gfin_d = din("g_final_bc", [128, D])
    out_d = nc.dram_tensor("out", [T, D], F32, kind="ExternalOutput").ap()

    dbg_outs = {}

    def dbg_out(name, ap, reads):
        if name not in dbg:
            return
        shape = list(ap.shape)
        dt = ap.dtype
        d = nc.dram_tensor("dbg_" + name, shape, dt, kind="ExternalOutput").ap()
        dbg_outs[name] = d
        P.add("sp", lambda e: e.dma_start(out=d, in_=ap), reads=reads, dma=True, grp="dbgout")

    psb = [nc.alloc_psum_tensor("ps%d" % i, [128, 512], F32) for i in range(8)]
    ps_rot = list(range(8))

    def next_ps():
        i = ps_rot.pop(0)
        ps_rot.append(i)
        return psb[i], ("ps%d" % i,)

    def hold_ps():
        i = ps_rot.pop(0)
        return psb[i], ("ps%d" % i,)

    def release_ps(key):
        ps_rot.append(int(key[0][2:]))

    ident_f = A.alloc("ident_f", [128], F32)
    ident_b = A.alloc("ident_b", [128], BF16)
    ones_b = A.alloc("ones_b", [128], BF16)
    ones_f = A.alloc("ones_f", [128], F32)
    mask_ut = A.alloc("mask_ut", [128], BF16)
    tri_f = A.alloc("tri_f", [128], F32)
    K_ID = ("ident_f", 0)
    P.add("pool", lambda e: e.memset(ident_f, 0.0), writes=[("ident_f", 0)])
    P.add("pool", lambda e: e.affine_select(out=ident_f, in_=ident_f, pattern=[[-1, 128]],
                                             compare_op=ALU.not_equal, fill=1.0, base=0, channel_multiplier=1),
          reads=[("ident_f", 0)], writes=[("ident_f", 0)])
    P.add("pool", lambda e: e.tensor_copy(out=ident_b, in_=ident_f), reads=[("ident_f", 0)], writes=[("ident_b", 0)])
    P.add("pool", lambda e: e.memset(ones_b, 1.0), writes=[("ones_b", 0)])
    P.add("pool", lambda e: e.memset(ones_f, 1.0), writes=[("ones_f", 0)])
    P.add("pool", lambda e: e.memset(tri_f, 1.0), writes=[("tri_f", 0)])
    P.add("pool", lambda e: e.affine_select(out=tri_f, in_=tri_f, pattern=[[1, 128]],
                                             compare_op=ALU.is_ge, fill=0.0, base=0, channel_multiplier=-1),
          reads=[("tri_f", 0)], writes=[("tri_f", 0)])
    P.add("pool", lambda e: e.tensor_copy(out=mask_ut, in_=tri_f), reads=[("tri_f", 0)], writes=[("mask_ut", 0)])

    def load_const(name, dram, shape, dt=F32):
        t = A.alloc(name, shape, dt)
        P.add("sp", lambda e: e.dma_start(out=t, in_=dram), writes=[(name, 0)], dma=True, grp="c_" + name)
        return t

    ccol = load_const("ccol", ccol_d, [8])
    bada = load_const("bada", bada_d, [48])
    g1c = load_const("g1c", g1_d, [8])
    g2c = load_const("g2c", g2_d, [8])

    silc = A.alloc("silc", [8], F32)
    silb = A.alloc("silb", [8], BF16)
    P.add("act", lambda e: e.activation(out=silc, in_=ccol, func=AF.Silu), reads=[("ccol", 0)], writes=[("silc", 0)])
    P.add("dve", lambda e: e.tensor_copy(out=silb, in_=silc), reads=[("silc", 0)], writes=[("silb", 0)])
    modT = A.alloc("modT", [48], F32)
    wada_v = wada_d.rearrange("(c p) n -> p c n", p=128)
    NWA = 2
    wab = [A.alloc("wada%d" % i, [8, 512], BF16) for i in range(NWA)]
    a1 = A.alloc("a1", [8], F32)
    a2 = A.alloc("a2", [8], F32)

    def adaln_block(blk, ps_mod, k_mod):
        s_ = blk % NWA
        buf = wab[s_]
        nm = "wada%d" % s_
        P.add("pool", (lambda e, buf=buf, blk=blk: e.dma_start(out=buf, in_=wada_v[:, :, blk * 512:(blk + 1) * 512])),
              writes=[(nm, 0)], dma=True, grp=nm)

        def mm(e, buf=buf, blk=blk):
            ins = None
            for jj in range(4):
                j = blk * 4 + jj
                for k in range(8):
                    ins = e.matmul(ps_mod[:, j:j + 1], lhsT=buf[:, k, jj * 128:(jj + 1) * 128], rhs=silb[:, k:k + 1],
                                   start=(k == 0), stop=(k == 7))
            return ins
        P.add("pe", mm, reads=[(nm, 0), ("silb", 0)], writes=[k_mod])

    def adaln_finish(ps_mod, k_mod, c0, c1, part):
        P.add("dve", lambda e: e.tensor_tensor(out=modT[:, c0:c1], in0=ps_mod[:, c0:c1], in1=bada[:, c0:c1], op=ALU.add),
              reads=[k_mod, ("bada", 0)], writes=[("modT", part)])
        release_ps(k_mod)

    pm0, km0 = hold_ps()
    for blk in range(4):
        adaln_block(blk, pm0, km0)
    adaln_finish(pm0, km0, 0, 16, 0)
    P.add("dve", lambda e: e.scalar_tensor_tensor(out=a1, in0=modT[:, 8:16], scalar=1.0, in1=g1c, op0=ALU.add, op1=ALU.mult),
          reads=[("modT", 0), ("g1c", 0)], writes=[("a1", 0)])
    p2state = {}

    def adaln_p2_block(blk):
        if "ps" not in p2state:
            p2state["ps"] = hold_ps()
        adaln_block(blk, *p2state["ps"])

    def adaln_p2_end():
        pm1, km1 = p2state["ps"]
        adaln_finish(pm1, km1, 16, 48, 1)
        P.add("dve", lambda e: e.scalar_tensor_tensor(out=a2, in0=modT[:, 32:40], scalar=1.0, in1=g2c, op0=ALU.add, op1=ALU.mult),
              reads=[("modT", 1), ("g2c", 0)], writes=[("a2", 0)])
        dbg_out("modT", modT, [("modT", 0), ("modT", 1)])
        for i in range(NWA):
            A.free("wada%d" % i)

    hT = A.alloc("hT", [8, T], BF16)
    merged = A.alloc("merged", [8, T], BF16)
    NWB = 3
    wbufs = [A.alloc("wblk%d" % i, [8, 512], BF16) for i in range(NWB)]
    NXB = 8
    xin = [A.alloc("xin%d" % i, [D], F32) for i in range(NXB)]
    xnb = [A.alloc("xnb%d" % i, [D], BF16) for i in range(NXB)]
    junk = A.alloc("junk", [D], F32)
    ss1 = A.alloc("ss1", [NT], F32)
    rs1 = A.alloc("rs1", [NT], F32)

    def p2_stats(nb):
        for tt in range(4):
            ti = nb * 4 + tt
            s = ti % NXB
            P.add("sp", (lambda e, s=s, ti=ti: e.dma_start(out=xin[s], in_=x_d[ti * 128:(ti + 1) * 128, :])),
                  writes=[("xin%d" % s, 0)], dma=True, grp="xin%d" % s)
            P.add("act", (lambda e, s=s, ti=ti: e.activation(out=junk, in_=xin[s], func=AF.Square,
                                                             accum_out=ss1[:, ti:ti + 1])),
                  reads=[("xin%d" % s, 0)], writes=[("junk", 0), ("ss1", ti)])
            P.add("act", (lambda e, ti=ti: e.activation(out=rs1[:, ti:ti + 1], in_=ss1[:, ti:ti + 1], func=AF.Sqrt,
                                                        scale=1.0 / D, bias=1e-6)),
                  reads=[("ss1", ti)], writes=[("rs1", ti)])
            P.add("dve", (lambda e, ti=ti: e.reciprocal(out=rs1[:, ti:ti + 1], in_=rs1[:, ti:ti + 1])),
                  reads=[("rs1", ti)], writes=[("rs1", ti)])
            P.add("dve", (lambda e, s=s, ti=ti: e.tensor_scalar(out=xnb[s], in0=xin[s], scalar1=rs1[:, ti:ti + 1],
                                                                scalar2=None, op0=ALU.mult)),
                  reads=[("xin%d" % s, 0), ("rs1", ti)], writes=[("xnb%d" % s, 0)])

    def p2_tr(nb):
        pst = [next_ps() for _ in range(4)]
        for tt in range(4):
            ti = nb * 4 + tt
            s = ti % NXB

            def tr(e, s=s, tt=tt, pst=pst):
                ins = None
                for c in range(8):
                    pb = pst[c // 2][0].bitcast(BF16)
                    ins = e.transpose(out=pb[:, (c % 2) * 512 + tt * 128:(c % 2) * 512 + (tt + 1) * 128],
                                      in_=xnb[s][:, c * 128:(c + 1) * 128], identity=ident_b)
                return ins
            P.add("pe", tr, reads=[("xnb%d" % s, 0), ("ident_b", 0)], writes=[pst[i][1] for i in range(4)])
        for c in range(8):
            pb = pst[c // 2][0].bitcast(BF16)
            P.add("act", (lambda e, c=c, pb=pb, nb=nb: e.activation(
                out=hT[:, c, nb * 512:(nb + 1) * 512], in_=pb[:, (c % 2) * 512:(c % 2 + 1) * 512],
                func=AF.Identity, scale=a1[:, c:c + 1], bias=modT[:, c:c + 1])),
                reads=[pst[c // 2][1], ("a1", 0), ("modT", 0)],
                writes=[("hT", c, nb)])

    p2_stats(0)
    for nb in range(NB):
        if nb + 1 < NB:
            p2_stats(nb + 1)
        p2_tr(nb)
    dbg_out("hT", hT, [("hT", c, nb) for c in range(8) for nb in range(NB)])
    for i in range(NXB):
        A.free("xin%d" % i); A.free("xnb%d" % i)

    def finish():
        P.emit(final_wait_groups=["dbgout"] if "dbgout" in P.dma_groups else [])
        return nc, dbg_outs

    GROWS = 256
    NG = -(-(2 * T + 32 * (GROWS - 1)) // GROWS)
    XS = nc.dram_tensor("xs_scratch", [NG * GROWS, D], BF16).ap()
    YS = nc.dram_tensor("ys_scratch", [NG * GROWS, D], BF16).ap()
    if stage <= 1:
        return finish()

    win_v = win_d.rearrange("(c p) n -> p c n", p=128)
    NWB = 3
    wb_ctr = [0]

    def load_wblock(col0, ncols=512):
        i = wb_ctr[0] % NWB
        wb_ctr[0] += 1
        buf = wbufs[i]
        nm = "wblk%d" % i
        P.add("pool", lambda e: e.dma_start(out=buf[:, :, 0:ncols], in_=win_v[:, :, col0:col0 + ncols]),
              writes=[(nm, 0)], dma=True, grp=nm)
        return buf, (nm, 0)

    def load_w4(dram_v):
        i = wb_ctr[0] % NWB
        wb_ctr[0] += 1
        nm = "wblk%d" % i
        v = wbufs[i].rearrange("p a b -> p (a b)").rearrange("p (a b) -> p a b", b=D)
        P.add("pool", lambda e: e.dma_start(out=v, in_=dram_v), writes=[(nm, 0)], dma=True, grp=nm)
        return v, (nm, 0)

    def load_cast(name, dram_ap, shape):
        t = A.alloc(name, shape, BF16)
        P.add("pool", lambda e: e.dma_start(out=t, in_=dram_ap), writes=[(name, 0)], dma=True, grp="c_" + name)
        return t

    hT_keys = lambda nb: [("hT", c, nb) for c in range(8)]

    def proj_fm(wb, wkey, mcol, nb):
        ps, pk = next_ps()

        def mm(e):
            ins = None
            for k in range(8):
                ins = e.matmul(ps[:, :], lhsT=wb[:, k, mcol * 128:(mcol + 1) * 128], rhs=hT[:, k, nb * 512:(nb + 1) * 512],
                               start=(k == 0), stop=(k == 7))
            return ins
        P.add("pe", mm, reads=[wkey] + hT_keys(nb), writes=[pk])
        return ps, pk

    cw = load_const("cw", cw_d, [4, 31])
    cb = load_const("cb", cb_d, [4])
    clg = load_const("clg", clg_d, [4])
    clb = load_const("clb", clb_d, [4])
    u = A.alloc("u", [4, 32 + T], BF16)
    PADU = 32
    for m in range(4):
        P.add("pool", (lambda e, m=m: e.memset(u[:, m, 0:PADU], 0.0)), writes=[("u", m, -1)])
    dg31 = A.alloc("dg31", [4, 31, 128], BF16)
    for m in range(4):
        P.add("pool", (lambda e, m=m: e.tensor_tensor(
            out=dg31[:, m], in0=ident_b.unsqueeze(1).to_broadcast([128, 31, 128]),
            in1=cw[:, m, :].unsqueeze(2).to_broadcast([128, 31, 128]), op=ALU.mult)),
            reads=[("ident_b", 0), ("cw", 0)], writes=[("dg31", m)])
    sgt = [A.alloc("sgt%d" % i, [512], BF16) for i in range(2)]
    sg_ctr = [0]

    def next_sgt():
        i = sg_ctr[0] % 2
        sg_ctr[0] += 1
        return sgt[i], ("sgt%d" % i, 0)

    wa, wak = load_wblock(0)
    wbk, wbkk = load_wblock(512)
    for m in range(4):
        for nb in range(NB):
            psa, pka = proj_fm(wa, wak, m, nb)
            psb_, pkb = proj_fm(wbk, wbkk, m, nb)
            sg, sgk = next_sgt()
            P.add("act", (lambda e, sg=sg, p=psb_: e.activation(out=sg, in_=p[:, :], func=AF.Sigmoid)),
                  reads=[pkb], writes=[sgk])
            P.add("dve", (lambda e, sg=sg, p=psa, m=m, nb=nb: e.tensor_tensor(
                out=u[:, m, PADU + nb * 512:PADU + (nb + 1) * 512], in0=p[:, :], in1=sg, op=ALU.mult)),
                reads=[pka, sgk], writes=[("u", m, nb)])
    dbg_out("u", u, [("u", m, nb) for m in range(4) for nb in range(-1, NB)])

    wco, wcok = load_w4(wco_d.rearrange("(c p) n -> p c n", p=128))
    gA_blocks = {0: load_wblock(3080)}
    cT = A.alloc("cT", [4, T], BF16)
    sqT = A.alloc("sqT", [4, T], BF16)
    for m in range(4):
        for nb in range(NB):
            ps, pk = next_ps()

            def cmm(e, ps=ps, m=m, nb=nb):
                ins = None
                for k in range(31):
                    o = PADU - 30 + nb * 512 + k
                    ins = e.matmul(ps[:, :], lhsT=dg31[:, m, k, :], rhs=u[:, m, o:o + 512], start=(k == 0), stop=(k == 30))
                return ins
            P.add("pe", cmm, reads=[("dg31", m), ("u", m, nb), ("u", m, nb - 1)], writes=[pk])
            P.add("act", (lambda e, ps=ps, m=m, nb=nb: e.activation(
                out=cT[:, m, nb * 512:(nb + 1) * 512], in_=ps[:, :], func=AF.Identity, bias=cb[:, m:m + 1])),
                reads=[pk, ("cb", 0)], writes=[("cT", m, nb)])
            P.add("act", (lambda e, ps=ps, m=m, nb=nb: e.activation(
                out=sqT[:, m, nb * 512:(nb + 1) * 512], in_=ps[:, :], func=AF.Square, bias=cb[:, m:m + 1])),
                reads=[pk, ("cb", 0)], writes=[("sqT", m, nb)])
            gi = m * NB + nb
            if gi % 2 == 1:
                adaln_p2_block(4 + gi // 2)
    adaln_p2_end()
    dbg_out("cT", cT, [("cT", m, nb) for m in range(4) for nb in range(NB)])
    A.free("u")
    A.free("dg31")

    actT = A.alloc("actT", [4, T], BF16)
    mean_t = A.alloc("mean_t", [512], F32)
    rstd_t = A.alloc("rstd_t", [512], F32)
    msq_t = A.alloc("msq_t", [512], F32)
    nrm_t = [A.alloc("nrm_t%d" % i, [512], F32) for i in range(2)]
    for nb in range(NB):
        ps1, pk1 = next_ps()
        ps2, pk2 = next_ps()

        def smm(e, ps1=ps1, ps2=ps2, nb=nb):
            ins = None
            for m in range(4):
                ins = e.matmul(ps1[:, :], lhsT=ones_b, rhs=cT[:, m, nb * 512:(nb + 1) * 512], start=(m == 0), stop=(m == 3))
            for m in range(4):
                ins = e.matmul(ps2[:, :], lhsT=ones_b, rhs=sqT[:, m, nb * 512:(nb + 1) * 512], start=(m == 0), stop=(m == 3))
            return ins
        P.add("pe", smm, reads=[("ones_b", 0)] + [("cT", m, nb) for m in range(4)] + [("sqT", m, nb) for m in range(4)],
              writes=[pk1, pk2])
        P.add("dve", (lambda e, ps1=ps1: e.tensor_scalar(out=mean_t, in0=ps1[:, :], scalar1=1.0 / 512, scalar2=None, op0=ALU.mult)),
              reads=[pk1], writes=[("mean_t", 0)])
        P.add("dve", lambda e: e.tensor_tensor(out=msq_t, in0=mean_t, in1=mean_t, op=ALU.mult),
              reads=[("mean_t", 0)], writes=[("msq_t", 0)])
        P.add("dve", (lambda e, ps2=ps2: e.scalar_tensor_tensor(out=rstd_t, in0=ps2[:, :], scalar=1.0 / 512, in1=msq_t,
                                                                op0=ALU.mult, op1=ALU.subtract)),
              reads=[pk2, ("msq_t", 0)], writes=[("rstd_t", 0)])
        P.add("act", lambda e: e.activation(out=rstd_t, in_=rstd_t, func=AF.Sqrt, bias=1e-5),
              reads=[("rstd_t", 0)], writes=[("rstd_t", 0)])
        P.add("dve", lambda e: e.reciprocal(out=rstd_t, in_=rstd_t), reads=[("rstd_t", 0)], writes=[("rstd_t", 0)])
        for m in range(4):
            nt = nrm_t[m % 2]
            ntk = ("nrm_t%d" % (m % 2), 0)
            P.add("dve", (lambda e, nt=nt, m=m, nb=nb: e.tensor_tensor(out=nt, in0=cT[:, m, nb * 512:(nb + 1) * 512], in1=mean_t,
                                                                      op=ALU.subtract)),
                  reads=[("cT", m, nb), ("mean_t", 0)], writes=[ntk])
            P.add("dve", (lambda e, nt=nt: e.tensor_tensor(out=nt, in0=nt, in1=rstd_t, op=ALU.mult)),
                  reads=[ntk, ("rstd_t", 0)], writes=[ntk])
            P.add("act", (lambda e, nt=nt, m=m, nb=nb: e.activation(
                out=actT[:, m, nb * 512:(nb + 1) * 512], in_=nt, func=AF.Silu, scale=clg[:, m:m + 1], bias=clb[:, m:m + 1])),
                reads=[ntk, ("clg", 0), ("clb", 0)], writes=[("actT", m, nb)])
    dbg_out("actT", actT, [("actT", m, nb) for m in range(4) for nb in range(NB)])
    A.free("cT"); A.free("sqT"); A.free("mean_t"); A.free("rstd_t"); A.free("msq_t"); A.free("nrm_t0"); A.free("nrm_t1")

    for jb in range(2):
        wg_, wgk = gA_blocks[jb] if jb in gA_blocks else load_wblock(3080 + jb * 512)
        for jj in range(4):
            j = jb * 4 + jj
            for nb in range(NB):
                psy, pky = next_ps()

                def ymm(e, psy=psy, j=j, nb=nb):
                    ins = None
                    for m in range(4):
                        ins = e.matmul(psy[:, :], lhsT=wco[:, m, j * 128:(j + 1) * 128], rhs=actT[:, m, nb * 512:(nb + 1) * 512],
                                       start=(m == 0), stop=(m == 3))
                    return ins
                P.add("pe", ymm, reads=[wcok] + [("actT", m, nb) for m in range(4)], writes=[pky])
                psg, pkg = proj_fm(wg_, wgk, jj, nb)
                sg, sgk = next_sgt()
                P.add("act", (lambda e, sg=sg, p=psg: e.activation(out=sg, in_=p[:, :], func=AF.Sigmoid)),
                      reads=[pkg], writes=[sgk])
                P.add("dve", (lambda e, sg=sg, p=psy, j=j, nb=nb: e.tensor_tensor(
                    out=merged[:, j, nb * 512:(nb + 1) * 512], in0=p[:, :], in1=sg, op=ALU.mult)),
                    reads=[pky, sgk], writes=[("merged", j, nb)])
    dbg_out("mergedA", merged, [("merged", j, nb) for j in range(8) for nb in range(NB)])
    A.free("actT")
    if stage <= 2:
        return finish()

    zt = A.alloc("zt", [D], BF16)
    P.add("pool", lambda e: e.memset(zt, 0.0), writes=[("zt", 0)])
    XSZ_KEYS = []
    for zi in range(NG * GROWS // 1024):
        P.add("sp", (lambda e, zi=zi: e.dma_start(out=XS[zi * 1024:(zi + 1) * 1024, :].rearrange("(n p) d -> p n d", p=128),
                                                  in_=zt.unsqueeze(1).to_broadcast([128, 8, D]))),
              reads=[("zt", 0)], writes=[("XSZ", zi)], dma=True, grp="xs_zero")
        XSZ_KEYS.append(("XSZ", zi))
    A.free("zt")
    PADQ = 4
    qkw = load_const("qkw", qkw_d, [8, 4])
    qkb = load_const("qkb", qkb_d, [8])
    bif = load_const("bif", bif_d, [8])
    mng = load_const("mng", mng_d, [4])
    qk_raw = A.alloc("qk_raw", [8, PADQ + T], BF16)
    for cc in range(8):
        P.add("pool", (lambda e, cc=cc: e.memset(qk_raw[:, cc, 0:PADQ], 0.0)), writes=[("qk_raw", cc, -1)])
    dg4 = A.alloc("dg4", [8, 4, 128], BF16)
    P.add("pool", lambda e: e.tensor_tensor(
        out=dg4.rearrange("p a b c -> p (a b) c"), in0=ident_b.unsqueeze(1).to_broadcast([128, 32, 128]),
        in1=qkw.rearrange("p a b -> p (a b)").unsqueeze(2).to_broadcast([128, 32, 128]), op=ALU.mult),
        reads=[("ident_b", 0), ("qkw", 0)], writes=[("dg4", 0)])
    for half in range(2):
        wq_, wqk = load_wblock(1024 + half * 512)
        for m in range(4):
            cc = half * 4 + m
            for nb in range(NB):
                ps, pk = proj_fm(wq_, wqk, m, nb)
                P.add("act", (lambda e, ps=ps, cc=cc, nb=nb: e.activation(
                    out=qk_raw[:, cc, PADQ + nb * 512:PADQ + (nb + 1) * 512], in_=ps[:, :], func=AF.Identity)),
                    reads=[pk], writes=[("qk_raw", cc, nb)])
    qkc = A.alloc("qkc", [8, T], BF16)
    for cc in range(8):
        for nb in range(NB):
            ps, pk = next_ps()

            def qmm(e, ps=ps, cc=cc, nb=nb):
                ins = None
                for k in range(4):
                    o = PADQ - 3 + nb * 512 + k
                    ins = e.matmul(ps[:, :], lhsT=dg4[:, cc, k, :], rhs=qk_raw[:, cc, o:o + 512], start=(k == 0), stop=(k == 3))
                return ins
            P.add("pe", qmm, reads=[("dg4", 0), ("qk_raw", cc, nb), ("qk_raw", cc, nb - 1)], writes=[pk])
            P.add("act", (lambda e, ps=ps, cc=cc, nb=nb: e.activation(
                out=qkc[:, cc, nb * 512:(nb + 1) * 512], in_=ps[:, :], func=AF.Silu, bias=qkb[:, cc:cc + 1])),
                reads=[pk, ("qkb", 0)], writes=[("qkc", cc, nb)])
    dbg_out("qkc", qkc, [("qkc", cc, nb) for cc in range(8) for nb in range(NB)])
    A.free("qk_raw"); A.free("dg4")
    if stage <= 2.2:
        return finish()

    wif = A.alloc("wif", [8, 8], BF16)
    wif_f = A.alloc("wif_f", [8, 8], F32)
    with nc.allow_non_contiguous_dma(reason="tiny gate-weight columns"):
        P.add("sp", lambda e: e.dma_start(out=wif_f, in_=win_v[:, :, 3072:3080]), writes=[("wif_f", 0)], dma=True, grp="c_wif")
    P.add("dve", lambda e: e.tensor_copy(out=wif, in_=wif_f), reads=[("wif_f", 0)], writes=[("wif", 0)])
    G = A.alloc("G", [NT, 8], F32)
    nlf = A.alloc("nlf", [NT, 4], F32)
    gtmp = A.alloc("gtmp", [NT, 4], F32)
    A_inv = A.alloc("A_inv", [NT, 4], F32)
    Bv = A.alloc("Bv", [NT, 4], F32)
    dec = A.alloc("dec", [NT, 4], F32)
    psg, pkg = hold_ps()

    def gmm(e):
        ins = None
        for ti in range(NT):
            for k in range(8):
                ins = e.matmul(psg[:, ti * 8:(ti + 1) * 8], lhsT=hT[:, k, ti * 128:(ti + 1) * 128], rhs=wif[:, k, :],
                               start=(k == 0), stop=(k == 7))
        return ins
    P.add("pe", gmm, reads=[("wif", 0)] + [("hT", c, nb) for c in range(8) for nb in range(NB)], writes=[pkg])
    P.add("dve", lambda e: e.tensor_tensor(out=G, in0=psg[:, 0:128].rearrange("p (a b) -> p a b", b=8),
                                           in1=bif.unsqueeze(1).to_broadcast([128, NT, 8]), op=ALU.add),
          reads=[pkg, ("bif", 0)], writes=[("G", 0)])
    release_ps(pkg)
    dbg_out("G", G, [("G", 0)])
    if stage <= 2.31:
        return finish()
    P.add("act", lambda e: e.activation(out=gtmp, in_=G[:, :, 4:8], func=AF.Exp, scale=-1.0),
          reads=[("G", 0)], writes=[("gtmp", 0)])
    P.add("act", lambda e: e.activation(out=nlf, in_=gtmp, func=AF.Ln, bias=1.0),
          reads=[("gtmp", 0)], writes=[("nlf", 0)])
    dbg_out("nlf", nlf, [("nlf", 0)])
    if stage <= 2.32:
        return finish()
    psc, pkc = next_ps()
    nlf2 = nlf.rearrange("p a b -> p (a b)")
    nl_hi = A.alloc("nl_hi", [64], BF16)
    nl_lo = A.alloc("nl_lo", [64], BF16)
    P.add("dve", lambda e: e.tensor_copy(out=nl_hi, in_=nlf2), reads=[("nlf", 0)], writes=[("nl_hi", 0)])
    P.add("dve", lambda e: e.tensor_tensor(out=nl_lo, in0=nlf2, in1=nl_hi, op=ALU.subtract),
          reads=[("nlf", 0), ("nl_hi", 0)], writes=[("nl_lo", 0)])

    def cmm2(e):
        e.matmul(psc[:, 0:64], lhsT=mask_ut, rhs=nl_hi, start=True, stop=False)
        e.matmul(psc[:, 0:64], lhsT=mask_ut, rhs=nl_lo, start=False, stop=True)
        e.matmul(psc[:, 64:128], lhsT=ones_b, rhs=nl_hi, start=True, stop=False)
        return e.matmul(psc[:, 64:128], lhsT=ones_b, rhs=nl_lo, start=False, stop=True)
    P.add("pe", cmm2, reads=[("mask_ut", 0), ("ones_b", 0), ("nl_hi", 0), ("nl_lo", 0)], writes=[pkc])
    if stage <= 2.33:
        P.add("dve", lambda e: e.tensor_copy(out=gtmp.rearrange("p a b -> p (a b)"), in_=psc[:, 0:64]), reads=[pkc], writes=[("gtmp", 0)])
        dbg_out("ncum", gtmp, [("gtmp", 0)])
        return finish()
    LNS = float(np.log(128.0 ** 0.5))
    cval = A.alloc("cval", [2], F32)
    P.add("pool", lambda e: e.memset(cval[:, 0:1], LNS), writes=[("cval", 0)])
    P.add("pool", lambda e: e.memset(cval[:, 1:2], -LNS), writes=[("cval", 1)])
    P.add("act", lambda e: e.activation(out=A_inv.rearrange("p a b -> p (a b)"), in_=psc[:, 0:64], func=AF.Exp, bias=cval[:, 0:1]),
          reads=[pkc, ("cval", 0)], writes=[("A_inv", 0)])
    A_ = A.alloc("A_", [NT, 4], F32)
    P.add("act", lambda e: e.activation(out=A_.rearrange("p a b -> p (a b)"), in_=psc[:, 0:64], func=AF.Exp, scale=-1.0, bias=cval[:, 1:2]),
          reads=[pkc, ("cval", 1)], writes=[("A_", 0)])
    if stage <= 2.34:
        dbg_out("A_", A_, [("A_", 0)])
        dbg_out("A_inv", A_inv, [("A_inv", 0)])
        return finish()
    P.add("dve", lambda e: e.tensor_tensor(out=gtmp, in0=psc[:, 0:64].rearrange("p (a b) -> p a b", b=4), in1=G[:, :, 0:4], op=ALU.add),
          reads=[pkc, ("G", 0), ("gtmp", 0)], writes=[("gtmp", 0)])
    P.add("act", lambda e: e.activation(out=Bv, in_=gtmp, func=AF.Exp), reads=[("gtmp", 0)], writes=[("Bv", 0)])
    if stage <= 2.36:
        dbg_out("Bv", Bv, [("Bv", 0)])
        return finish()
    P.add("act", lambda e: e.activation(out=dec.rearrange("p a b -> p (a b)"), in_=psc[:, 64:128], func=AF.Exp, scale=-1.0),
          reads=[pkc], writes=[("dec", 0)])
    dbg_out("Bv", Bv, [("Bv", 0)])
    dbg_out("A_", A_, [("A_", 0)])
    dbg_out("decay", dec, [("dec", 0)])

    if stage <= 2.4:
        return finish()
    ktok = A.alloc("ktok", [NT, 512], BF16)
    for c in range(NT):
        ps, pk = next_ps()
        pb = ps.bitcast(BF16)

        def ktr(e, pb=pb, c=c):
            ins = None
            for h in range(4):
                ins = e.transpose(out=pb[:, h * 128:(h + 1) * 128], in_=qkc[:, 4 + h, c * 128:(c + 1) * 128], identity=ident_b)
            return ins
        P.add("pe", ktr, reads=[("ident_b", 0)] + [("qkc", 4 + h, c // 4) for h in range(4)], writes=[pk])
        P.add("act", (lambda e, pb=pb, c=c: e.activation(out=ktok[:, c, :], in_=pb[:, 0:512], func=AF.Identity)),
              reads=[pk], writes=[("ktok", c)])

    vB = A.alloc("vB", [NT, 4, 129], BF16)
    wv_, wvk = load_wblock(2048)
    for c in range(NT):
        ps, pk = next_ps()

        def vmm(e, ps=ps, c=c):
            ins = None
            for k in range(8):
                ins = e.matmul(ps[:, :], lhsT=hT[:, k, c * 128:(c + 1) * 128], rhs=wv_[:, k, 0:512], start=(k == 0), stop=(k == 7))
            return ins
        P.add("pe", vmm, reads=[wvk] + hT_keys(c // 4), writes=[pk])
        P.add("dve", (lambda e, ps=ps, c=c: e.tensor_tensor(
            out=vB[:, c, :, 0:128], in0=ps[:, :].rearrange("p (a b) -> p a b", b=128),
            in1=Bv[:, c, :].unsqueeze(2).to_broadcast([128, 4, 128]), op=ALU.mult)),
            reads=[pk, ("Bv", 0)], writes=[("vB", c, 0)])
        P.add("dve", (lambda e, c=c: e.tensor_copy(out=vB[:, c, :, 128], in_=Bv[:, c, :])),
              reads=[("Bv", 0)], writes=[("vB", c, 1)])

    if stage <= 2.6:
        return finish()
    E = A.alloc("E", [4, 129], F32)
    Cb = [A.alloc("Cb%d" % i, [4, 129], BF16) for i in range(2)]
    sm = [A.alloc("sm%d" % i, [4, 128], BF16) for i in range(2)]
    hn = [A.alloc("hn%d" % i, [4, 128], BF16) for i in range(2)]
    st6 = A.alloc("st6", [4, 6], F32)
    mv = A.alloc("mv", [4, 2], F32)
    den = A.alloc("den", [4], F32)
    qq = A.alloc("qq", [4], F32)
    rstd = A.alloc("rstd", [4], F32)
    sgo = [A.alloc("sgo%d" % i, [4, 512], BF16) for i in range(2)]
    hmT = A.alloc("hmT", [4, T], BF16)
    wo_, wok = load_wblock(2560)
    CW = 256

    chs = {}

    def chunk_A(c):
        nb = c // 4
        cs = slice(c * 128, (c + 1) * 128)
        if c % 4 == 0:
            for h in range(4):
                ps, pk = proj_fm(wo_, wok, h, nb)
                P.add("act", (lambda e, ps=ps, h=h, nb=nb: e.activation(out=sgo[nb % 2][:, h, :], in_=ps[:, :], func=AF.Sigmoid)),
                      reads=[pk], writes=[("sgo%d" % (nb % 2), h)])
        pss, pks = next_ps()

        def smm2(e, pss=pss, cs=cs):
            ins = None
            for h in range(4):
                ins = e.matmul(pss[:, h * 128:(h + 1) * 128], lhsT=qkc[:, 4 + h, cs], rhs=qkc[:, h, cs], start=True, stop=True)
            return ins
        P.add("pe", smm2, reads=[("qkc", cc, nb) for cc in range(8)], writes=[pks])
        smc = sm[c % 2]
        smk = ("sm%d" % (c % 2), 0)
        P.add("dve", (lambda e, pss=pss, smc=smc: e.tensor_tensor(
            out=smc, in0=pss[:, :].rearrange("p (a b) -> p a b", b=128),
            in1=mask_ut.unsqueeze(1).to_broadcast([128, 4, 128]), op=ALU.mult)),
            reads=[pks, ("mask_ut", 0)], writes=[smk])
        pu = [hold_ps(), hold_ps()]

        def umm(e, pu=pu, c=c):
            ins = None
            for h in range(4):
                o = pu[h // 2][0][:, (h % 2) * CW:(h % 2) * CW + 129]
                ins = e.matmul(o, lhsT=ktok[:, c, h * 128:(h + 1) * 128], rhs=vB[:, c, h, :], start=True, stop=True)
            return ins
        P.add("pe", umm, reads=[("ktok", c), ("vB", c, 0), ("vB", c, 1)], writes=[pu[0][1], pu[1][1]])
        chs[c] = (smc, smk, pu)

    def chunk_B(c):
        nb = c // 4
        cs = slice(c * 128, (c + 1) * 128)
        smc, smk, pu = chs[c]
        pn = [next_ps(), next_ps()]

        def nmm(e, pn=pn, smc=smc, c=c, cs=cs):
            ins = None
            for h in range(4):
                o = pn[h // 2][0][:, (h % 2) * CW:(h % 2) * CW + 129]
                ins = e.matmul(o, lhsT=smc[:, h, :], rhs=vB[:, c, h, :], start=True, stop=(c == 0))
                if c > 0:
                    ins = e.matmul(o, lhsT=qkc[:, h, cs], rhs=Cb[(c - 1) % 2][:, h, :], start=False, stop=True)
            return ins
        rd = [smk, ("vB", c, 0), ("vB", c, 1)] + [("qkc", h, nb) for h in range(4)]
        if c > 0:
            rd += [("Cb%d" % ((c - 1) % 2), h) for h in range(4)]
        P.add("pe", nmm, reads=rd, writes=[pn[0][1], pn[1][1]])
        for h in range(4):
            src = pu[h // 2][0][:, (h % 2) * CW:(h % 2) * CW + 129]
            if c == 0:
                P.add("dve", (lambda e, src=src, h=h: e.tensor_copy(out=E[:, h, :], in_=src)),
                      reads=[pu[h // 2][1]], writes=[("E", h)])
            else:
                P.add("dve", (lambda e, src=src, h=h, c=c: e.scalar_tensor_tensor(
                    out=E[:, h, :], in0=E[:, h, :], scalar=dec[:, c - 1, h:h + 1], in1=src, op0=ALU.mult, op1=ALU.add)),
                    reads=[pu[h // 2][1], ("E", h), ("dec", 0)], writes=[("E", h)])
            if c < NT - 1:
                P.add("act", (lambda e, h=h, c=c: e.activation(out=Cb[c % 2][:, h, :], in_=E[:, h, :], func=AF.Identity,
                                                               scale=dec[:, c, h:h + 1])),
                      reads=[("E", h), ("dec", 0)], writes=[("Cb%d" % (c % 2), h)])
        release_ps(pu[0][1]); release_ps(pu[1][1])
        chs[c] = pn

    def chunk_C(c):
        nb = c // 4
        cs = slice(c * 128, (c + 1) * 128)
        pn = chs[c]
        for h in range(4):
            src = pn[h // 2][0][:, (h % 2) * CW:(h % 2) * CW + 128]
            P.add("dve", (lambda e, src=src, h=h: e.bn_stats(out=st6[:, h, :], in_=src)),
                  reads=[pn[h // 2][1]], writes=[("st6", h)])
            P.add("dve", (lambda e, h=h: e.bn_aggr(out=mv[:, h, :], in_=st6[:, h, :])),
                  reads=[("st6", h)], writes=[("mv", h)])
        for b2 in range(2):
            dsrc = pn[b2][0][:, 0:512].rearrange("p (a b) -> p a b", b=CW)[:, :, 128]
            P.add("dve", (lambda e, dsrc=dsrc, b2=b2, c=c: e.tensor_tensor(
                out=den[:, 2 * b2:2 * b2 + 2], in0=dsrc, in1=A_[:, c, 2 * b2:2 * b2 + 2], op=ALU.mult)),
                reads=[pn[b2][1], ("A_", 0)], writes=[("den", b2)])
        P.add("dve", lambda e: e.scalar_tensor_tensor(out=den, in0=den, scalar=-1.0, in1=den, op0=ALU.mult, op1=ALU.max),
              reads=[("den", 0), ("den", 1)], writes=[("den", 0), ("den", 1)])
        P.add("dve", lambda e: e.tensor_scalar(out=den, in0=den, scalar1=1.0, scalar2=None, op0=ALU.max),
              reads=[("den", 0), ("den", 1)], writes=[("den", 0), ("den", 1)])
        P.add("dve", (lambda e, c=c: e.tensor_tensor(out=qq, in0=den, in1=A_inv[:, c, :], op=ALU.mult)),
              reads=[("den", 0), ("den", 1), ("A_inv", 0)], writes=[("qq", 0)])
        P.add("dve", lambda e: e.tensor_tensor(out=qq, in0=qq, in1=qq, op=ALU.mult), reads=[("qq", 0)], writes=[("qq", 0)])
        P.add("dve", lambda e: e.scalar_tensor_tensor(out=rstd, in0=qq, scalar=1e-5, in1=mv[:, :, 1], op0=ALU.mult, op1=ALU.add),
              reads=[("qq", 0)] + [("mv", h) for h in range(4)], writes=[("rstd", 0)])
        P.add("act", lambda e: e.activation(out=rstd, in_=rstd, func=AF.Sqrt), reads=[("rstd", 0)], writes=[("rstd", 0)])
        P.add("dve", lambda e: e.reciprocal(out=rstd, in_=rstd), reads=[("rstd", 0)], writes=[("rstd", 0)])
        hnc = hn[c % 2]
        hnk = "hn%d" % (c % 2)
        for h in range(4):
            src = pn[h // 2][0][:, (h % 2) * CW:(h % 2) * CW + 128]
            P.add("dve", (lambda e, src=src, h=h, hnc=hnc: e.tensor_scalar(
                out=hnc[:, h, :], in0=src, scalar1=mv[:, h, 0:1], scalar2=rstd[:, h:h + 1], op0=ALU.subtract, op1=ALU.mult)),
                reads=[pn[h // 2][1], ("mv", h), ("rstd", 0)], writes=[(hnk, h)])
        pt, pkt = next_ps()
        ptb = pt.bitcast(BF16)

        def htr(e, ptb=ptb, hnc=hnc):
            ins = None
            for h in range(4):
                ins = e.transpose(out=ptb[:, h * 128:(h + 1) * 128], in_=hnc[:, h, :], identity=ident_b)
            return ins
        P.add("pe", htr, reads=[("ident_b", 0)] + [(hnk, h) for h in range(4)], writes=[pkt])
        P.add("dve", (lambda e, ptb=ptb, c=c, nb=nb, cs=cs: e.tensor_tensor(
            out=hmT[:, :, cs], in0=ptb[:, 0:512].rearrange("p (a b) -> p a b", b=128),
            in1=sgo[nb % 2][:, :, (c % 4) * 128:(c % 4 + 1) * 128], op=ALU.mult)),
            reads=[pkt] + [("sgo%d" % (nb % 2), h) for h in range(4)], writes=[("hmT", c)])

    chunk_A(0)
    for c in range(NT):
        if c + 1 < NT:
            chunk_A(c + 1)
        chunk_B(c)
        chunk_C(c)
    dbg_out("hmT", hmT, [("hmT", c) for c in range(NT)])
    for nm in ("qkc", "wif", "wif_f", "nl_hi", "nl_lo", "G", "nlf", "gtmp", "A_inv", "Bv", "dec", "A_", "ktok", "vB", "E", "Cb0", "Cb1", "sm0", "sm1",
               "hn0", "hn1", "st6", "mv", "den", "qq", "rstd", "sgo0", "sgo1"):
        A.free(nm)

    wmo = load_cast("wmo", wmo_d.rearrange("(c p) n -> p c n", p=128), [4, D])
    for h in range(4):
        P.add("dve", (lambda e, h=h: e.tensor_scalar(out=wmo[:, h, :], in0=wmo[:, h, :], scalar1=mng[:, h:h + 1], scalar2=None,
                                                     op0=ALU.mult)),
              reads=[("wmo", 0), ("wmo", 1 + h), ("mng", 0)], writes=[("wmo", 1 + h)])
    mtmp = [A.alloc("mtmp%d" % i, [512], BF16) for i in range(2)]
    for jb in range(2):
        wg_, wgk = load_wblock(4104 + jb * 512)
        for jj in range(4):
            j = jb * 4 + jj
            for nb in range(NB):
                psy, pky = next_ps()

                def ymm2(e, psy=psy, j=j, nb=nb):
                    ins = None
                    for h in range(4):
                        ins = e.matmul(psy[:, :], lhsT=wmo[:, h, j * 128:(j + 1) * 128], rhs=hmT[:, h, nb * 512:(nb + 1) * 512],
                                       start=(h == 0), stop=(h == 3))
                    return ins
                P.add("pe", ymm2, reads=[("wmo", 1 + h) for h in range(4)] + [("hmT", c) for c in range(nb * 4, nb * 4 + 4)],
                      writes=[pky])
                psg2, pkg2 = proj_fm(wg_, wgk, jj, nb)
                sg, sgk = next_sgt()
                P.add("act", (lambda e, sg=sg, p=psg2: e.activation(out=sg, in_=p[:, :], func=AF.Sigmoid)),
                      reads=[pkg2], writes=[sgk])
                mt = mtmp[(j * NB + nb) % 2]
                mtk = ("mtmp%d" % ((j * NB + nb) % 2), 0)
                P.add("dve", (lambda e, sg=sg, p=psy, mt=mt: e.tensor_tensor(out=mt, in0=p[:, :], in1=sg, op=ALU.mult)),
                      reads=[pky, sgk], writes=[mtk])
                P.add("dve", (lambda e, mt=mt, j=j, nb=nb: e.tensor_tensor(
                    out=merged[:, j, nb * 512:(nb + 1) * 512], in0=merged[:, j, nb * 512:(nb + 1) * 512], in1=mt, op=ALU.add)),
                    reads=[mtk, ("merged", j, nb)], writes=[("merged", j, nb)])
    dbg_out("merged", merged, [("merged", j, nb) for j in range(8) for nb in range(NB)])
    for nm in ("hmT", "wmo", "mtmp0", "mtmp1", "sgt0", "sgt1", "hT", "wblk0", "wblk1", "wblk2"):
        A.free(nm)
    if stage <= 3:
        return finish()

    dgf = A.alloc("dgf", [128], F32)
    dgh = A.alloc("dgh", [128], BF16)
    dgl = A.alloc("dgl", [128], BF16)

    def row_bcast(name, col0, src=None, srckey=("modT", 1)):
        src = modT if src is None else src
        row = A.alloc(name, [D], F32)
        banks = [next_ps(), next_ps()]
        for j in range(8):
            P.add("dve", (lambda e, j=j: e.tensor_scalar(out=dgf, in0=ident_f, scalar1=src[:, col0 + j:col0 + j + 1], scalar2=None,
                                                         op0=ALU.mult)),
                  reads=[("ident_f", 0), srckey], writes=[("dgf", 0)])
            P.add("dve", lambda e: e.tensor_copy(out=dgh, in_=dgf), reads=[("dgf", 0)], writes=[("dgh", 0)])
            P.add("dve", lambda e: e.tensor_tensor(out=dgl, in0=dgf, in1=dgh, op=ALU.subtract),
                  reads=[("dgf", 0), ("dgh", 0)], writes=[("dgl", 0)])
            bk, bkk = banks[j // 4]

            def bmm(e, bk=bk, j=j):
                o = bk[:, (j % 4) * 128:(j % 4 + 1) * 128]
                e.matmul(o, lhsT=ones_b, rhs=dgh, start=True, stop=False)
                return e.matmul(o, lhsT=ones_b, rhs=dgl, start=False, stop=True)
            P.add("pe", bmm, reads=[("ones_b", 0), ("dgh", 0), ("dgl", 0)], writes=[bkk])
        for b2 in range(2):
            bk, bkk = banks[b2]
            P.add("act", (lambda e, bk=bk, b2=b2: e.activation(out=row[:, b2 * 512:(b2 + 1) * 512], in_=bk[:, :], func=AF.Identity)),
                  reads=[bkk], writes=[(name, b2)])
        return row

    gt1row = row_bcast("gt1row", 16)
    a2row = row_bcast("a2row", 0, src=a2, srckey=("a2", 0))
    sh2row = row_bcast("sh2row", 24)
    gt2row = row_bcast("gt2row", 40)
    wout = load_cast("wout", wout_d.rearrange("(c p) n -> p c n", p=128), [8, D])
    for k in range(8):
        P.add("dve", (lambda e, k=k: e.tensor_tensor(out=wout[:, k, :], in0=wout[:, k, :], in1=gt1row, op=ALU.mult)),
              reads=[("wout", 0), ("wout", 1 + k), ("gt1row", 0), ("gt1row", 1)], writes=[("wout", 1 + k)])
    x1 = A.alloc("x1", [NT, D], F32)
    for ti in range(NT):
        P.add("sp", (lambda e, ti=ti: e.dma_start(out=x1[:, ti, :], in_=x_d[ti * 128:(ti + 1) * 128, :])),
              writes=[("x1", ti)], dma=True, grp="x1ld%d" % ti)
    wr_f = A.alloc("wr_f", [8, 36], F32)
    wr_b = A.alloc("wr_b", [8, 36], BF16)
    with nc.allow_non_contiguous_dma(reason="small router weight rows"):
        P.add("sp", lambda e: e.dma_start(out=wr_f, in_=wr_d.rearrange("(c p) n -> p c n", p=128)), writes=[("wr_f", 0)],
              dma=True, grp="c_wr")
    P.add("dve", lambda e: e.tensor_copy(out=wr_b, in_=wr_f), reads=[("wr_f", 0)], writes=[("wr_b", 0)])
    brt = load_const("brt", br_d, [36])
    h2tok = A.alloc("h2tok", [NT, D], BF16)
    xn2 = [A.alloc("xn2_%d" % i, [D], F32) for i in range(2)]
    h2T = [A.alloc("h2T%d" % i, [8, 128], BF16) for i in range(2)]
    ss2 = A.alloc("ss2", [NT], F32)
    rs2 = A.alloc("rs2", [NT], F32)
    psr = [hold_ps(), hold_ps()]
    def emit_p5(ti):
        s2 = ti % 2
        P.add("act", (lambda e, ti=ti: e.activation(out=junk, in_=x1[:, ti, :], func=AF.Square, accum_out=ss2[:, ti:ti + 1])),
              reads=[("x1", ti)], writes=[("junk", 0), ("ss2", ti)])
        P.add("act", (lambda e, ti=ti: e.activation(out=rs2[:, ti:ti + 1], in_=ss2[:, ti:ti + 1], func=AF.Sqrt, scale=1.0 / D, bias=1e-6)),
              reads=[("ss2", ti)], writes=[("rs2", ti)])
        P.add("dve", (lambda e, ti=ti: e.reciprocal(out=rs2[:, ti:ti + 1], in_=rs2[:, ti:ti + 1])),
              reads=[("rs2", ti)], writes=[("rs2", ti)])
        P.add("act", (lambda e, ti=ti, s2=s2: e.activation(out=xn2[s2], in_=x1[:, ti, :], func=AF.Identity, scale=rs2[:, ti:ti + 1])),
              reads=[("x1", ti), ("rs2", ti)], writes=[("xn2_%d" % s2, 0)])
        P.add("dve", (lambda e, s2=s2: e.tensor_tensor(out=xn2[s2], in0=xn2[s2], in1=a2row, op=ALU.mult)),
              reads=[("xn2_%d" % s2, 0), ("a2row", 0), ("a2row", 1)], writes=[("xn2_%d" % s2, 0)])
        P.add("dve", (lambda e, s2=s2, ti=ti: e.tensor_tensor(out=h2tok[:, ti, :], in0=xn2[s2], in1=sh2row, op=ALU.add)),
              reads=[("xn2_%d" % s2, 0), ("sh2row", 0), ("sh2row", 1)], writes=[("h2tok", ti)])
        pt, pkt = next_ps()
        ptb = pt.bitcast(BF16)

        def h2tr(e, ptb=ptb, ti=ti):
            ins = None
            for c in range(8):
                ins = e.transpose(out=ptb[:, c * 128:(c + 1) * 128], in_=h2tok[:, ti, c * 128:(c + 1) * 128], identity=ident_b)
            return ins
        P.add("pe", h2tr, reads=[("ident_b", 0), ("h2tok", ti)], writes=[pkt])
        P.add("act", (lambda e, ptb=ptb, s2=s2: e.activation(out=h2T[s2].rearrange("p a b -> p (a b)"), in_=ptb[:, 0:1024], func=AF.Identity)),
              reads=[pkt], writes=[("h2T%d" % s2, 0)])
        bk, bkk = psr[ti // 8]

        def rmm(e, bk=bk, ti=ti, s2=s2):
            ins = None
            o = bk[:, (ti % 8) * 36:(ti % 8 + 1) * 36]
            for k in range(8):
                ins = e.matmul(o, lhsT=h2T[s2][:, k, :], rhs=wr_b[:, k, :], start=(k == 0), stop=(k == 7))
            return ins
        P.add("pe", rmm, reads=[("h2T%d" % s2, 0), ("wr_b", 0)], writes=[bkk])

    def emit_p4(ti):
        for half in range(2):
            ps, pk = next_ps()

            def omm(e, ps=ps, ti=ti, half=half):
                ins = None
                for k in range(8):
                    ins = e.matmul(ps[:, :], lhsT=merged[:, k, ti * 128:(ti + 1) * 128], rhs=wout[:, k, half * 512:(half + 1) * 512],
                                   start=(k == 0), stop=(k == 7))
                return ins
            P.add("pe", omm, reads=[("wout", 1 + k) for k in range(8)] + [("merged", k, ti // 4) for k in range(8)], writes=[pk])
            P.add("dve", (lambda e, ps=ps, ti=ti, half=half: e.tensor_tensor(
                out=x1[:, ti, half * 512:(half + 1) * 512], in0=x1[:, ti, half * 512:(half + 1) * 512], in1=ps[:, :], op=ALU.add)),
                reads=[pk, ("x1", ti)], writes=[("x1", ti)])

    emit_p4(0)
    for ti in range(NT):
        if ti + 1 < NT:
            emit_p4(ti + 1)
        emit_p5(ti)
    dbg_out("x1", x1, [("x1", ti) for ti in range(NT)])
    A.free("merged"); A.free("wout"); A.free("gt1row")

    Lg = A.alloc("Lg", [NT, 36], F32)
    for b2 in range(2):
        bk, bkk = psr[b2]
        P.add("dve", (lambda e, bk=bk, b2=b2: e.tensor_tensor(
            out=Lg[:, b2 * 8:(b2 + 1) * 8, :], in0=bk[:, 0:288].rearrange("p (a b) -> p a b", b=36),
            in1=brt.unsqueeze(1).to_broadcast([128, 8, 36]), op=ALU.add)),
            reads=[bkk, ("brt", 0)], writes=[("Lg", b2)])
    release_ps(psr[0][1]); release_ps(psr[1][1])
    dbg_out("Lg", Lg, [("Lg", 0), ("Lg", 1)])
    dbg_out("h2tok", h2tok, [("h2tok", ti) for ti in range(NT)])
    for nm in ("xn2_0", "xn2_1", "h2T0", "h2T1", "a2row", "sh2row", "wr_f", "wr_b"):
        A.free(nm)
    if stage <= 5:
        return finish()

    NCHK0 = NG - 15
    def T_(name, shape, dt=F32):
        return A.alloc(name, shape, dt)
    LK = [("Lg", 0), ("Lg", 1)]
    lg = Lg[:, :, 0:4]
    le = Lg[:, :, 4:36]
    gmax = T_("gmax", [NT])
    G1h = T_("G1h", [NT, 4])
    egs = T_("egs", [NT, 4])
    p_g = T_("p_g", [NT])
    P.add("dve", lambda e: e.tensor_reduce(out=gmax, in_=lg, axis=AX.X, op=ALU.max), reads=LK, writes=[("gmax", 0)])
    gmb = gmax.unsqueeze(2).to_broadcast([128, NT, 4])
    P.add("dve", lambda e: e.tensor_tensor(out=G1h, in0=lg, in1=gmb, op=ALU.is_equal), reads=LK + [("gmax", 0)], writes=[("G1h", 0)])
    P.add("dve", lambda e: e.tensor_tensor(out=egs, in0=lg, in1=gmb, op=ALU.subtract), reads=LK + [("gmax", 0)], writes=[("egs", 0)])
    P.add("act", lambda e: e.activation(out=egs, in_=egs, func=AF.Exp), reads=[("egs", 0)], writes=[("egs", 0)])
    P.add("dve", lambda e: e.tensor_reduce(out=p_g, in_=egs, axis=AX.X, op=ALU.add), reads=[("egs", 0)], writes=[("p_g", 0)])
    P.add("dve", lambda e: e.reciprocal(out=p_g, in_=p_g), reads=[("p_g", 0)], writes=[("p_g", 0)])
    tmp32 = T_("tmp32", [NT, 32])
    lsel = T_("lsel", [NT, 8])
    P.add("dve", lambda e: e.tensor_tensor(
        out=tmp32.rearrange("p t (g j) -> p t g j", j=8), in0=le.rearrange("p t (g j) -> p t g j", j=8),
        in1=G1h.unsqueeze(3).to_broadcast([128, NT, 4, 8]), op=ALU.mult),
        reads=LK + [("G1h", 0)], writes=[("tmp32", 0)])
    P.add("dve", lambda e: e.tensor_reduce(out=lsel, in_=tmp32.rearrange("p t (g j) -> p t j g", j=8), axis=AX.X, op=ALU.add),
          reads=[("tmp32", 0)], writes=[("lsel", 0)])
    m1 = T_("m1", [NT])
    m2 = T_("m2", [NT])
    E1 = T_("E1", [NT, 8])
    E2 = T_("E2", [NT, 8])
    ls2 = T_("ls2", [NT, 8])
    P.add("dve", lambda e: e.tensor_reduce(out=m1, in_=lsel, axis=AX.X, op=ALU.max), reads=[("lsel", 0)], writes=[("m1", 0)])
    P.add("dve", lambda e: e.tensor_tensor(out=E1, in0=lsel, in1=m1.unsqueeze(2).to_broadcast([128, NT, 8]), op=ALU.is_equal),
          reads=[("lsel", 0), ("m1", 0)], writes=[("E1", 0)])
    P.add("dve", lambda e: e.scalar_tensor_tensor(out=ls2.rearrange("p a b -> p (a b)"), in0=E1.rearrange("p a b -> p (a b)"),
                                                  scalar=-1e30, in1=lsel.rearrange("p a b -> p (a b)"), op0=ALU.mult, op1=ALU.add),
          reads=[("E1", 0), ("lsel", 0)], writes=[("ls2", 0)])
    P.add("dve", lambda e: e.tensor_reduce(out=m2, in_=ls2, axis=AX.X, op=ALU.max), reads=[("ls2", 0)], writes=[("m2", 0)])
    P.add("dve", lambda e: e.tensor_tensor(out=E2, in0=ls2, in1=m2.unsqueeze(2).to_broadcast([128, NT, 8]), op=ALU.is_equal),
          reads=[("ls2", 0), ("m2", 0)], writes=[("E2", 0)])
    w1 = T_("w1", [NT])
    w2 = T_("w2", [NT])
    P.add("dve", lambda e: e.tensor_tensor(out=w2, in0=m1, in1=m2, op=ALU.subtract), reads=[("m1", 0), ("m2", 0)], writes=[("w2", 0)])
    P.add("act", lambda e: e.activation(out=w1, in_=w2, func=AF.Sigmoid), reads=[("w2", 0)], writes=[("w1", 0)])
    P.add("dve", lambda e: e.tensor_tensor(out=w1, in0=w1, in1=p_g, op=ALU.mult), reads=[("w1", 0), ("p_g", 0)], writes=[("w1", 0)])
    P.add("dve", lambda e: e.tensor_tensor(out=w2, in0=p_g, in1=w1, op=ALU.subtract), reads=[("w1", 0), ("p_g", 0), ("w2", 0)], writes=[("w2", 0)])
    A1 = T_("A1", [NT, 32])
    A2 = T_("A2", [NT, 32])
    A12b = T_("A12b", [NT, 32], BF16)
    for g in range(4):
        gb = G1h[:, :, g].unsqueeze(2).to_broadcast([128, NT, 8])
        P.add("dve", (lambda e, g=g, gb=gb: e.tensor_tensor(out=A1[:, :, g * 8:(g + 1) * 8], in0=E1, in1=gb, op=ALU.mult)),
              reads=[("E1", 0), ("G1h", 0)], writes=[("A1", g)])
        P.add("dve", (lambda e, g=g, gb=gb: e.tensor_tensor(out=A2[:, :, g * 8:(g + 1) * 8], in0=E2, in1=gb, op=ALU.mult)),
              reads=[("E2", 0), ("G1h", 0)], writes=[("A2", g)])
    AK = [("A1", g) for g in range(4)] + [("A2", g) for g in range(4)]
    P.add("dve", lambda e: e.tensor_tensor(out=A12b, in0=A1, in1=A2, op=ALU.add), reads=AK, writes=[("A12b", 0)])
    lstrict = T_("lstrict", [128], BF16)
    lsf = T_("lsf", [128], F32)
    P.add("pool", lambda e: e.memset(lsf, 1.0), writes=[("lsf", 0)])
    P.add("pool", lambda e: e.affine_select(out=lsf, in_=lsf, pattern=[[1, 128]], compare_op=ALU.is_ge, fill=0.0, base=-1,
                                             channel_multiplier=-1), reads=[("lsf", 0)], writes=[("lsf", 0)])
    P.add("pool", lambda e: e.tensor_copy(out=lstrict, in_=lsf), reads=[("lsf", 0)], writes=[("lstrict", 0)])
    psw, pkw = next_ps()
    pst_, pkt_ = next_ps()
    A12f = A12b.rearrange("p a b -> p (a b)")
    P.add("pe", lambda e: e.matmul(psw[:, :], lhsT=lstrict, rhs=A12f, start=True, stop=True),
          reads=[("lstrict", 0), ("A12b", 0)], writes=[pkw])
    P.add("pe", lambda e: e.matmul(pst_[:, :], lhsT=ones_b, rhs=A12f, start=True, stop=True),
          reads=[("ones_b", 0), ("A12b", 0)], writes=[pkt_])
    carry = T_("carry", [NT + 1, 32])
    P.add("dve", lambda e: e.memset(carry[:, 0, :], 0.0), writes=[("carry", 0)])
    for ti in range(NT):
        P.add("dve", (lambda e, ti=ti: e.tensor_tensor(out=carry[:, ti + 1, :], in0=carry[:, ti, :], in1=pst_[:, ti * 32:(ti + 1) * 32],
                                                      op=ALU.add)),
              reads=[("carry", ti), pkt_], writes=[("carry", ti + 1)])
    counts = carry[:, NT, :]
    CK = [("carry", ti) for ti in range(NT + 1)]
    thr = T_("thr", [64])
    thr_i = T_("thr_i", [64], I32)
    P.add("pool", lambda e: e.iota(thr_i, pattern=[[1, 64]], base=0, channel_multiplier=0), writes=[("thr_i", 0)])
    P.add("pool", lambda e: e.tensor_copy(out=thr, in_=thr_i), reads=[("thr_i", 0)], writes=[("thr", 0)])
    thr128 = T_("thr128", [8])
    P.add("pool", lambda e: e.tensor_scalar(out=thr128, in0=thr[:, 0:8], scalar1=float(GROWS), scalar2=None, op0=ALU.mult),
          reads=[("thr", 0)], writes=[("thr128", 0)])
    cmp1 = T_("cmp1", [32, 8])
    ngrp = T_("ngrp", [32])
    P.add("dve", lambda e: e.tensor_tensor(out=cmp1, in0=counts.unsqueeze(2).to_broadcast([128, 32, 8]),
                                           in1=thr128.unsqueeze(1).to_broadcast([128, 32, 8]), op=ALU.is_gt),
          reads=CK + [("thr128", 0)], writes=[("cmp1", 0)])
    P.add("dve", lambda e: e.tensor_reduce(out=ngrp, in_=cmp1, axis=AX.X, op=ALU.add), reads=[("cmp1", 0)], writes=[("ngrp", 0)])
    cs = [T_("cs0", [32]), T_("cs1", [32])]
    src, srck = ngrp, ("ngrp", 0)
    for si, sh in enumerate((1, 2, 4, 8, 16)):
        dst = cs[si % 2]
        dk = ("cs%d" % (si % 2),)
        P.add("dve", (lambda e, dst=dst, src=src, sh=sh: e.tensor_copy(out=dst[:, 0:sh], in_=src[:, 0:sh])),
              reads=[srck], writes=[dk + (0,)])
        P.add("dve", (lambda e, dst=dst, src=src, sh=sh: e.tensor_tensor(out=dst[:, sh:32], in0=src[:, sh:32], in1=src[:, 0:32 - sh],
                                                                        op=ALU.add)),
              reads=[srck], writes=[dk + (1,)])
        src, srck = dst, dk + (1,)
        if si > 0:
            pass
    pend = src
    PK = [("cs0", 0), ("cs0", 1), ("cs1", 0), ("cs1", 1)]
    pstart = T_("pstart", [32])
    P.add("dve", lambda e: e.tensor_tensor(out=pstart, in0=pend, in1=ngrp, op=ALU.subtract), reads=PK + [("ngrp", 0)],
          writes=[("pstart", 0)])
    P.add("dve", lambda e: e.tensor_scalar(out=pstart, in0=pstart, scalar1=float(GROWS), scalar2=None, op0=ALU.mult),
          reads=[("pstart", 0)], writes=[("pstart", 0)])
    cmp2 = T_("cmp2", [NG, 32])
    grpf = T_("grpf", [NG])
    grpi = T_("grpi", [NG], I32)
    P.add("dve", lambda e: e.tensor_tensor(out=cmp2, in0=pend.unsqueeze(1).to_broadcast([128, NG, 32]),
                                           in1=thr[:, 0:NG].unsqueeze(2).to_broadcast([128, NG, 32]), op=ALU.is_le),
          reads=PK + [("thr", 0)], writes=[("cmp2", 0)])
    P.add("dve", lambda e: e.tensor_reduce(out=grpf, in_=cmp2, axis=AX.X, op=ALU.add), reads=[("cmp2", 0)], writes=[("grpf", 0)])
    P.add("dve", lambda e: e.tensor_scalar(out=grpf, in0=grpf, scalar1=31.0, scalar2=None, op0=ALU.min),
          reads=[("grpf", 0)], writes=[("grpf", 0)])
    P.add("dve", lambda e: e.tensor_copy(out=grpi, in_=grpf), reads=[("grpf", 0)], writes=[("grpi", 0)])
    pidx_i = T_("pidx_i", [1], I32)
    pidx = T_("pidx", [1])
    idxf = T_("idxf", [NG])
    inval = T_("inval", [NG])
    idxw = T_("idxw", [NG], I32)
    idxs = T_("idxs", [NG], I32)
    P.add("pool", lambda e: e.iota(pidx_i, pattern=[[0, 1]], base=0, channel_multiplier=1), writes=[("pidx_i", 0)])
    P.add("pool", lambda e: e.tensor_copy(out=pidx, in_=pidx_i), reads=[("pidx_i", 0)], writes=[("pidx", 0)])
    P.add("dve", lambda e: e.tensor_scalar(out=idxf, in0=grpf, scalar1=128.0, scalar2=pidx[:, 0:1], op0=ALU.mult, op1=ALU.add),
          reads=[("grpf", 0), ("pidx", 0)], writes=[("idxf", 0)])
    P.add("dve", lambda e: e.tensor_scalar(out=inval, in0=thr[:, 0:NG], scalar1=pend[:, 31:32], scalar2=None, op0=ALU.is_ge),
          reads=PK + [("thr", 0)], writes=[("inval", 0)])
    P.add("dve", lambda e: e.tensor_copy(out=idxw, in_=idxf), reads=[("idxf", 0)], writes=[("idxw", 0)])
    P.add("dve", lambda e: e.scalar_tensor_tensor(out=idxf, in0=inval, scalar=1.0e6, in1=idxf, op0=ALU.mult, op1=ALU.add),
          reads=[("inval", 0), ("idxf", 0), ("idxw", 0)], writes=[("idxf", 0)])
    P.add("dve", lambda e: e.tensor_copy(out=idxs, in_=idxf), reads=[("idxf", 0)], writes=[("idxs", 0)])
    stg = {wn: A.alloc("stg_" + wn, [4096], F32) for wn in ("wg", "wu", "wd")}
    for (wsrc_, wn_) in ((weg_d, "wg"), (weu_d, "wu"), (wed_d, "wd")):
        P.add("pool", (lambda e, wsrc_=wsrc_, wn_=wn_: e.indirect_dma_start(
            out=stg[wn_], out_offset=None, in_=wsrc_, in_offset=bass.IndirectOffsetOnAxis(ap=idxw[:, 0:1], axis=0))),
            reads=[("idxw", 0)], writes=[("stg_" + wn_, 0)], dma=True, grp="stg_" + wn_)
    slot = T_("slot", [NT, 32])
    P.add("dve", lambda e: e.tensor_tensor(out=slot, in0=psw[:, :].rearrange("p (a b) -> p a b", b=32), in1=carry[:, 0:NT, :], op=ALU.add),
          reads=[pkw] + CK, writes=[("slot", 0)])
    P.add("dve", lambda e: e.tensor_tensor(out=slot, in0=slot, in1=pstart.unsqueeze(1).to_broadcast([128, NT, 32]), op=ALU.add),
          reads=[("slot", 0), ("pstart", 0)], writes=[("slot", 0)])
    dstf = T_("dstf", [2, NT])
    dsti = T_("dsti", [2, NT], I32)
    for q, (Aq, qk) in enumerate(((A1, "A1"), (A2, "A2"))):
        P.add("dve", (lambda e, Aq=Aq: e.tensor_tensor(out=tmp32, in0=Aq, in1=slot, op=ALU.mult)),
              reads=[(qk, g) for g in range(4)] + [("slot", 0), ("tmp32", 0)], writes=[("tmp32", 0)])
        P.add("dve", (lambda e, q=q: e.tensor_reduce(out=dstf[:, q, :], in_=tmp32, axis=AX.X, op=ALU.add)),
              reads=[("tmp32", 0)], writes=[("dstf", q)])
    P.add("dve", lambda e: e.tensor_copy(out=dsti, in_=dstf), reads=[("dstf", 0), ("dstf", 1)], writes=[("dsti", 0)])
    dbg_out("dstf", dstf, [("dstf", 0), ("dstf", 1)])
    dbg_out("grpf", grpf, [("grpf", 0)])
    dbg_out("w1", w1, [("w1", 0)])
    dbg_out("w2", w2, [("w2", 0)])
    for nm in ("gmax", "G1h", "egs", "p_g", "tmp32", "lsel", "m1", "m2", "E1", "E2", "ls2", "A1", "A2", "A12b", "lstrict", "lsf",
               "carry", "thr", "thr_i", "thr128", "cmp1", "ngrp", "cs0", "cs1", "pstart", "slot", "cmp2", "Lg"):
        A.free(nm)
    if stage <= 5.5:
        return finish()

    for ti in range(NT):
        for q in range(2):
            P.add("pool", (lambda e, ti=ti, q=q: e.indirect_dma_start(
                out=XS, out_offset=bass.IndirectOffsetOnAxis(ap=dsti[:, q, ti:ti + 1], axis=0),
                in_=h2tok[:, ti, :], in_offset=None)),
                reads=[("h2tok", ti), ("dsti", 0)] + XSZ_KEYS, writes=[("XS", ti, q)], dma=True, grp="xs_sc")
    XS_KEYS = [("XS", ti, q) for ti in range(NT) for q in range(2)]
    A.free("h2tok")
    NS = 2
    wgs = [A.alloc("wg%d" % i, [8, 512], BF16) for i in range(NS)]
    wus = [A.alloc("wu%d" % i, [8, 512], BF16) for i in range(NS)]
    wds = [A.alloc("wd%d" % i, [4, D], BF16) for i in range(NS)]
    xgt = [A.alloc("xgt%d" % i, [D], BF16) for i in range(2)]
    xgT = [A.alloc("xgT%d" % i, [8, 256], BF16) for i in range(2)]
    sgl = [A.alloc("sgl%d" % i, [4, 256], BF16) for i in range(1)]
    aT = [A.alloc("aT%d" % i, [4, 256], BF16) for i in range(2)]
    ysb = [A.alloc("ysb%d" % i, [D], BF16) for i in range(2)]
    def emit_load(g, part="both"):
        sl = g % NS
        s2 = g % 2
        for (wt, wsrc, wn, ceng) in ((wgs, weg_d, "wg", "act"), (wus, weu_d, "wu", "dve"), (wds, wed_d, "wd", "pool")):
            st_ = stg[wn]
            if part in ("both", "dma") and g >= NCHK0:
                P.add("pool", (lambda e, g=g, st_=st_, wsrc=wsrc: e.indirect_dma_start(
                    out=st_, out_offset=None, in_=wsrc,
                    in_offset=bass.IndirectOffsetOnAxis(ap=idxs[:, g:g + 1], axis=0), bounds_check=32 * 128 - 1, oob_is_err=False)),
                    reads=[("idxs", 0)], writes=[("stg_" + wn, 0)], dma=True, grp="stg_" + wn)
            elif part in ("both", "dma"):
                P.add("pool", (lambda e, g=g, st_=st_, wsrc=wsrc: e.indirect_dma_start(
                    out=st_, out_offset=None, in_=wsrc,
                    in_offset=bass.IndirectOffsetOnAxis(ap=idxw[:, g:g + 1], axis=0))),
                    reads=[("idxw", 0)], writes=[("stg_" + wn, 0)], dma=True, grp="stg_" + wn)
            if part == "dma":
                continue
            dstv = wt[sl].rearrange("p a b -> p (a b)")
            if ceng == "act":
                P.add("act", (lambda e, dstv=dstv, st_=st_: e.activation(out=dstv, in_=st_, func=AF.Identity)),
                      reads=[("stg_" + wn, 0)], writes=[("%s%d" % (wn, sl), 0)])
            elif ceng == "dve":
                P.add("dve", (lambda e, dstv=dstv, st_=st_: e.tensor_copy(out=dstv, in_=st_)),
                      reads=[("stg_" + wn, 0)], writes=[("%s%d" % (wn, sl), 0)])
            else:
                P.add("act", (lambda e, dstv=dstv, st_=st_: e.activation(out=dstv[:, 0:2048], in_=st_[:, 0:2048], func=AF.Identity)),
                      reads=[("stg_" + wn, 0)], writes=[("%s%d" % (wn, sl), 0)])
                P.add("dve", (lambda e, dstv=dstv, st_=st_: e.tensor_copy(out=dstv[:, 2048:4096], in_=st_[:, 2048:4096])),
                      reads=[("stg_" + wn, 0)], writes=[("%s%d" % (wn, sl), 1)])

    def emit_compute_a(g):
        s2 = g % 2
        for hf in range(2):
            xi = (2 * g + hf) % 2
            r0 = g * GROWS + hf * 128
            P.add("sp", (lambda e, r0=r0, xi=xi: e.dma_start(out=xgt[xi], in_=XS[r0:r0 + 128, :])),
                  reads=XS_KEYS, writes=[("xgt%d" % xi, 0)], dma=True, grp="xgt%d" % xi)
            pt, pkt = next_ps()
            ptb = pt.bitcast(BF16)

            def xtr(e, ptb=ptb, xi=xi):
                ins = None
                for c in range(8):
                    ins = e.transpose(out=ptb[:, c * 128:(c + 1) * 128], in_=xgt[xi][:, c * 128:(c + 1) * 128], identity=ident_b)
                return ins
            P.add("pe", xtr, reads=[("ident_b", 0), ("xgt%d" % xi, 0)], writes=[pkt])
            P.add("act", (lambda e, ptb=ptb, s2=s2, hf=hf: e.activation(
                out=xgT[s2][:, :, hf * 128:(hf + 1) * 128], in_=ptb[:, 0:1024].rearrange("p (a b) -> p a b", b=128), func=AF.Identity)),
                reads=[pkt], writes=[("xgT%d" % s2, hf)])

    def emit_compute_b1(g):
        sl = g % NS
        s2 = g % 2
        pg_ = [next_ps(), next_ps()]
        pu_ = [next_ps(), next_ps()]

        def gumm(e, pg_=pg_, pu_=pu_, sl=sl, s2=s2):
            ins = None
            for (pp, ww) in ((pg_, wgs), (pu_, wus)):
                for fc in range(4):
                    o = pp[fc // 2][0][:, (fc % 2) * 256:(fc % 2 + 1) * 256]
                    for k in range(8):
                        ins = e.matmul(o, lhsT=ww[sl][:, k, fc * 128:(fc + 1) * 128], rhs=xgT[s2][:, k, :], start=(k == 0), stop=(k == 7))
            return ins
        P.add("pe", gumm, reads=[("wg%d" % sl, 0), ("wu%d" % sl, 0), ("xgT%d" % s2, 0), ("xgT%d" % s2, 1)],
              writes=[pg_[0][1], pg_[1][1], pu_[0][1], pu_[1][1]])
        for b2 in range(2):
            P.add("act", (lambda e, pg_=pg_, s2=s2, b2=b2: e.activation(
                out=sgl[0][:, 2 * b2:2 * b2 + 2, :].rearrange("p a b -> p (a b)"), in_=pg_[b2][0][:, :], func=AF.Silu)),
                reads=[pg_[b2][1]], writes=[("sgl0", b2)])
            P.add("dve", (lambda e, pu_=pu_, s2=s2, b2=b2: e.tensor_tensor(
                out=aT[s2][:, 2 * b2:2 * b2 + 2, :].rearrange("p a b -> p (a b)"), in0=pu_[b2][0][:, :],
                in1=sgl[0][:, 2 * b2:2 * b2 + 2, :].rearrange("p a b -> p (a b)"), op=ALU.mult)),
                reads=[pu_[b2][1], ("sgl0", b2)], writes=[("aT%d" % s2, b2)])

    def emit_compute_b2(g):
        sl = g % NS
        s2 = g % 2
        for hf in range(2):
            yi = (2 * g + hf) % 2
            py = [next_ps(), next_ps()]

            def dmm(e, py=py, sl=sl, s2=s2, hf=hf):
                ins = None
                for half in range(2):
                    for fc in range(4):
                        ins = e.matmul(py[half][0][:, :], lhsT=aT[s2][:, fc, hf * 128:(hf + 1) * 128],
                                       rhs=wds[sl][:, fc, half * 512:(half + 1) * 512], start=(fc == 0), stop=(fc == 3))
                return ins
            P.add("pe", dmm, reads=[("wd%d" % sl, 0), ("wd%d" % sl, 1), ("aT%d" % s2, 0), ("aT%d" % s2, 1)], writes=[py[0][1], py[1][1]])
            for half in range(2):
                P.add("dve", (lambda e, py=py, yi=yi, half=half: e.tensor_tensor(
                    out=ysb[yi][:, half * 512:(half + 1) * 512], in0=py[half][0][:, :], in1=gt2row[:, half * 512:(half + 1) * 512],
                    op=ALU.mult)),
                    reads=[py[half][1], ("gt2row", half)], writes=[("ysb%d" % yi, half)])
            r0 = g * GROWS + hf * 128
            P.add("sp", (lambda e, r0=r0, yi=yi: e.dma_start(out=YS[r0:r0 + 128, :], in_=ysb[yi])),
                  reads=[("ysb%d" % yi, 0), ("ysb%d" % yi, 1)], writes=[("YS", g, hf)], dma=True, grp="ys_st")

    emit_load(0, "cast")
    emit_compute_a(0)
    for g in range(NG):
        emit_compute_b1(g)
        if g + 1 < NG:
            emit_load(g + 1)
            emit_compute_a(g + 1)
        emit_compute_b2(g)
    YS_KEYS = [("YS", g, hf) for g in range(NG) for hf in range(2)]
    for i in range(NS):
        A.free("wg%d" % i); A.free("wu%d" % i); A.free("wd%d" % i)
    for nm in ("stg_wg", "stg_wu", "stg_wd", "xgt0", "xgt1", "xgT0", "xgT1", "sgl0", "aT0", "aT1", "ysb0", "ysb1"):
        A.free(nm)

    gfin = load_const("gfin", gfin_d, [D])
    NYG = 4
    yg = [[A.alloc("yg%d_%d" % (q, i), [D], BF16) for i in range(NYG)] for q in range(2)]
    acc = [A.alloc("acc%d" % i, [D], F32) for i in range(2)]
    outt = [A.alloc("outt%d" % i, [D], F32) for i in range(2)]
    ssf = A.alloc("ssf", [NT], F32)
    rsf = A.alloc("rsf", [NT], F32)

    def emit_g(ti):
        s4 = ti % NYG
        for q in range(2):
            P.add("pool", (lambda e, ti=ti, q=q, s4=s4: e.indirect_dma_start(
                out=yg[q][s4], out_offset=None, in_=YS,
                in_offset=bass.IndirectOffsetOnAxis(ap=dsti[:, q, ti:ti + 1], axis=0))),
                reads=YS_KEYS + [("dsti", 0)], writes=[("yg%d_%d" % (q, s4), 0)], dma=True, grp="yg%d_%d" % (q, s4))

    def emit_c1(ti):
        s4 = ti % NYG
        s2 = ti % 2
        y1, y2 = yg[0][s4], yg[1][s4]
        k1, k2 = ("yg0_%d" % s4, 0), ("yg1_%d" % s4, 0)
        ac = acc[s2]
        ak = ("acc%d" % s2, 0)
        P.add("act", (lambda e, y1=y1, ac=ac, ti=ti: e.activation(out=ac, in_=y1, func=AF.Identity, scale=w1[:, ti:ti + 1])),
              reads=[k1, ("w1", 0)], writes=[ak])
        P.add("dve", (lambda e, ac=ac, y2=y2, ti=ti: e.scalar_tensor_tensor(out=ac, in0=y2, scalar=w2[:, ti:ti + 1], in1=ac,
                                                                          op0=ALU.mult, op1=ALU.add)),
              reads=[ak, k2, ("w2", 0)], writes=[ak])
        P.add("dve", (lambda e, ac=ac, ti=ti: e.tensor_tensor(out=x1[:, ti, :], in0=x1[:, ti, :], in1=ac, op=ALU.add)),
              reads=[ak, ("x1", ti)], writes=[("x1", ti)])

    def emit_c2(ti):
        s2 = ti % 2
        P.add("act", (lambda e, ti=ti: e.activation(out=junk, in_=x1[:, ti, :], func=AF.Square, accum_out=ssf[:, ti:ti + 1])),
              reads=[("x1", ti)], writes=[("junk", 0), ("ssf", ti)])
        P.add("act", (lambda e, ti=ti: e.activation(out=rsf[:, ti:ti + 1], in_=ssf[:, ti:ti + 1], func=AF.Sqrt, scale=1.0 / D, bias=1e-6)),
              reads=[("ssf", ti)], writes=[("rsf", ti)])
        P.add("dve", (lambda e, ti=ti: e.reciprocal(out=rsf[:, ti:ti + 1], in_=rsf[:, ti:ti + 1])),
              reads=[("rsf", ti)], writes=[("rsf", ti)])
        ot = outt[s2]
        ok = ("outt%d" % s2, 0)
        P.add("act", (lambda e, ot=ot, ti=ti: e.activation(out=ot, in_=x1[:, ti, :], func=AF.Identity, scale=rsf[:, ti:ti + 1])),
              reads=[("x1", ti), ("rsf", ti)], writes=[ok])
        P.add("dve", (lambda e, ot=ot: e.tensor_tensor(out=ot, in0=ot, in1=gfin, op=ALU.mult)),
              reads=[ok, ("gfin", 0)], writes=[ok])
        P.add("sp", (lambda e, ot=ot, ti=ti: e.dma_start(out=out_d[ti * 128:(ti + 1) * 128, :], in_=ot)),
              reads=[ok], dma=True, grp="out")

    for ti in range(min(3, NT)):
        emit_g(ti)
    for ti in range(NT):
        if ti + 3 < NT:
            emit_g(ti + 3)
        emit_c1(ti)
        if ti > 0:
            emit_c2(ti - 1)
    emit_c2(NT - 1)
    P.emit(final_wait_groups=["out"] + (["dbgout"] if "dbgout" in P.dma_groups else []))
    build.stats = dict(peak_kb=A.peak * 4 / 1024.0, n_ops=len(P.all_ops), n_groups=len(P.dma_groups))
    return nc, dbg_outs


def host_layout(inp, b):
    f = lambda a: np.ascontiguousarray(a, dtype=np.float32)
    col = lambda v, n: f(np.asarray(v).reshape(n, 128).T)
    m = {}
    m["x"] = f(inp["x"][b])
    m["c_col"] = col(inp["c"][b], 8)
    m["w_ada"] = f(inp["w_ada"][0])
    m["b_ada_col"] = col(inp["b_ada"][0], 48)
    m["g1_col"] = col(inp["g_norm1"][0], 8)
    m["g2_col"] = col(inp["g_norm2"][0], 8)
    m["w_in"] = f(inp["w_in"][0])
    m["b_if_bc"] = f(np.broadcast_to(inp["b_if"][0][None, :], (128, 8)))
    m["conv_w_col"] = f(inp["conv_dw_w"][0].reshape(31, 4, 128).transpose(2, 1, 0))
    m["conv_b_col"] = col(inp["conv_dw_b"][0], 4)
    m["conv_lng_col"] = col(inp["conv_ln_g"][0], 4)
    m["conv_lnb_col"] = col(inp["conv_ln_b"][0], 4)
    m["w_conv_out"] = f(inp["w_conv_out"][0])
    m["qk_w_col"] = f(inp["qk_conv_w"][0].reshape(4, 8, 128).transpose(2, 1, 0))
    m["qk_b_col"] = col(inp["qk_conv_b"][0], 8)
    m["mng_col"] = col(inp["m_norm_g"][0], 4)
    m["w_m_out"] = f(inp["w_m_out"][0])
    m["w_out"] = f(inp["w_out"][0])
    m["w_router"] = f(np.concatenate([inp["w_rg"][0], inp["w_re"][0]], axis=1))
    m["b_router_bc"] = f(np.broadcast_to(np.concatenate([inp["b_rg"][0], inp["b_re"][0]])[None, :], (128, 36)))
    m["w_e_gate_l"] = f(inp["w_e_gate"][0].reshape(32, 8, 128, 512).transpose(0, 2, 1, 3).reshape(32 * 128, 8 * 512))
    m["w_e_up_l"] = f(inp["w_e_up"][0].reshape(32, 8, 128, 512).transpose(0, 2, 1, 3).reshape(32 * 128, 8 * 512))
    m["w_e_down_l"] = f(inp["w_e_down"][0].reshape(32, 4, 128, D).transpose(0, 2, 1, 3).reshape(32 * 128, 4 * D))
    m["g_final_bc"] = f(np.broadcast_to(np.asarray(inp["g_final"])[None, :], (128, D)))
    return m


def kernel(**inputs):
    nc, _ = build()
    shared = host_layout(inputs, 0)
    in_maps = []
    for b in range(8):
        m = dict(shared)
        m["x"] = np.ascontiguousarray(inputs["x"][b], dtype=np.float32)
        m["c_col"] = np.ascontiguousarray(np.asarray(inputs["c"][b]).reshape(8, 128).T, dtype=np.float32)
        in_maps.append(m)
    res = run_bass_kernel_spmd(nc, in_maps, core_ids=list(range(8)))
    return np.stack([np.asarray(r["out"]) for r in res.results], axis=0).astype(np.float32)
```

```python
import contextlib
import numpy as np
import concourse.bass as bass
import concourse.mybir as mybir
from concourse.bass_utils import run_bass_kernel_spmd

F32 = mybir.dt.float32
BF16 = mybir.dt.bfloat16
I32 = mybir.dt.int32
AF = mybir.ActivationFunctionType
ALU = mybir.AluOpType
AX = mybir.AxisListType

T = 2048
D = 1024
NT = 16
NB = 4
DIN = 5128
ENG_NAMES = ("pe", "act", "dve", "pool", "sp")


class Op:
    __slots__ = ("eng", "fn", "is_dma", "grp", "signal", "val", "idx", "deps")

    def __init__(self, eng, fn, is_dma, grp):
        self.eng = eng
        self.fn = fn
        self.is_dma = is_dma
        self.grp = grp
        self.signal = False
        self.val = None
        self.idx = None
        self.deps = []


def _reduce_ops(ops):
    latest = {}
    dm = {}
    for o in ops:
        if o.is_dma:
            if o.grp not in dm or dm[o.grp].idx < o.idx:
                dm[o.grp] = o
        else:
            if o.eng not in latest or latest[o.eng].idx < o.idx:
                latest[o.eng] = o
    return list(latest.values()) + list(dm.values())


class Prog:
    def __init__(self, nc):
        self.nc = nc
        self.ops = {e: [] for e in ENG_NAMES}
        self.all_ops = []
        self.last_writer = {}
        self.readers = {}
        self.dma_groups = {}
        self.buf_pred = {}
        self.keys_by_buf = {}
        self.wait_all_groups = set()

    def _touch(self, k):
        if k not in self.readers:
            self.readers[k] = list(self.buf_pred.get(k[0], ()))
            self.last_writer[k] = None
            self.keys_by_buf.setdefault(k[0], set()).add(k)

    def ops_touching(self, bufname):
        s = list(self.buf_pred.get(bufname, ()))
        for k in self.keys_by_buf.get(bufname, ()):
            w = self.last_writer.get(k)
            if w is not None:
                s.append(w)
            s.extend(self.readers.get(k, ()))
        return _reduce_ops(s)

    def add(self, eng, fn, reads=(), writes=(), dma=False, grp=None):
        op = Op(eng, fn, dma, grp)
        op.idx = len(self.all_ops)
        self.all_ops.append(op)
        self.ops[eng].append(op)
        if dma:
            assert grp is not None
            self.dma_groups.setdefault(grp, []).append(op)
        deps = []
        for k in reads:
            self._touch(k)
            w = self.last_writer[k]
            if w is not None:
                deps.append((w, "raw"))
            elif self.readers[k] and k[0] in self.buf_pred:
                pass
        for k in writes:
            self._touch(k)
            w = self.last_writer[k]
            if w is not None:
                deps.append((w, "waw"))
            for r in self.readers[k]:
                deps.append((r, "war"))
        for d, kind in deps:
            if d is op:
                continue
            if (not d.is_dma) and (not dma) and d.eng == eng:
                if eng == "pe":
                    continue
            op.deps.append(d)
        for k in reads:
            self.readers[k].append(op)
        for k in writes:
            self.last_writer[k] = op
            self.readers[k] = []
        return op

    def emit(self, final_wait_groups=()):
        nc = self.nc
        for op in self.all_ops:
            op.deps = _reduce_ops(op.deps)
            for d in op.deps:
                d.signal = True
        for e in ENG_NAMES:
            c = 0
            for op in self.ops[e]:
                if (not op.is_dma) and op.signal:
                    c += 1
                    op.val = c
        gtotal = {}
        for g, lst in self.dma_groups.items():
            c = 0
            for op in lst:
                c += 16
                op.val = c
            gtotal[g] = c
        with contextlib.ExitStack() as st:
            esem = {e: st.enter_context(nc.semaphore("s_" + e)) for e in ENG_NAMES}
            gsem = {g: st.enter_context(nc.semaphore("d_%d" % i))
                    for i, g in enumerate(self.dma_groups)}
            block = st.enter_context(nc.Block())

            def run(e, engobj):
                seen = {}
                for op in self.ops[e]:
                    for d in op.deps:
                        if d.is_dma:
                            key = ("g", d.grp)
                            sem = gsem[d.grp]
                            v = gtotal[d.grp] if d.grp in self.wait_all_groups else d.val
                        else:
                            key = ("e", d.eng)
                            sem = esem[d.eng]
                            v = d.val
                        if seen.get(key, 0) >= v:
                            continue
                        seen[key] = v
                        engobj.wait_ge(sem, v)
                    ins = op.fn(engobj)
                    if op.is_dma:
                        ins.then_inc(gsem[op.grp], 16)
                    elif op.signal:
                        ins.then_inc(esem[e], 1)
                if e == "sp":
                    for g in final_wait_groups:
                        engobj.wait_ge(gsem[g], gtotal[g])

            block.tensor(lambda eng: run("pe", eng))
            block.scalar(lambda eng: run("act", eng))
            block.vector(lambda eng: run("dve", eng))
            block.gpsimd(lambda eng: run("pool", eng))
            block.sync(lambda eng: run("sp", eng))


class Arena:
    def __init__(self, nc, prog, words):
        self.t = nc.alloc_sbuf_tensor("arena", [128, words], F32)
        self.P = prog
        self.free_list = [(0, words)]
        self.live = {}
        self.dead = []
        self.peak = 0

    def alloc(self, name, shape, dt, parts=128):
        n = int(np.prod(shape))
        esz = 2 if dt == BF16 else 4
        words = (n * esz + 31) // 32 * 8
        small = words <= 1100
        order = range(len(self.free_list) - 1, -1, -1) if small else range(len(self.free_list))
        for i in order:
            o, w = self.free_list[i]
            if w >= words:
                if w == words:
                    off = o
                    self.free_list.pop(i)
                elif small:
                    off = o + w - words
                    self.free_list[i] = (o, w - words)
                else:
                    off = o
                    self.free_list[i] = (o + words, w - words)
                break
        else:
            raise RuntimeError("SBUF arena full allocating %s (%d words); live=%s" % (
                name, words, {k: v[1] for k, v in self.live.items()}))
        self.live[name] = (off, words)
        self.peak = max(self.peak, off + words)
        preds = []
        for (o, w, nm) in self.dead:
            if o < off + words and off < o + w:
                preds.extend(self.P.ops_touching(nm))
        assert name not in self.P.keys_by_buf, name
        self.P.buf_pred[name] = _reduce_ops(preds)
        v = self.t[0:parts, off:off + words]
        if dt != F32:
            v = v.bitcast(dt)
        v = v[:, 0:n]
        if len(shape) == 2:
            v = v.rearrange("p (a b) -> p a b", b=shape[1])
        elif len(shape) == 3:
            v = v.rearrange("p (a b c) -> p a b c", b=shape[1], c=shape[2])
        return v

    def free(self, name):
        off, words = self.live.pop(name)
        self.dead.append((off, words, name))
        fl = self.free_list + [(off, words)]
        fl.sort()
        merged = []
        for o, w in fl:
            if merged and merged[-1][0] + merged[-1][1] == o:
                merged[-1] = (merged[-1][0], merged[-1][1] + w)
            else:
                merged.append((o, w))
        self.free_list = merged


def build(stage=99, dbg=()):
    nc = bass.Bass("TRN2", target_bir_lowering=False)
    P = Prog(nc)
    A = Arena(nc, P, 52992)

    def din(name, shape, dt=F32):
        return nc.dram_tensor(name, list(shape), dt, kind="ExternalInput").ap()

    x_d = din("x", [T, D])
    ccol_d = din("c_col", [128, 8])
    wada_d = din("w_ada", [D, 6 * D])
    bada_d = din("b_ada_col", [128, 48])
    g1_d = din("g1_col", [128, 8])
    g2_d = din("g2_col", [128, 8])
    win_d = din("w_in", [D, DIN])
    bif_d = din("b_if_bc", [128, 8])
    cw_d = din("conv_w_col", [128, 4, 31])
    cb_d = din("conv_b_col", [128, 4])
    clg_d = din("conv_lng_col", [128, 4])
    clb_d = din("conv_lnb_col", [128, 4])
    wco_d = din("w_conv_out", [512, D])
    qkw_d = din("qk_w_col", [128, 8, 4])
    qkb_d = din("qk_b_col", [128, 8])
    mng_d = din("mng_col", [128, 4])
    wmo_d = din("w_m_out", [512, D])
    wout_d = din("w_out", [D, D])
    wr_d = din("w_router", [D, 36])
    br_d = din("b_router_bc", [128, 36])
    weg_d = din("w_e_gate_l", [32 * 128, 8 * 512])
    weu_d = din("w_e_up_l", [32 * 128, 8 * 512])
    wed_d = din("w_e_down_l", [32 * 128, 4 * D])
    gfin_d = din("g_final_bc", [128, D])
    out_d = nc.dram_tensor("out", [T, D], F32, kind="ExternalOutput").ap()

    dbg_outs = {}

    def dbg_out(name, ap, reads):
        if name not in dbg:
            return
        shape = list(ap.shape)
        dt = ap.dtype
        d = nc.dram_tensor("dbg_" + name, shape, dt, kind="ExternalOutput").ap()
        dbg_outs[name] = d
        P.add("sp", lambda e: e.dma_start(out=d, in_=ap), reads=reads, dma=True, grp="dbgout")

    psb = [nc.alloc_psum_tensor("ps%d" % i, [128, 512], F32) for i in range(8)]
    ps_rot = list(range(8))

    def next_ps():
        i = ps_rot.pop(0)
        ps_rot.append(i)
        return psb[i], ("ps%d" % i,)

    def hold_ps():
        i = ps_rot.pop(0)
        return psb[i], ("ps%d" % i,)

    def release_ps(key):
        ps_rot.append(int(key[0][2:]))

    ident_f = A.alloc("ident_f", [128], F32)
    ident_b = A.alloc("ident_b", [128], BF16)
    ones_b = A.alloc("ones_b", [128], BF16)
    ones_f = A.alloc("ones_f", [128], F32)
    mask_ut = A.alloc("mask_ut", [128], BF16)
    tri_f = A.alloc("tri_f", [128], F32)
    K_ID = ("ident_f", 0)
    P.add("pool", lambda e: e.memset(ident_f, 0.0), writes=[("ident_f", 0)])
    P.add("pool", lambda e: e.affine_select(out=ident_f, in_=ident_f, pattern=[[-1, 128]],
                                             compare_op=ALU.not_equal, fill=1.0, base=0, channel_multiplier=1),
          reads=[("ident_f", 0)], writes=[("ident_f", 0)])
    P.add("pool", lambda e: e.tensor_copy(out=ident_b, in_=ident_f), reads=[("ident_f", 0)], writes=[("ident_b", 0)])
    P.add("pool", lambda e: e.memset(ones_b, 1.0), writes=[("ones_b", 0)])
    P.add("pool", lambda e: e.memset(ones_f, 1.0), writes=[("ones_f", 0)])
    P.add("pool", lambda e: e.memset(tri_f, 1.0), writes=[("tri_f", 0)])
    P.add("pool", lambda e: e.affine_select(out=tri_f, in_=tri_f, pattern=[[1, 128]],
                                             compare_op=ALU.is_ge, fill=0.0, base=0, channel_multiplier=-1),
          reads=[("tri_f", 0)], writes=[("tri_f", 0)])
    P.add("pool", lambda e: e.tensor_copy(out=mask_ut, in_=tri_f), reads=[("tri_f", 0)], writes=[("mask_ut", 0)])

    def load_const(name, dram, shape, dt=F32):
        t = A.alloc(name, shape, dt)
        P.add("sp", lambda e: e.dma_start(out=t, in_=dram), writes=[(name, 0)], dma=True, grp="c_" + name)
        return t

    ccol = load_const("ccol", ccol_d, [8])
    bada = load_const("bada", bada_d, [48])
    g1c = load_const("g1c", g1_d, [8])
    g2c = load_const("g2c", g2_d, [8])

    silc = A.alloc("silc", [8], F32)
    silb = A.alloc("silb", [8], BF16)
    P.add("act", lambda e: e.activation(out=silc, in_=ccol, func=AF.Silu), reads=[("ccol", 0)], writes=[("silc", 0)])
    P.add("dve", lambda e: e.tensor_copy(out=silb, in_=silc), reads=[("silc", 0)], writes=[("silb", 0)])
    modT = A.alloc("modT", [48], F32)
    wada_v = wada_d.rearrange("(c p) n -> p c n", p=128)
    NWA = 2
    wab = [A.alloc("wada%d" % i, [8, 512], BF16) for i in range(NWA)]
    a1 = A.alloc("a1", [8], F32)
    a2 = A.alloc("a2", [8], F32)

    def adaln_block(blk, ps_mod, k_mod):
        s_ = blk % NWA
        buf = wab[s_]
        nm = "wada%d" % s_
        P.add("pool", (lambda e, buf=buf, blk=blk: e.dma_start(out=buf, in_=wada_v[:, :, blk * 512:(blk + 1) * 512])),
              writes=[(nm, 0)], dma=True, grp=nm)

        def mm(e, buf=buf, blk=blk):
            ins = None
            for jj in range(4):
                j = blk * 4 + jj
                for k in range(8):
                    ins = e.matmul(ps_mod[:, j:j + 1], lhsT=buf[:, k, jj * 128:(jj + 1) * 128], rhs=silb[:, k:k + 1],
                                   start=(k == 0), stop=(k == 7))
            return ins
        P.add("pe", mm, reads=[(nm, 0), ("silb", 0)], writes=[k_mod])

    def adaln_finish(ps_mod, k_mod, c0, c1, part):
        P.add("dve", lambda e: e.tensor_tensor(out=modT[:, c0:c1], in0=ps_mod[:, c0:c1], in1=bada[:, c0:c1], op=ALU.add),
              reads=[k_mod, ("bada", 0)], writes=[("modT", part)])
        release_ps(k_mod)

    pm0, km0 = hold_ps()
    for blk in range(4):
        adaln_block(blk, pm0, km0)
    adaln_finish(pm0, km0, 0, 16, 0)
    P.add("dve", lambda e: e.scalar_tensor_tensor(out=a1, in0=modT[:, 8:16], scalar=1.0, in1=g1c, op0=ALU.add, op1=ALU.mult),
          reads=[("modT", 0), ("g1c", 0)], writes=[("a1", 0)])
    p2state = {}

    def adaln_p2_block(blk):
        if "ps" not in p2state:
            p2state["ps"] = hold_ps()
        adaln_block(blk, *p2state["ps"])

    def adaln_p2_end():
        pm1, km1 = p2state["ps"]
        adaln_finish(pm1, km1, 16, 48, 1)
        P.add("dve", lambda e: e.scalar_tensor_tensor(out=a2, in0=modT[:, 32:40], scalar=1.0, in1=g2c, op0=ALU.add, op1=ALU.mult),
              reads=[("modT", 1), ("g2c", 0)], writes=[("a2", 0)])
        dbg_out("modT", modT, [("modT", 0), ("modT", 1)])
        for i in range(NWA):
            A.free("wada%d" % i)

    hT = A.alloc("hT", [8, T], BF16)
    merged = A.alloc("merged", [8, T], BF16)
    NWB = 3
    wbufs = [A.alloc("wblk%d" % i, [8, 512], BF16) for i in range(NWB)]
    NXB = 8
    xin = [A.alloc("xin%d" % i, [D], F32) for i in range(NXB)]
    xnb = [A.alloc("xnb%d" % i, [D], BF16) for i in range(NXB)]
    junk = A.alloc("junk", [D], F32)
    ss1 = A.alloc("ss1", [NT], F32)
    rs1 = A.alloc("rs1", [NT], F32)

    def p2_stats(nb):
        tis = [nb * 4 + tt for tt in range(4)]
        for ti in tis:
            s = ti % NXB
            P.add("sp", (lambda e, s=s, ti=ti: e.dma_start(out=xin[s], in_=x_d[ti * 128:(ti + 1) * 128, :])),
                  writes=[("xin%d" % s, 0)], dma=True, grp="xin%d" % s)
        for ti in tis:
            s = ti % NXB
            P.add("act", (lambda e, s=s, ti=ti: e.activation(out=junk, in_=xin[s], func=AF.Square,
                                                             accum_out=ss1[:, ti:ti + 1])),
                  reads=[("xin%d" % s, 0)], writes=[("junk", 0), ("ss1", ti)])
        t0_, t1_ = tis[0], tis[-1] + 1
        P.add("act", (lambda e, t0_=t0_, t1_=t1_: e.activation(out=rs1[:, t0_:t1_], in_=ss1[:, t0_:t1_], func=AF.Sqrt,
                                                               scale=1.0 / D, bias=1e-6)),
              reads=[("ss1", ti) for ti in tis], writes=[("rs1", ti) for ti in tis])
        P.add("dve", (lambda e, t0_=t0_, t1_=t1_: e.reciprocal(out=rs1[:, t0_:t1_], in_=rs1[:, t0_:t1_])),
              reads=[("rs1", ti) for ti in tis], writes=[("rs1", ti) for ti in tis])
        for ti in tis:
            s = ti % NXB
            P.add("dve", (lambda e, s=s, ti=ti: e.tensor_scalar(out=xnb[s], in0=xin[s], scalar1=rs1[:, ti:ti + 1],
                                                                scalar2=None, op0=ALU.mult)),
                  reads=[("xin%d" % s, 0), ("rs1", ti)], writes=[("xnb%d" % s, 0)])

    def p2_tr(nb):
        pst = [next_ps() for _ in range(4)]
        for tt in range(4):
            ti = nb * 4 + tt
            s = ti % NXB

            def tr(e, s=s, tt=tt, pst=pst):
                ins = None
                for c in range(8):
                    pb = pst[c // 2][0].bitcast(BF16)
                    ins = e.transpose(out=pb[:, (c % 2) * 512 + tt * 128:(c % 2) * 512 + (tt + 1) * 128],
                                      in_=xnb[s][:, c * 128:(c + 1) * 128], identity=ident_b)
                return ins
            P.add("pe", tr, reads=[("xnb%d" % s, 0), ("ident_b", 0)], writes=[pst[i][1] for i in range(4)])
        for c in range(8):
            pb = pst[c // 2][0].bitcast(BF16)
            P.add("act", (lambda e, c=c, pb=pb, nb=nb: e.activation(
                out=hT[:, c, nb * 512:(nb + 1) * 512], in_=pb[:, (c % 2) * 512:(c % 2 + 1) * 512],
                func=AF.Identity, scale=a1[:, c:c + 1], bias=modT[:, c:c + 1])),
                reads=[pst[c // 2][1], ("a1", 0), ("modT", 0)],
                writes=[("hT", c, nb)])

    p2_stats(0)
    for nb in range(NB):
        if nb + 1 < NB:
            p2_stats(nb + 1)
        p2_tr(nb)
    dbg_out("hT", hT, [("hT", c, nb) for c in range(8) for nb in range(NB)])
    for i in range(NXB):
        A.free("xin%d" % i); A.free("xnb%d" % i)

    def finish():
        P.emit(final_wait_groups=["dbgout"] if "dbgout" in P.dma_groups else [])
        return nc, dbg_outs

    GROWS = 256
    NG = -(-(2 * T + 32 * (GROWS - 1)) // GROWS)
    P.wait_all_groups.add("x1_spill")
    X1S = nc.dram_tensor("x1_spill", [T, D], F32).ap()
    XS = nc.dram_tensor("xs_scratch", [NG * GROWS, D], BF16).ap()
    YS = nc.dram_tensor("ys_scratch", [NG * GROWS, D], BF16).ap()
    if stage <= 1:
        return finish()

    win_v = win_d.rearrange("(c p) n -> p c n", p=128)
    NWB = 3
    wb_ctr = [0]

    def load_wblock(col0, ncols=512):
        i = wb_ctr[0] % NWB
        wb_ctr[0] += 1
        buf = wbufs[i]
        nm = "wblk%d" % i
        P.add("pool", lambda e: e.dma_start(out=buf[:, :, 0:ncols], in_=win_v[:, :, col0:col0 + ncols]),
              writes=[(nm, 0)], dma=True, grp=nm)
        return buf, (nm, 0)

    def load_w4(dram_v):
        i = wb_ctr[0] % NWB
        wb_ctr[0] += 1
        nm = "wblk%d" % i
        v = wbufs[i].rearrange("p a b -> p (a b)").rearrange("p (a b) -> p a b", b=D)
        P.add("pool", lambda e: e.dma_start(out=v, in_=dram_v), writes=[(nm, 0)], dma=True, grp=nm)
        return v, (nm, 0)

    def load_cast(name, dram_ap, shape):
        t = A.alloc(name, shape, BF16)
        P.add("pool", lambda e: e.dma_start(out=t, in_=dram_ap), writes=[(name, 0)], dma=True, grp="c_" + name)
        return t

    hT_keys = lambda nb: [("hT", c, nb) for c in range(8)]

    def proj_fm(wb, wkey, mcol, nb):
        ps, pk = next_ps()

        def mm(e):
            ins = None
            for k in range(8):
                ins = e.matmul(ps[:, :], lhsT=wb[:, k, mcol * 128:(mcol + 1) * 128], rhs=hT[:, k, nb * 512:(nb + 1) * 512],
                               start=(k == 0), stop=(k == 7))
            return ins
        P.add("pe", mm, reads=[wkey] + hT_keys(nb), writes=[pk])
        return ps, pk

    cw = load_const("cw", cw_d, [4, 31])
    cb = load_const("cb", cb_d, [4])
    clg = load_const("clg", clg_d, [4])
    clb = load_const("clb", clb_d, [4])
    u = A.alloc("u", [4, 32 + T], BF16)
    PADU = 32
    for m in range(4):
        P.add("pool", (lambda e, m=m: e.memset(u[:, m, 0:PADU], 0.0)), writes=[("u", m, -1)])
    dg31 = A.alloc("dg31", [4, 31, 128], BF16)
    for m in range(4):
        P.add("pool", (lambda e, m=m: e.tensor_tensor(
            out=dg31[:, m], in0=ident_b.unsqueeze(1).to_broadcast([128, 31, 128]),
            in1=cw[:, m, :].unsqueeze(2).to_broadcast([128, 31, 128]), op=ALU.mult)),
            reads=[("ident_b", 0), ("cw", 0)], writes=[("dg31", m)])
    sgt = [A.alloc("sgt%d" % i, [512], BF16) for i in range(2)]
    sg_ctr = [0]

    def next_sgt():
        i = sg_ctr[0] % 2
        sg_ctr[0] += 1
        return sgt[i], ("sgt%d" % i, 0)

    wa, wak = load_wblock(0)
    wbk, wbkk = load_wblock(512)
    for m in range(4):
        for nb in range(NB):
            psa, pka = proj_fm(wa, wak, m, nb)
            psb_, pkb = proj_fm(wbk, wbkk, m, nb)
            sg, sgk = next_sgt()
            P.add("act", (lambda e, sg=sg, p=psb_: e.activation(out=sg, in_=p[:, :], func=AF.Sigmoid)),
                  reads=[pkb], writes=[sgk])
            P.add("dve", (lambda e, sg=sg, p=psa, m=m, nb=nb: e.tensor_tensor(
                out=u[:, m, PADU + nb * 512:PADU + (nb + 1) * 512], in0=p[:, :], in1=sg, op=ALU.mult)),
                reads=[pka, sgk], writes=[("u", m, nb)])
    dbg_out("u", u, [("u", m, nb) for m in range(4) for nb in range(-1, NB)])

    wco, wcok = load_w4(wco_d.rearrange("(c p) n -> p c n", p=128))
    gA_blocks = {0: load_wblock(3080)}
    cT = A.alloc("cT", [4, T], BF16)
    sqT = A.alloc("sqT", [4, T], BF16)
    for m in range(4):
        for nb in range(NB):
            ps, pk = next_ps()

            def cmm(e, ps=ps, m=m, nb=nb):
                ins = None
                for k in range(31):
                    o = PADU - 30 + nb * 512 + k
                    ins = e.matmul(ps[:, :], lhsT=dg31[:, m, k, :], rhs=u[:, m, o:o + 512], start=(k == 0), stop=(k == 30))
                return ins
            P.add("pe", cmm, reads=[("dg31", m), ("u", m, nb), ("u", m, nb - 1)], writes=[pk])
            P.add("act", (lambda e, ps=ps, m=m, nb=nb: e.activation(
                out=cT[:, m, nb * 512:(nb + 1) * 512], in_=ps[:, :], func=AF.Identity, bias=cb[:, m:m + 1])),
                reads=[pk, ("cb", 0)], writes=[("cT", m, nb)])
            P.add("act", (lambda e, ps=ps, m=m, nb=nb: e.activation(
                out=sqT[:, m, nb * 512:(nb + 1) * 512], in_=ps[:, :], func=AF.Square, bias=cb[:, m:m + 1])),
                reads=[pk, ("cb", 0)], writes=[("sqT", m, nb)])
            gi = m * NB + nb
            if gi % 2 == 1:
                adaln_p2_block(4 + gi // 2)
    adaln_p2_end()
    dbg_out("cT", cT, [("cT", m, nb) for m in range(4) for nb in range(NB)])
    A.free("u")
    A.free("dg31")

    actT = A.alloc("actT", [4, T], BF16)
    mean_t = A.alloc("mean_t", [512], F32)
    rstd_t = A.alloc("rstd_t", [512], F32)
    msq_t = A.alloc("msq_t", [512], F32)
    nrm_t = [A.alloc("nrm_t%d" % i, [512], F32) for i in range(2)]
    for nb in range(NB):
        ps1, pk1 = next_ps()
        ps2, pk2 = next_ps()

        def smm(e, ps1=ps1, ps2=ps2, nb=nb):
            ins = None
            for m in range(4):
                ins = e.matmul(ps1[:, :], lhsT=ones_b, rhs=cT[:, m, nb * 512:(nb + 1) * 512], start=(m == 0), stop=(m == 3))
            for m in range(4):
                ins = e.matmul(ps2[:, :], lhsT=ones_b, rhs=sqT[:, m, nb * 512:(nb + 1) * 512], start=(m == 0), stop=(m == 3))
            return ins
        P.add("pe", smm, reads=[("ones_b", 0)] + [("cT", m, nb) for m in range(4)] + [("sqT", m, nb) for m in range(4)],
              writes=[pk1, pk2])
        P.add("dve", (lambda e, ps1=ps1: e.tensor_scalar(out=mean_t, in0=ps1[:, :], scalar1=1.0 / 512, scalar2=None, op0=ALU.mult)),
              reads=[pk1], writes=[("mean_t", 0)])
        P.add("dve", lambda e: e.tensor_tensor(out=msq_t, in0=mean_t, in1=mean_t, op=ALU.mult),
              reads=[("mean_t", 0)], writes=[("msq_t", 0)])
        P.add("dve", (lambda e, ps2=ps2: e.scalar_tensor_tensor(out=rstd_t, in0=ps2[:, :], scalar=1.0 / 512, in1=msq_t,
                                                                op0=ALU.mult, op1=ALU.subtract)),
              reads=[pk2, ("msq_t", 0)], writes=[("rstd_t", 0)])
        P.add("act", lambda e: e.activation(out=rstd_t, in_=rstd_t, func=AF.Sqrt, bias=1e-5),
              reads=[("rstd_t", 0)], writes=[("rstd_t", 0)])
        P.add("dve", lambda e: e.reciprocal(out=rstd_t, in_=rstd_t), reads=[("rstd_t", 0)], writes=[("rstd_t", 0)])
        for m in range(4):
            nt = nrm_t[m % 2]
            ntk = ("nrm_t%d" % (m % 2), 0)
            P.add("dve", (lambda e, nt=nt, m=m, nb=nb: e.tensor_tensor(out=nt, in0=cT[:, m, nb * 512:(nb + 1) * 512], in1=mean_t,
                                                                      op=ALU.subtract)),
                  reads=[("cT", m, nb), ("mean_t", 0)], writes=[ntk])
            P.add("dve", (lambda e, nt=nt: e.tensor_tensor(out=nt, in0=nt, in1=rstd_t, op=ALU.mult)),
                  reads=[ntk, ("rstd_t", 0)], writes=[ntk])
            P.add("act", (lambda e, nt=nt, m=m, nb=nb: e.activation(
                out=actT[:, m, nb * 512:(nb + 1) * 512], in_=nt, func=AF.Silu, scale=clg[:, m:m + 1], bias=clb[:, m:m + 1])),
                reads=[ntk, ("clg", 0), ("clb", 0)], writes=[("actT", m, nb)])
    dbg_out("actT", actT, [("actT", m, nb) for m in range(4) for nb in range(NB)])
    A.free("cT"); A.free("sqT"); A.free("mean_t"); A.free("rstd_t"); A.free("msq_t"); A.free("nrm_t0"); A.free("nrm_t1")

    for jb in range(2):
        wg_, wgk = gA_blocks[jb] if jb in gA_blocks else load_wblock(3080 + jb * 512)
        for jj in range(4):
            j = jb * 4 + jj
            for nb in range(NB):
                psy, pky = next_ps()

                def ymm(e, psy=psy, j=j, nb=nb):
                    ins = None
                    for m in range(4):
                        ins = e.matmul(psy[:, :], lhsT=wco[:, m, j * 128:(j + 1) * 128], rhs=actT[:, m, nb * 512:(nb + 1) * 512],
                                       start=(m == 0), stop=(m == 3))
                    return ins
                P.add("pe", ymm, reads=[wcok] + [("actT", m, nb) for m in range(4)], writes=[pky])
                psg, pkg = proj_fm(wg_, wgk, jj, nb)
                sg, sgk = next_sgt()
                P.add("act", (lambda e, sg=sg, p=psg: e.activation(out=sg, in_=p[:, :], func=AF.Sigmoid)),
                      reads=[pkg], writes=[sgk])
                P.add("dve", (lambda e, sg=sg, p=psy, j=j, nb=nb: e.tensor_tensor(
                    out=merged[:, j, nb * 512:(nb + 1) * 512], in0=p[:, :], in1=sg, op=ALU.mult)),
                    reads=[pky, sgk], writes=[("merged", j, nb)])
    dbg_out("mergedA", merged, [("merged", j, nb) for j in range(8) for nb in range(NB)])
    A.free("actT")
    if stage <= 2:
        return finish()

    zt = A.alloc("zt", [D], BF16)
    P.add("pool", lambda e: e.memset(zt, 0.0), writes=[("zt", 0)])
    XSZ_KEYS = []
    for zi in range(NG * GROWS // 1024):
        P.add("sp", (lambda e, zi=zi: e.dma_start(out=XS[zi * 1024:(zi + 1) * 1024, :].rearrange("(n p) d -> p n d", p=128),
                                                  in_=zt.unsqueeze(1).to_broadcast([128, 8, D]))),
              reads=[("zt", 0)], writes=[("XSZ", zi)], dma=True, grp="xs_zero")
        XSZ_KEYS.append(("XSZ", zi))
    A.free("zt")
    PADQ = 4
    qkw = load_const("qkw", qkw_d, [8, 4])
    qkb = load_const("qkb", qkb_d, [8])
    bif = load_const("bif", bif_d, [8])
    mng = load_const("mng", mng_d, [4])
    qk_raw = A.alloc("qk_raw", [8, PADQ + T], BF16)
    for cc in range(8):
        P.add("pool", (lambda e, cc=cc: e.memset(qk_raw[:, cc, 0:PADQ], 0.0)), writes=[("qk_raw", cc, -1)])
    dg4 = A.alloc("dg4", [8, 4, 128], BF16)
    P.add("pool", lambda e: e.tensor_tensor(
        out=dg4.rearrange("p a b c -> p (a b) c"), in0=ident_b.unsqueeze(1).to_broadcast([128, 32, 128]),
        in1=qkw.rearrange("p a b -> p (a b)").unsqueeze(2).to_broadcast([128, 32, 128]), op=ALU.mult),
        reads=[("ident_b", 0), ("qkw", 0)], writes=[("dg4", 0)])
    for half in range(2):
        wq_, wqk = load_wblock(1024 + half * 512)
        for m in range(4):
            cc = half * 4 + m
            for nb in range(NB):
                ps, pk = proj_fm(wq_, wqk, m, nb)
                P.add("act", (lambda e, ps=ps, cc=cc, nb=nb: e.activation(
                    out=qk_raw[:, cc, PADQ + nb * 512:PADQ + (nb + 1) * 512], in_=ps[:, :], func=AF.Identity)),
                    reads=[pk], writes=[("qk_raw", cc, nb)])
    qkc = A.alloc("qkc", [8, T], BF16)
    for cc in range(8):
        for nb in range(NB):
            ps, pk = next_ps()

            def qmm(e, ps=ps, cc=cc, nb=nb):
                ins = None
                for k in range(4):
                    o = PADQ - 3 + nb * 512 + k
                    ins = e.matmul(ps[:, :], lhsT=dg4[:, cc, k, :], rhs=qk_raw[:, cc, o:o + 512], start=(k == 0), stop=(k == 3))
                return ins
            P.add("pe", qmm, reads=[("dg4", 0), ("qk_raw", cc, nb), ("qk_raw", cc, nb - 1)], writes=[pk])
            P.add("act", (lambda e, ps=ps, cc=cc, nb=nb: e.activation(
                out=qkc[:, cc, nb * 512:(nb + 1) * 512], in_=ps[:, :], func=AF.Silu, bias=qkb[:, cc:cc + 1])),
                reads=[pk, ("qkb", 0)], writes=[("qkc", cc, nb)])
    dbg_out("qkc", qkc, [("qkc", cc, nb) for cc in range(8) for nb in range(NB)])
    A.free("qk_raw"); A.free("dg4")
    if stage <= 2.2:
        return finish()

    wif = A.alloc("wif", [8, 8], BF16)
    wif_f = A.alloc("wif_f", [8, 8], F32)
    with nc.allow_non_contiguous_dma(reason="tiny gate-weight columns"):
        P.add("sp", lambda e: e.dma_start(out=wif_f, in_=win_v[:, :, 3072:3080]), writes=[("wif_f", 0)], dma=True, grp="c_wif")
    P.add("dve", lambda e: e.tensor_copy(out=wif, in_=wif_f), reads=[("wif_f", 0)], writes=[("wif", 0)])
    G = A.alloc("G", [NT, 8], F32)
    nlf = A.alloc("nlf", [NT, 4], F32)
    gtmp = A.alloc("gtmp", [NT, 4], F32)
    A_inv = A.alloc("A_inv", [NT, 4], F32)
    Bv = A.alloc("Bv", [NT, 4], F32)
    dec = A.alloc("dec", [NT, 4], F32)
    psg, pkg = hold_ps()

    def gmm(e):
        ins = None
        for ti in range(NT):
            for k in range(8):
                ins = e.matmul(psg[:, ti * 8:(ti + 1) * 8], lhsT=hT[:, k, ti * 128:(ti + 1) * 128], rhs=wif[:, k, :],
                               start=(k == 0), stop=(k == 7))
        return ins
    P.add("pe", gmm, reads=[("wif", 0)] + [("hT", c, nb) for c in range(8) for nb in range(NB)], writes=[pkg])
    P.add("dve", lambda e: e.tensor_tensor(out=G, in0=psg[:, 0:128].rearrange("p (a b) -> p a b", b=8),
                                           in1=bif.unsqueeze(1).to_broadcast([128, NT, 8]), op=ALU.add),
          reads=[pkg, ("bif", 0)], writes=[("G", 0)])
    release_ps(pkg)
    dbg_out("G", G, [("G", 0)])
    if stage <= 2.31:
        return finish()
    P.add("act", lambda e: e.activation(out=gtmp, in_=G[:, :, 4:8], func=AF.Exp, scale=-1.0),
          reads=[("G", 0)], writes=[("gtmp", 0)])
    P.add("act", lambda e: e.activation(out=nlf, in_=gtmp, func=AF.Ln, bias=1.0),
          reads=[("gtmp", 0)], writes=[("nlf", 0)])
    dbg_out("nlf", nlf, [("nlf", 0)])
    if stage <= 2.32:
        return finish()
    psc, pkc = next_ps()
    nlf2 = nlf.rearrange("p a b -> p (a b)")
    nl_hi = A.alloc("nl_hi", [64], BF16)
    nl_lo = A.alloc("nl_lo", [64], BF16)
    P.add("dve", lambda e: e.tensor_copy(out=nl_hi, in_=nlf2), reads=[("nlf", 0)], writes=[("nl_hi", 0)])
    P.add("dve", lambda e: e.tensor_tensor(out=nl_lo, in0=nlf2, in1=nl_hi, op=ALU.subtract),
          reads=[("nlf", 0), ("nl_hi", 0)], writes=[("nl_lo", 0)])

    def cmm2(e):
        e.matmul(psc[:, 0:64], lhsT=mask_ut, rhs=nl_hi, start=True, stop=False)
        e.matmul(psc[:, 0:64], lhsT=mask_ut, rhs=nl_lo, start=False, stop=True)
        e.matmul(psc[:, 64:128], lhsT=ones_b, rhs=nl_hi, start=True, stop=False)
        return e.matmul(psc[:, 64:128], lhsT=ones_b, rhs=nl_lo, start=False, stop=True)
    P.add("pe", cmm2, reads=[("mask_ut", 0), ("ones_b", 0), ("nl_hi", 0), ("nl_lo", 0)], writes=[pkc])
    if stage <= 2.33:
        P.add("dve", lambda e: e.tensor_copy(out=gtmp.rearrange("p a b -> p (a b)"), in_=psc[:, 0:64]), reads=[pkc], writes=[("gtmp", 0)])
        dbg_out("ncum", gtmp, [("gtmp", 0)])
        return finish()
    LNS = float(np.log(128.0 ** 0.5))
    cval = A.alloc("cval", [2], F32)
    P.add("pool", lambda e: e.memset(cval[:, 0:1], LNS), writes=[("cval", 0)])
    P.add("pool", lambda e: e.memset(cval[:, 1:2], -LNS), writes=[("cval", 1)])
    P.add("act", lambda e: e.activation(out=A_inv.rearrange("p a b -> p (a b)"), in_=psc[:, 0:64], func=AF.Exp, bias=cval[:, 0:1]),
          reads=[pkc, ("cval", 0)], writes=[("A_inv", 0)])
    A_ = A.alloc("A_", [NT, 4], F32)
    P.add("act", lambda e: e.activation(out=A_.rearrange("p a b -> p (a b)"), in_=psc[:, 0:64], func=AF.Exp, scale=-1.0, bias=cval[:, 1:2]),
          reads=[pkc, ("cval", 1)], writes=[("A_", 0)])
    if stage <= 2.34:
        dbg_out("A_", A_, [("A_", 0)])
        dbg_out("A_inv", A_inv, [("A_inv", 0)])
        return finish()
    P.add("dve", lambda e: e.tensor_tensor(out=gtmp, in0=psc[:, 0:64].rearrange("p (a b) -> p a b", b=4), in1=G[:, :, 0:4], op=ALU.add),
          reads=[pkc, ("G", 0), ("gtmp", 0)], writes=[("gtmp", 0)])
    P.add("act", lambda e: e.activation(out=Bv, in_=gtmp, func=AF.Exp), reads=[("gtmp", 0)], writes=[("Bv", 0)])
    if stage <= 2.36:
        dbg_out("Bv", Bv, [("Bv", 0)])
        return finish()
    P.add("act", lambda e: e.activation(out=dec.rearrange("p a b -> p (a b)"), in_=psc[:, 64:128], func=AF.Exp, scale=-1.0),
          reads=[pkc], writes=[("dec", 0)])
    dbg_out("Bv", Bv, [("Bv", 0)])
    dbg_out("A_", A_, [("A_", 0)])
    dbg_out("decay", dec, [("dec", 0)])

    if stage <= 2.4:
        return finish()
    ktok = A.alloc("ktok", [NT, 512], BF16)
    for c in range(NT):
        ps, pk = next_ps()
        pb = ps.bitcast(BF16)

        def ktr(e, pb=pb, c=c):
            ins = None
            for h in range(4):
                ins = e.transpose(out=pb[:, h * 128:(h + 1) * 128], in_=qkc[:, 4 + h, c * 128:(c + 1) * 128], identity=ident_b)
            return ins
        P.add("pe", ktr, reads=[("ident_b", 0)] + [("qkc", 4 + h, c // 4) for h in range(4)], writes=[pk])
        P.add("act", (lambda e, pb=pb, c=c: e.activation(out=ktok[:, c, :], in_=pb[:, 0:512], func=AF.Identity)),
              reads=[pk], writes=[("ktok", c)])

    vB = A.alloc("vB", [NT, 4, 129], BF16)
    wv_, wvk = load_wblock(2048)
    for c in range(NT):
        ps, pk = next_ps()

        def vmm(e, ps=ps, c=c):
            ins = None
            for k in range(8):
                ins = e.matmul(ps[:, :], lhsT=hT[:, k, c * 128:(c + 1) * 128], rhs=wv_[:, k, 0:512], start=(k == 0), stop=(k == 7))
            return ins
        P.add("pe", vmm, reads=[wvk] + hT_keys(c // 4), writes=[pk])
        P.add("dve", (lambda e, ps=ps, c=c: e.tensor_tensor(
            out=vB[:, c, :, 0:128], in0=ps[:, :].rearrange("p (a b) -> p a b", b=128),
            in1=Bv[:, c, :].unsqueeze(2).to_broadcast([128, 4, 128]), op=ALU.mult)),
            reads=[pk, ("Bv", 0)], writes=[("vB", c, 0)])
        P.add("dve", (lambda e, c=c: e.tensor_copy(out=vB[:, c, :, 128], in_=Bv[:, c, :])),
              reads=[("Bv", 0)], writes=[("vB", c, 1)])

    if stage <= 2.6:
        return finish()
    E = A.alloc("E", [4, 129], F32)
    Cb = [A.alloc("Cb%d" % i, [4, 129], BF16) for i in range(2)]
    sm = [A.alloc("sm%d" % i, [4, 128], BF16) for i in range(2)]
    hn = [A.alloc("hn%d" % i, [4, 128], BF16) for i in range(2)]
    st6 = A.alloc("st6", [4, 6], F32)
    mv = A.alloc("mv", [4, 2], F32)
    den = A.alloc("den", [4], F32)
    qq = A.alloc("qq", [4], F32)
    rstd = A.alloc("rstd", [4], F32)
    sgo = [A.alloc("sgo%d" % i, [4, 512], BF16) for i in range(2)]
    hmT = A.alloc("hmT", [4, T], BF16)
    wo_, wok = load_wblock(2560)
    CW = 256

    chs = {}

    def chunk_A(c):
        nb = c // 4
        cs = slice(c * 128, (c + 1) * 128)
        if c % 4 == 0:
            for h in range(4):
                ps, pk = proj_fm(wo_, wok, h, nb)
                P.add("act", (lambda e, ps=ps, h=h, nb=nb: e.activation(out=sgo[nb % 2][:, h, :], in_=ps[:, :], func=AF.Sigmoid)),
                      reads=[pk], writes=[("sgo%d" % (nb % 2), h)])
        pss, pks = next_ps()

        def smm2(e, pss=pss, cs=cs):
            ins = None
            for h in range(4):
                ins = e.matmul(pss[:, h * 128:(h + 1) * 128], lhsT=qkc[:, 4 + h, cs], rhs=qkc[:, h, cs], start=True, stop=True)
            return ins
        P.add("pe", smm2, reads=[("qkc", cc, nb) for cc in range(8)], writes=[pks])
        smc = sm[c % 2]
        smk = ("sm%d" % (c % 2), 0)
        P.add("dve", (lambda e, pss=pss, smc=smc: e.tensor_tensor(
            out=smc, in0=pss[:, :].rearrange("p (a b) -> p a b", b=128),
            in1=mask_ut.unsqueeze(1).to_broadcast([128, 4, 128]), op=ALU.mult)),
            reads=[pks, ("mask_ut", 0)], writes=[smk])
        pu = [hold_ps(), hold_ps()]

        def umm(e, pu=pu, c=c):
            ins = None
            for h in range(4):
                o = pu[h // 2][0][:, (h % 2) * CW:(h % 2) * CW + 129]
                ins = e.matmul(o, lhsT=ktok[:, c, h * 128:(h + 1) * 128], rhs=vB[:, c, h, :], start=True, stop=True)
            return ins
        P.add("pe", umm, reads=[("ktok", c), ("vB", c, 0), ("vB", c, 1)], writes=[pu[0][1], pu[1][1]])
        chs[c] = (smc, smk, pu)

    def chunk_B(c):
        nb = c // 4
        cs = slice(c * 128, (c + 1) * 128)
        smc, smk, pu = chs[c]
        pn = [next_ps(), next_ps()]

        def nmm(e, pn=pn, smc=smc, c=c, cs=cs):
            ins = None
            for h in range(4):
                o = pn[h // 2][0][:, (h % 2) * CW:(h % 2) * CW + 129]
                ins = e.matmul(o, lhsT=smc[:, h, :], rhs=vB[:, c, h, :], start=True, stop=(c == 0))
                if c > 0:
                    ins = e.matmul(o, lhsT=qkc[:, h, cs], rhs=Cb[(c - 1) % 2][:, h, :], start=False, stop=True)
            return ins
        rd = [smk, ("vB", c, 0), ("vB", c, 1)] + [("qkc", h, nb) for h in range(4)]
        if c > 0:
            rd += [("Cb%d" % ((c - 1) % 2), h) for h in range(4)]
        P.add("pe", nmm, reads=rd, writes=[pn[0][1], pn[1][1]])
        for h in range(4):
            src = pu[h // 2][0][:, (h % 2) * CW:(h % 2) * CW + 129]
            if c == 0:
                P.add("dve", (lambda e, src=src, h=h: e.tensor_copy(out=E[:, h, :], in_=src)),
                      reads=[pu[h // 2][1]], writes=[("E", h)])
            else:
                P.add("dve", (lambda e, src=src, h=h, c=c: e.scalar_tensor_tensor(
                    out=E[:, h, :], in0=E[:, h, :], scalar=dec[:, c - 1, h:h + 1], in1=src, op0=ALU.mult, op1=ALU.add)),
                    reads=[pu[h // 2][1], ("E", h), ("dec", 0)], writes=[("E", h)])
            if c < NT - 1:
                P.add("act", (lambda e, h=h, c=c: e.activation(out=Cb[c % 2][:, h, :], in_=E[:, h, :], func=AF.Identity,
                                                               scale=dec[:, c, h:h + 1])),
                      reads=[("E", h), ("dec", 0)], writes=[("Cb%d" % (c % 2), h)])
        release_ps(pu[0][1]); release_ps(pu[1][1])
        chs[c] = pn

    def chunk_C(c):
        nb = c // 4
        cs = slice(c * 128, (c + 1) * 128)
        pn = chs[c]
        for h in range(4):
            src = pn[h // 2][0][:, (h % 2) * CW:(h % 2) * CW + 128]
            P.add("dve", (lambda e, src=src, h=h: e.bn_stats(out=st6[:, h, :], in_=src)),
                  reads=[pn[h // 2][1]], writes=[("st6", h)])
            P.add("dve", (lambda e, h=h: e.bn_aggr(out=mv[:, h, :], in_=st6[:, h, :])),
                  reads=[("st6", h)], writes=[("mv", h)])
        for b2 in range(2):
            dsrc = pn[b2][0][:, 0:512].rearrange("p (a b) -> p a b", b=CW)[:, :, 128]
            P.add("dve", (lambda e, dsrc=dsrc, b2=b2, c=c: e.tensor_tensor(
                out=den[:, 2 * b2:2 * b2 + 2], in0=dsrc, in1=A_[:, c, 2 * b2:2 * b2 + 2], op=ALU.mult)),
                reads=[pn[b2][1], ("A_", 0)], writes=[("den", b2)])
        P.add("dve", lambda e: e.scalar_tensor_tensor(out=den, in0=den, scalar=-1.0, in1=den, op0=ALU.mult, op1=ALU.max),
              reads=[("den", 0), ("den", 1)], writes=[("den", 0), ("den", 1)])
        P.add("dve", lambda e: e.tensor_scalar(out=den, in0=den, scalar1=1.0, scalar2=None, op0=ALU.max),
              reads=[("den", 0), ("den", 1)], writes=[("den", 0), ("den", 1)])
        P.add("dve", (lambda e, c=c: e.tensor_tensor(out=qq, in0=den, in1=A_inv[:, c, :], op=ALU.mult)),
              reads=[("den", 0), ("den", 1), ("A_inv", 0)], writes=[("qq", 0)])
        P.add("dve", lambda e: e.tensor_tensor(out=qq, in0=qq, in1=qq, op=ALU.mult), reads=[("qq", 0)], writes=[("qq", 0)])
        P.add("dve", lambda e: e.scalar_tensor_tensor(out=rstd, in0=qq, scalar=1e-5, in1=mv[:, :, 1], op0=ALU.mult, op1=ALU.add),
              reads=[("qq", 0)] + [("mv", h) for h in range(4)], writes=[("rstd", 0)])
        P.add("act", lambda e: e.activation(out=rstd, in_=rstd, func=AF.Sqrt), reads=[("rstd", 0)], writes=[("rstd", 0)])
        P.add("dve", lambda e: e.reciprocal(out=rstd, in_=rstd), reads=[("rstd", 0)], writes=[("rstd", 0)])
        hnc = hn[c % 2]
        hnk = "hn%d" % (c % 2)
        for h in range(4):
            src = pn[h // 2][0][:, (h % 2) * CW:(h % 2) * CW + 128]
            P.add("dve", (lambda e, src=src, h=h, hnc=hnc: e.tensor_scalar(
                out=hnc[:, h, :], in0=src, scalar1=mv[:, h, 0:1], scalar2=rstd[:, h:h + 1], op0=ALU.subtract, op1=ALU.mult)),
                reads=[pn[h // 2][1], ("mv", h), ("rstd", 0)], writes=[(hnk, h)])
        pt, pkt = next_ps()
        ptb = pt.bitcast(BF16)

        def htr(e, ptb=ptb, hnc=hnc):
            ins = None
            for h in range(4):
                ins = e.transpose(out=ptb[:, h * 128:(h + 1) * 128], in_=hnc[:, h, :], identity=ident_b)
            return ins
        P.add("pe", htr, reads=[("ident_b", 0)] + [(hnk, h) for h in range(4)], writes=[pkt])
        P.add("dve", (lambda e, ptb=ptb, c=c, nb=nb, cs=cs: e.tensor_tensor(
            out=hmT[:, :, cs], in0=ptb[:, 0:512].rearrange("p (a b) -> p a b", b=128),
            in1=sgo[nb % 2][:, :, (c % 4) * 128:(c % 4 + 1) * 128], op=ALU.mult)),
            reads=[pkt] + [("sgo%d" % (nb % 2), h) for h in range(4)], writes=[("hmT", c)])

    chunk_A(0)
    for c in range(NT):
        if c + 1 < NT:
            chunk_A(c + 1)
        chunk_B(c)
        chunk_C(c)
    dbg_out("hmT", hmT, [("hmT", c) for c in range(NT)])
    for nm in ("qkc", "wif", "wif_f", "nl_hi", "nl_lo", "G", "nlf", "gtmp", "A_inv", "Bv", "dec", "A_", "ktok", "vB", "E", "Cb0", "Cb1", "sm0", "sm1",
               "hn0", "hn1", "st6", "mv", "den", "qq", "rstd", "sgo0", "sgo1"):
        A.free(nm)

    wmo = load_cast("wmo", wmo_d.rearrange("(c p) n -> p c n", p=128), [4, D])
    for h in range(4):
        P.add("dve", (lambda e, h=h: e.tensor_scalar(out=wmo[:, h, :], in0=wmo[:, h, :], scalar1=mng[:, h:h + 1], scalar2=None,
                                                     op0=ALU.mult)),
              reads=[("wmo", 0), ("wmo", 1 + h), ("mng", 0)], writes=[("wmo", 1 + h)])
    mtmp = [A.alloc("mtmp%d" % i, [512], BF16) for i in range(2)]
    for jb in range(2):
        wg_, wgk = load_wblock(4104 + jb * 512)
        for jj in range(4):
            j = jb * 4 + jj
            for nb in range(NB):
                psy, pky = next_ps()

                def ymm2(e, psy=psy, j=j, nb=nb):
                    ins = None
                    for h in range(4):
                        ins = e.matmul(psy[:, :], lhsT=wmo[:, h, j * 128:(j + 1) * 128], rhs=hmT[:, h, nb * 512:(nb + 1) * 512],
                                       start=(h == 0), stop=(h == 3))
                    return ins
                P.add("pe", ymm2, reads=[("wmo", 1 + h) for h in range(4)] + [("hmT", c) for c in range(nb * 4, nb * 4 + 4)],
                      writes=[pky])
                psg2, pkg2 = proj_fm(wg_, wgk, jj, nb)
                sg, sgk = next_sgt()
                P.add("act", (lambda e, sg=sg, p=psg2: e.activation(out=sg, in_=p[:, :], func=AF.Sigmoid)),
                      reads=[pkg2], writes=[sgk])
                mt = mtmp[(j * NB + nb) % 2]
                mtk = ("mtmp%d" % ((j * NB + nb) % 2), 0)
                P.add("dve", (lambda e, sg=sg, p=psy, mt=mt: e.tensor_tensor(out=mt, in0=p[:, :], in1=sg, op=ALU.mult)),
                      reads=[pky, sgk], writes=[mtk])
                P.add("dve", (lambda e, mt=mt, j=j, nb=nb: e.tensor_tensor(
                    out=merged[:, j, nb * 512:(nb + 1) * 512], in0=merged[:, j, nb * 512:(nb + 1) * 512], in1=mt, op=ALU.add)),
                    reads=[mtk, ("merged", j, nb)], writes=[("merged", j, nb)])
    dbg_out("merged", merged, [("merged", j, nb) for j in range(8) for nb in range(NB)])
    for nm in ("hmT", "wmo", "mtmp0", "mtmp1", "sgt0", "sgt1", "hT", "wblk0", "wblk1", "wblk2"):
        A.free(nm)
    if stage <= 3:
        return finish()

    dgf = A.alloc("dgf", [128], F32)
    dgh = A.alloc("dgh", [128], BF16)
    dgl = A.alloc("dgl", [128], BF16)

    def row_bcast(name, col0, src=None, srckey=("modT", 1)):
        src = modT if src is None else src
        row = A.alloc(name, [D], F32)
        banks = [next_ps(), next_ps()]
        for j in range(8):
            P.add("dve", (lambda e, j=j: e.tensor_scalar(out=dgf, in0=ident_f, scalar1=src[:, col0 + j:col0 + j + 1], scalar2=None,
                                                         op0=ALU.mult)),
                  reads=[("ident_f", 0), srckey], writes=[("dgf", 0)])
            P.add("dve", lambda e: e.tensor_copy(out=dgh, in_=dgf), reads=[("dgf", 0)], writes=[("dgh", 0)])
            P.add("dve", lambda e: e.tensor_tensor(out=dgl, in0=dgf, in1=dgh, op=ALU.subtract),
                  reads=[("dgf", 0), ("dgh", 0)], writes=[("dgl", 0)])
            bk, bkk = banks[j // 4]

            def bmm(e, bk=bk, j=j):
                o = bk[:, (j % 4) * 128:(j % 4 + 1) * 128]
                e.matmul(o, lhsT=ones_b, rhs=dgh, start=True, stop=False)
                return e.matmul(o, lhsT=ones_b, rhs=dgl, start=False, stop=True)
            P.add("pe", bmm, reads=[("ones_b", 0), ("dgh", 0), ("dgl", 0)], writes=[bkk])
        for b2 in range(2):
            bk, bkk = banks[b2]
            P.add("act", (lambda e, bk=bk, b2=b2: e.activation(out=row[:, b2 * 512:(b2 + 1) * 512], in_=bk[:, :], func=AF.Identity)),
                  reads=[bkk], writes=[(name, b2)])
        return row

    gt1row = row_bcast("gt1row", 16)
    a2row = row_bcast("a2row", 0, src=a2, srckey=("a2", 0))
    sh2row = row_bcast("sh2row", 24)
    gt2row = row_bcast("gt2row", 40)
    wout = load_cast("wout", wout_d.rearrange("(c p) n -> p c n", p=128), [8, D])
    for k in range(8):
        P.add("dve", (lambda e, k=k: e.tensor_tensor(out=wout[:, k, :], in0=wout[:, k, :], in1=gt1row, op=ALU.mult)),
              reads=[("wout", 0), ("wout", 1 + k), ("gt1row", 0), ("gt1row", 1)], writes=[("wout", 1 + k)])
    x1 = A.alloc("x1", [NT, D], F32)
    for ti in range(NT):
        P.add("sp", (lambda e, ti=ti: e.dma_start(out=x1[:, ti, :], in_=x_d[ti * 128:(ti + 1) * 128, :])),
              writes=[("x1", ti)], dma=True, grp="x1ld%d" % ti)
    wr_f = A.alloc("wr_f", [8, 36], F32)
    wr_b = A.alloc("wr_b", [8, 36], BF16)
    with nc.allow_non_contiguous_dma(reason="small router weight rows"):
        P.add("sp", lambda e: e.dma_start(out=wr_f, in_=wr_d.rearrange("(c p) n -> p c n", p=128)), writes=[("wr_f", 0)],
              dma=True, grp="c_wr")
    P.add("dve", lambda e: e.tensor_copy(out=wr_b, in_=wr_f), reads=[("wr_f", 0)], writes=[("wr_b", 0)])
    brt = load_const("brt", br_d, [36])
    h2tok = A.alloc("h2tok", [NT, D], BF16)
    xn2 = [A.alloc("xn2_%d" % i, [D], F32) for i in range(2)]
    h2T = [A.alloc("h2T%d" % i, [8, 128], BF16) for i in range(2)]
    ss2 = A.alloc("ss2", [NT], F32)
    rs2 = A.alloc("rs2", [NT], F32)
    psr = [hold_ps(), hold_ps()]
    def emit_p5(ti):
        s2 = ti % 2
        P.add("act", (lambda e, ti=ti: e.activation(out=junk, in_=x1[:, ti, :], func=AF.Square, accum_out=ss2[:, ti:ti + 1])),
              reads=[("x1", ti)], writes=[("junk", 0), ("ss2", ti)])
        P.add("act", (lambda e, ti=ti: e.activation(out=rs2[:, ti:ti + 1], in_=ss2[:, ti:ti + 1], func=AF.Sqrt, scale=1.0 / D, bias=1e-6)),
              reads=[("ss2", ti)], writes=[("rs2", ti)])
        P.add("dve", (lambda e, ti=ti: e.reciprocal(out=rs2[:, ti:ti + 1], in_=rs2[:, ti:ti + 1])),
              reads=[("rs2", ti)], writes=[("rs2", ti)])
        P.add("act", (lambda e, ti=ti, s2=s2: e.activation(out=xn2[s2], in_=x1[:, ti, :], func=AF.Identity, scale=rs2[:, ti:ti + 1])),
              reads=[("x1", ti), ("rs2", ti)], writes=[("xn2_%d" % s2, 0)])
        P.add("dve", (lambda e, s2=s2: e.tensor_tensor(out=xn2[s2], in0=xn2[s2], in1=a2row, op=ALU.mult)),
              reads=[("xn2_%d" % s2, 0), ("a2row", 0), ("a2row", 1)], writes=[("xn2_%d" % s2, 0)])
        P.add("dve", (lambda e, s2=s2, ti=ti: e.tensor_tensor(out=h2tok[:, ti, :], in0=xn2[s2], in1=sh2row, op=ALU.add)),
              reads=[("xn2_%d" % s2, 0), ("sh2row", 0), ("sh2row", 1)], writes=[("h2tok", ti)])
        pt, pkt = next_ps()
        ptb = pt.bitcast(BF16)

        def h2tr(e, ptb=ptb, ti=ti):
            ins = None
            for c in range(8):
                ins = e.transpose(out=ptb[:, c * 128:(c + 1) * 128], in_=h2tok[:, ti, c * 128:(c + 1) * 128], identity=ident_b)
            return ins
        P.add("pe", h2tr, reads=[("ident_b", 0), ("h2tok", ti)], writes=[pkt])
        P.add("act", (lambda e, ptb=ptb, s2=s2: e.activation(out=h2T[s2].rearrange("p a b -> p (a b)"), in_=ptb[:, 0:1024], func=AF.Identity)),
              reads=[pkt], writes=[("h2T%d" % s2, 0)])
        bk, bkk = psr[ti // 8]

        def rmm(e, bk=bk, ti=ti, s2=s2):
            ins = None
            o = bk[:, (ti % 8) * 36:(ti % 8 + 1) * 36]
            for k in range(8):
                ins = e.matmul(o, lhsT=h2T[s2][:, k, :], rhs=wr_b[:, k, :], start=(k == 0), stop=(k == 7))
            return ins
        P.add("pe", rmm, reads=[("h2T%d" % s2, 0), ("wr_b", 0)], writes=[bkk])

    def emit_p4(ti):
        for half in range(2):
            ps, pk = next_ps()

            def omm(e, ps=ps, ti=ti, half=half):
                ins = None
                for k in range(8):
                    ins = e.matmul(ps[:, :], lhsT=merged[:, k, ti * 128:(ti + 1) * 128], rhs=wout[:, k, half * 512:(half + 1) * 512],
                                   start=(k == 0), stop=(k == 7))
                return ins
            P.add("pe", omm, reads=[("wout", 1 + k) for k in range(8)] + [("merged", k, ti // 4) for k in range(8)], writes=[pk])
            P.add("dve", (lambda e, ps=ps, ti=ti, half=half: e.tensor_tensor(
                out=x1[:, ti, half * 512:(half + 1) * 512], in0=x1[:, ti, half * 512:(half + 1) * 512], in1=ps[:, :], op=ALU.add)),
                reads=[pk, ("x1", ti)], writes=[("x1", ti)])

    emit_p4(0)
    for ti in range(NT):
        if ti + 1 < NT:
            emit_p4(ti + 1)
        emit_p5(ti)
        P.add("sp", (lambda e, ti=ti: e.dma_start(out=X1S[ti * 128:(ti + 1) * 128, :], in_=x1[:, ti, :])),
              reads=[("x1", ti)], writes=[("X1S", ti)], dma=True, grp="x1_spill")
    dbg_out("x1", x1, [("x1", ti) for ti in range(NT)])
    A.free("merged"); A.free("wout"); A.free("gt1row")

    Lg = A.alloc("Lg", [NT, 36], F32)
    for b2 in range(2):
        bk, bkk = psr[b2]
        P.add("dve", (lambda e, bk=bk, b2=b2: e.tensor_tensor(
            out=Lg[:, b2 * 8:(b2 + 1) * 8, :], in0=bk[:, 0:288].rearrange("p (a b) -> p a b", b=36),
            in1=brt.unsqueeze(1).to_broadcast([128, 8, 36]), op=ALU.add)),
            reads=[bkk, ("brt", 0)], writes=[("Lg", b2)])
    release_ps(psr[0][1]); release_ps(psr[1][1])
    dbg_out("Lg", Lg, [("Lg", 0), ("Lg", 1)])
    dbg_out("h2tok", h2tok, [("h2tok", ti) for ti in range(NT)])
    for nm in ("xn2_0", "xn2_1", "h2T0", "h2T1", "a2row", "sh2row", "wr_f", "wr_b"):
        A.free(nm)
    if stage <= 5:
        return finish()

    NCHK0 = NG - 15
    def T_(name, shape, dt=F32):
        return A.alloc(name, shape, dt)
    LK = [("Lg", 0), ("Lg", 1)]
    lg = Lg[:, :, 0:4]
    le = Lg[:, :, 4:36]
    gmax = T_("gmax", [NT])
    G1h = T_("G1h", [NT, 4])
    egs = T_("egs", [NT, 4])
    p_g = T_("p_g", [NT])
    P.add("dve", lambda e: e.tensor_reduce(out=gmax, in_=lg, axis=AX.X, op=ALU.max), reads=LK, writes=[("gmax", 0)])
    gmb = gmax.unsqueeze(2).to_broadcast([128, NT, 4])
    P.add("dve", lambda e: e.tensor_tensor(out=G1h, in0=lg, in1=gmb, op=ALU.is_equal), reads=LK + [("gmax", 0)], writes=[("G1h", 0)])
    P.add("dve", lambda e: e.tensor_tensor(out=egs, in0=lg, in1=gmb, op=ALU.subtract), reads=LK + [("gmax", 0)], writes=[("egs", 0)])
    P.add("act", lambda e: e.activation(out=egs, in_=egs, func=AF.Exp), reads=[("egs", 0)], writes=[("egs", 0)])
    P.add("dve", lambda e: e.tensor_reduce(out=p_g, in_=egs, axis=AX.X, op=ALU.add), reads=[("egs", 0)], writes=[("p_g", 0)])
    P.add("dve", lambda e: e.reciprocal(out=p_g, in_=p_g), reads=[("p_g", 0)], writes=[("p_g", 0)])
    tmp32 = T_("tmp32", [NT, 32])
    lsel = T_("lsel", [NT, 8])
    P.add("dve", lambda e: e.tensor_tensor(
        out=tmp32.rearrange("p t (g j) -> p t g j", j=8), in0=le.rearrange("p t (g j) -> p t g j", j=8),
        in1=G1h.unsqueeze(3).to_broadcast([128, NT, 4, 8]), op=ALU.mult),
        reads=LK + [("G1h", 0)], writes=[("tmp32", 0)])
    P.add("dve", lambda e: e.tensor_reduce(out=lsel, in_=tmp32.rearrange("p t (g j) -> p t j g", j=8), axis=AX.X, op=ALU.add),
          reads=[("tmp32", 0)], writes=[("lsel", 0)])
    m1 = T_("m1", [NT])
    m2 = T_("m2", [NT])
    E1 = T_("E1", [NT, 8])
    E2 = T_("E2", [NT, 8])
    ls2 = T_("ls2", [NT, 8])
    P.add("dve", lambda e: e.tensor_reduce(out=m1, in_=lsel, axis=AX.X, op=ALU.max), reads=[("lsel", 0)], writes=[("m1", 0)])
    P.add("dve", lambda e: e.tensor_tensor(out=E1, in0=lsel, in1=m1.unsqueeze(2).to_broadcast([128, NT, 8]), op=ALU.is_equal),
          reads=[("lsel", 0), ("m1", 0)], writes=[("E1", 0)])
    P.add("dve", lambda e: e.scalar_tensor_tensor(out=ls2.rearrange("p a b -> p (a b)"), in0=E1.rearrange("p a b -> p (a b)"),
                                                  scalar=-1e30, in1=lsel.rearrange("p a b -> p (a b)"), op0=ALU.mult, op1=ALU.add),
          reads=[("E1", 0), ("lsel", 0)], writes=[("ls2", 0)])
    P.add("dve", lambda e: e.tensor_reduce(out=m2, in_=ls2, axis=AX.X, op=ALU.max), reads=[("ls2", 0)], writes=[("m2", 0)])
    P.add("dve", lambda e: e.tensor_tensor(out=E2, in0=ls2, in1=m2.unsqueeze(2).to_broadcast([128, NT, 8]), op=ALU.is_equal),
          reads=[("ls2", 0), ("m2", 0)], writes=[("E2", 0)])
    w1 = T_("w1", [NT])
    w2 = T_("w2", [NT])
    P.add("dve", lambda e: e.tensor_tensor(out=w2, in0=m1, in1=m2, op=ALU.subtract), reads=[("m1", 0), ("m2", 0)], writes=[("w2", 0)])
    P.add("act", lambda e: e.activation(out=w1, in_=w2, func=AF.Sigmoid), reads=[("w2", 0)], writes=[("w1", 0)])
    P.add("dve", lambda e: e.tensor_tensor(out=w1, in0=w1, in1=p_g, op=ALU.mult), reads=[("w1", 0), ("p_g", 0)], writes=[("w1", 0)])
    P.add("dve", lambda e: e.tensor_tensor(out=w2, in0=p_g, in1=w1, op=ALU.subtract), reads=[("w1", 0), ("p_g", 0), ("w2", 0)], writes=[("w2", 0)])
    A1 = T_("A1", [NT, 32])
    A2 = T_("A2", [NT, 32])
    A12b = T_("A12b", [NT, 32], BF16)
    for g in range(4):
        gb = G1h[:, :, g].unsqueeze(2).to_broadcast([128, NT, 8])
        P.add("dve", (lambda e, g=g, gb=gb: e.tensor_tensor(out=A1[:, :, g * 8:(g + 1) * 8], in0=E1, in1=gb, op=ALU.mult)),
              reads=[("E1", 0), ("G1h", 0)], writes=[("A1", g)])
        P.add("dve", (lambda e, g=g, gb=gb: e.tensor_tensor(out=A2[:, :, g * 8:(g + 1) * 8], in0=E2, in1=gb, op=ALU.mult)),
              reads=[("E2", 0), ("G1h", 0)], writes=[("A2", g)])
    AK = [("A1", g) for g in range(4)] + [("A2", g) for g in range(4)]
    P.add("dve", lambda e: e.tensor_tensor(out=A12b, in0=A1, in1=A2, op=ALU.add), reads=AK, writes=[("A12b", 0)])
    lstrict = T_("lstrict", [128], BF16)
    lsf = T_("lsf", [128], F32)
    P.add("pool", lambda e: e.memset(lsf, 1.0), writes=[("lsf", 0)])
    P.add("pool", lambda e: e.affine_select(out=lsf, in_=lsf, pattern=[[1, 128]], compare_op=ALU.is_ge, fill=0.0, base=-1,
                                             channel_multiplier=-1), reads=[("lsf", 0)], writes=[("lsf", 0)])
    P.add("pool", lambda e: e.tensor_copy(out=lstrict, in_=lsf), reads=[("lsf", 0)], writes=[("lstrict", 0)])
    psw, pkw = next_ps()
    pst_, pkt_ = next_ps()
    A12f = A12b.rearrange("p a b -> p (a b)")
    P.add("pe", lambda e: e.matmul(psw[:, :], lhsT=lstrict, rhs=A12f, start=True, stop=True),
          reads=[("lstrict", 0), ("A12b", 0)], writes=[pkw])
    P.add("pe", lambda e: e.matmul(pst_[:, :], lhsT=ones_b, rhs=A12f, start=True, stop=True),
          reads=[("ones_b", 0), ("A12b", 0)], writes=[pkt_])
    carry = T_("carry", [NT + 1, 32])
    P.add("dve", lambda e: e.memset(carry[:, 0, :], 0.0), writes=[("carry", 0)])
    for ti in range(NT):
        P.add("dve", (lambda e, ti=ti: e.tensor_tensor(out=carry[:, ti + 1, :], in0=carry[:, ti, :], in1=pst_[:, ti * 32:(ti + 1) * 32],
                                                      op=ALU.add)),
              reads=[("carry", ti), pkt_], writes=[("carry", ti + 1)])
    counts = carry[:, NT, :]
    CK = [("carry", ti) for ti in range(NT + 1)]
    thr = T_("thr", [64])
    thr_i = T_("thr_i", [64], I32)
    P.add("pool", lambda e: e.iota(thr_i, pattern=[[1, 64]], base=0, channel_multiplier=0), writes=[("thr_i", 0)])
    P.add("pool", lambda e: e.tensor_copy(out=thr, in_=thr_i), reads=[("thr_i", 0)], writes=[("thr", 0)])
    thr128 = T_("thr128", [8])
    P.add("pool", lambda e: e.tensor_scalar(out=thr128, in0=thr[:, 0:8], scalar1=float(GROWS), scalar2=None, op0=ALU.mult),
          reads=[("thr", 0)], writes=[("thr128", 0)])
    cmp1 = T_("cmp1", [32, 8])
    ngrp = T_("ngrp", [32])
    P.add("dve", lambda e: e.tensor_tensor(out=cmp1, in0=counts.unsqueeze(2).to_broadcast([128, 32, 8]),
                                           in1=thr128.unsqueeze(1).to_broadcast([128, 32, 8]), op=ALU.is_gt),
          reads=CK + [("thr128", 0)], writes=[("cmp1", 0)])
    P.add("dve", lambda e: e.tensor_reduce(out=ngrp, in_=cmp1, axis=AX.X, op=ALU.add), reads=[("cmp1", 0)], writes=[("ngrp", 0)])
    cs = [T_("cs0", [32]), T_("cs1", [32])]
    src, srck = ngrp, ("ngrp", 0)
    for si, sh in enumerate((1, 2, 4, 8, 16)):
        dst = cs[si % 2]
        dk = ("cs%d" % (si % 2),)
        P.add("dve", (lambda e, dst=dst, src=src, sh=sh: e.tensor_copy(out=dst[:, 0:sh], in_=src[:, 0:sh])),
              reads=[srck], writes=[dk + (0,)])
        P.add("dve", (lambda e, dst=dst, src=src, sh=sh: e.tensor_tensor(out=dst[:, sh:32], in0=src[:, sh:32], in1=src[:, 0:32 - sh],
                                                                        op=ALU.add)),
              reads=[srck], writes=[dk + (1,)])
        src, srck = dst, dk + (1,)
        if si > 0:
            pass
    pend = src
    PK = [("cs0", 0), ("cs0", 1), ("cs1", 0), ("cs1", 1)]
    pstart = T_("pstart", [32])
    P.add("dve", lambda e: e.tensor_tensor(out=pstart, in0=pend, in1=ngrp, op=ALU.subtract), reads=PK + [("ngrp", 0)],
          writes=[("pstart", 0)])
    P.add("dve", lambda e: e.tensor_scalar(out=pstart, in0=pstart, scalar1=float(GROWS), scalar2=None, op0=ALU.mult),
          reads=[("pstart", 0)], writes=[("pstart", 0)])
    cmp2 = T_("cmp2", [NG, 32])
    grpf = T_("grpf", [NG])
    grpi = T_("grpi", [NG], I32)
    P.add("dve", lambda e: e.tensor_tensor(out=cmp2, in0=pend.unsqueeze(1).to_broadcast([128, NG, 32]),
                                           in1=thr[:, 0:NG].unsqueeze(2).to_broadcast([128, NG, 32]), op=ALU.is_le),
          reads=PK + [("thr", 0)], writes=[("cmp2", 0)])
    P.add("dve", lambda e: e.tensor_reduce(out=grpf, in_=cmp2, axis=AX.X, op=ALU.add), reads=[("cmp2", 0)], writes=[("grpf", 0)])
    P.add("dve", lambda e: e.tensor_scalar(out=grpf, in0=grpf, scalar1=31.0, scalar2=None, op0=ALU.min),
          reads=[("grpf", 0)], writes=[("grpf", 0)])
    P.add("dve", lambda e: e.tensor_copy(out=grpi, in_=grpf), reads=[("grpf", 0)], writes=[("grpi", 0)])
    pidx_i = T_("pidx_i", [1], I32)
    pidx = T_("pidx", [1])
    idxf = T_("idxf", [NG])
    inval = T_("inval", [NG])
    idxw = T_("idxw", [NG], I32)
    idxs = T_("idxs", [NG], I32)
    P.add("pool", lambda e: e.iota(pidx_i, pattern=[[0, 1]], base=0, channel_multiplier=1), writes=[("pidx_i", 0)])
    P.add("pool", lambda e: e.tensor_copy(out=pidx, in_=pidx_i), reads=[("pidx_i", 0)], writes=[("pidx", 0)])
    P.add("dve", lambda e: e.tensor_scalar(out=idxf, in0=grpf, scalar1=128.0, scalar2=pidx[:, 0:1], op0=ALU.mult, op1=ALU.add),
          reads=[("grpf", 0), ("pidx", 0)], writes=[("idxf", 0)])
    P.add("dve", lambda e: e.tensor_scalar(out=inval, in0=thr[:, 0:NG], scalar1=pend[:, 31:32], scalar2=None, op0=ALU.is_ge),
          reads=PK + [("thr", 0)], writes=[("inval", 0)])
    P.add("dve", lambda e: e.tensor_copy(out=idxw, in_=idxf), reads=[("idxf", 0)], writes=[("idxw", 0)])
    P.add("dve", lambda e: e.scalar_tensor_tensor(out=idxf, in0=inval, scalar=1.0e6, in1=idxf, op0=ALU.mult, op1=ALU.add),
          reads=[("inval", 0), ("idxf", 0), ("idxw", 0)], writes=[("idxf", 0)])
    P.add("dve", lambda e: e.tensor_copy(out=idxs, in_=idxf), reads=[("idxf", 0)], writes=[("idxs", 0)])
    stg = {wn: [A.alloc("stg_" + wn + "0", [4096], F32)] for wn in ("wg", "wu", "wd")}
    for (wsrc_, wn_) in ((weg_d, "wg"), (weu_d, "wu"), (wed_d, "wd")):
        P.add("pool", (lambda e, wsrc_=wsrc_, wn_=wn_: e.indirect_dma_start(
            out=stg[wn_][0], out_offset=None, in_=wsrc_, in_offset=bass.IndirectOffsetOnAxis(ap=idxw[:, 0:1], axis=0))),
            reads=[("idxw", 0)], writes=[("stg_" + wn_ + "0", 0)], dma=True, grp="stg_" + wn_ + "0")
    slot = T_("slot", [NT, 32])
    P.add("dve", lambda e: e.tensor_tensor(out=slot, in0=psw[:, :].rearrange("p (a b) -> p a b", b=32), in1=carry[:, 0:NT, :], op=ALU.add),
          reads=[pkw] + CK, writes=[("slot", 0)])
    P.add("dve", lambda e: e.tensor_tensor(out=slot, in0=slot, in1=pstart.unsqueeze(1).to_broadcast([128, NT, 32]), op=ALU.add),
          reads=[("slot", 0), ("pstart", 0)], writes=[("slot", 0)])
    dstf = T_("dstf", [2, NT])
    dsti = T_("dsti", [2, NT], I32)
    for q, (Aq, qk) in enumerate(((A1, "A1"), (A2, "A2"))):
        P.add("dve", (lambda e, Aq=Aq: e.tensor_tensor(out=tmp32, in0=Aq, in1=slot, op=ALU.mult)),
              reads=[(qk, g) for g in range(4)] + [("slot", 0), ("tmp32", 0)], writes=[("tmp32", 0)])
        P.add("dve", (lambda e, q=q: e.tensor_reduce(out=dstf[:, q, :], in_=tmp32, axis=AX.X, op=ALU.add)),
              reads=[("tmp32", 0)], writes=[("dstf", q)])
    P.add("dve", lambda e: e.tensor_copy(out=dsti, in_=dstf), reads=[("dstf", 0), ("dstf", 1)], writes=[("dsti", 0)])
    dbg_out("dstf", dstf, [("dstf", 0), ("dstf", 1)])
    dbg_out("grpf", grpf, [("grpf", 0)])
    dbg_out("w1", w1, [("w1", 0)])
    dbg_out("w2", w2, [("w2", 0)])
    for nm in ("gmax", "G1h", "egs", "p_g", "tmp32", "lsel", "m1", "m2", "E1", "E2", "ls2", "A1", "A2", "A12b", "lstrict", "lsf",
               "carry", "thr", "thr_i", "thr128", "cmp1", "ngrp", "cs0", "cs1", "pstart", "slot", "cmp2", "Lg"):
        A.free(nm)
    if stage <= 5.5:
        return finish()

    for ti in range(NT):
        for q in range(2):
            P.add("pool", (lambda e, ti=ti, q=q: e.indirect_dma_start(
                out=XS, out_offset=bass.IndirectOffsetOnAxis(ap=dsti[:, q, ti:ti + 1], axis=0),
                in_=h2tok[:, ti, :], in_offset=None)),
                reads=[("h2tok", ti), ("dsti", 0)] + XSZ_KEYS, writes=[("XS", ti, q)], dma=True, grp="xs_sc")
    XS_KEYS = [("XS", ti, q) for ti in range(NT) for q in range(2)]
    A.free("h2tok")
    A.free("x1")
    for wn in ("wg", "wu", "wd"):
        stg[wn].append(A.alloc("stg_" + wn + "1", [4096], F32))
    NS = 2
    wgs = [A.alloc("wg%d" % i, [8, 512], BF16) for i in range(NS)]
    wus = [A.alloc("wu%d" % i, [8, 512], BF16) for i in range(NS)]
    wds = [A.alloc("wd%d" % i, [4, D], BF16) for i in range(NS)]
    xgt = [A.alloc("xgt%d" % i, [D], BF16) for i in range(2)]
    xgT = [A.alloc("xgT%d" % i, [8, 256], BF16) for i in range(2)]
    sgl = [A.alloc("sgl%d" % i, [4, 256], BF16) for i in range(1)]
    aT = [A.alloc("aT%d" % i, [4, 256], BF16) for i in range(2)]
    ysb = [A.alloc("ysb%d" % i, [D], BF16) for i in range(2)]
    def emit_load(g, part="both"):
        sl = g % NS
        s2 = g % 2
        for (wt, wsrc, wn, ceng) in ((wgs, weg_d, "wg", "act"), (wus, weu_d, "wu", "dve"), (wds, wed_d, "wd", "pool")):
            st_ = stg[wn][g % 2]
            sk_ = "stg_%s%d" % (wn, g % 2)
            if part in ("both", "dma") and g >= NCHK0:
                P.add("pool", (lambda e, g=g, st_=st_, wsrc=wsrc: e.indirect_dma_start(
                    out=st_, out_offset=None, in_=wsrc,
                    in_offset=bass.IndirectOffsetOnAxis(ap=idxs[:, g:g + 1], axis=0), bounds_check=32 * 128 - 1, oob_is_err=False)),
                    reads=[("idxs", 0)], writes=[(sk_, 0)], dma=True, grp=sk_)
            elif part in ("both", "dma"):
                P.add("pool", (lambda e, g=g, st_=st_, wsrc=wsrc: e.indirect_dma_start(
                    out=st_, out_offset=None, in_=wsrc,
                    in_offset=bass.IndirectOffsetOnAxis(ap=idxw[:, g:g + 1], axis=0))),
                    reads=[("idxw", 0)], writes=[(sk_, 0)], dma=True, grp=sk_)
            if part == "dma":
                continue
            dstv = wt[sl].rearrange("p a b -> p (a b)")
            if ceng == "act":
                P.add("act", (lambda e, dstv=dstv, st_=st_: e.activation(out=dstv, in_=st_, func=AF.Identity)),
                      reads=[(sk_, 0)], writes=[("%s%d" % (wn, sl), 0)])
            elif ceng == "dve":
                P.add("dve", (lambda e, dstv=dstv, st_=st_: e.tensor_copy(out=dstv, in_=st_)),
                      reads=[(sk_, 0)], writes=[("%s%d" % (wn, sl), 0)])
            else:
                P.add("act", (lambda e, dstv=dstv, st_=st_: e.activation(out=dstv[:, 0:2048], in_=st_[:, 0:2048], func=AF.Identity)),
                      reads=[(sk_, 0)], writes=[("%s%d" % (wn, sl), 0)])
                P.add("dve", (lambda e, dstv=dstv, st_=st_: e.tensor_copy(out=dstv[:, 2048:4096], in_=st_[:, 2048:4096])),
                      reads=[(sk_, 0)], writes=[("%s%d" % (wn, sl), 1)])

    def emit_compute_a(g):
        s2 = g % 2
        for hf in range(2):
            xi = (2 * g + hf) % 2
            r0 = g * GROWS + hf * 128
            P.add("sp", (lambda e, r0=r0, xi=xi: e.dma_start(out=xgt[xi], in_=XS[r0:r0 + 128, :])),
                  reads=XS_KEYS, writes=[("xgt%d" % xi, 0)], dma=True, grp="xgt%d" % xi)
            pt, pkt = next_ps()
            ptb = pt.bitcast(BF16)

            def xtr(e, ptb=ptb, xi=xi):
                ins = None
                for c in range(8):
                    ins = e.transpose(out=ptb[:, c * 128:(c + 1) * 128], in_=xgt[xi][:, c * 128:(c + 1) * 128], identity=ident_b)
                return ins
            P.add("pe", xtr, reads=[("ident_b", 0), ("xgt%d" % xi, 0)], writes=[pkt])
            P.add("act", (lambda e, ptb=ptb, s2=s2, hf=hf: e.activation(
                out=xgT[s2][:, :, hf * 128:(hf + 1) * 128], in_=ptb[:, 0:1024].rearrange("p (a b) -> p a b", b=128), func=AF.Identity)),
                reads=[pkt], writes=[("xgT%d" % s2, hf)])

    def emit_compute_b1(g):
        sl = g % NS
        s2 = g % 2
        pg_ = [next_ps(), next_ps()]
        pu_ = [next_ps(), next_ps()]

        def gumm(e, pg_=pg_, pu_=pu_, sl=sl, s2=s2):
            ins = None
            for (pp, ww) in ((pg_, wgs), (pu_, wus)):
                for fc in range(4):
                    o = pp[fc // 2][0][:, (fc % 2) * 256:(fc % 2 + 1) * 256]
                    for k in range(8):
                        ins = e.matmul(o, lhsT=ww[sl][:, k, fc * 128:(fc + 1) * 128], rhs=xgT[s2][:, k, :], start=(k == 0), stop=(k == 7))
            return ins
        P.add("pe", gumm, reads=[("wg%d" % sl, 0), ("wu%d" % sl, 0), ("xgT%d" % s2, 0), ("xgT%d" % s2, 1)],
              writes=[pg_[0][1], pg_[1][1], pu_[0][1], pu_[1][1]])
        for b2 in range(2):
            P.add("act", (lambda e, pg_=pg_, s2=s2, b2=b2: e.activation(
                out=sgl[0][:, 2 * b2:2 * b2 + 2, :].rearrange("p a b -> p (a b)"), in_=pg_[b2][0][:, :], func=AF.Silu)),
                reads=[pg_[b2][1]], writes=[("sgl0", b2)])
            P.add("dve", (lambda e, pu_=pu_, s2=s2, b2=b2: e.tensor_tensor(
                out=aT[s2][:, 2 * b2:2 * b2 + 2, :].rearrange("p a b -> p (a b)"), in0=pu_[b2][0][:, :],
                in1=sgl[0][:, 2 * b2:2 * b2 + 2, :].rearrange("p a b -> p (a b)"), op=ALU.mult)),
                reads=[pu_[b2][1], ("sgl0", b2)], writes=[("aT%d" % s2, b2)])

    def emit_compute_b2(g):
        sl = g % NS
        s2 = g % 2
        for hf in range(2):
            yi = (2 * g + hf) % 2
            py = [next_ps(), next_ps()]

            def dmm(e, py=py, sl=sl, s2=s2, hf=hf):
                ins = None
                for half in range(2):
                    for fc in range(4):
                        ins = e.matmul(py[half][0][:, :], lhsT=aT[s2][:, fc, hf * 128:(hf + 1) * 128],
                                       rhs=wds[sl][:, fc, half * 512:(half + 1) * 512], start=(fc == 0), stop=(fc == 3))
                return ins
            P.add("pe", dmm, reads=[("wd%d" % sl, 0), ("wd%d" % sl, 1), ("aT%d" % s2, 0), ("aT%d" % s2, 1)], writes=[py[0][1], py[1][1]])
            for half in range(2):
                P.add("dve", (lambda e, py=py, yi=yi, half=half: e.tensor_tensor(
                    out=ysb[yi][:, half * 512:(half + 1) * 512], in0=py[half][0][:, :], in1=gt2row[:, half * 512:(half + 1) * 512],
                    op=ALU.mult)),
                    reads=[py[half][1], ("gt2row", half)], writes=[("ysb%d" % yi, half)])
            r0 = g * GROWS + hf * 128
            P.add("sp", (lambda e, r0=r0, yi=yi: e.dma_start(out=YS[r0:r0 + 128, :], in_=ysb[yi])),
                  reads=[("ysb%d" % yi, 0), ("ysb%d" % yi, 1)], writes=[("YS", g, hf)], dma=True, grp="ys_st%d" % yi)

    emit_load(0, "cast")
    emit_compute_a(0)
    for g in range(NG):
        emit_compute_b1(g)
        if g + 1 < NG:
            emit_load(g + 1)
            emit_compute_a(g + 1)
        emit_compute_b2(g)
    YS_KEYS = [("YS", g, hf) for g in range(NG) for hf in range(2)]
    for i in range(NS):
        A.free("wg%d" % i); A.free("wu%d" % i); A.free("wd%d" % i)
    for nm in ("stg_wg0", "stg_wu0", "stg_wd0", "stg_wg1", "stg_wu1", "stg_wd1", "xgt0", "xgt1", "xgT0", "xgT1", "sgl0", "aT0", "aT1", "ysb0", "ysb1"):
        A.free(nm)

    gfin = load_const("gfin", gfin_d, [D])
    NYG = 4
    yg = [[A.alloc("yg%d_%d" % (q, i), [D], BF16) for i in range(NYG)] for q in range(2)]
    acc = [A.alloc("acc%d" % i, [D], F32) for i in range(2)]
    outt = [A.alloc("outt%d" % i, [D], F32) for i in range(2)]
    ssf = A.alloc("ssf", [NT], F32)
    rsf = A.alloc("rsf", [NT], F32)
    xr = [A.alloc("xr%d" % i, [D], F32) for i in range(NYG)]

    def emit_g(ti):
        s4 = ti % NYG
        P.add("sp", (lambda e, ti=ti, s4=s4: e.dma_start(out=xr[s4], in_=X1S[ti * 128:(ti + 1) * 128, :])),
              reads=[("X1S", ti)], writes=[("xr%d" % s4, 0)], dma=True, grp="xr%d" % s4)
        for q in range(2):
            P.add("pool", (lambda e, ti=ti, q=q, s4=s4: e.indirect_dma_start(
                out=yg[q][s4], out_offset=None, in_=YS,
                in_offset=bass.IndirectOffsetOnAxis(ap=dsti[:, q, ti:ti + 1], axis=0))),
                reads=YS_KEYS + [("dsti", 0)], writes=[("yg%d_%d" % (q, s4), 0)], dma=True, grp="yg%d_%d" % (q, s4))

    def emit_c1(ti):
        s4 = ti % NYG
        s2 = ti % 2
        y1, y2 = yg[0][s4], yg[1][s4]
        k1, k2 = ("yg0_%d" % s4, 0), ("yg1_%d" % s4, 0)
        ac = acc[s2]
        ak = ("acc%d" % s2, 0)
        P.add("act", (lambda e, y1=y1, ac=ac, ti=ti: e.activation(out=ac, in_=y1, func=AF.Identity, scale=w1[:, ti:ti + 1])),
              reads=[k1, ("w1", 0)], writes=[ak])
        P.add("dve", (lambda e, ac=ac, y2=y2, ti=ti: e.scalar_tensor_tensor(out=ac, in0=y2, scalar=w2[:, ti:ti + 1], in1=ac,
                                                                          op0=ALU.mult, op1=ALU.add)),
              reads=[ak, k2, ("w2", 0)], writes=[ak])
        P.add("dve", (lambda e, ac=ac, ti=ti: e.tensor_tensor(out=xr[ti % NYG], in0=xr[ti % NYG], in1=ac, op=ALU.add)),
              reads=[ak, ("xr%d" % (ti % NYG), 0)], writes=[("xr%d" % (ti % NYG), 0)])

    def emit_c2(ti):
        s2 = ti % 2
        P.add("act", (lambda e, ti=ti: e.activation(out=junk, in_=xr[ti % NYG], func=AF.Square, accum_out=ssf[:, ti:ti + 1])),
              reads=[("xr%d" % (ti % NYG), 0)], writes=[("junk", 0), ("ssf", ti)])
        P.add("act", (lambda e, ti=ti: e.activation(out=rsf[:, ti:ti + 1], in_=ssf[:, ti:ti + 1], func=AF.Sqrt, scale=1.0 / D, bias=1e-6)),
              reads=[("ssf", ti)], writes=[("rsf", ti)])
        P.add("dve", (lambda e, ti=ti: e.reciprocal(out=rsf[:, ti:ti + 1], in_=rsf[:, ti:ti + 1])),
              reads=[("rsf", ti)], writes=[("rsf", ti)])
        ot = outt[s2]
        ok = ("outt%d" % s2, 0)
        P.add("act", (lambda e, ot=ot, ti=ti: e.activation(out=ot, in_=xr[ti % NYG], func=AF.Identity, scale=rsf[:, ti:ti + 1])),
              reads=[("xr%d" % (ti % NYG), 0), ("rsf", ti)], writes=[ok])
        P.add("dve", (lambda e, ot=ot: e.tensor_tensor(out=ot, in0=ot, in1=gfin, op=ALU.mult)),
              reads=[ok, ("gfin", 0)], writes=[ok])
        P.add("sp", (lambda e, ot=ot, ti=ti: e.dma_start(out=out_d[ti * 128:(ti + 1) * 128, :], in_=ot)),
              reads=[ok], dma=True, grp="out")

    for ti in range(min(3, NT)):
        emit_g(ti)
    for ti in range(NT):
        emit_c1(ti)
        if ti > 0:
            emit_c2(ti - 1)
        if ti + 3 < NT:
            emit_g(ti + 3)
    emit_c2(NT - 1)
    P.emit(final_wait_groups=["out"] + (["dbgout"] if "dbgout" in P.dma_groups else []))
    build.stats = dict(peak_kb=A.peak * 4 / 1024.0, n_ops=len(P.all_ops), n_groups=len(P.dma_groups))
    return nc, dbg_outs


def host_layout(inp, b):
    f = lambda a: np.ascontiguousarray(a, dtype=np.float32)
    col = lambda v, n: f(np.asarray(v).reshape(n, 128).T)
    m = {}
    m["x"] = f(inp["x"][b])
    m["c_col"] = col(inp["c"][b], 8)
    m["w_ada"] = f(inp["w_ada"][0])
    m["b_ada_col"] = col(inp["b_ada"][0], 48)
    m["g1_col"] = col(inp["g_norm1"][0], 8)
    m["g2_col"] = col(inp["g_norm2"][0], 8)
    m["w_in"] = f(inp["w_in"][0])
    m["b_if_bc"] = f(np.broadcast_to(inp["b_if"][0][None, :], (128, 8)))
    m["conv_w_col"] = f(inp["conv_dw_w"][0].reshape(31, 4, 128).transpose(2, 1, 0))
    m["conv_b_col"] = col(inp["conv_dw_b"][0], 4)
    m["conv_lng_col"] = col(inp["conv_ln_g"][0], 4)
    m["conv_lnb_col"] = col(inp["conv_ln_b"][0], 4)
    m["w_conv_out"] = f(inp["w_conv_out"][0])
    m["qk_w_col"] = f(inp["qk_conv_w"][0].reshape(4, 8, 128).transpose(2, 1, 0))
    m["qk_b_col"] = col(inp["qk_conv_b"][0], 8)
    m["mng_col"] = col(inp["m_norm_g"][0], 4)
    m["w_m_out"] = f(inp["w_m_out"][0])
    m["w_out"] = f(inp["w_out"][0])
    m["w_router"] = f(np.concatenate([inp["w_rg"][0], inp["w_re"][0]], axis=1))
    m["b_router_bc"] = f(np.broadcast_to(np.concatenate([inp["b_rg"][0], inp["b_re"][0]])[None, :], (128, 36)))
    m["w_e_gate_l"] = f(inp["w_e_gate"][0].reshape(32, 8, 128, 512).transpose(0, 2, 1, 3).reshape(32 * 128, 8 * 512))
    m["w_e_up_l"] = f(inp["w_e_up"][0].reshape(32, 8, 128, 512).transpose(0, 2, 1, 3).reshape(32 * 128, 8 * 512))
    m["w_e_down_l"] = f(inp["w_e_down"][0].reshape(32, 4, 128, D).transpose(0, 2, 1, 3).reshape(32 * 128, 4 * D))
    m["g_final_bc"] = f(np.broadcast_to(np.asarray(inp["g_final"])[None, :], (128, D)))
    return m


def kernel(**inputs):
    nc, _ = build()
    shared = host_layout(inputs, 0)
    in_maps = []
    for b in range(8):
        m = dict(shared)
        m["x"] = np.ascontiguousarray(inputs["x"][b], dtype=np.float32)
        m["c_col"] = np.ascontiguousarray(np.asarray(inputs["c"][b]).reshape(8, 128).T, dtype=np.float32)
        in_maps.append(m)
    res = run_bass_kernel_spmd(nc, in_maps, core_ids=list(range(8)))
    return np.stack([np.asarray(r["out"]) for r in res.results], axis=0).astype(np.float32)
```

```python
import contextlib
import numpy as np
import concourse.bass as bass
import concourse.mybir as mybir
from concourse.bass_utils import run_bass_kernel_spmd

F32 = mybir.dt.float32
BF16 = mybir.dt.bfloat16
I32 = mybir.dt.int32
AF = mybir.ActivationFunctionType
ALU = mybir.AluOpType
AX = mybir.AxisListType

T = 2048
D = 1024
NT = 16
NB = 4
DIN = 5128
ENG_NAMES = ("pe", "act", "dve", "pool", "sp")


class Op:
    __slots__ = ("eng", "fn", "is_dma", "grp", "signal", "val", "idx", "deps")

    def __init__(self, eng, fn, is_dma, grp):
        self.eng = eng
        self.fn = fn
        self.is_dma = is_dma
        self.grp = grp
        self.signal = False
        self.val = None
        self.idx = None
        self.deps = []


def _reduce_ops(ops):
    latest = {}
    dm = {}
    for o in ops:
        if o.is_dma:
            if o.grp not in dm or dm[o.grp].idx < o.idx:
                dm[o.grp] = o
        else:
            if o.eng not in latest or latest[o.eng].idx < o.idx:
                latest[o.eng] = o
    return list(latest.values()) + list(dm.values())


class Prog:
    def __init__(self, nc):
        self.nc = nc
        self.ops = {e: [] for e in ENG_NAMES}
        self.all_ops = []
        self.last_writer = {}
        self.readers = {}
        self.dma_groups = {}
        self.buf_pred = {}
        self.keys_by_buf = {}
        self.wait_all_groups = set()

    def _touch(self, k):
        if k not in self.readers:
            self.readers[k] = list(self.buf_pred.get(k[0], ()))
            self.last_writer[k] = None
            self.keys_by_buf.setdefault(k[0], set()).add(k)

    def ops_touching(self, bufname):
        s = list(self.buf_pred.get(bufname, ()))
        for k in self.keys_by_buf.get(bufname, ()):
            w = self.last_writer.get(k)
            if w is not None:
                s.append(w)
            s.extend(self.readers.get(k, ()))
        return _reduce_ops(s)

    def add(self, eng, fn, reads=(), writes=(), dma=False, grp=None):
        op = Op(eng, fn, dma, grp)
        op.idx = len(self.all_ops)
        self.all_ops.append(op)
        self.ops[eng].append(op)
        if dma:
            assert grp is not None
            self.dma_groups.setdefault(grp, []).append(op)
        deps = []
        for k in reads:
            self._touch(k)
            w = self.last_writer[k]
            if w is not None:
                deps.append((w, "raw"))
            elif self.readers[k] and k[0] in self.buf_pred:
                pass
        for k in writes:
            self._touch(k)
            w = self.last_writer[k]
            if w is not None:
                deps.append((w, "waw"))
            for r in self.readers[k]:
                deps.append((r, "war"))
        for d, kind in deps:
            if d is op:
                continue
            if (not d.is_dma) and (not dma) and d.eng == eng:
                if eng == "pe":
                    continue
            op.deps.append(d)
        for k in reads:
            self.readers[k].append(op)
        for k in writes:
            self.last_writer[k] = op
            self.readers[k] = []
        return op

    def emit(self, final_wait_groups=()):
        nc = self.nc
        for op in self.all_ops:
            op.deps = _reduce_ops(op.deps)
            for d in op.deps:
                d.signal = True
        for e in ENG_NAMES:
            c = 0
            for op in self.ops[e]:
                if (not op.is_dma) and op.signal:
                    c += 1
                    op.val = c
        gtotal = {}
        for g, lst in self.dma_groups.items():
            c = 0
            for op in lst:
                c += 16
                op.val = c
            gtotal[g] = c
        with contextlib.ExitStack() as st:
            esem = {e: st.enter_context(nc.semaphore("s_" + e)) for e in ENG_NAMES}
            gsem = {g: st.enter_context(nc.semaphore("d_%d" % i))
                    for i, g in enumerate(self.dma_groups)}
            block = st.enter_context(nc.Block())

            def run(e, engobj):
                seen = {}
                for op in self.ops[e]:
                    for d in op.deps:
                        if d.is_dma:
                            key = ("g", d.grp)
                            sem = gsem[d.grp]
                            v = gtotal[d.grp] if d.grp in self.wait_all_groups else d.val
                        else:
                            key = ("e", d.eng)
                            sem = esem[d.eng]
                            v = d.val
                        if seen.get(key, 0) >= v:
                            continue
                        seen[key] = v
                        engobj.wait_ge(sem, v)
                    ins = op.fn(engobj)
                    if op.is_dma:
                        ins.then_inc(gsem[op.grp], 16)
                    elif op.signal:
                        ins.then_inc(esem[e], 1)
                if e == "sp":
                    for g in final_wait_groups:
                        engobj.wait_ge(gsem[g], gtotal[g])

            block.tensor(lambda eng: run("pe", eng))
            block.scalar(lambda eng: run("act", eng))
            block.vector(lambda eng: run("dve", eng))
            block.gpsimd(lambda eng: run("pool", eng))
            block.sync(lambda eng: run("sp", eng))


class Arena:
    def __init__(self, nc, prog, words):
        self.t = nc.alloc_sbuf_tensor("arena", [128, words], F32)
        self.P = prog
        self.free_list = [(0, words)]
        self.live = {}
        self.dead = []
        self.peak = 0

    def alloc(self, name, shape, dt, parts=128):
        n = int(np.prod(shape))
        esz = 2 if dt == BF16 else 4
        words = (n * esz + 31) // 32 * 8
        small = words <= 1100
        order = range(len(self.free_list) - 1, -1, -1) if small else range(len(self.free_list))
        for i in order:
            o, w = self.free_list[i]
            if w >= words:
                if w == words:
                    off = o
                    self.free_list.pop(i)
                elif small:
                    off = o + w - words
                    self.free_list[i] = (o, w - words)
                else:
                    off = o
                    self.free_list[i] = (o + words, w - words)
                break
        else:
            raise RuntimeError("SBUF arena full allocating %s (%d words); live=%s" % (
                name, words, {k: v[1] for k, v in self.live.items()}))
        self.live[name] = (off, words)
        self.peak = max(self.peak, off + words)
        preds = []
        for (o, w, nm) in self.dead:
            if o < off + words and off < o + w:
                preds.extend(self.P.ops_touching(nm))
        assert name not in self.P.keys_by_buf, name
        self.P.buf_pred[name] = _reduce_ops(preds)
        v = self.t[0:parts, off:off + words]
        if dt != F32:
            v = v.bitcast(dt)
        v = v[:, 0:n]
        if len(shape) == 2:
            v = v.rearrange("p (a b) -> p a b", b=shape[1])
        elif len(shape) == 3:
            v = v.rearrange("p (a b c) -> p a b c", b=shape[1], c=shape[2])
        return v

    def free(self, name):
        off, words = self.live.pop(name)
        self.dead.append((off, words, name))
        fl = self.free_list + [(off, words)]
        fl.sort()
        merged = []
        for o, w in fl:
            if merged and merged[-1][0] + merged[-1][1] == o:
                merged[-1] = (merged[-1][0], merged[-1][1] + w)
            else:
                merged.append((o, w))
        self.free_list = merged


def build(stage=99, dbg=()):
    nc = bass.Bass("TRN2", target_bir_lowering=False)
    P = Prog(nc)
    A = Arena(nc, P, 52992)

    def din(name, shape, dt=F32):
        return nc.dram_tensor(name, list(shape), dt, kind="ExternalInput").ap()

    x_d = din("x", [T, D])
    ccol_d = din("c_col", [128, 8])
    wada_d = din("w_ada", [D, 6 * D])
    bada_d = din("b_ada_col", [128, 48])
    g1_d = din("g1_col", [128, 8])
    g2_d = din("g2_col", [128, 8])
    win_d = din("w_in", [D, DIN])
    bif_d = din("b_if_bc", [128, 8])
    cw_d = din("conv_w_col", [128, 4, 31])
    cb_d = din("conv_b_col", [128, 4])
    clg_d = din("conv_lng_col", [128, 4])
    clb_d = din("conv_lnb_col", [128, 4])
    wco_d = din("w_conv_out", [512, D])
    qkw_d = din("qk_w_col", [128, 8, 4])
    qkb_d = din("qk_b_col", [128, 8])
    mng_d = din("mng_col", [128, 4])
    wmo_d = din("w_m_out", [512, D])
    wout_d = din("w_out", [D, D])
    wr_d = din("w_router", [D, 36])
    br_d = din("b_router_bc", [128, 36])
    weg_d = din("w_e_gate_l", [32 * 128, 8 * 512])
    weu_d = din("w_e_up_l", [32 * 128, 8 * 512])
    wed_d = din("w_e_down_l", [32 * 128, 4 * D])
    gfin_d = din("g_final_bc", [128, D])
    out_d = nc.dram_tensor("out", [T, D], F32, kind="ExternalOutput").ap()

    dbg_outs = {}

    def dbg_out(name, ap, reads):
        if name not in dbg:
            return
        shape = list(ap.shape)
        dt = ap.dtype
        d = nc.dram_tensor("dbg_" + name, shape, dt, kind="ExternalOutput").ap()
        dbg_outs[name] = d
        P.add("sp", lambda e: e.dma_start(out=d, in_=ap), reads=reads, dma=True, grp="dbgout")

    psb = [nc.alloc_psum_tensor("ps%d" % i, [128, 512], F32) for i in range(8)]
    ps_rot = list(range(8))

    def next_ps():
        i = ps_rot.pop(0)
        ps_rot.append(i)
        return psb[i], ("ps%d" % i,)

    def hold_ps():
        i = ps_rot.pop(0)
        return psb[i], ("ps%d" % i,)

    def release_ps(key):
        ps_rot.append(int(key[0][2:]))

    ident_f = A.alloc("ident_f", [128], F32)
    ident_b = A.alloc("ident_b", [128], BF16)
    ones_b = A.alloc("ones_b", [128], BF16)
    ones_f = A.alloc("ones_f", [128], F32)
    mask_ut = A.alloc("mask_ut", [128], BF16)
    tri_f = A.alloc("tri_f", [128], F32)
    K_ID = ("ident_f", 0)
    P.add("pool", lambda e: e.memset(ident_f, 0.0), writes=[("ident_f", 0)])
    P.add("pool", lambda e: e.affine_select(out=ident_f, in_=ident_f, pattern=[[-1, 128]],
                                             compare_op=ALU.not_equal, fill=1.0, base=0, channel_multiplier=1),
          reads=[("ident_f", 0)], writes=[("ident_f", 0)])
    P.add("pool", lambda e: e.tensor_copy(out=ident_b, in_=ident_f), reads=[("ident_f", 0)], writes=[("ident_b", 0)])
    P.add("pool", lambda e: e.memset(ones_b, 1.0), writes=[("ones_b", 0)])
    P.add("pool", lambda e: e.memset(ones_f, 1.0), writes=[("ones_f", 0)])
    P.add("pool", lambda e: e.memset(tri_f, 1.0), writes=[("tri_f", 0)])
    P.add("pool", lambda e: e.affine_select(out=tri_f, in_=tri_f, pattern=[[1, 128]],
                                             compare_op=ALU.is_ge, fill=0.0, base=0, channel_multiplier=-1),
          reads=[("tri_f", 0)], writes=[("tri_f", 0)])
    P.add("pool", lambda e: e.tensor_copy(out=mask_ut, in_=tri_f), reads=[("tri_f", 0)], writes=[("mask_ut", 0)])

    def load_const(name, dram, shape, dt=F32):
        t = A.alloc(name, shape, dt)
        P.add("sp", lambda e: e.dma_start(out=t, in_=dram), writes=[(name, 0)], dma=True, grp="c_" + name)
        return t

    ccol = load_const("ccol", ccol_d, [8])
    bada = load_const("bada", bada_d, [48])
    g1c = load_const("g1c", g1_d, [8])
    g2c = load_const("g2c", g2_d, [8])

    silc = A.alloc("silc", [8], F32)
    silb = A.alloc("silb", [8], BF16)
    P.add("act", lambda e: e.activation(out=silc, in_=ccol, func=AF.Silu), reads=[("ccol", 0)], writes=[("silc", 0)])
    P.add("dve", lambda e: e.tensor_copy(out=silb, in_=silc), reads=[("silc", 0)], writes=[("silb", 0)])
    modT = A.alloc("modT", [48], F32)
    wada_v = wada_d.rearrange("(c p) n -> p c n", p=128)
    NWA = 2
    wab = [A.alloc("wada%d" % i, [8, 512], BF16) for i in range(NWA)]
    a1 = A.alloc("a1", [8], F32)
    a2 = A.alloc("a2", [8], F32)

    def adaln_block(blk, ps_mod, k_mod):
        s_ = blk % NWA
        buf = wab[s_]
        nm = "wada%d" % s_
        P.add("pool", (lambda e, buf=buf, blk=blk: e.dma_start(out=buf, in_=wada_v[:, :, blk * 512:(blk + 1) * 512])),
              writes=[(nm, 0)], dma=True, grp=nm)

        def mm(e, buf=buf, blk=blk):
            ins = None
            for jj in range(4):
                j = blk * 4 + jj
                for k in range(8):
                    ins = e.matmul(ps_mod[:, j:j + 1], lhsT=buf[:, k, jj * 128:(jj + 1) * 128], rhs=silb[:, k:k + 1],
                                   start=(k == 0), stop=(k == 7))
            return ins
        P.add("pe", mm, reads=[(nm, 0), ("silb", 0)], writes=[k_mod])

    def adaln_finish(ps_mod, k_mod, c0, c1, part):
        P.add("dve", lambda e: e.tensor_tensor(out=modT[:, c0:c1], in0=ps_mod[:, c0:c1], in1=bada[:, c0:c1], op=ALU.add),
              reads=[k_mod, ("bada", 0)], writes=[("modT", part)])
        release_ps(k_mod)

    pm0, km0 = hold_ps()
    for blk in range(4):
        adaln_block(blk, pm0, km0)
    adaln_finish(pm0, km0, 0, 16, 0)
    P.add("dve", lambda e: e.scalar_tensor_tensor(out=a1, in0=modT[:, 8:16], scalar=1.0, in1=g1c, op0=ALU.add, op1=ALU.mult),
          reads=[("modT", 0), ("g1c", 0)], writes=[("a1", 0)])
    p2state = {}

    def adaln_p2_block(blk):
        if "ps" not in p2state:
            p2state["ps"] = hold_ps()
        adaln_block(blk, *p2state["ps"])

    def adaln_p2_end():
        pm1, km1 = p2state["ps"]
        adaln_finish(pm1, km1, 16, 48, 1)
        P.add("dve", lambda e: e.scalar_tensor_tensor(out=a2, in0=modT[:, 32:40], scalar=1.0, in1=g2c, op0=ALU.add, op1=ALU.mult),
              reads=[("modT", 1), ("g2c", 0)], writes=[("a2", 0)])
        dbg_out("modT", modT, [("modT", 0), ("modT", 1)])
        for i in range(NWA):
            A.free("wada%d" % i)

    hT = A.alloc("hT", [8, T], BF16)
    merged = A.alloc("merged", [8, T], BF16)
    NWB = 3
    wbufs = [A.alloc("wblk%d" % i, [8, 512], BF16) for i in range(NWB)]
    NXB = 8
    xin = [A.alloc("xin%d" % i, [D], F32) for i in range(NXB)]
    xnb = [A.alloc("xnb%d" % i, [D], BF16) for i in range(NXB)]
    junk = A.alloc("junk", [D], F32)
    ss1 = A.alloc("ss1", [NT], F32)
    rs1 = A.alloc("rs1", [NT], F32)

    def p2_stats(nb):
        tis = [nb * 4 + tt for tt in range(4)]
        for ti in tis:
            s = ti % NXB
            P.add("sp", (lambda e, s=s, ti=ti: e.dma_start(out=xin[s], in_=x_d[ti * 128:(ti + 1) * 128, :])),
                  writes=[("xin%d" % s, 0)], dma=True, grp="xin%d" % s)
        for ti in tis:
            s = ti % NXB
            P.add("act", (lambda e, s=s, ti=ti: e.activation(out=junk, in_=xin[s], func=AF.Square,
                                                             accum_out=ss1[:, ti:ti + 1])),
                  reads=[("xin%d" % s, 0)], writes=[("junk", 0), ("ss1", ti)])
        t0_, t1_ = tis[0], tis[-1] + 1
        P.add("act", (lambda e, t0_=t0_, t1_=t1_: e.activation(out=rs1[:, t0_:t1_], in_=ss1[:, t0_:t1_], func=AF.Sqrt,
                                                               scale=1.0 / D, bias=1e-6)),
              reads=[("ss1", ti) for ti in tis], writes=[("rs1", ti) for ti in tis])
        P.add("dve", (lambda e, t0_=t0_, t1_=t1_: e.reciprocal(out=rs1[:, t0_:t1_], in_=rs1[:, t0_:t1_])),
              reads=[("rs1", ti) for ti in tis], writes=[("rs1", ti) for ti in tis])
        for ti in tis:
            s = ti % NXB
            P.add("dve", (lambda e, s=s, ti=ti: e.tensor_scalar(out=xnb[s], in0=xin[s], scalar1=rs1[:, ti:ti + 1],
                                                                scalar2=None, op0=ALU.mult)),
                  reads=[("xin%d" % s, 0), ("rs1", ti)], writes=[("xnb%d" % s, 0)])

    def p2_tr(nb):
        pst = [next_ps() for _ in range(4)]
        for tt in range(4):
            ti = nb * 4 + tt
            s = ti % NXB

            def tr(e, s=s, tt=tt, pst=pst):
                ins = None
                for c in range(8):
                    pb = pst[c // 2][0].bitcast(BF16)
                    ins = e.transpose(out=pb[:, (c % 2) * 512 + tt * 128:(c % 2) * 512 + (tt + 1) * 128],
                                      in_=xnb[s][:, c * 128:(c + 1) * 128], identity=ident_b)
                return ins
            P.add("pe", tr, reads=[("xnb%d" % s, 0), ("ident_b", 0)], writes=[pst[i][1] for i in range(4)])
        for c in range(8):
            pb = pst[c // 2][0].bitcast(BF16)
            P.add("act", (lambda e, c=c, pb=pb, nb=nb: e.activation(
                out=hT[:, c, nb * 512:(nb + 1) * 512], in_=pb[:, (c % 2) * 512:(c % 2 + 1) * 512],
                func=AF.Identity, scale=a1[:, c:c + 1], bias=modT[:, c:c + 1])),
                reads=[pst[c // 2][1], ("a1", 0), ("modT", 0)],
                writes=[("hT", c, nb)])

    p2_stats(0)
    for nb in range(NB):
        if nb + 1 < NB:
            p2_stats(nb + 1)
        p2_tr(nb)
    dbg_out("hT", hT, [("hT", c, nb) for c in range(8) for nb in range(NB)])
    for i in range(NXB):
        A.free("xin%d" % i); A.free("xnb%d" % i)

    def finish():
        P.emit(final_wait_groups=["dbgout"] if "dbgout" in P.dma_groups else [])
        return nc, dbg_outs

    GROWS = 256
    NG = -(-(2 * T + 32 * (GROWS - 1)) // GROWS)
    XS = nc.dram_tensor("xs_scratch", [NG * GROWS, D], BF16).ap()
    YS = nc.dram_tensor("ys_scratch", [NG * GROWS, D], BF16).ap()
    if stage <= 1:
        return finish()

    win_v = win_d.rearrange("(c p) n -> p c n", p=128)
    NWB = 3
    wb_ctr = [0]

    def load_wblock(col0, ncols=512):
        i = wb_ctr[0] % NWB
        wb_ctr[0] += 1
        buf = wbufs[i]
        nm = "wblk%d" % i
        P.add("pool", lambda e: e.dma_start(out=buf[:, :, 0:ncols], in_=win_v[:, :, col0:col0 + ncols]),
              writes=[(nm, 0)], dma=True, grp=nm)
        return buf, (nm, 0)

    def load_w4(dram_v):
        i = wb_ctr[0] % NWB
        wb_ctr[0] += 1
        nm = "wblk%d" % i
        v = wbufs[i].rearrange("p a b -> p (a b)").rearrange("p (a b) -> p a b", b=D)
        P.add("pool", lambda e: e.dma_start(out=v, in_=dram_v), writes=[(nm, 0)], dma=True, grp=nm)
        return v, (nm, 0)

    def load_cast(name, dram_ap, shape):
        t = A.alloc(name, shape, BF16)
        P.add("pool", lambda e: e.dma_start(out=t, in_=dram_ap), writes=[(name, 0)], dma=True, grp="c_" + name)
        return t

    hT_keys = lambda nb: [("hT", c, nb) for c in range(8)]

    def proj_fm(wb, wkey, mcol, nb):
        ps, pk = next_ps()

        def mm(e):
            ins = None
            for k in range(8):
                ins = e.matmul(ps[:, :], lhsT=wb[:, k, mcol * 128:(mcol + 1) * 128], rhs=hT[:, k, nb * 512:(nb + 1) * 512],
                               start=(k == 0), stop=(k == 7))
            return ins
        P.add("pe", mm, reads=[wkey] + hT_keys(nb), writes=[pk])
        return ps, pk

    cw = load_const("cw", cw_d, [4, 31])
    cb = load_const("cb", cb_d, [4])
    clg = load_const("clg", clg_d, [4])
    clb = load_const("clb", clb_d, [4])
    u = A.alloc("u", [4, 32 + T], BF16)
    PADU = 32
    for m in range(4):
        P.add("pool", (lambda e, m=m: e.memset(u[:, m, 0:PADU], 0.0)), writes=[("u", m, -1)])
    dg31 = A.alloc("dg31", [4, 31, 128], BF16)
    for m in range(4):
        P.add("pool", (lambda e, m=m: e.tensor_tensor(
            out=dg31[:, m], in0=ident_b.unsqueeze(1).to_broadcast([128, 31, 128]),
            in1=cw[:, m, :].unsqueeze(2).to_broadcast([128, 31, 128]), op=ALU.mult)),
            reads=[("ident_b", 0), ("cw", 0)], writes=[("dg31", m)])
    sgt = [A.alloc("sgt%d" % i, [512], BF16) for i in range(2)]
    sg_ctr = [0]

    def next_sgt():
        i = sg_ctr[0] % 2
        sg_ctr[0] += 1
        return sgt[i], ("sgt%d" % i, 0)

    wa, wak = load_wblock(0)
    wbk, wbkk = load_wblock(512)
    for m in range(4):
        for nb in range(NB):
            psa, pka = proj_fm(wa, wak, m, nb)
            psb_, pkb = proj_fm(wbk, wbkk, m, nb)
            sg, sgk = next_sgt()
            P.add("act", (lambda e, sg=sg, p=psb_: e.activation(out=sg, in_=p[:, :], func=AF.Sigmoid)),
                  reads=[pkb], writes=[sgk])
            P.add("dve", (lambda e, sg=sg, p=psa, m=m, nb=nb: e.tensor_tensor(
                out=u[:, m, PADU + nb * 512:PADU + (nb + 1) * 512], in0=p[:, :], in1=sg, op=ALU.mult)),
                reads=[pka, sgk], writes=[("u", m, nb)])
    dbg_out("u", u, [("u", m, nb) for m in range(4) for nb in range(-1, NB)])

    wco, wcok = load_w4(wco_d.rearrange("(c p) n -> p c n", p=128))
    gA_blocks = {0: load_wblock(3080)}
    cT = A.alloc("cT", [4, T], BF16)
    sqT = A.alloc("sqT", [4, T], BF16)
    for m in range(4):
        for nb in range(NB):
            ps, pk = next_ps()

            def cmm(e, ps=ps, m=m, nb=nb):
                ins = None
                for k in range(31):
                    o = PADU - 30 + nb * 512 + k
                    ins = e.matmul(ps[:, :], lhsT=dg31[:, m, k, :], rhs=u[:, m, o:o + 512], start=(k == 0), stop=(k == 30))
                return ins
            P.add("pe", cmm, reads=[("dg31", m), ("u", m, nb), ("u", m, nb - 1)], writes=[pk])
            P.add("act", (lambda e, ps=ps, m=m, nb=nb: e.activation(
                out=cT[:, m, nb * 512:(nb + 1) * 512], in_=ps[:, :], func=AF.Identity, bias=cb[:, m:m + 1])),
                reads=[pk, ("cb", 0)], writes=[("cT", m, nb)])
            P.add("act", (lambda e, ps=ps, m=m, nb=nb: e.activation(
                out=sqT[:, m, nb * 512:(nb + 1) * 512], in_=ps[:, :], func=AF.Square, bias=cb[:, m:m + 1])),
                reads=[pk, ("cb", 0)], writes=[("sqT", m, nb)])
            gi = m * NB + nb
            if gi % 2 == 1:
                adaln_p2_block(4 + gi // 2)
    adaln_p2_end()
    dbg_out("cT", cT, [("cT", m, nb) for m in range(4) for nb in range(NB)])
    A.free("u")
    A.free("dg31")

    actT = A.alloc("actT", [4, T], BF16)
    mean_t = A.alloc("mean_t", [512], F32)
    rstd_t = A.alloc("rstd_t", [512], F32)
    msq_t = A.alloc("msq_t", [512], F32)
    nrm_t = [A.alloc("nrm_t%d" % i, [512], F32) for i in range(2)]
    for nb in range(NB):
        ps1, pk1 = next_ps()
        ps2, pk2 = next_ps()

        def smm(e, ps1=ps1, ps2=ps2, nb=nb):
            ins = None
            for m in range(4):
                ins = e.matmul(ps1[:, :], lhsT=ones_b, rhs=cT[:, m, nb * 512:(nb + 1) * 512], start=(m == 0), stop=(m == 3))
            for m in range(4):
                ins = e.matmul(ps2[:, :], lhsT=ones_b, rhs=sqT[:, m, nb * 512:(nb + 1) * 512], start=(m == 0), stop=(m == 3))
            return ins
        P.add("pe", smm, reads=[("ones_b", 0)] + [("cT", m, nb) for m in range(4)] + [("sqT", m, nb) for m in range(4)],
              writes=[pk1, pk2])
        P.add("dve", (lambda e, ps1=ps1: e.tensor_scalar(out=mean_t, in0=ps1[:, :], scalar1=1.0 / 512, scalar2=None, op0=ALU.mult)),
              reads=[pk1], writes=[("mean_t", 0)])
        P.add("dve", lambda e: e.tensor_tensor(out=msq_t, in0=mean_t, in1=mean_t, op=ALU.mult),
              reads=[("mean_t", 0)], writes=[("msq_t", 0)])
        P.add("dve", (lambda e, ps2=ps2: e.scalar_tensor_tensor(out=rstd_t, in0=ps2[:, :], scalar=1.0 / 512, in1=msq_t,
                                                                op0=ALU.mult, op1=ALU.subtract)),
              reads=[pk2, ("msq_t", 0)], writes=[("rstd_t", 0)])
        P.add("act", lambda e: e.activation(out=rstd_t, in_=rstd_t, func=AF.Sqrt, bias=1e-5),
              reads=[("rstd_t", 0)], writes=[("rstd_t", 0)])
        P.add("dve", lambda e: e.reciprocal(out=rstd_t, in_=rstd_t), reads=[("rstd_t", 0)], writes=[("rstd_t", 0)])
        for m in range(4):
            nt = nrm_t[m % 2]
            ntk = ("nrm_t%d" % (m % 2), 0)
            P.add("dve", (lambda e, nt=nt, m=m, nb=nb: e.tensor_tensor(out=nt, in0=cT[:, m, nb * 512:(nb + 1) * 512], in1=mean_t,
                                                                      op=ALU.subtract)),
                  reads=[("cT", m, nb), ("mean_t", 0)], writes=[ntk])
            P.add("dve", (lambda e, nt=nt: e.tensor_tensor(out=nt, in0=nt, in1=rstd_t, op=ALU.mult)),
                  reads=[ntk, ("rstd_t", 0)], writes=[ntk])
            P.add("act", (lambda e, nt=nt, m=m, nb=nb: e.activation(
                out=actT[:, m, nb * 512:(nb + 1) * 512], in_=nt, func=AF.Silu, scale=clg[:, m:m + 1], bias=clb[:, m:m + 1])),
                reads=[ntk, ("clg", 0), ("clb", 0)], writes=[("actT", m, nb)])
    dbg_out("actT", actT, [("actT", m, nb) for m in range(4) for nb in range(NB)])
    A.free("cT"); A.free("sqT"); A.free("mean_t"); A.free("rstd_t"); A.free("msq_t"); A.free("nrm_t0"); A.free("nrm_t1")

    for jb in range(2):
        wg_, wgk = gA_blocks[jb] if jb in gA_blocks else load_wblock(3080 + jb * 512)
        for jj in range(4):
            j = jb * 4 + jj
            for nb in range(NB):
                psy, pky = next_ps()

                def ymm(e, psy=psy, j=j, nb=nb):
                    ins = None
                    for m in range(4):
                        ins = e.matmul(psy[:, :], lhsT=wco[:, m, j * 128:(j + 1) * 128], rhs=actT[:, m, nb * 512:(nb + 1) * 512],
                                       start=(m == 0), stop=(m == 3))
                    return ins
                P.add("pe", ymm, reads=[wcok] + [("actT", m, nb) for m in range(4)], writes=[pky])
                psg, pkg = proj_fm(wg_, wgk, jj, nb)
                sg, sgk = next_sgt()
                P.add("act", (lambda e, sg=sg, p=psg: e.activation(out=sg, in_=p[:, :], func=AF.Sigmoid)),
                      reads=[pkg], writes=[sgk])
                P.add("dve", (lambda e, sg=sg, p=psy, j=j, nb=nb: e.tensor_tensor(
                    out=merged[:, j, nb * 512:(nb + 1) * 512], in0=p[:, :], in1=sg, op=ALU.mult)),
                    reads=[pky, sgk], writes=[("merged", j, nb)])
    dbg_out("mergedA", merged, [("merged", j, nb) for j in range(8) for nb in range(NB)])
    A.free("actT")
    if stage <= 2:
        return finish()

    zt = A.alloc("zt", [D], BF16)
    P.add("pool", lambda e: e.memset(zt, 0.0), writes=[("zt", 0)])
    XSZ_KEYS = []
    for zi in range(NG * GROWS // 1024):
        P.add("sp", (lambda e, zi=zi: e.dma_start(out=XS[zi * 1024:(zi + 1) * 1024, :].rearrange("(n p) d -> p n d", p=128),
                                                  in_=zt.unsqueeze(1).to_broadcast([128, 8, D]))),
              reads=[("zt", 0)], writes=[("XSZ", zi)], dma=True, grp="xs_zero")
        XSZ_KEYS.append(("XSZ", zi))
    A.free("zt")
    PADQ = 4
    qkw = load_const("qkw", qkw_d, [8, 4])
    qkb = load_const("qkb", qkb_d, [8])
    bif = load_const("bif", bif_d, [8])
    mng = load_const("mng", mng_d, [4])
    qk_raw = A.alloc("qk_raw", [8, PADQ + T], BF16)
    for cc in range(8):
        P.add("pool", (lambda e, cc=cc: e.memset(qk_raw[:, cc, 0:PADQ], 0.0)), writes=[("qk_raw", cc, -1)])
    dg4 = A.alloc("dg4", [8, 4, 128], BF16)
    P.add("pool", lambda e: e.tensor_tensor(
        out=dg4.rearrange("p a b c -> p (a b) c"), in0=ident_b.unsqueeze(1).to_broadcast([128, 32, 128]),
        in1=qkw.rearrange("p a b -> p (a b)").unsqueeze(2).to_broadcast([128, 32, 128]), op=ALU.mult),
        reads=[("ident_b", 0), ("qkw", 0)], writes=[("dg4", 0)])
    for half in range(2):
        wq_, wqk = load_wblock(1024 + half * 512)
        for m in range(4):
            cc = half * 4 + m
            for nb in range(NB):
                ps, pk = proj_fm(wq_, wqk, m, nb)
                P.add("act", (lambda e, ps=ps, cc=cc, nb=nb: e.activation(
                    out=qk_raw[:, cc, PADQ + nb * 512:PADQ + (nb + 1) * 512], in_=ps[:, :], func=AF.Identity)),
                    reads=[pk], writes=[("qk_raw", cc, nb)])
    qkc = A.alloc("qkc", [8, T], BF16)
    for cc in range(8):
        for nb in range(NB):
            ps, pk = next_ps()

            def qmm(e, ps=ps, cc=cc, nb=nb):
                ins = None
                for k in range(4):
                    o = PADQ - 3 + nb * 512 + k
                    ins = e.matmul(ps[:, :], lhsT=dg4[:, cc, k, :], rhs=qk_raw[:, cc, o:o + 512], start=(k == 0), stop=(k == 3))
                return ins
            P.add("pe", qmm, reads=[("dg4", 0), ("qk_raw", cc, nb), ("qk_raw", cc, nb - 1)], writes=[pk])
            P.add("act", (lambda e, ps=ps, cc=cc, nb=nb: e.activation(
                out=qkc[:, cc, nb * 512:(nb + 1) * 512], in_=ps[:, :], func=AF.Silu, bias=qkb[:, cc:cc + 1])),
                reads=[pk, ("qkb", 0)], writes=[("qkc", cc, nb)])
    dbg_out("qkc", qkc, [("qkc", cc, nb) for cc in range(8) for nb in range(NB)])
    A.free("qk_raw"); A.free("dg4")
    if stage <= 2.2:
        return finish()

    wif = A.alloc("wif", [8, 8], BF16)
    wif_f = A.alloc("wif_f", [8, 8], F32)
    with nc.allow_non_contiguous_dma(reason="tiny gate-weight columns"):
        P.add("sp", lambda e: e.dma_start(out=wif_f, in_=win_v[:, :, 3072:3080]), writes=[("wif_f", 0)], dma=True, grp="c_wif")
    P.add("dve", lambda e: e.tensor_copy(out=wif, in_=wif_f), reads=[("wif_f", 0)], writes=[("wif", 0)])
    G = A.alloc("G", [NT, 8], F32)
    nlf = A.alloc("nlf", [NT, 4], F32)
    gtmp = A.alloc("gtmp", [NT, 4], F32)
    A_inv = A.alloc("A_inv", [NT, 4], F32)
    Bv = A.alloc("Bv", [NT, 4], F32)
    dec = A.alloc("dec", [NT, 4], F32)
    psg, pkg = hold_ps()

    def gmm(e):
        ins = None
        for ti in range(NT):
            for k in range(8):
                ins = e.matmul(psg[:, ti * 8:(ti + 1) * 8], lhsT=hT[:, k, ti * 128:(ti + 1) * 128], rhs=wif[:, k, :],
                               start=(k == 0), stop=(k == 7))
        return ins
    P.add("pe", gmm, reads=[("wif", 0)] + [("hT", c, nb) for c in range(8) for nb in range(NB)], writes=[pkg])
    P.add("dve", lambda e: e.tensor_tensor(out=G, in0=psg[:, 0:128].rearrange("p (a b) -> p a b", b=8),
                                           in1=bif.unsqueeze(1).to_broadcast([128, NT, 8]), op=ALU.add),
          reads=[pkg, ("bif", 0)], writes=[("G", 0)])
    release_ps(pkg)
    dbg_out("G", G, [("G", 0)])
    if stage <= 2.31:
        return finish()
    P.add("act", lambda e: e.activation(out=gtmp, in_=G[:, :, 4:8], func=AF.Exp, scale=-1.0),
          reads=[("G", 0)], writes=[("gtmp", 0)])
    P.add("act", lambda e: e.activation(out=nlf, in_=gtmp, func=AF.Ln, bias=1.0),
          reads=[("gtmp", 0)], writes=[("nlf", 0)])
    dbg_out("nlf", nlf, [("nlf", 0)])
    if stage <= 2.32:
        return finish()
    psc, pkc = next_ps()
    nlf2 = nlf.rearrange("p a b -> p (a b)")
    nl_hi = A.alloc("nl_hi", [64], BF16)
    nl_lo = A.alloc("nl_lo", [64], BF16)
    P.add("dve", lambda e: e.tensor_copy(out=nl_hi, in_=nlf2), reads=[("nlf", 0)], writes=[("nl_hi", 0)])
    P.add("dve", lambda e: e.tensor_tensor(out=nl_lo, in0=nlf2, in1=nl_hi, op=ALU.subtract),
          reads=[("nlf", 0), ("nl_hi", 0)], writes=[("nl_lo", 0)])

    def cmm2(e):
        e.matmul(psc[:, 0:64], lhsT=mask_ut, rhs=nl_hi, start=True, stop=False)
        e.matmul(psc[:, 0:64], lhsT=mask_ut, rhs=nl_lo, start=False, stop=True)
        e.matmul(psc[:, 64:128], lhsT=ones_b, rhs=nl_hi, start=True, stop=False)
        return e.matmul(psc[:, 64:128], lhsT=ones_b, rhs=nl_lo, start=False, stop=True)
    P.add("pe", cmm2, reads=[("mask_ut", 0), ("ones_b", 0), ("nl_hi", 0), ("nl_lo", 0)], writes=[pkc])
    if stage <= 2.33:
        P.add("dve", lambda e: e.tensor_copy(out=gtmp.rearrange("p a b -> p (a b)"), in_=psc[:, 0:64]), reads=[pkc], writes=[("gtmp", 0)])
        dbg_out("ncum", gtmp, [("gtmp", 0)])
        return finish()
    LNS = float(np.log(128.0 ** 0.5))
    cval = A.alloc("cval", [2], F32)
    P.add("pool", lambda e: e.memset(cval[:, 0:1], LNS), writes=[("cval", 0)])
    P.add("pool", lambda e: e.memset(cval[:, 1:2], -LNS), writes=[("cval", 1)])
    P.add("act", lambda e: e.activation(out=A_inv.rearrange("p a b -> p (a b)"), in_=psc[:, 0:64], func=AF.Exp, bias=cval[:, 0:1]),
          reads=[pkc, ("cval", 0)], writes=[("A_inv", 0)])
    A_ = A.alloc("A_", [NT, 4], F32)
    P.add("act", lambda e: e.activation(out=A_.rearrange("p a b -> p (a b)"), in_=psc[:, 0:64], func=AF.Exp, scale=-1.0, bias=cval[:, 1:2]),
          reads=[pkc, ("cval", 1)], writes=[("A_", 0)])
    if stage <= 2.34:
        dbg_out("A_", A_, [("A_", 0)])
        dbg_out("A_inv", A_inv, [("A_inv", 0)])
        return finish()
    P.add("dve", lambda e: e.tensor_tensor(out=gtmp, in0=psc[:, 0:64].rearrange("p (a b) -> p a b", b=4), in1=G[:, :, 0:4], op=ALU.add),
          reads=[pkc, ("G", 0), ("gtmp", 0)], writes=[("gtmp", 0)])
    P.add("act", lambda e: e.activation(out=Bv, in_=gtmp, func=AF.Exp), reads=[("gtmp", 0)], writes=[("Bv", 0)])
    if stage <= 2.36:
        dbg_out("Bv", Bv, [("Bv", 0)])
        return finish()
    P.add("act", lambda e: e.activation(out=dec.rearrange("p a b -> p (a b)"), in_=psc[:, 64:128], func=AF.Exp, scale=-1.0),
          reads=[pkc], writes=[("dec", 0)])
    dbg_out("Bv", Bv, [("Bv", 0)])
    dbg_out("A_", A_, [("A_", 0)])
    dbg_out("decay", dec, [("dec", 0)])

    if stage <= 2.4:
        return finish()
    ktok = A.alloc("ktok", [NT, 512], BF16)
    for c in range(NT):
        ps, pk = next_ps()
        pb = ps.bitcast(BF16)

        def ktr(e, pb=pb, c=c):
            ins = None
            for h in range(4):
                ins = e.transpose(out=pb[:, h * 128:(h + 1) * 128], in_=qkc[:, 4 + h, c * 128:(c + 1) * 128], identity=ident_b)
            return ins
        P.add("pe", ktr, reads=[("ident_b", 0)] + [("qkc", 4 + h, c // 4) for h in range(4)], writes=[pk])
        P.add("act", (lambda e, pb=pb, c=c: e.activation(out=ktok[:, c, :], in_=pb[:, 0:512], func=AF.Identity)),
              reads=[pk], writes=[("ktok", c)])

    vB = A.alloc("vB", [NT, 4, 129], BF16)
    wv_, wvk = load_wblock(2048)
    for c in range(NT):
        ps, pk = next_ps()

        def vmm(e, ps=ps, c=c):
            ins = None
            for k in range(8):
                ins = e.matmul(ps[:, :], lhsT=hT[:, k, c * 128:(c + 1) * 128], rhs=wv_[:, k, 0:512], start=(k == 0), stop=(k == 7))
            return ins
        P.add("pe", vmm, reads=[wvk] + hT_keys(c // 4), writes=[pk])
        P.add("dve", (lambda e, ps=ps, c=c: e.tensor_tensor(
            out=vB[:, c, :, 0:128], in0=ps[:, :].rearrange("p (a b) -> p a b", b=128),
            in1=Bv[:, c, :].unsqueeze(2).to_broadcast([128, 4, 128]), op=ALU.mult)),
            reads=[pk, ("Bv", 0)], writes=[("vB", c, 0)])
        P.add("dve", (lambda e, c=c: e.tensor_copy(out=vB[:, c, :, 128], in_=Bv[:, c, :])),
              reads=[("Bv", 0)], writes=[("vB", c, 1)])

    if stage <= 2.6:
        return finish()
    E = A.alloc("E", [4, 129], F32)
    Cb = [A.alloc("Cb%d" % i, [4, 129], BF16) for i in range(2)]
    sm = [A.alloc("sm%d" % i, [4, 128], BF16) for i in range(2)]
    hn = [A.alloc("hn%d" % i, [4, 128], BF16) for i in range(2)]
    st6 = A.alloc("st6", [4, 6], F32)
    mv = A.alloc("mv", [4, 2], F32)
    den = A.alloc("den", [4], F32)
    qq = A.alloc("qq", [4], F32)
    rstd = A.alloc("rstd", [4], F32)
    sgo = [A.alloc("sgo%d" % i, [4, 512], BF16) for i in range(2)]
    hmT = A.alloc("hmT", [4, T], BF16)
    wo_, wok = load_wblock(2560)
    CW = 256

    chs = {}

    def chunk_A(c):
        nb = c // 4
        cs = slice(c * 128, (c + 1) * 128)
        if c % 4 == 0:
            for h in range(4):
                ps, pk = proj_fm(wo_, wok, h, nb)
                P.add("act", (lambda e, ps=ps, h=h, nb=nb: e.activation(out=sgo[nb % 2][:, h, :], in_=ps[:, :], func=AF.Sigmoid)),
                      reads=[pk], writes=[("sgo%d" % (nb % 2), h)])
        pss, pks = next_ps()

        def smm2(e, pss=pss, cs=cs):
            ins = None
            for h in range(4):
                ins = e.matmul(pss[:, h * 128:(h + 1) * 128], lhsT=qkc[:, 4 + h, cs], rhs=qkc[:, h, cs], start=True, stop=True)
            return ins
        P.add("pe", smm2, reads=[("qkc", cc, nb) for cc in range(8)], writes=[pks])
        smc = sm[c % 2]
        smk = ("sm%d" % (c % 2), 0)
        P.add("dve", (lambda e, pss=pss, smc=smc: e.tensor_tensor(
            out=smc, in0=pss[:, :].rearrange("p (a b) -> p a b", b=128),
            in1=mask_ut.unsqueeze(1).to_broadcast([128, 4, 128]), op=ALU.mult)),
            reads=[pks, ("mask_ut", 0)], writes=[smk])
        pu = [hold_ps(), hold_ps()]

        def umm(e, pu=pu, c=c):
            ins = None
            for h in range(4):
                o = pu[h // 2][0][:, (h % 2) * CW:(h % 2) * CW + 129]
                ins = e.matmul(o, lhsT=ktok[:, c, h * 128:(h + 1) * 128], rhs=vB[:, c, h, :], start=True, stop=True)
            return ins
        P.add("pe", umm, reads=[("ktok", c), ("vB", c, 0), ("vB", c, 1)], writes=[pu[0][1], pu[1][1]])
        chs[c] = (smc, smk, pu)

    def chunk_B(c):
        nb = c // 4
        cs = slice(c * 128, (c + 1) * 128)
        smc, smk, pu = chs[c]
        pn = [next_ps(), next_ps()]

        def nmm(e, pn=pn, smc=smc, c=c, cs=cs):
            ins = None
            for h in range(4):
                o = pn[h // 2][0][:, (h % 2) * CW:(h % 2) * CW + 129]
                ins = e.matmul(o, lhsT=smc[:, h, :], rhs=vB[:, c, h, :], start=True, stop=(c == 0))
                if c > 0:
                    ins = e.matmul(o, lhsT=qkc[:, h, cs], rhs=Cb[(c - 1) % 2][:, h, :], start=False, stop=True)
            return ins
        rd = [smk, ("vB", c, 0), ("vB", c, 1)] + [("qkc", h, nb) for h in range(4)]
        if c > 0:
            rd += [("Cb%d" % ((c - 1) % 2), h) for h in range(4)]
        P.add("pe", nmm, reads=rd, writes=[pn[0][1], pn[1][1]])
        for h in range(4):
            src = pu[h // 2][0][:, (h % 2) * CW:(h % 2) * CW + 129]
            if c == 0:
                P.add("dve", (lambda e, src=src, h=h: e.tensor_copy(out=E[:, h, :], in_=src)),
                      reads=[pu[h // 2][1]], writes=[("E", h)])
            else:
                P.add("dve", (lambda e, src=src, h=h, c=c: e.scalar_tensor_tensor(
                    out=E[:, h, :], in0=E[:, h, :], scalar=dec[:, c - 1, h:h + 1], in1=src, op0=ALU.mult, op1=ALU.add)),
                    reads=[pu[h // 2][1], ("E", h), ("dec", 0)], writes=[("E", h)])
            if c < NT - 1:
                P.add("act", (lambda e, h=h, c=c: e.activation(out=Cb[c % 2][:, h, :], in_=E[:, h, :], func=AF.Identity,
                                                               scale=dec[:, c, h:h + 1])),
                      reads=[("E", h), ("dec", 0)], writes=[("Cb%d" % (c % 2), h)])
        release_ps(pu[0][1]); release_ps(pu[1][1])
        chs[c] = pn

    def chunk_C(c):
        nb = c // 4
        cs = slice(c * 128, (c + 1) * 128)
        pn = chs[c]
        for h in range(4):
            src = pn[h // 2][0][:, (h % 2) * CW:(h % 2) * CW + 128]
            P.add("dve", (lambda e, src=src, h=h: e.bn_stats(out=st6[:, h, :], in_=src)),
                  reads=[pn[h // 2][1]], writes=[("st6", h)])
            P.add("dve", (lambda e, h=h: e.bn_aggr(out=mv[:, h, :], in_=st6[:, h, :])),
                  reads=[("st6", h)], writes=[("mv", h)])
        for b2 in range(2):
            dsrc = pn[b2][0][:, 0:512].rearrange("p (a b) -> p a b", b=CW)[:, :, 128]
            P.add("dve", (lambda e, dsrc=dsrc, b2=b2, c=c: e.tensor_tensor(
                out=den[:, 2 * b2:2 * b2 + 2], in0=dsrc, in1=A_[:, c, 2 * b2:2 * b2 + 2], op=ALU.mult)),
                reads=[pn[b2][1], ("A_", 0)], writes=[("den", b2)])
        P.add("dve", lambda e: e.scalar_tensor_tensor(out=den, in0=den, scalar=-1.0, in1=den, op0=ALU.mult, op1=ALU.max),
              reads=[("den", 0), ("den", 1)], writes=[("den", 0), ("den", 1)])
        P.add("dve", lambda e: e.tensor_scalar(out=den, in0=den, scalar1=1.0, scalar2=None, op0=ALU.max),
              reads=[("den", 0), ("den", 1)], writes=[("den", 0), ("den", 1)])
        P.add("dve", (lambda e, c=c: e.tensor_tensor(out=qq, in0=den, in1=A_inv[:, c, :], op=ALU.mult)),
              reads=[("den", 0), ("den", 1), ("A_inv", 0)], writes=[("qq", 0)])
        P.add("dve", lambda e: e.tensor_tensor(out=qq, in0=qq, in1=qq, op=ALU.mult), reads=[("qq", 0)], writes=[("qq", 0)])
        P.add("dve", lambda e: e.scalar_tensor_tensor(out=rstd, in0=qq, scalar=1e-5, in1=mv[:, :, 1], op0=ALU.mult, op1=ALU.add),
              reads=[("qq", 0)] + [("mv", h) for h in range(4)], writes=[("rstd", 0)])
        P.add("act", lambda e: e.activation(out=rstd, in_=rstd, func=AF.Sqrt), reads=[("rstd", 0)], writes=[("rstd", 0)])
        P.add("dve", lambda e: e.reciprocal(out=rstd, in_=rstd), reads=[("rstd", 0)], writes=[("rstd", 0)])
        hnc = hn[c % 2]
        hnk = "hn%d" % (c % 2)
        for h in range(4):
            src = pn[h // 2][0][:, (h % 2) * CW:(h % 2) * CW + 128]
            P.add("dve", (lambda e, src=src, h=h, hnc=hnc: e.tensor_scalar(
                out=hnc[:, h, :], in0=src, scalar1=mv[:, h, 0:1], scalar2=rstd[:, h:h + 1], op0=ALU.subtract, op1=ALU.mult)),
                reads=[pn[h // 2][1], ("mv", h), ("rstd", 0)], writes=[(hnk, h)])
        pt, pkt = next_ps()
        ptb = pt.bitcast(BF16)

        def htr(e, ptb=ptb, hnc=hnc):
            ins = None
            for h in range(4):
                ins = e.transpose(out=ptb[:, h * 128:(h + 1) * 128], in_=hnc[:, h, :], identity=ident_b)
            return ins
        P.add("pe", htr, reads=[("ident_b", 0)] + [(hnk, h) for h in range(4)], writes=[pkt])
        P.add("dve", (lambda e, ptb=ptb, c=c, nb=nb, cs=cs: e.tensor_tensor(
            out=hmT[:, :, cs], in0=ptb[:, 0:512].rearrange("p (a b) -> p a b", b=128),
            in1=sgo[nb % 2][:, :, (c % 4) * 128:(c % 4 + 1) * 128], op=ALU.mult)),
            reads=[pkt] + [("sgo%d" % (nb % 2), h) for h in range(4)], writes=[("hmT", c)])

    chunk_A(0)
    for c in range(NT):
        if c + 1 < NT:
            chunk_A(c + 1)
        chunk_B(c)
        chunk_C(c)
    dbg_out("hmT", hmT, [("hmT", c) for c in range(NT)])
    for nm in ("qkc", "wif", "wif_f", "nl_hi", "nl_lo", "G", "nlf", "gtmp", "A_inv", "Bv", "dec", "A_", "ktok", "vB", "E", "Cb0", "Cb1", "sm0", "sm1",
               "hn0", "hn1", "st6", "mv", "den", "qq", "rstd", "sgo0", "sgo1"):
        A.free(nm)

    wmo = load_cast("wmo", wmo_d.rearrange("(c p) n -> p c n", p=128), [4, D])
    for h in range(4):
        P.add("dve", (lambda e, h=h: e.tensor_scalar(out=wmo[:, h, :], in0=wmo[:, h, :], scalar1=mng[:, h:h + 1], scalar2=None,
                                                     op0=ALU.mult)),
              reads=[("wmo", 0), ("wmo", 1 + h), ("mng", 0)], writes=[("wmo", 1 + h)])
    mtmp = [A.alloc("mtmp%d" % i, [512], BF16) for i in range(2)]
    for jb in range(2):
        wg_, wgk = load_wblock(4104 + jb * 512)
        for jj in range(4):
            j = jb * 4 + jj
            for nb in range(NB):
                psy, pky = next_ps()

                def ymm2(e, psy=psy, j=j, nb=nb):
                    ins = None
                    for h in range(4):
                        ins = e.matmul(psy[:, :], lhsT=wmo[:, h, j * 128:(j + 1) * 128], rhs=hmT[:, h, nb * 512:(nb + 1) * 512],
                                       start=(h == 0), stop=(h == 3))
                    return ins
                P.add("pe", ymm2, reads=[("wmo", 1 + h) for h in range(4)] + [("hmT", c) for c in range(nb * 4, nb * 4 + 4)],
                      writes=[pky])
                psg2, pkg2 = proj_fm(wg_, wgk, jj, nb)
                sg, sgk = next_sgt()
                P.add("act", (lambda e, sg=sg, p=psg2: e.activation(out=sg, in_=p[:, :], func=AF.Sigmoid)),
                      reads=[pkg2], writes=[sgk])
                mt = mtmp[(j * NB + nb) % 2]
                mtk = ("mtmp%d" % ((j * NB + nb) % 2), 0)
                P.add("dve", (lambda e, sg=sg, p=psy, mt=mt: e.tensor_tensor(out=mt, in0=p[:, :], in1=sg, op=ALU.mult)),
                      reads=[pky, sgk], writes=[mtk])
                P.add("dve", (lambda e, mt=mt, j=j, nb=nb: e.tensor_tensor(
                    out=merged[:, j, nb * 512:(nb + 1) * 512], in0=merged[:, j, nb * 512:(nb + 1) * 512], in1=mt, op=ALU.add)),
                    reads=[mtk, ("merged", j, nb)], writes=[("merged", j, nb)])
    dbg_out("merged", merged, [("merged", j, nb) for j in range(8) for nb in range(NB)])
    for nm in ("hmT", "wmo", "mtmp0", "mtmp1", "sgt0", "sgt1", "hT", "wblk0", "wblk1", "wblk2"):
        A.free(nm)
    if stage <= 3:
        return finish()

    dgf = A.alloc("dgf", [128], F32)
    dgh = A.alloc("dgh", [128], BF16)
    dgl = A.alloc("dgl", [128], BF16)

    def row_bcast(name, col0, src=None, srckey=("modT", 1)):
        src = modT if src is None else src
        row = A.alloc(name, [D], F32)
        banks = [next_ps(), next_ps()]
        for j in range(8):
            P.add("dve", (lambda e, j=j: e.tensor_scalar(out=dgf, in0=ident_f, scalar1=src[:, col0 + j:col0 + j + 1], scalar2=None,
                                                         op0=ALU.mult)),
                  reads=[("ident_f", 0), srckey], writes=[("dgf", 0)])
            P.add("dve", lambda e: e.tensor_copy(out=dgh, in_=dgf), reads=[("dgf", 0)], writes=[("dgh", 0)])
            P.add("dve", lambda e: e.tensor_tensor(out=dgl, in0=dgf, in1=dgh, op=ALU.subtract),
                  reads=[("dgf", 0), ("dgh", 0)], writes=[("dgl", 0)])
            bk, bkk = banks[j // 4]

            def bmm(e, bk=bk, j=j):
                o = bk[:, (j % 4) * 128:(j % 4 + 1) * 128]
                e.matmul(o, lhsT=ones_b, rhs=dgh, start=True, stop=False)
                return e.matmul(o, lhsT=ones_b, rhs=dgl, start=False, stop=True)
            P.add("pe", bmm, reads=[("ones_b", 0), ("dgh", 0), ("dgl", 0)], writes=[bkk])
        for b2 in range(2):
            bk, bkk = banks[b2]
            P.add("act", (lambda e, bk=bk, b2=b2: e.activation(out=row[:, b2 * 512:(b2 + 1) * 512], in_=bk[:, :], func=AF.Identity)),
                  reads=[bkk], writes=[(name, b2)])
        return row

    gt1row = row_bcast("gt1row", 16)
    a2row = row_bcast("a2row", 0, src=a2, srckey=("a2", 0))
    sh2row = row_bcast("sh2row", 24)
    gt2row = row_bcast("gt2row", 40)
    wout = load_cast("wout", wout_d.rearrange("(c p) n -> p c n", p=128), [8, D])
    for k in range(8):
        P.add("dve", (lambda e, k=k: e.tensor_tensor(out=wout[:, k, :], in0=wout[:, k, :], in1=gt1row, op=ALU.mult)),
              reads=[("wout", 0), ("wout", 1 + k), ("gt1row", 0), ("gt1row", 1)], writes=[("wout", 1 + k)])
    x1 = A.alloc("x1", [NT, D], F32)
    for ti in range(NT):
        P.add("sp", (lambda e, ti=ti: e.dma_start(out=x1[:, ti, :], in_=x_d[ti * 128:(ti + 1) * 128, :])),
              writes=[("x1", ti)], dma=True, grp="x1ld%d" % ti)
    wr_f = A.alloc("wr_f", [8, 36], F32)
    wr_b = A.alloc("wr_b", [8, 36], BF16)
    with nc.allow_non_contiguous_dma(reason="small router weight rows"):
        P.add("sp", lambda e: e.dma_start(out=wr_f, in_=wr_d.rearrange("(c p) n -> p c n", p=128)), writes=[("wr_f", 0)],
              dma=True, grp="c_wr")
    P.add("dve", lambda e: e.tensor_copy(out=wr_b, in_=wr_f), reads=[("wr_f", 0)], writes=[("wr_b", 0)])
    brt = load_const("brt", br_d, [36])
    h2tok = A.alloc("h2tok", [NT, D], BF16)
    xn2 = [A.alloc("xn2_%d" % i, [D], F32) for i in range(2)]
    h2T = [A.alloc("h2T%d" % i, [8, 128], BF16) for i in range(2)]
    ss2 = A.alloc("ss2", [NT], F32)
    rs2 = A.alloc("rs2", [NT], F32)
    psr = [hold_ps(), hold_ps()]
    def emit_p5(ti):
        s2 = ti % 2
        P.add("act", (lambda e, ti=ti: e.activation(out=junk, in_=x1[:, ti, :], func=AF.Square, accum_out=ss2[:, ti:ti + 1])),
              reads=[("x1", ti)], writes=[("junk", 0), ("ss2", ti)])
        P.add("act", (lambda e, ti=ti: e.activation(out=rs2[:, ti:ti + 1], in_=ss2[:, ti:ti + 1], func=AF.Sqrt, scale=1.0 / D, bias=1e-6)),
              reads=[("ss2", ti)], writes=[("rs2", ti)])
        P.add("dve", (lambda e, ti=ti: e.reciprocal(out=rs2[:, ti:ti + 1], in_=rs2[:, ti:ti + 1])),
              reads=[("rs2", ti)], writes=[("rs2", ti)])
        P.add("act", (lambda e, ti=ti, s2=s2: e.activation(out=xn2[s2], in_=x1[:, ti, :], func=AF.Identity, scale=rs2[:, ti:ti + 1])),
              reads=[("x1", ti), ("rs2", ti)], writes=[("xn2_%d" % s2, 0)])
        P.add("dve", (lambda e, s2=s2: e.tensor_tensor(out=xn2[s2], in0=xn2[s2], in1=a2row, op=ALU.mult)),
              reads=[("xn2_%d" % s2, 0), ("a2row", 0), ("a2row", 1)], writes=[("xn2_%d" % s2, 0)])
        P.add("dve", (lambda e, s2=s2, ti=ti: e.tensor_tensor(out=h2tok[:, ti, :], in0=xn2[s2], in1=sh2row, op=ALU.add)),
              reads=[("xn2_%d" % s2, 0), ("sh2row", 0), ("sh2row", 1)], writes=[("h2tok", ti)])
        pt, pkt = next_ps()
        ptb = pt.bitcast(BF16)

        def h2tr(e, ptb=ptb, ti=ti):
            ins = None
            for c in range(8):
                ins = e.transpose(out=ptb[:, c * 128:(c + 1) * 128], in_=h2tok[:, ti, c * 128:(c + 1) * 128], identity=ident_b)
            return ins
        P.add("pe", h2tr, reads=[("ident_b", 0), ("h2tok", ti)], writes=[pkt])
        P.add("act", (lambda e, ptb=ptb, s2=s2: e.activation(out=h2T[s2].rearrange("p a b -> p (a b)"), in_=ptb[:, 0:1024], func=AF.Identity)),
              reads=[pkt], writes=[("h2T%d" % s2, 0)])
        bk, bkk = psr[ti // 8]

        def rmm(e, bk=bk, ti=ti, s2=s2):
            ins = None
            o = bk[:, (ti % 8) * 36:(ti % 8 + 1) * 36]
            for k in range(8):
                ins = e.matmul(o, lhsT=h2T[s2][:, k, :], rhs=wr_b[:, k, :], start=(k == 0), stop=(k == 7))
            return ins
        P.add("pe", rmm, reads=[("h2T%d" % s2, 0), ("wr_b", 0)], writes=[bkk])

    def emit_p4(ti):
        for half in range(2):
            ps, pk = next_ps()

            def omm(e, ps=ps, ti=ti, half=half):
                ins = None
                for k in range(8):
                    ins = e.matmul(ps[:, :], lhsT=merged[:, k, ti * 128:(ti + 1) * 128], rhs=wout[:, k, half * 512:(half + 1) * 512],
                                   start=(k == 0), stop=(k == 7))
                return ins
            P.add("pe", omm, reads=[("wout", 1 + k) for k in range(8)] + [("merged", k, ti // 4) for k in range(8)], writes=[pk])
            P.add("dve", (lambda e, ps=ps, ti=ti, half=half: e.tensor_tensor(
                out=x1[:, ti, half * 512:(half + 1) * 512], in0=x1[:, ti, half * 512:(half + 1) * 512], in1=ps[:, :], op=ALU.add)),
                reads=[pk, ("x1", ti)], writes=[("x1", ti)])

    emit_p4(0)
    for ti in range(NT):
        if ti + 1 < NT:
            emit_p4(ti + 1)
        emit_p5(ti)
    dbg_out("x1", x1, [("x1", ti) for ti in range(NT)])
    A.free("merged"); A.free("wout"); A.free("gt1row")

    Lg = A.alloc("Lg", [NT, 36], F32)
    for b2 in range(2):
        bk, bkk = psr[b2]
        P.add("dve", (lambda e, bk=bk, b2=b2: e.tensor_tensor(
            out=Lg[:, b2 * 8:(b2 + 1) * 8, :], in0=bk[:, 0:288].rearrange("p (a b) -> p a b", b=36),
            in1=brt.unsqueeze(1).to_broadcast([128, 8, 36]), op=ALU.add)),
            reads=[bkk, ("brt", 0)], writes=[("Lg", b2)])
    release_ps(psr[0][1]); release_ps(psr[1][1])
    dbg_out("Lg", Lg, [("Lg", 0), ("Lg", 1)])
    dbg_out("h2tok", h2tok, [("h2tok", ti) for ti in range(NT)])
    for nm in ("xn2_0", "xn2_1", "h2T0", "h2T1", "a2row", "sh2row", "wr_f", "wr_b"):
        A.free(nm)
    if stage <= 5:
        return finish()

    NCHK0 = NG - 15
    def T_(name, shape, dt=F32):
        return A.alloc(name, shape, dt)
    LK = [("Lg", 0), ("Lg", 1)]
    lg = Lg[:, :, 0:4]
    le = Lg[:, :, 4:36]
    gmax = T_("gmax", [NT])
    G1h = T_("G1h", [NT, 4])
    egs = T_("egs", [NT, 4])
    p_g = T_("p_g", [NT])
    P.add("dve", lambda e: e.tensor_reduce(out=gmax, in_=lg, axis=AX.X, op=ALU.max), reads=LK, writes=[("gmax", 0)])
    gmb = gmax.unsqueeze(2).to_broadcast([128, NT, 4])
    P.add("dve", lambda e: e.tensor_tensor(out=G1h, in0=lg, in1=gmb, op=ALU.is_equal), reads=LK + [("gmax", 0)], writes=[("G1h", 0)])
    P.add("dve", lambda e: e.tensor_tensor(out=egs, in0=lg, in1=gmb, op=ALU.subtract), reads=LK + [("gmax", 0)], writes=[("egs", 0)])
    P.add("act", lambda e: e.activation(out=egs, in_=egs, func=AF.Exp), reads=[("egs", 0)], writes=[("egs", 0)])
    P.add("dve", lambda e: e.tensor_reduce(out=p_g, in_=egs, axis=AX.X, op=ALU.add), reads=[("egs", 0)], writes=[("p_g", 0)])
    P.add("dve", lambda e: e.reciprocal(out=p_g, in_=p_g), reads=[("p_g", 0)], writes=[("p_g", 0)])
    tmp32 = T_("tmp32", [NT, 32])
    lsel = T_("lsel", [NT, 8])
    P.add("dve", lambda e: e.tensor_tensor(
        out=tmp32.rearrange("p t (g j) -> p t g j", j=8), in0=le.rearrange("p t (g j) -> p t g j", j=8),
        in1=G1h.unsqueeze(3).to_broadcast([128, NT, 4, 8]), op=ALU.mult),
        reads=LK + [("G1h", 0)], writes=[("tmp32", 0)])
    P.add("dve", lambda e: e.tensor_reduce(out=lsel, in_=tmp32.rearrange("p t (g j) -> p t j g", j=8), axis=AX.X, op=ALU.add),
          reads=[("tmp32", 0)], writes=[("lsel", 0)])
    m1 = T_("m1", [NT])
    m2 = T_("m2", [NT])
    E1 = T_("E1", [NT, 8])
    E2 = T_("E2", [NT, 8])
    ls2 = T_("ls2", [NT, 8])
    P.add("dve", lambda e: e.tensor_reduce(out=m1, in_=lsel, axis=AX.X, op=ALU.max), reads=[("lsel", 0)], writes=[("m1", 0)])
    P.add("dve", lambda e: e.tensor_tensor(out=E1, in0=lsel, in1=m1.unsqueeze(2).to_broadcast([128, NT, 8]), op=ALU.is_equal),
          reads=[("lsel", 0), ("m1", 0)], writes=[("E1", 0)])
    P.add("dve", lambda e: e.scalar_tensor_tensor(out=ls2.rearrange("p a b -> p (a b)"), in0=E1.rearrange("p a b -> p (a b)"),
                                                  scalar=-1e30, in1=lsel.rearrange("p a b -> p (a b)"), op0=ALU.mult, op1=ALU.add),
          reads=[("E1", 0), ("lsel", 0)], writes=[("ls2", 0)])
    P.add("dve", lambda e: e.tensor_reduce(out=m2, in_=ls2, axis=AX.X, op=ALU.max), reads=[("ls2", 0)], writes=[("m2", 0)])
    P.add("dve", lambda e: e.tensor_tensor(out=E2, in0=ls2, in1=m2.unsqueeze(2).to_broadcast([128, NT, 8]), op=ALU.is_equal),
          reads=[("ls2", 0), ("m2", 0)], writes=[("E2", 0)])
    w1 = T_("w1", [NT])
    w2 = T_("w2", [NT])
    P.add("dve", lambda e: e.tensor_tensor(out=w2, in0=m1, in1=m2, op=ALU.subtract), reads=[("m1", 0), ("m2", 0)], writes=[("w2", 0)])
    P.add("act", lambda e: e.activation(out=w1, in_=w2, func=AF.Sigmoid), reads=[("w2", 0)], writes=[("w1", 0)])
    P.add("dve", lambda e: e.tensor_tensor(out=w1, in0=w1, in1=p_g, op=ALU.mult), reads=[("w1", 0), ("p_g", 0)], writes=[("w1", 0)])
    P.add("dve", lambda e: e.tensor_tensor(out=w2, in0=p_g, in1=w1, op=ALU.subtract), reads=[("w1", 0), ("p_g", 0), ("w2", 0)], writes=[("w2", 0)])
    A1 = T_("A1", [NT, 32])
    A2 = T_("A2", [NT, 32])
    A12b = T_("A12b", [NT, 32], BF16)
    for g in range(4):
        gb = G1h[:, :, g].unsqueeze(2).to_broadcast([128, NT, 8])
        P.add("dve", (lambda e, g=g, gb=gb: e.tensor_tensor(out=A1[:, :, g * 8:(g + 1) * 8], in0=E1, in1=gb, op=ALU.mult)),
              reads=[("E1", 0), ("G1h", 0)], writes=[("A1", g)])
        P.add("dve", (lambda e, g=g, gb=gb: e.tensor_tensor(out=A2[:, :, g * 8:(g + 1) * 8], in0=E2, in1=gb, op=ALU.mult)),
              reads=[("E2", 0), ("G1h", 0)], writes=[("A2", g)])
    AK = [("A1", g) for g in range(4)] + [("A2", g) for g in range(4)]
    P.add("dve", lambda e: e.tensor_tensor(out=A12b, in0=A1, in1=A2, op=ALU.add), reads=AK, writes=[("A12b", 0)])
    lstrict = T_("lstrict", [128], BF16)
    lsf = T_("lsf", [128], F32)
    P.add("pool", lambda e: e.memset(lsf, 1.0), writes=[("lsf", 0)])
    P.add("pool", lambda e: e.affine_select(out=lsf, in_=lsf, pattern=[[1, 128]], compare_op=ALU.is_ge, fill=0.0, base=-1,
                                             channel_multiplier=-1), reads=[("lsf", 0)], writes=[("lsf", 0)])
    P.add("pool", lambda e: e.tensor_copy(out=lstrict, in_=lsf), reads=[("lsf", 0)], writes=[("lstrict", 0)])
    psw, pkw = next_ps()
    pst_, pkt_ = next_ps()
    A12f = A12b.rearrange("p a b -> p (a b)")
    P.add("pe", lambda e: e.matmul(psw[:, :], lhsT=lstrict, rhs=A12f, start=True, stop=True),
          reads=[("lstrict", 0), ("A12b", 0)], writes=[pkw])
    P.add("pe", lambda e: e.matmul(pst_[:, :], lhsT=ones_b, rhs=A12f, start=True, stop=True),
          reads=[("ones_b", 0), ("A12b", 0)], writes=[pkt_])
    carry = T_("carry", [NT + 1, 32])
    P.add("dve", lambda e: e.memset(carry[:, 0, :], 0.0), writes=[("carry", 0)])
    for ti in range(NT):
        P.add("dve", (lambda e, ti=ti: e.tensor_tensor(out=carry[:, ti + 1, :], in0=carry[:, ti, :], in1=pst_[:, ti * 32:(ti + 1) * 32],
                                                      op=ALU.add)),
              reads=[("carry", ti), pkt_], writes=[("carry", ti + 1)])
    counts = carry[:, NT, :]
    CK = [("carry", ti) for ti in range(NT + 1)]
    thr = T_("thr", [64])
    thr_i = T_("thr_i", [64], I32)
    P.add("pool", lambda e: e.iota(thr_i, pattern=[[1, 64]], base=0, channel_multiplier=0), writes=[("thr_i", 0)])
    P.add("pool", lambda e: e.tensor_copy(out=thr, in_=thr_i), reads=[("thr_i", 0)], writes=[("thr", 0)])
    thr128 = T_("thr128", [8])
    P.add("pool", lambda e: e.tensor_scalar(out=thr128, in0=thr[:, 0:8], scalar1=float(GROWS), scalar2=None, op0=ALU.mult),
          reads=[("thr", 0)], writes=[("thr128", 0)])
    cmp1 = T_("cmp1", [32, 8])
    ngrp = T_("ngrp", [32])
    P.add("dve", lambda e: e.tensor_tensor(out=cmp1, in0=counts.unsqueeze(2).to_broadcast([128, 32, 8]),
                                           in1=thr128.unsqueeze(1).to_broadcast([128, 32, 8]), op=ALU.is_gt),
          reads=CK + [("thr128", 0)], writes=[("cmp1", 0)])
    P.add("dve", lambda e: e.tensor_reduce(out=ngrp, in_=cmp1, axis=AX.X, op=ALU.add), reads=[("cmp1", 0)], writes=[("ngrp", 0)])
    cs = [T_("cs0", [32]), T_("cs1", [32])]
    src, srck = ngrp, ("ngrp", 0)
    for si, sh in enumerate((1, 2, 4, 8, 16)):
        dst = cs[si % 2]
        dk = ("cs%d" % (si % 2),)
        P.add("dve", (lambda e, dst=dst, src=src, sh=sh: e.tensor_copy(out=dst[:, 0:sh], in_=src[:, 0:sh])),
              reads=[srck], writes=[dk + (0,)])
        P.add("dve", (lambda e, dst=dst, src=src, sh=sh: e.tensor_tensor(out=dst[:, sh:32], in0=src[:, sh:32], in1=src[:, 0:32 - sh],
                                                                        op=ALU.add)),
              reads=[srck], writes=[dk + (1,)])
        src, srck = dst, dk + (1,)
        if si > 0:
            pass
    pend = src
    PK = [("cs0", 0), ("cs0", 1), ("cs1", 0), ("cs1", 1)]
    pstart = T_("pstart", [32])
    P.add("dve", lambda e: e.tensor_tensor(out=pstart, in0=pend, in1=ngrp, op=ALU.subtract), reads=PK + [("ngrp", 0)],
          writes=[("pstart", 0)])
    P.add("dve", lambda e: e.tensor_scalar(out=pstart, in0=pstart, scalar1=float(GROWS), scalar2=None, op0=ALU.mult),
          reads=[("pstart", 0)], writes=[("pstart", 0)])
    cmp2 = T_("cmp2", [NG, 32])
    grpf = T_("grpf", [NG])
    grpi = T_("grpi", [NG], I32)
    P.add("dve", lambda e: e.tensor_tensor(out=cmp2, in0=pend.unsqueeze(1).to_broadcast([128, NG, 32]),
                                           in1=thr[:, 0:NG].unsqueeze(2).to_broadcast([128, NG, 32]), op=ALU.is_le),
          reads=PK + [("thr", 0)], writes=[("cmp2", 0)])
    P.add("dve", lambda e: e.tensor_reduce(out=grpf, in_=cmp2, axis=AX.X, op=ALU.add), reads=[("cmp2", 0)], writes=[("grpf", 0)])
    P.add("dve", lambda e: e.tensor_scalar(out=grpf, in0=grpf, scalar1=31.0, scalar2=None, op0=ALU.min),
          reads=[("grpf", 0)], writes=[("grpf", 0)])
    P.add("dve", lambda e: e.tensor_copy(out=grpi, in_=grpf), reads=[("grpf", 0)], writes=[("grpi", 0)])
    pidx_i = T_("pidx_i", [1], I32)
    pidx = T_("pidx", [1])
    idxf = T_("idxf", [NG])
    inval = T_("inval", [NG])
    idxw = T_("idxw", [NG], I32)
    idxs = T_("idxs", [NG], I32)
    P.add("pool", lambda e: e.iota(pidx_i, pattern=[[0, 1]], base=0, channel_multiplier=1), writes=[("pidx_i", 0)])
    P.add("pool", lambda e: e.tensor_copy(out=pidx, in_=pidx_i), reads=[("pidx_i", 0)], writes=[("pidx", 0)])
    P.add("dve", lambda e: e.tensor_scalar(out=idxf, in0=grpf, scalar1=128.0, scalar2=pidx[:, 0:1], op0=ALU.mult, op1=ALU.add),
          reads=[("grpf", 0), ("pidx", 0)], writes=[("idxf", 0)])
    P.add("dve", lambda e: e.tensor_scalar(out=inval, in0=thr[:, 0:NG], scalar1=pend[:, 31:32], scalar2=None, op0=ALU.is_ge),
          reads=PK + [("thr", 0)], writes=[("inval", 0)])
    P.add("dve", lambda e: e.tensor_copy(out=idxw, in_=idxf), reads=[("idxf", 0)], writes=[("idxw", 0)])
    P.add("dve", lambda e: e.scalar_tensor_tensor(out=idxf, in0=inval, scalar=1.0e6, in1=idxf, op0=ALU.mult, op1=ALU.add),
          reads=[("inval", 0), ("idxf", 0), ("idxw", 0)], writes=[("idxf", 0)])
    P.add("dve", lambda e: e.tensor_copy(out=idxs, in_=idxf), reads=[("idxf", 0)], writes=[("idxs", 0)])
    stg = {wn: A.alloc("stg_" + wn, [4096], F32) for wn in ("wg", "wu", "wd")}
    for (wsrc_, wn_) in ((weg_d, "wg"), (weu_d, "wu"), (wed_d, "wd")):
        P.add("pool", (lambda e, wsrc_=wsrc_, wn_=wn_: e.indirect_dma_start(
            out=stg[wn_], out_offset=None, in_=wsrc_, in_offset=bass.IndirectOffsetOnAxis(ap=idxw[:, 0:1], axis=0))),
            reads=[("idxw", 0)], writes=[("stg_" + wn_, 0)], dma=True, grp="stg_" + wn_)
    slot = T_("slot", [NT, 32])
    P.add("dve", lambda e: e.tensor_tensor(out=slot, in0=psw[:, :].rearrange("p (a b) -> p a b", b=32), in1=carry[:, 0:NT, :], op=ALU.add),
          reads=[pkw] + CK, writes=[("slot", 0)])
    P.add("dve", lambda e: e.tensor_tensor(out=slot, in0=slot, in1=pstart.unsqueeze(1).to_broadcast([128, NT, 32]), op=ALU.add),
          reads=[("slot", 0), ("pstart", 0)], writes=[("slot", 0)])
    dstf = T_("dstf", [2, NT])
    dsti = T_("dsti", [2, NT], I32)
    for q, (Aq, qk) in enumerate(((A1, "A1"), (A2, "A2"))):
        P.add("dve", (lambda e, Aq=Aq: e.tensor_tensor(out=tmp32, in0=Aq, in1=slot, op=ALU.mult)),
              reads=[(qk, g) for g in range(4)] + [("slot", 0), ("tmp32", 0)], writes=[("tmp32", 0)])
        P.add("dve", (lambda e, q=q: e.tensor_reduce(out=dstf[:, q, :], in_=tmp32, axis=AX.X, op=ALU.add)),
              reads=[("tmp32", 0)], writes=[("dstf", q)])
    P.add("dve", lambda e: e.tensor_copy(out=dsti, in_=dstf), reads=[("dstf", 0), ("dstf", 1)], writes=[("dsti", 0)])
    dbg_out("dstf", dstf, [("dstf", 0), ("dstf", 1)])
    dbg_out("grpf", grpf, [("grpf", 0)])
    dbg_out("w1", w1, [("w1", 0)])
    dbg_out("w2", w2, [("w2", 0)])
    for nm in ("gmax", "G1h", "egs", "p_g", "tmp32", "lsel", "m1", "m2", "E1", "E2", "ls2", "A1", "A2", "A12b", "lstrict", "lsf",
               "carry", "thr", "thr_i", "thr128", "cmp1", "ngrp", "cs0", "cs1", "pstart", "slot", "cmp2", "Lg"):
        A.free(nm)
    if stage <= 5.5:
        return finish()

    for ti in range(NT):
        for q in range(2):
            P.add("pool", (lambda e, ti=ti, q=q: e.indirect_dma_start(
                out=XS, out_offset=bass.IndirectOffsetOnAxis(ap=dsti[:, q, ti:ti + 1], axis=0),
                in_=h2tok[:, ti, :], in_offset=None)),
                reads=[("h2tok", ti), ("dsti", 0)] + XSZ_KEYS, writes=[("XS", ti, q)], dma=True, grp="xs_sc")
    XS_KEYS = [("XS", ti, q) for ti in range(NT) for q in range(2)]
    A.free("h2tok")
    NS = 2
    wgs = [A.alloc("wg%d" % i, [8, 512], BF16) for i in range(NS)]
    wus = [A.alloc("wu%d" % i, [8, 512], BF16) for i in range(NS)]
    wds = [A.alloc("wd%d" % i, [4, D], BF16) for i in range(NS)]
    xgt = [A.alloc("xgt%d" % i, [D], BF16) for i in range(2)]
    xgT = [A.alloc("xgT%d" % i, [8, 256], BF16) for i in range(2)]
    sgl = [A.alloc("sgl%d" % i, [4, 256], BF16) for i in range(1)]
    aT = [A.alloc("aT%d" % i, [4, 256], BF16) for i in range(2)]
    ysb = [A.alloc("ysb%d" % i, [D], BF16) for i in range(4)]
    def emit_load(g, part="both"):
        sl = g % NS
        s2 = g % 2
        for (wt, wsrc, wn, ceng) in ((wgs, weg_d, "wg", "act"), (wus, weu_d, "wu", "dve"), (wds, wed_d, "wd", "pool")):
            st_ = stg[wn]
            if part in ("both", "dma") and g >= NCHK0:
                P.add("pool", (lambda e, g=g, st_=st_, wsrc=wsrc: e.indirect_dma_start(
                    out=st_, out_offset=None, in_=wsrc,
                    in_offset=bass.IndirectOffsetOnAxis(ap=idxs[:, g:g + 1], axis=0), bounds_check=32 * 128 - 1, oob_is_err=False)),
                    reads=[("idxs", 0)], writes=[("stg_" + wn, 0)], dma=True, grp="stg_" + wn)
            elif part in ("both", "dma"):
                P.add("pool", (lambda e, g=g, st_=st_, wsrc=wsrc: e.indirect_dma_start(
                    out=st_, out_offset=None, in_=wsrc,
                    in_offset=bass.IndirectOffsetOnAxis(ap=idxw[:, g:g + 1], axis=0))),
                    reads=[("idxw", 0)], writes=[("stg_" + wn, 0)], dma=True, grp="stg_" + wn)
            if part == "dma":
                continue
            dstv = wt[sl].rearrange("p a b -> p (a b)")
            if ceng == "act":
                P.add("act", (lambda e, dstv=dstv, st_=st_: e.activation(out=dstv, in_=st_, func=AF.Identity)),
                      reads=[("stg_" + wn, 0)], writes=[("%s%d" % (wn, sl), 0)])
            elif ceng == "dve":
                P.add("dve", (lambda e, dstv=dstv, st_=st_: e.tensor_copy(out=dstv, in_=st_)),
                      reads=[("stg_" + wn, 0)], writes=[("%s%d" % (wn, sl), 0)])
            else:
                P.add("act", (lambda e, dstv=dstv, st_=st_: e.activation(out=dstv[:, 0:2048], in_=st_[:, 0:2048], func=AF.Identity)),
                      reads=[("stg_" + wn, 0)], writes=[("%s%d" % (wn, sl), 0)])
                P.add("dve", (lambda e, dstv=dstv, st_=st_: e.tensor_copy(out=dstv[:, 2048:4096], in_=st_[:, 2048:4096])),
                      reads=[("stg_" + wn, 0)], writes=[("%s%d" % (wn, sl), 1)])

    def emit_compute_a(g):
        s2 = g % 2
        for hf in range(2):
            xi = (2 * g + hf) % 2
            r0 = g * GROWS + hf * 128
            P.add("sp", (lambda e, r0=r0, xi=xi: e.dma_start(out=xgt[xi], in_=XS[r0:r0 + 128, :])),
                  reads=XS_KEYS, writes=[("xgt%d" % xi, 0)], dma=True, grp="xgt%d" % xi)
            pt, pkt = next_ps()
            ptb = pt.bitcast(BF16)

            def xtr(e, ptb=ptb, xi=xi):
                ins = None
                for c in range(8):
                    ins = e.transpose(out=ptb[:, c * 128:(c + 1) * 128], in_=xgt[xi][:, c * 128:(c + 1) * 128], identity=ident_b)
                return ins
            P.add("pe", xtr, reads=[("ident_b", 0), ("xgt%d" % xi, 0)], writes=[pkt])
            P.add("act", (lambda e, ptb=ptb, s2=s2, hf=hf: e.activation(
                out=xgT[s2][:, :, hf * 128:(hf + 1) * 128], in_=ptb[:, 0:1024].rearrange("p (a b) -> p a b", b=128), func=AF.Identity)),
                reads=[pkt], writes=[("xgT%d" % s2, hf)])

    def emit_compute_b1(g):
        sl = g % NS
        s2 = g % 2
        pg_ = [next_ps(), next_ps()]
        pu_ = [next_ps(), next_ps()]

        def gumm(e, pg_=pg_, pu_=pu_, sl=sl, s2=s2):
            ins = None
            for (pp, ww) in ((pg_, wgs), (pu_, wus)):
                for fc in range(4):
                    o = pp[fc // 2][0][:, (fc % 2) * 256:(fc % 2 + 1) * 256]
                    for k in range(8):
                        ins = e.matmul(o, lhsT=ww[sl][:, k, fc * 128:(fc + 1) * 128], rhs=xgT[s2][:, k, :], start=(k == 0), stop=(k == 7))
            return ins
        P.add("pe", gumm, reads=[("wg%d" % sl, 0), ("wu%d" % sl, 0), ("xgT%d" % s2, 0), ("xgT%d" % s2, 1)],
              writes=[pg_[0][1], pg_[1][1], pu_[0][1], pu_[1][1]])
        for b2 in range(2):
            P.add("act", (lambda e, pg_=pg_, s2=s2, b2=b2: e.activation(
                out=sgl[0][:, 2 * b2:2 * b2 + 2, :].rearrange("p a b -> p (a b)"), in_=pg_[b2][0][:, :], func=AF.Silu)),
                reads=[pg_[b2][1]], writes=[("sgl0", b2)])
            P.add("dve", (lambda e, pu_=pu_, s2=s2, b2=b2: e.tensor_tensor(
                out=aT[s2][:, 2 * b2:2 * b2 + 2, :].rearrange("p a b -> p (a b)"), in0=pu_[b2][0][:, :],
                in1=sgl[0][:, 2 * b2:2 * b2 + 2, :].rearrange("p a b -> p (a b)"), op=ALU.mult)),
                reads=[pu_[b2][1], ("sgl0", b2)], writes=[("aT%d" % s2, b2)])

    def emit_compute_b2(g):
        sl = g % NS
        s2 = g % 2
        for hf in range(2):
            yi = (2 * g + hf) % 4
            py = [next_ps(), next_ps()]

            def dmm(e, py=py, sl=sl, s2=s2, hf=hf):
                ins = None
                for half in range(2):
                    for fc in range(4):
                        ins = e.matmul(py[half][0][:, :], lhsT=aT[s2][:, fc, hf * 128:(hf + 1) * 128],
                                       rhs=wds[sl][:, fc, half * 512:(half + 1) * 512], start=(fc == 0), stop=(fc == 3))
                return ins
            P.add("pe", dmm, reads=[("wd%d" % sl, 0), ("wd%d" % sl, 1), ("aT%d" % s2, 0), ("aT%d" % s2, 1)], writes=[py[0][1], py[1][1]])
            for half in range(2):
                P.add("dve", (lambda e, py=py, yi=yi, half=half: e.tensor_tensor(
                    out=ysb[yi][:, half * 512:(half + 1) * 512], in0=py[half][0][:, :], in1=gt2row[:, half * 512:(half + 1) * 512],
                    op=ALU.mult)),
                    reads=[py[half][1], ("gt2row", half)], writes=[("ysb%d" % yi, half)])
            r0 = g * GROWS + hf * 128
            P.add("sp", (lambda e, r0=r0, yi=yi: e.dma_start(out=YS[r0:r0 + 128, :], in_=ysb[yi])),
                  reads=[("ysb%d" % yi, 0), ("ysb%d" % yi, 1)], writes=[("YS", g, hf)], dma=True, grp="ys_st%d" % yi)

    emit_load(0, "cast")
    emit_compute_a(0)
    for g in range(NG):
        emit_compute_b1(g)
        if g + 1 < NG:
            emit_load(g + 1)
            emit_compute_a(g + 1)
        emit_compute_b2(g)
    YS_KEYS = [("YS", g, hf) for g in range(NG) for hf in range(2)]
    for i in range(NS):
        A.free("wg%d" % i); A.free("wu%d" % i); A.free("wd%d" % i)
    for nm in ("stg_wg", "stg_wu", "stg_wd", "xgt0", "xgt1", "xgT0", "xgT1", "sgl0", "aT0", "aT1", "ysb0", "ysb1", "ysb2", "ysb3"):
        A.free(nm)

    gfin = load_const("gfin", gfin_d, [D])
    NYG = 4
    yg = [[A.alloc("yg%d_%d" % (q, i), [D], BF16) for i in range(NYG)] for q in range(2)]
    acc = [A.alloc("acc%d" % i, [D], F32) for i in range(2)]
    outt = [A.alloc("outt%d" % i, [D], F32) for i in range(2)]
    ssf = A.alloc("ssf", [NT], F32)
    rsf = A.alloc("rsf", [NT], F32)

    def emit_g(ti):
        s4 = ti % NYG
        for q in range(2):
            P.add("pool", (lambda e, ti=ti, q=q, s4=s4: e.indirect_dma_start(
                out=yg[q][s4], out_offset=None, in_=YS,
                in_offset=bass.IndirectOffsetOnAxis(ap=dsti[:, q, ti:ti + 1], axis=0))),
                reads=YS_KEYS + [("dsti", 0)], writes=[("yg%d_%d" % (q, s4), 0)], dma=True, grp="yg%d_%d" % (q, s4))

    def emit_c1(ti):
        s4 = ti % NYG
        s2 = ti % 2
        y1, y2 = yg[0][s4], yg[1][s4]
        k1, k2 = ("yg0_%d" % s4, 0), ("yg1_%d" % s4, 0)
        ac = acc[s2]
        ak = ("acc%d" % s2, 0)
        P.add("act", (lambda e, y1=y1, ac=ac, ti=ti: e.activation(out=ac, in_=y1, func=AF.Identity, scale=w1[:, ti:ti + 1])),
              reads=[k1, ("w1", 0)], writes=[ak])
        P.add("dve", (lambda e, ac=ac, y2=y2, ti=ti: e.scalar_tensor_tensor(out=ac, in0=y2, scalar=w2[:, ti:ti + 1], in1=ac,
                                                                          op0=ALU.mult, op1=ALU.add)),
              reads=[ak, k2, ("w2", 0)], writes=[ak])
        P.add("dve", (lambda e, ac=ac, ti=ti: e.tensor_tensor(out=x1[:, ti, :], in0=x1[:, ti, :], in1=ac, op=ALU.add)),
              reads=[ak, ("x1", ti)], writes=[("x1", ti)])

    def emit_c2(ti):
        s2 = ti % 2
        P.add("act", (lambda e, ti=ti: e.activation(out=junk, in_=x1[:, ti, :], func=AF.Square, accum_out=ssf[:, ti:ti + 1])),
              reads=[("x1", ti)], writes=[("junk", 0), ("ssf", ti)])
        P.add("act", (lambda e, ti=ti: e.activation(out=rsf[:, ti:ti + 1], in_=ssf[:, ti:ti + 1], func=AF.Sqrt, scale=1.0 / D, bias=1e-6)),
              reads=[("ssf", ti)], writes=[("rsf", ti)])
        P.add("dve", (lambda e, ti=ti: e.reciprocal(out=rsf[:, ti:ti + 1], in_=rsf[:, ti:ti + 1])),
              reads=[("rsf", ti)], writes=[("rsf", ti)])
        ot = outt[s2]
        ok = ("outt%d" % s2, 0)
        P.add("act", (lambda e, ot=ot, ti=ti: e.activation(out=ot, in_=x1[:, ti, :], func=AF.Identity, scale=rsf[:, ti:ti + 1])),
              reads=[("x1", ti), ("rsf", ti)], writes=[ok])
        P.add("dve", (lambda e, ot=ot: e.tensor_tensor(out=ot, in0=ot, in1=gfin, op=ALU.mult)),
              reads=[ok, ("gfin", 0)], writes=[ok])
        P.add("sp", (lambda e, ot=ot, ti=ti: e.dma_start(out=out_d[ti * 128:(ti + 1) * 128, :], in_=ot)),
              reads=[ok], dma=True, grp="out")

    for ti in range(min(3, NT)):
        emit_g(ti)
    for ti in range(NT):
        if ti + 3 < NT:
            emit_g(ti + 3)
        emit_c1(ti)
        if ti > 0:
            emit_c2(ti - 1)
    emit_c2(NT - 1)
    P.emit(final_wait_groups=["out"] + (["dbgout"] if "dbgout" in P.dma_groups else []))
    build.stats = dict(peak_kb=A.peak * 4 / 1024.0, n_ops=len(P.all_ops), n_groups=len(P.dma_groups))
    return nc, dbg_outs


def host_layout(inp, b):
    f = lambda a: np.ascontiguousarray(a, dtype=np.float32)
    col = lambda v, n: f(np.asarray(v).reshape(n, 128).T)
    m = {}
    m["x"] = f(inp["x"][b])
    m["c_col"] = col(inp["c"][b], 8)
    m["w_ada"] = f(inp["w_ada"][0])
    m["b_ada_col"] = col(inp["b_ada"][0], 48)
    m["g1_col"] = col(inp["g_norm1"][0], 8)
    m["g2_col"] = col(inp["g_norm2"][0], 8)
    m["w_in"] = f(inp["w_in"][0])
    m["b_if_bc"] = f(np.broadcast_to(inp["b_if"][0][None, :], (128, 8)))
    m["conv_w_col"] = f(inp["conv_dw_w"][0].reshape(31, 4, 128).transpose(2, 1, 0))
    m["conv_b_col"] = col(inp["conv_dw_b"][0], 4)
    m["conv_lng_col"] = col(inp["conv_ln_g"][0], 4)
    m["conv_lnb_col"] = col(inp["conv_ln_b"][0], 4)
    m["w_conv_out"] = f(inp["w_conv_out"][0])
    m["qk_w_col"] = f(inp["qk_conv_w"][0].reshape(4, 8, 128).transpose(2, 1, 0))
    m["qk_b_col"] = col(inp["qk_conv_b"][0], 8)
    m["mng_col"] = col(inp["m_norm_g"][0], 4)
    m["w_m_out"] = f(inp["w_m_out"][0])
    m["w_out"] = f(inp["w_out"][0])
    m["w_router"] = f(np.concatenate([inp["w_rg"][0], inp["w_re"][0]], axis=1))
    m["b_router_bc"] = f(np.broadcast_to(np.concatenate([inp["b_rg"][0], inp["b_re"][0]])[None, :], (128, 36)))
    m["w_e_gate_l"] = f(inp["w_e_gate"][0].reshape(32, 8, 128, 512).transpose(0, 2, 1, 3).reshape(32 * 128, 8 * 512))
    m["w_e_up_l"] = f(inp["w_e_up"][0].reshape(32, 8, 128, 512).transpose(0, 2, 1, 3).reshape(32 * 128, 8 * 512))
    m["w_e_down_l"] = f(inp["w_e_down"][0].reshape(32, 4, 128, D).transpose(0, 2, 1, 3).reshape(32 * 128, 4 * D))
    m["g_final_bc"] = f(np.broadcast_to(np.asarray(inp["g_final"])[None, :], (128, D)))
    return m


def kernel(**inputs):
    nc, _ = build()
    shared = host_layout(inputs, 0)
    in_maps = []
    for b in range(8):
        m = dict(shared)
        m["x"] = np.ascontiguousarray(inputs["x"][b], dtype=np.float32)
        m["c_col"] = np.ascontiguousarray(np.asarray(inputs["c"][b]).reshape(8, 128).T, dtype=np.float32)
        in_maps.append(m)
    res = run_bass_kernel_spmd(nc, in_maps, core_ids=list(range(8)))
    return np.stack([np.asarray(r["out"]) for r in res.results], axis=0).astype(np.float32)
```

```python
import contextlib
import numpy as np
import concourse.bass as bass
import concourse.mybir as mybir
from concourse.bass_utils import run_bass_kernel_spmd

F32 = mybir.dt.float32
BF16 = mybir.dt.bfloat16
I32 = mybir.dt.int32
AF = mybir.ActivationFunctionType
ALU = mybir.AluOpType
AX = mybir.AxisListType

T = 2048
D = 1024
NT = 16
NB = 4
DIN = 5128
ENG_NAMES = ("pe", "act", "dve", "pool", "sp")


class Op:
    __slots__ = ("eng", "fn", "is_dma", "grp", "signal", "val", "idx", "deps")

    def __init__(self, eng, fn, is_dma, grp):
        self.eng = eng
        self.fn = fn
        self.is_dma = is_dma
        self.grp = grp
        self.signal = False
        self.val = None
        self.idx = None
        self.deps = []


def _reduce_ops(ops):
    latest = {}
    dm = {}
    for o in ops:
        if o.is_dma:
            if o.grp not in dm or dm[o.grp].idx < o.idx:
                dm[o.grp] = o
        else:
            if o.eng not in latest or latest[o.eng].idx < o.idx:
                latest[o.eng] = o
    return list(latest.values()) + list(dm.values())


class Prog:
    def __init__(self, nc):
        self.nc = nc
        self.ops = {e: [] for e in ENG_NAMES}
        self.all_ops = []
        self.last_writer = {}
        self.readers = {}
        self.dma_groups = {}
        self.buf_pred = {}
        self.keys_by_buf = {}
        self.wait_all_groups = set()

    def _touch(self, k):
        if k not in self.readers:
            self.readers[k] = list(self.buf_pred.get(k[0], ()))
            self.last_writer[k] = None
            self.keys_by_buf.setdefault(k[0], set()).add(k)

    def ops_touching(self, bufname):
        s = list(self.buf_pred.get(bufname, ()))
        for k in self.keys_by_buf.get(bufname, ()):
            w = self.last_writer.get(k)
            if w is not None:
                s.append(w)
            s.extend(self.readers.get(k, ()))
        return _reduce_ops(s)

    def add(self, eng, fn, reads=(), writes=(), dma=False, grp=None):
        op = Op(eng, fn, dma, grp)
        op.idx = len(self.all_ops)
        self.all_ops.append(op)
        self.ops[eng].append(op)
        if dma:
            assert grp is not None
            self.dma_groups.setdefault(grp, []).append(op)
        deps = []
        for k in reads:
            self._touch(k)
            w = self.last_writer[k]
            if w is not None:
                deps.append((w, "raw"))
            elif self.readers[k] and k[0] in self.buf_pred:
                pass
        for k in writes:
            self._touch(k)
            w = self.last_writer[k]
            if w is not None:
                deps.append((w, "waw"))
            for r in self.readers[k]:
                deps.append((r, "war"))
        for d, kind in deps:
            if d is op:
                continue
            if (not d.is_dma) and (not dma) and d.eng == eng:
                if eng == "pe":
                    continue
            op.deps.append(d)
        for k in reads:
            self.readers[k].append(op)
        for k in writes:
            self.last_writer[k] = op
            self.readers[k] = []
        return op

    def emit(self, final_wait_groups=()):
        nc = self.nc
        for op in self.all_ops:
            op.deps = _reduce_ops(op.deps)
            for d in op.deps:
                d.signal = True
        for e in ENG_NAMES:
            c = 0
            for op in self.ops[e]:
                if (not op.is_dma) and op.signal:
                    c += 1
                    op.val = c
        gtotal = {}
        for g, lst in self.dma_groups.items():
            c = 0
            for op in lst:
                c += 16
                op.val = c
            gtotal[g] = c
        with contextlib.ExitStack() as st:
            esem = {e: st.enter_context(nc.semaphore("s_" + e)) for e in ENG_NAMES}
            gsem = {g: st.enter_context(nc.semaphore("d_%d" % i))
                    for i, g in enumerate(self.dma_groups)}
            block = st.enter_context(nc.Block())

            def run(e, engobj):
                seen = {}
                for op in self.ops[e]:
                    for d in op.deps:
                        if d.is_dma:
                            key = ("g", d.grp)
                            sem = gsem[d.grp]
                            v = gtotal[d.grp] if d.grp in self.wait_all_groups else d.val
                        else:
                            key = ("e", d.eng)
                            sem = esem[d.eng]
                            v = d.val
                        if seen.get(key, 0) >= v:
                            continue
                        seen[key] = v
                        engobj.wait_ge(sem, v)
                    ins = op.fn(engobj)
                    if op.is_dma:
                        ins.then_inc(gsem[op.grp], 16)
                    elif op.signal:
                        ins.then_inc(esem[e], 1)
                if e == "sp":
                    for g in final_wait_groups:
                        engobj.wait_ge(gsem[g], gtotal[g])

            block.tensor(lambda eng: run("pe", eng))
            block.scalar(lambda eng: run("act", eng))
            block.vector(lambda eng: run("dve", eng))
            block.gpsimd(lambda eng: run("pool", eng))
            block.sync(lambda eng: run("sp", eng))


class Arena:
    def __init__(self, nc, prog, words):
        self.t = nc.alloc_sbuf_tensor("arena", [128, words], F32)
        self.P = prog
        self.free_list = [(0, words)]
        self.live = {}
        self.dead = []
        self.peak = 0

    def alloc(self, name, shape, dt, parts=128):
        n = int(np.prod(shape))
        esz = 2 if dt == BF16 else 4
        words = (n * esz + 31) // 32 * 8
        small = words <= 1100
        order = range(len(self.free_list) - 1, -1, -1) if small else range(len(self.free_list))
        for i in order:
            o, w = self.free_list[i]
            if w >= words:
                if w == words:
                    off = o
                    self.free_list.pop(i)
                elif small:
                    off = o + w - words
                    self.free_list[i] = (o, w - words)
                else:
                    off = o
                    self.free_list[i] = (o + words, w - words)
                break
        else:
            raise RuntimeError("SBUF arena full allocating %s (%d words); live=%s" % (
                name, words, {k: v[1] for k, v in self.live.items()}))
        self.live[name] = (off, words)
        self.peak = max(self.peak, off + words)
        preds = []
        for (o, w, nm) in self.dead:
            if o < off + words and off < o + w:
                preds.extend(self.P.ops_touching(nm))
        assert name not in self.P.keys_by_buf, name
        self.P.buf_pred[name] = _reduce_ops(preds)
        v = self.t[0:parts, off:off + words]
        if dt != F32:
            v = v.bitcast(dt)
        v = v[:, 0:n]
        if len(shape) == 2:
            v = v.rearrange("p (a b) -> p a b", b=shape[1])
        elif len(shape) == 3:
            v = v.rearrange("p (a b c) -> p a b c", b=shape[1], c=shape[2])
        return v

    def free(self, name):
        off, words = self.live.pop(name)
        self.dead.append((off, words, name))
        fl = self.free_list + [(off, words)]
        fl.sort()
        merged = []
        for o, w in fl:
            if merged and merged[-1][0] + merged[-1][1] == o:
                merged[-1] = (merged[-1][0], merged[-1][1] + w)
            else:
                merged.append((o, w))
        self.free_list = merged


def build(stage=99, dbg=()):
    nc = bass.Bass("TRN2", target_bir_lowering=False)
    P = Prog(nc)
    A = Arena(nc, P, 52992)

    def din(name, shape, dt=F32):
        return nc.dram_tensor(name, list(shape), dt, kind="ExternalInput").ap()

    x_d = din("x", [T, D])
    ccol_d = din("c_col", [128, 8])
    wada_d = din("w_ada", [D, 6 * D])
    bada_d = din("b_ada_col", [128, 48])
    g1_d = din("g1_col", [128, 8])
    g2_d = din("g2_col", [128, 8])
    win_d = din("w_in", [D, DIN])
    bif_d = din("b_if_bc", [128, 8])
    cw_d = din("conv_w_col", [128, 4, 31])
    cb_d = din("conv_b_col", [128, 4])
    clg_d = din("conv_lng_col", [128, 4])
    clb_d = din("conv_lnb_col", [128, 4])
    wco_d = din("w_conv_out", [512, D])
    qkw_d = din("qk_w_col", [128, 8, 4])
    qkb_d = din("qk_b_col", [128, 8])
    mng_d = din("mng_col", [128, 4])
    wmo_d = din("w_m_out", [512, D])
    wout_d = din("w_out", [D, D])
    wr_d = din("w_router", [D, 36])
    br_d = din("b_router_bc", [128, 36])
    weg_d = din("w_e_gate_l", [32 * 128, 8 * 512])
    weu_d = din("w_e_up_l", [32 * 128, 8 * 512])
    wed_d = din("w_e_down_l", [32 * 128, 4 * D])
    gfin_d = din("g_final_bc", [128, D])
    out_d = nc.dram_tensor("out", [T, D], F32, kind="ExternalOutput").ap()

    dbg_outs = {}

    def dbg_out(name, ap, reads):
        if name not in dbg:
            return
        shape = list(ap.shape)
        dt = ap.dtype
        d = nc.dram_tensor("dbg_" + name, shape, dt, kind="ExternalOutput").ap()
        dbg_outs[name] = d
        P.add("sp", lambda e: e.dma_start(out=d, in_=ap), reads=reads, dma=True, grp="dbgout")

    psb = [nc.alloc_psum_tensor("ps%d" % i, [128, 512], F32) for i in range(8)]
    ps_rot = list(range(8))

    def next_ps():
        i = ps_rot.pop(0)
        ps_rot.append(i)
        return psb[i], ("ps%d" % i,)

    def hold_ps():
        i = ps_rot.pop(0)
        return psb[i], ("ps%d" % i,)

    def release_ps(key):
        ps_rot.append(int(key[0][2:]))

    ident_f = A.alloc("ident_f", [128], F32)
    ident_b = A.alloc("ident_b", [128], BF16)
    ones_b = A.alloc("ones_b", [128], BF16)
    ones_f = A.alloc("ones_f", [128], F32)
    mask_ut = A.alloc("mask_ut", [128], BF16)
    tri_f = A.alloc("tri_f", [128], F32)
    K_ID = ("ident_f", 0)
    P.add("pool", lambda e: e.memset(ident_f, 0.0), writes=[("ident_f", 0)])
    P.add("pool", lambda e: e.affine_select(out=ident_f, in_=ident_f, pattern=[[-1, 128]],
                                             compare_op=ALU.not_equal, fill=1.0, base=0, channel_multiplier=1),
          reads=[("ident_f", 0)], writes=[("ident_f", 0)])
    P.add("pool", lambda e: e.tensor_copy(out=ident_b, in_=ident_f), reads=[("ident_f", 0)], writes=[("ident_b", 0)])
    P.add("pool", lambda e: e.memset(ones_b, 1.0), writes=[("ones_b", 0)])
    P.add("pool", lambda e: e.memset(ones_f, 1.0), writes=[("ones_f", 0)])
    P.add("pool", lambda e: e.memset(tri_f, 1.0), writes=[("tri_f", 0)])
    P.add("pool", lambda e: e.affine_select(out=tri_f, in_=tri_f, pattern=[[1, 128]],
                                             compare_op=ALU.is_ge, fill=0.0, base=0, channel_multiplier=-1),
          reads=[("tri_f", 0)], writes=[("tri_f", 0)])
    P.add("pool", lambda e: e.tensor_copy(out=mask_ut, in_=tri_f), reads=[("tri_f", 0)], writes=[("mask_ut", 0)])

    def load_const(name, dram, shape, dt=F32):
        t = A.alloc(name, shape, dt)
        P.add("sp", lambda e: e.dma_start(out=t, in_=dram), writes=[(name, 0)], dma=True, grp="c_" + name)
        return t

    ccol = load_const("ccol", ccol_d, [8])
    bada = load_const("bada", bada_d, [48])
    g1c = load_const("g1c", g1_d, [8])
    g2c = load_const("g2c", g2_d, [8])

    silc = A.alloc("silc", [8], F32)
    silb = A.alloc("silb", [8], BF16)
    P.add("act", lambda e: e.activation(out=silc, in_=ccol, func=AF.Silu), reads=[("ccol", 0)], writes=[("silc", 0)])
    P.add("dve", lambda e: e.tensor_copy(out=silb, in_=silc), reads=[("silc", 0)], writes=[("silb", 0)])
    modT = A.alloc("modT", [48], F32)
    wada_v = wada_d.rearrange("(c p) n -> p c n", p=128)
    NWA = 2
    wab = [A.alloc("wada%d" % i, [8, 512], BF16) for i in range(NWA)]
    a1 = A.alloc("a1", [8], F32)
    a2 = A.alloc("a2", [8], F32)

    def adaln_block(blk, ps_mod, k_mod):
        s_ = blk % NWA
        buf = wab[s_]
        nm = "wada%d" % s_
        P.add("pool", (lambda e, buf=buf, blk=blk: e.dma_start(out=buf, in_=wada_v[:, :, blk * 512:(blk + 1) * 512])),
              writes=[(nm, 0)], dma=True, grp=nm)

        def mm(e, buf=buf, blk=blk):
            ins = None
            for jj in range(4):
                j = blk * 4 + jj
                for k in range(8):
                    ins = e.matmul(ps_mod[:, j:j + 1], lhsT=buf[:, k, jj * 128:(jj + 1) * 128], rhs=silb[:, k:k + 1],
                                   start=(k == 0), stop=(k == 7))
            return ins
        P.add("pe", mm, reads=[(nm, 0), ("silb", 0)], writes=[k_mod])

    def adaln_finish(ps_mod, k_mod, c0, c1, part):
        P.add("dve", lambda e: e.tensor_tensor(out=modT[:, c0:c1], in0=ps_mod[:, c0:c1], in1=bada[:, c0:c1], op=ALU.add),
              reads=[k_mod, ("bada", 0)], writes=[("modT", part)])
        release_ps(k_mod)

    pm0, km0 = hold_ps()
    for blk in range(4):
        adaln_block(blk, pm0, km0)
    adaln_finish(pm0, km0, 0, 16, 0)
    P.add("dve", lambda e: e.scalar_tensor_tensor(out=a1, in0=modT[:, 8:16], scalar=1.0, in1=g1c, op0=ALU.add, op1=ALU.mult),
          reads=[("modT", 0), ("g1c", 0)], writes=[("a1", 0)])
    p2state = {}

    def adaln_p2_block(blk):
        if "ps" not in p2state:
            p2state["ps"] = hold_ps()
        adaln_block(blk, *p2state["ps"])

    def adaln_p2_end():
        pm1, km1 = p2state["ps"]
        adaln_finish(pm1, km1, 16, 48, 1)
        P.add("dve", lambda e: e.scalar_tensor_tensor(out=a2, in0=modT[:, 32:40], scalar=1.0, in1=g2c, op0=ALU.add, op1=ALU.mult),
              reads=[("modT", 1), ("g2c", 0)], writes=[("a2", 0)])
        dbg_out("modT", modT, [("modT", 0), ("modT", 1)])
        for i in range(NWA):
            A.free("wada%d" % i)

    hT = A.alloc("hT", [8, T], BF16)
    merged = A.alloc("merged", [8, T], BF16)
    NWB = 3
    wbufs = [A.alloc("wblk%d" % i, [8, 512], BF16) for i in range(NWB)]
    NXB = 8
    xin = [A.alloc("xin%d" % i, [D], F32) for i in range(NXB)]
    xnb = [A.alloc("xnb%d" % i, [D], BF16) for i in range(NXB)]
    junk = A.alloc("junk", [D], F32)
    ss1 = A.alloc("ss1", [NT], F32)
    rs1 = A.alloc("rs1", [NT], F32)

    def p2_stats(nb):
        for tt in range(4):
            ti = nb * 4 + tt
            s = ti % NXB
            P.add("sp", (lambda e, s=s, ti=ti: e.dma_start(out=xin[s], in_=x_d[ti * 128:(ti + 1) * 128, :])),
                  writes=[("xin%d" % s, 0)], dma=True, grp="xin%d" % s)
            P.add("act", (lambda e, s=s, ti=ti: e.activation(out=junk, in_=xin[s], func=AF.Square,
                                                             accum_out=ss1[:, ti:ti + 1])),
                  reads=[("xin%d" % s, 0)], writes=[("junk", 0), ("ss1", ti)])
            P.add("act", (lambda e, ti=ti: e.activation(out=rs1[:, ti:ti + 1], in_=ss1[:, ti:ti + 1], func=AF.Sqrt,
                                                        scale=1.0 / D, bias=1e-6)),
                  reads=[("ss1", ti)], writes=[("rs1", ti)])
            P.add("dve", (lambda e, ti=ti: e.reciprocal(out=rs1[:, ti:ti + 1], in_=rs1[:, ti:ti + 1])),
                  reads=[("rs1", ti)], writes=[("rs1", ti)])
            P.add("dve", (lambda e, s=s, ti=ti: e.tensor_scalar(out=xnb[s], in0=xin[s], scalar1=rs1[:, ti:ti + 1],
                                                                scalar2=None, op0=ALU.mult)),
                  reads=[("xin%d" % s, 0), ("rs1", ti)], writes=[("xnb%d" % s, 0)])

    def p2_tr(nb):
        pst = [next_ps() for _ in range(4)]
        for tt in range(4):
            ti = nb * 4 + tt
            s = ti % NXB

            def tr(e, s=s, tt=tt, pst=pst):
                ins = None
                for c in range(8):
                    pb = pst[c // 2][0].bitcast(BF16)
                    ins = e.transpose(out=pb[:, (c % 2) * 512 + tt * 128:(c % 2) * 512 + (tt + 1) * 128],
                                      in_=xnb[s][:, c * 128:(c + 1) * 128], identity=ident_b)
                return ins
            P.add("pe", tr, reads=[("xnb%d" % s, 0), ("ident_b", 0)], writes=[pst[i][1] for i in range(4)])
        for c in range(8):
            pb = pst[c // 2][0].bitcast(BF16)
            P.add("act", (lambda e, c=c, pb=pb, nb=nb: e.activation(
                out=hT[:, c, nb * 512:(nb + 1) * 512], in_=pb[:, (c % 2) * 512:(c % 2 + 1) * 512],
                func=AF.Identity, scale=a1[:, c:c + 1], bias=modT[:, c:c + 1])),
                reads=[pst[c // 2][1], ("a1", 0), ("modT", 0)],
                writes=[("hT", c, nb)])

    p2_stats(0)
    for nb in range(NB):
        if nb + 1 < NB:
            p2_stats(nb + 1)
        p2_tr(nb)
    dbg_out("hT", hT, [("hT", c, nb) for c in range(8) for nb in range(NB)])
    for i in range(NXB):
        A.free("xin%d" % i); A.free("xnb%d" % i)

    def finish():
        P.emit(final_wait_groups=["dbgout"] if "dbgout" in P.dma_groups else [])
        return nc, dbg_outs

    GROWS = 256
    NG = -(-(2 * T + 32 * (GROWS - 1)) // GROWS)
    XS = nc.dram_tensor("xs_scratch", [NG * GROWS, D], BF16).ap()
    YS = nc.dram_tensor("ys_scratch", [NG * GROWS, D], BF16).ap()
    if stage <= 1:
        return finish()

    win_v = win_d.rearrange("(c p) n -> p c n", p=128)
    NWB = 3
    wb_ctr = [0]

    def load_wblock(col0, ncols=512):
        i = wb_ctr[0] % NWB
        wb_ctr[0] += 1
        buf = wbufs[i]
        nm = "wblk%d" % i
        P.add("pool", lambda e: e.dma_start(out=buf[:, :, 0:ncols], in_=win_v[:, :, col0:col0 + ncols]),
              writes=[(nm, 0)], dma=True, grp=nm)
        return buf, (nm, 0)

    def load_w4(dram_v):
        i = wb_ctr[0] % NWB
        wb_ctr[0] += 1
        nm = "wblk%d" % i
        v = wbufs[i].rearrange("p a b -> p (a b)").rearrange("p (a b) -> p a b", b=D)
        P.add("pool", lambda e: e.dma_start(out=v, in_=dram_v), writes=[(nm, 0)], dma=True, grp=nm)
        return v, (nm, 0)

    def load_cast(name, dram_ap, shape):
        t = A.alloc(name, shape, BF16)
        P.add("pool", lambda e: e.dma_start(out=t, in_=dram_ap), writes=[(name, 0)], dma=True, grp="c_" + name)
        return t

    hT_keys = lambda nb: [("hT", c, nb) for c in range(8)]

    def proj_fm(wb, wkey, mcol, nb):
        ps, pk = next_ps()

        def mm(e):
            ins = None
            for k in range(8):
                ins = e.matmul(ps[:, :], lhsT=wb[:, k, mcol * 128:(mcol + 1) * 128], rhs=hT[:, k, nb * 512:(nb + 1) * 512],
                               start=(k == 0), stop=(k == 7))
            return ins
        P.add("pe", mm, reads=[wkey] + hT_keys(nb), writes=[pk])
        return ps, pk

    cw = load_const("cw", cw_d, [4, 31])
    cb = load_const("cb", cb_d, [4])
    clg = load_const("clg", clg_d, [4])
    clb = load_const("clb", clb_d, [4])
    u = A.alloc("u", [4, 32 + T], BF16)
    PADU = 32
    for m in range(4):
        P.add("pool", (lambda e, m=m: e.memset(u[:, m, 0:PADU], 0.0)), writes=[("u", m, -1)])
    dg31 = A.alloc("dg31", [4, 31, 128], BF16)
    for m in range(4):
        P.add("pool", (lambda e, m=m: e.tensor_tensor(
            out=dg31[:, m], in0=ident_b.unsqueeze(1).to_broadcast([128, 31, 128]),
            in1=cw[:, m, :].unsqueeze(2).to_broadcast([128, 31, 128]), op=ALU.mult)),
            reads=[("ident_b", 0), ("cw", 0)], writes=[("dg31", m)])
    sgt = [A.alloc("sgt%d" % i, [512], BF16) for i in range(2)]
    sg_ctr = [0]

    def next_sgt():
        i = sg_ctr[0] % 2
        sg_ctr[0] += 1
        return sgt[i], ("sgt%d" % i, 0)

    wa, wak = load_wblock(0)
    wbk, wbkk = load_wblock(512)
    for m in range(4):
        for nb in range(NB):
            psa, pka = proj_fm(wa, wak, m, nb)
            psb_, pkb = proj_fm(wbk, wbkk, m, nb)
            sg, sgk = next_sgt()
            P.add("act", (lambda e, sg=sg, p=psb_: e.activation(out=sg, in_=p[:, :], func=AF.Sigmoid)),
                  reads=[pkb], writes=[sgk])
            P.add("dve", (lambda e, sg=sg, p=psa, m=m, nb=nb: e.tensor_tensor(
                out=u[:, m, PADU + nb * 512:PADU + (nb + 1) * 512], in0=p[:, :], in1=sg, op=ALU.mult)),
                reads=[pka, sgk], writes=[("u", m, nb)])
    dbg_out("u", u, [("u", m, nb) for m in range(4) for nb in range(-1, NB)])

    wco, wcok = load_w4(wco_d.rearrange("(c p) n -> p c n", p=128))
    gA_blocks = {0: load_wblock(3080)}
    cT = A.alloc("cT", [4, T], BF16)
    sqT = A.alloc("sqT", [4, T], BF16)
    for m in range(4):
        for nb in range(NB):
            ps, pk = next_ps()

            def cmm(e, ps=ps, m=m, nb=nb):
                ins = None
                for k in range(31):
                    o = PADU - 30 + nb * 512 + k
                    ins = e.matmul(ps[:, :], lhsT=dg31[:, m, k, :], rhs=u[:, m, o:o + 512], start=(k == 0), stop=(k == 30))
                return ins
            P.add("pe", cmm, reads=[("dg31", m), ("u", m, nb), ("u", m, nb - 1)], writes=[pk])
            P.add("act", (lambda e, ps=ps, m=m, nb=nb: e.activation(
                out=cT[:, m, nb * 512:(nb + 1) * 512], in_=ps[:, :], func=AF.Identity, bias=cb[:, m:m + 1])),
                reads=[pk, ("cb", 0)], writes=[("cT", m, nb)])
            P.add("act", (lambda e, ps=ps, m=m, nb=nb: e.activation(
                out=sqT[:, m, nb * 512:(nb + 1) * 512], in_=ps[:, :], func=AF.Square, bias=cb[:, m:m + 1])),
                reads=[pk, ("cb", 0)], writes=[("sqT", m, nb)])
            gi = m * NB + nb
            if gi % 2 == 1:
                adaln_p2_block(4 + gi // 2)
    adaln_p2_end()
    dbg_out("cT", cT, [("cT", m, nb) for m in range(4) for nb in range(NB)])
    A.free("u")
    A.free("dg31")

    actT = A.alloc("actT", [4, T], BF16)
    mean_t = A.alloc("mean_t", [512], F32)
    rstd_t = A.alloc("rstd_t", [512], F32)
    msq_t = A.alloc("msq_t", [512], F32)
    nrm_t = [A.alloc("nrm_t%d" % i, [512], F32) for i in range(2)]
    for nb in range(NB):
        ps1, pk1 = next_ps()
        ps2, pk2 = next_ps()

        def smm(e, ps1=ps1, ps2=ps2, nb=nb):
            ins = None
            for m in range(4):
                ins = e.matmul(ps1[:, :], lhsT=ones_b, rhs=cT[:, m, nb * 512:(nb + 1) * 512], start=(m == 0), stop=(m == 3))
            for m in range(4):
                ins = e.matmul(ps2[:, :], lhsT=ones_b, rhs=sqT[:, m, nb * 512:(nb + 1) * 512], start=(m == 0), stop=(m == 3))
            return ins
        P.add("pe", smm, reads=[("ones_b", 0)] + [("cT", m, nb) for m in range(4)] + [("sqT", m, nb) for m in range(4)],
              writes=[pk1, pk2])
        P.add("dve", (lambda e, ps1=ps1: e.tensor_scalar(out=mean_t, in0=ps1[:, :], scalar1=1.0 / 512, scalar2=None, op0=ALU.mult)),
              reads=[pk1], writes=[("mean_t", 0)])
        P.add("dve", lambda e: e.tensor_tensor(out=msq_t, in0=mean_t, in1=mean_t, op=ALU.mult),
              reads=[("mean_t", 0)], writes=[("msq_t", 0)])
        P.add("dve", (lambda e, ps2=ps2: e.scalar_tensor_tensor(out=rstd_t, in0=ps2[:, :], scalar=1.0 / 512, in1=msq_t,
                                                                op0=ALU.mult, op1=ALU.subtract)),
              reads=[pk2, ("msq_t", 0)], writes=[("rstd_t", 0)])
        P.add("act", lambda e: e.activation(out=rstd_t, in_=rstd_t, func=AF.Sqrt, bias=1e-5),
              reads=[("rstd_t", 0)], writes=[("rstd_t", 0)])
        P.add("dve", lambda e: e.reciprocal(out=rstd_t, in_=rstd_t), reads=[("rstd_t", 0)], writes=[("rstd_t", 0)])
        for m in range(4):
            nt = nrm_t[m % 2]
            ntk = ("nrm_t%d" % (m % 2), 0)
            P.add("dve", (lambda e, nt=nt, m=m, nb=nb: e.tensor_tensor(out=nt, in0=cT[:, m, nb * 512:(nb + 1) * 512], in1=mean_t,
                                                                      op=ALU.subtract)),
                  reads=[("cT", m, nb), ("mean_t", 0)], writes=[ntk])
            P.add("dve", (lambda e, nt=nt: e.tensor_tensor(out=nt, in0=nt, in1=rstd_t, op=ALU.mult)),
                  reads=[ntk, ("rstd_t", 0)], writes=[ntk])
            P.add("act", (lambda e, nt=nt, m=m, nb=nb: e.activation(
                out=actT[:, m, nb * 512:(nb + 1) * 512], in_=nt, func=AF.Silu, scale=clg[:, m:m + 1], bias=clb[:, m:m + 1])),
                reads=[ntk, ("clg", 0), ("clb", 0)], writes=[("actT", m, nb)])
    dbg_out("actT", actT, [("actT", m, nb) for m in range(4) for nb in range(NB)])
    A.free("cT"); A.free("sqT"); A.free("mean_t"); A.free("rstd_t"); A.free("msq_t"); A.free("nrm_t0"); A.free("nrm_t1")

    for jb in range(2):
        wg_, wgk = gA_blocks[jb] if jb in gA_blocks else load_wblock(3080 + jb * 512)
        for jj in range(4):
            j = jb * 4 + jj
            for nb in range(NB):
                psy, pky = next_ps()

                def ymm(e, psy=psy, j=j, nb=nb):
                    ins = None
                    for m in range(4):
                        ins = e.matmul(psy[:, :], lhsT=wco[:, m, j * 128:(j + 1) * 128], rhs=actT[:, m, nb * 512:(nb + 1) * 512],
                                       start=(m == 0), stop=(m == 3))
                    return ins
                P.add("pe", ymm, reads=[wcok] + [("actT", m, nb) for m in range(4)], writes=[pky])
                psg, pkg = proj_fm(wg_, wgk, jj, nb)
                sg, sgk = next_sgt()
                P.add("act", (lambda e, sg=sg, p=psg: e.activation(out=sg, in_=p[:, :], func=AF.Sigmoid)),
                      reads=[pkg], writes=[sgk])
                P.add("dve", (lambda e, sg=sg, p=psy, j=j, nb=nb: e.tensor_tensor(
                    out=merged[:, j, nb * 512:(nb + 1) * 512], in0=p[:, :], in1=sg, op=ALU.mult)),
                    reads=[pky, sgk], writes=[("merged", j, nb)])
    dbg_out("mergedA", merged, [("merged", j, nb) for j in range(8) for nb in range(NB)])
    A.free("actT")
    if stage <= 2:
        return finish()

    zt = A.alloc("zt", [D], BF16)
    P.add("pool", lambda e: e.memset(zt, 0.0), writes=[("zt", 0)])
    XSZ_KEYS = []
    for zi in range(NG * GROWS // 1024):
        P.add("sp", (lambda e, zi=zi: e.dma_start(out=XS[zi * 1024:(zi + 1) * 1024, :].rearrange("(n p) d -> p n d", p=128),
                                                  in_=zt.unsqueeze(1).to_broadcast([128, 8, D]))),
              reads=[("zt", 0)], writes=[("XSZ", zi)], dma=True, grp="xs_zero")
        XSZ_KEYS.append(("XSZ", zi))
    A.free("zt")
    PADQ = 4
    qkw = load_const("qkw", qkw_d, [8, 4])
    qkb = load_const("qkb", qkb_d, [8])
    bif = load_const("bif", bif_d, [8])
    mng = load_const("mng", mng_d, [4])
    qk_raw = A.alloc("qk_raw", [8, PADQ + T], BF16)
    for cc in range(8):
        P.add("pool", (lambda e, cc=cc: e.memset(qk_raw[:, cc, 0:PADQ], 0.0)), writes=[("qk_raw", cc, -1)])
    dg4 = A.alloc("dg4", [8, 4, 128], BF16)
    P.add("pool", lambda e: e.tensor_tensor(
        out=dg4.rearrange("p a b c -> p (a b) c"), in0=ident_b.unsqueeze(1).to_broadcast([128, 32, 128]),
        in1=qkw.rearrange("p a b -> p (a b)").unsqueeze(2).to_broadcast([128, 32, 128]), op=ALU.mult),
        reads=[("ident_b", 0), ("qkw", 0)], writes=[("dg4", 0)])
    for half in range(2):
        wq_, wqk = load_wblock(1024 + half * 512)
        for m in range(4):
            cc = half * 4 + m
            for nb in range(NB):
                ps, pk = proj_fm(wq_, wqk, m, nb)
                P.add("act", (lambda e, ps=ps, cc=cc, nb=nb: e.activation(
                    out=qk_raw[:, cc, PADQ + nb * 512:PADQ + (nb + 1) * 512], in_=ps[:, :], func=AF.Identity)),
                    reads=[pk], writes=[("qk_raw", cc, nb)])
    qkc = A.alloc("qkc", [8, T], BF16)
    for cc in range(8):
        for nb in range(NB):
            ps, pk = next_ps()

            def qmm(e, ps=ps, cc=cc, nb=nb):
                ins = None
                for k in range(4):
                    o = PADQ - 3 + nb * 512 + k
                    ins = e.matmul(ps[:, :], lhsT=dg4[:, cc, k, :], rhs=qk_raw[:, cc, o:o + 512], start=(k == 0), stop=(k == 3))
                return ins
            P.add("pe", qmm, reads=[("dg4", 0), ("qk_raw", cc, nb), ("qk_raw", cc, nb - 1)], writes=[pk])
            P.add("act", (lambda e, ps=ps, cc=cc, nb=nb: e.activation(
                out=qkc[:, cc, nb * 512:(nb + 1) * 512], in_=ps[:, :], func=AF.Silu, bias=qkb[:, cc:cc + 1])),
                reads=[pk, ("qkb", 0)], writes=[("qkc", cc, nb)])
    dbg_out("qkc", qkc, [("qkc", cc, nb) for cc in range(8) for nb in range(NB)])
    A.free("qk_raw"); A.free("dg4")
    if stage <= 2.2:
        return finish()

    wif = A.alloc("wif", [8, 8], BF16)
    wif_f = A.alloc("wif_f", [8, 8], F32)
    with nc.allow_non_contiguous_dma(reason="tiny gate-weight columns"):
        P.add("sp", lambda e: e.dma_start(out=wif_f, in_=win_v[:, :, 3072:3080]), writes=[("wif_f", 0)], dma=True, grp="c_wif")
    P.add("dve", lambda e: e.tensor_copy(out=wif, in_=wif_f), reads=[("wif_f", 0)], writes=[("wif", 0)])
    G = A.alloc("G", [NT, 8], F32)
    nlf = A.alloc("nlf", [NT, 4], F32)
    gtmp = A.alloc("gtmp", [NT, 4], F32)
    A_inv = A.alloc("A_inv", [NT, 4], F32)
    Bv = A.alloc("Bv", [NT, 4], F32)
    dec = A.alloc("dec", [NT, 4], F32)
    psg, pkg = hold_ps()

    def gmm(e):
        ins = None
        for ti in range(NT):
            for k in range(8):
                ins = e.matmul(psg[:, ti * 8:(ti + 1) * 8], lhsT=hT[:, k, ti * 128:(ti + 1) * 128], rhs=wif[:, k, :],
                               start=(k == 0), stop=(k == 7))
        return ins
    P.add("pe", gmm, reads=[("wif", 0)] + [("hT", c, nb) for c in range(8) for nb in range(NB)], writes=[pkg])
    P.add("dve", lambda e: e.tensor_tensor(out=G, in0=psg[:, 0:128].rearrange("p (a b) -> p a b", b=8),
                                           in1=bif.unsqueeze(1).to_broadcast([128, NT, 8]), op=ALU.add),
          reads=[pkg, ("bif", 0)], writes=[("G", 0)])
    release_ps(pkg)
    dbg_out("G", G, [("G", 0)])
    if stage <= 2.31:
        return finish()
    P.add("act", lambda e: e.activation(out=gtmp, in_=G[:, :, 4:8], func=AF.Exp, scale=-1.0),
          reads=[("G", 0)], writes=[("gtmp", 0)])
    P.add("act", lambda e: e.activation(out=nlf, in_=gtmp, func=AF.Ln, bias=1.0),
          reads=[("gtmp", 0)], writes=[("nlf", 0)])
    dbg_out("nlf", nlf, [("nlf", 0)])
    if stage <= 2.32:
        return finish()
    psc, pkc = next_ps()
    nlf2 = nlf.rearrange("p a b -> p (a b)")
    nl_hi = A.alloc("nl_hi", [64], BF16)
    nl_lo = A.alloc("nl_lo", [64], BF16)
    P.add("dve", lambda e: e.tensor_copy(out=nl_hi, in_=nlf2), reads=[("nlf", 0)], writes=[("nl_hi", 0)])
    P.add("dve", lambda e: e.tensor_tensor(out=nl_lo, in0=nlf2, in1=nl_hi, op=ALU.subtract),
          reads=[("nlf", 0), ("nl_hi", 0)], writes=[("nl_lo", 0)])

    def cmm2(e):
        e.matmul(psc[:, 0:64], lhsT=mask_ut, rhs=nl_hi, start=True, stop=False)
        e.matmul(psc[:, 0:64], lhsT=mask_ut, rhs=nl_lo, start=False, stop=True)
        e.matmul(psc[:, 64:128], lhsT=ones_b, rhs=nl_hi, start=True, stop=False)
        return e.matmul(psc[:, 64:128], lhsT=ones_b, rhs=nl_lo, start=False, stop=True)
    P.add("pe", cmm2, reads=[("mask_ut", 0), ("ones_b", 0), ("nl_hi", 0), ("nl_lo", 0)], writes=[pkc])
    if stage <= 2.33:
        P.add("dve", lambda e: e.tensor_copy(out=gtmp.rearrange("p a b -> p (a b)"), in_=psc[:, 0:64]), reads=[pkc], writes=[("gtmp", 0)])
        dbg_out("ncum", gtmp, [("gtmp", 0)])
        return finish()
    LNS = float(np.log(128.0 ** 0.5))
    cval = A.alloc("cval", [2], F32)
    P.add("pool", lambda e: e.memset(cval[:, 0:1], LNS), writes=[("cval", 0)])
    P.add("pool", lambda e: e.memset(cval[:, 1:2], -LNS), writes=[("cval", 1)])
    P.add("act", lambda e: e.activation(out=A_inv.rearrange("p a b -> p (a b)"), in_=psc[:, 0:64], func=AF.Exp, bias=cval[:, 0:1]),
          reads=[pkc, ("cval", 0)], writes=[("A_inv", 0)])
    A_ = A.alloc("A_", [NT, 4], F32)
    P.add("act", lambda e: e.activation(out=A_.rearrange("p a b -> p (a b)"), in_=psc[:, 0:64], func=AF.Exp, scale=-1.0, bias=cval[:, 1:2]),
          reads=[pkc, ("cval", 1)], writes=[("A_", 0)])
    if stage <= 2.34:
        dbg_out("A_", A_, [("A_", 0)])
        dbg_out("A_inv", A_inv, [("A_inv", 0)])
        return finish()
    P.add("dve", lambda e: e.tensor_tensor(out=gtmp, in0=psc[:, 0:64].rearrange("p (a b) -> p a b", b=4), in1=G[:, :, 0:4], op=ALU.add),
          reads=[pkc, ("G", 0), ("gtmp", 0)], writes=[("gtmp", 0)])
    P.add("act", lambda e: e.activation(out=Bv, in_=gtmp, func=AF.Exp), reads=[("gtmp", 0)], writes=[("Bv", 0)])
    if stage <= 2.36:
        dbg_out("Bv", Bv, [("Bv", 0)])
        return finish()
    P.add("act", lambda e: e.activation(out=dec.rearrange("p a b -> p (a b)"), in_=psc[:, 64:128], func=AF.Exp, scale=-1.0),
          reads=[pkc], writes=[("dec", 0)])
    dbg_out("Bv", Bv, [("Bv", 0)])
    dbg_out("A_", A_, [("A_", 0)])
    dbg_out("decay", dec, [("dec", 0)])

    if stage <= 2.4:
        return finish()
    ktok = A.alloc("ktok", [NT, 512], BF16)
    for c in range(NT):
        ps, pk = next_ps()
        pb = ps.bitcast(BF16)

        def ktr(e, pb=pb, c=c):
            ins = None
            for h in range(4):
                ins = e.transpose(out=pb[:, h * 128:(h + 1) * 128], in_=qkc[:, 4 + h, c * 128:(c + 1) * 128], identity=ident_b)
            return ins
        P.add("pe", ktr, reads=[("ident_b", 0)] + [("qkc", 4 + h, c // 4) for h in range(4)], writes=[pk])
        P.add("act", (lambda e, pb=pb, c=c: e.activation(out=ktok[:, c, :], in_=pb[:, 0:512], func=AF.Identity)),
              reads=[pk], writes=[("ktok", c)])

    vB = A.alloc("vB", [NT, 4, 129], BF16)
    wv_, wvk = load_wblock(2048)
    for c in range(NT):
        ps, pk = next_ps()

        def vmm(e, ps=ps, c=c):
            ins = None
            for k in range(8):
                ins = e.matmul(ps[:, :], lhsT=hT[:, k, c * 128:(c + 1) * 128], rhs=wv_[:, k, 0:512], start=(k == 0), stop=(k == 7))
            return ins
        P.add("pe", vmm, reads=[wvk] + hT_keys(c // 4), writes=[pk])
        P.add("dve", (lambda e, ps=ps, c=c: e.tensor_tensor(
            out=vB[:, c, :, 0:128], in0=ps[:, :].rearrange("p (a b) -> p a b", b=128),
            in1=Bv[:, c, :].unsqueeze(2).to_broadcast([128, 4, 128]), op=ALU.mult)),
            reads=[pk, ("Bv", 0)], writes=[("vB", c, 0)])
        P.add("dve", (lambda e, c=c: e.tensor_copy(out=vB[:, c, :, 128], in_=Bv[:, c, :])),
              reads=[("Bv", 0)], writes=[("vB", c, 1)])

    if stage <= 2.6:
        return finish()
    E = A.alloc("E", [4, 129], F32)
    Cb = [A.alloc("Cb%d" % i, [4, 129], BF16) for i in range(2)]
    sm = [A.alloc("sm%d" % i, [4, 128], BF16) for i in range(2)]
    hn = [A.alloc("hn%d" % i, [4, 128], BF16) for i in range(2)]
    st6 = A.alloc("st6", [4, 6], F32)
    mv = A.alloc("mv", [4, 2], F32)
    den = A.alloc("den", [4], F32)
    qq = A.alloc("qq", [4], F32)
    rstd = A.alloc("rstd", [4], F32)
    sgo = [A.alloc("sgo%d" % i, [4, 512], BF16) for i in range(2)]
    hmT = A.alloc("hmT", [4, T], BF16)
    wo_, wok = load_wblock(2560)
    CW = 256

    chs = {}

    def chunk_A(c):
        nb = c // 4
        cs = slice(c * 128, (c + 1) * 128)
        if c % 4 == 0:
            for h in range(4):
                ps, pk = proj_fm(wo_, wok, h, nb)
                P.add("act", (lambda e, ps=ps, h=h, nb=nb: e.activation(out=sgo[nb % 2][:, h, :], in_=ps[:, :], func=AF.Sigmoid)),
                      reads=[pk], writes=[("sgo%d" % (nb % 2), h)])
        pss, pks = next_ps()

        def smm2(e, pss=pss, cs=cs):
            ins = None
            for h in range(4):
                ins = e.matmul(pss[:, h * 128:(h + 1) * 128], lhsT=qkc[:, 4 + h, cs], rhs=qkc[:, h, cs], start=True, stop=True)
            return ins
        P.add("pe", smm2, reads=[("qkc", cc, nb) for cc in range(8)], writes=[pks])
        smc = sm[c % 2]
        smk = ("sm%d" % (c % 2), 0)
        P.add("dve", (lambda e, pss=pss, smc=smc: e.tensor_tensor(
            out=smc, in0=pss[:, :].rearrange("p (a b) -> p a b", b=128),
            in1=mask_ut.unsqueeze(1).to_broadcast([128, 4, 128]), op=ALU.mult)),
            reads=[pks, ("mask_ut", 0)], writes=[smk])
        pu = [hold_ps(), hold_ps()]

        def umm(e, pu=pu, c=c):
            ins = None
            for h in range(4):
                o = pu[h // 2][0][:, (h % 2) * CW:(h % 2) * CW + 129]
                ins = e.matmul(o, lhsT=ktok[:, c, h * 128:(h + 1) * 128], rhs=vB[:, c, h, :], start=True, stop=True)
            return ins
        P.add("pe", umm, reads=[("ktok", c), ("vB", c, 0), ("vB", c, 1)], writes=[pu[0][1], pu[1][1]])
        chs[c] = (smc, smk, pu)

    def chunk_B(c):
        nb = c // 4
        cs = slice(c * 128, (c + 1) * 128)
        smc, smk, pu = chs[c]
        pn = [next_ps(), next_ps()]

        def nmm(e, pn=pn, smc=smc, c=c, cs=cs):
            ins = None
            for h in range(4):
                o = pn[h // 2][0][:, (h % 2) * CW:(h % 2) * CW + 129]
                ins = e.matmul(o, lhsT=smc[:, h, :], rhs=vB[:, c, h, :], start=True, stop=(c == 0))
                if c > 0:
                    ins = e.matmul(o, lhsT=qkc[:, h, cs], rhs=Cb[(c - 1) % 2][:, h, :], start=False, stop=True)
            return ins
        rd = [smk, ("vB", c, 0), ("vB", c, 1)] + [("qkc", h, nb) for h in range(4)]
        if c > 0:
            rd += [("Cb%d" % ((c - 1) % 2), h) for h in range(4)]
        P.add("pe", nmm, reads=rd, writes=[pn[0][1], pn[1][1]])
        for h in range(4):
            src = pu[h // 2][0][:, (h % 2) * CW:(h % 2) * CW + 129]
            if c == 0:
                P.add("dve", (lambda e, src=src, h=h: e.tensor_copy(out=E[:, h, :], in_=src)),
                      reads=[pu[h // 2][1]], writes=[("E", h)])
            else:
                P.add("dve", (lambda e, src=src, h=h, c=c: e.scalar_tensor_tensor(
                    out=E[:, h, :], in0=E[:, h, :], scalar=dec[:, c - 1, h:h + 1], in1=src, op0=ALU.mult, op1=ALU.add)),
                    reads=[pu[h // 2][1], ("E", h), ("dec", 0)], writes=[("E", h)])
            if c < NT - 1:
                P.add("act", (lambda e, h=h, c=c: e.activation(out=Cb[c % 2][:, h, :], in_=E[:, h, :], func=AF.Identity,
                                                               scale=dec[:, c, h:h + 1])),
                      reads=[("E", h), ("dec", 0)], writes=[("Cb%d" % (c % 2), h)])
        release_ps(pu[0][1]); release_ps(pu[1][1])
        chs[c] = pn

    def chunk_C(c):
        nb = c // 4
        cs = slice(c * 128, (c + 1) * 128)
        pn = chs[c]
        for h in range(4):
            src = pn[h // 2][0][:, (h % 2) * CW:(h % 2) * CW + 128]
            P.add("dve", (lambda e, src=src, h=h: e.bn_stats(out=st6[:, h, :], in_=src)),
                  reads=[pn[h // 2][1]], writes=[("st6", h)])
            P.add("dve", (lambda e, h=h: e.bn_aggr(out=mv[:, h, :], in_=st6[:, h, :])),
                  reads=[("st6", h)], writes=[("mv", h)])
        for b2 in range(2):
            dsrc = pn[b2][0][:, 0:512].rearrange("p (a b) -> p a b", b=CW)[:, :, 128]
            P.add("dve", (lambda e, dsrc=dsrc, b2=b2, c=c: e.tensor_tensor(
                out=den[:, 2 * b2:2 * b2 + 2], in0=dsrc, in1=A_[:, c, 2 * b2:2 * b2 + 2], op=ALU.mult)),
                reads=[pn[b2][1], ("A_", 0)], writes=[("den", b2)])
        P.add("dve", lambda e: e.scalar_tensor_tensor(out=den, in0=den, scalar=-1.0, in1=den, op0=ALU.mult, op1=ALU.max),
              reads=[("den", 0), ("den", 1)], writes=[("den", 0), ("den", 1)])
        P.add("dve", lambda e: e.tensor_scalar(out=den, in0=den, scalar1=1.0, scalar2=None, op0=ALU.max),
              reads=[("den", 0), ("den", 1)], writes=[("den", 0), ("den", 1)])
        P.add("dve", (lambda e, c=c: e.tensor_tensor(out=qq, in0=den, in1=A_inv[:, c, :], op=ALU.mult)),
              reads=[("den", 0), ("den", 1), ("A_inv", 0)], writes=[("qq", 0)])
        P.add("dve", lambda e: e.tensor_tensor(out=qq, in0=qq, in1=qq, op=ALU.mult), reads=[("qq", 0)], writes=[("qq", 0)])
        P.add("dve", lambda e: e.scalar_tensor_tensor(out=rstd, in0=qq, scalar=1e-5, in1=mv[:, :, 1], op0=ALU.mult, op1=ALU.add),
              reads=[("qq", 0)] + [("mv", h) for h in range(4)], writes=[("rstd", 0)])
        P.add("act", lambda e: e.activation(out=rstd, in_=rstd, func=AF.Sqrt), reads=[("rstd", 0)], writes=[("rstd", 0)])
        P.add("dve", lambda e: e.reciprocal(out=rstd, in_=rstd), reads=[("rstd", 0)], writes=[("rstd", 0)])
        hnc = hn[c % 2]
        hnk = "hn%d" % (c % 2)
        for h in range(4):
            src = pn[h // 2][0][:, (h % 2) * CW:(h % 2) * CW + 128]
            P.add("dve", (lambda e, src=src, h=h, hnc=hnc: e.tensor_scalar(
                out=hnc[:, h, :], in0=src, scalar1=mv[:, h, 0:1], scalar2=rstd[:, h:h + 1], op0=ALU.subtract, op1=ALU.mult)),
                reads=[pn[h // 2][1], ("mv", h), ("rstd", 0)], writes=[(hnk, h)])
        pt, pkt = next_ps()
        ptb = pt.bitcast(BF16)

        def htr(e, ptb=ptb, hnc=hnc):
            ins = None
            for h in range(4):
                ins = e.transpose(out=ptb[:, h * 128:(h + 1) * 128], in_=hnc[:, h, :], identity=ident_b)
            return ins
        P.add("pe", htr, reads=[("ident_b", 0)] + [(hnk, h) for h in range(4)], writes=[pkt])
        P.add("dve", (lambda e, ptb=ptb, c=c, nb=nb, cs=cs: e.tensor_tensor(
            out=hmT[:, :, cs], in0=ptb[:, 0:512].rearrange("p (a b) -> p a b", b=128),
            in1=sgo[nb % 2][:, :, (c % 4) * 128:(c % 4 + 1) * 128], op=ALU.mult)),
            reads=[pkt] + [("sgo%d" % (nb % 2), h) for h in range(4)], writes=[("hmT", c)])

    chunk_A(0)
    for c in range(NT):
        if c + 1 < NT:
            chunk_A(c + 1)
        chunk_B(c)
        chunk_C(c)
    dbg_out("hmT", hmT, [("hmT", c) for c in range(NT)])
    for nm in ("qkc", "wif", "wif_f", "nl_hi", "nl_lo", "G", "nlf", "gtmp", "A_inv", "Bv", "dec", "A_", "ktok", "vB", "E", "Cb0", "Cb1", "sm0", "sm1",
               "hn0", "hn1", "st6", "mv", "den", "qq", "rstd", "sgo0", "sgo1"):
        A.free(nm)

    wmo = load_cast("wmo", wmo_d.rearrange("(c p) n -> p c n", p=128), [4, D])
    for h in range(4):
        P.add("dve", (lambda e, h=h: e.tensor_scalar(out=wmo[:, h, :], in0=wmo[:, h, :], scalar1=mng[:, h:h + 1], scalar2=None,
                                                     op0=ALU.mult)),
              reads=[("wmo", 0), ("wmo", 1 + h), ("mng", 0)], writes=[("wmo", 1 + h)])
    mtmp = [A.alloc("mtmp%d" % i, [512], BF16) for i in range(2)]
    for jb in range(2):
        wg_, wgk = load_wblock(4104 + jb * 512)
        for jj in range(4):
            j = jb * 4 + jj
            for nb in range(NB):
                psy, pky = next_ps()

                def ymm2(e, psy=psy, j=j, nb=nb):
                    ins = None
                    for h in range(4):
                        ins = e.matmul(psy[:, :], lhsT=wmo[:, h, j * 128:(j + 1) * 128], rhs=hmT[:, h, nb * 512:(nb + 1) * 512],
                                       start=(h == 0), stop=(h == 3))
                    return ins
                P.add("pe", ymm2, reads=[("wmo", 1 + h) for h in range(4)] + [("hmT", c) for c in range(nb * 4, nb * 4 + 4)],
                      writes=[pky])
                psg2, pkg2 = proj_fm(wg_, wgk, jj, nb)
                sg, sgk = next_sgt()
                P.add("act", (lambda e, sg=sg, p=psg2: e.activation(out=sg, in_=p[:, :], func=AF.Sigmoid)),
                      reads=[pkg2], writes=[sgk])
                mt = mtmp[(j * NB + nb) % 2]
                mtk = ("mtmp%d" % ((j * NB + nb) % 2), 0)
                P.add("dve", (lambda e, sg=sg, p=psy, mt=mt: e.tensor_tensor(out=mt, in0=p[:, :], in1=sg, op=ALU.mult)),
                      reads=[pky, sgk], writes=[mtk])
                P.add("dve", (lambda e, mt=mt, j=j, nb=nb: e.tensor_tensor(
                    out=merged[:, j, nb * 512:(nb + 1) * 512], in0=merged[:, j, nb * 512:(nb + 1) * 512], in1=mt, op=ALU.add)),
                    reads=[mtk, ("merged", j, nb)], writes=[("merged", j, nb)])
    dbg_out("merged", merged, [("merged", j, nb) for j in range(8) for nb in range(NB)])
    for nm in ("hmT", "wmo", "mtmp0", "mtmp1", "sgt0", "sgt1", "hT", "wblk0", "wblk1", "wblk2"):
        A.free(nm)
    if stage <= 3:
        return finish()

    dgf = A.alloc("dgf", [128], F32)
    dgh = A.alloc("dgh", [128], BF16)
    dgl = A.alloc("dgl", [128], BF16)

    def row_bcast(name, col0, src=None, srckey=("modT", 1)):
        src = modT if src is None else src
        row = A.alloc(name, [D], F32)
        banks = [next_ps(), next_ps()]
        for j in range(8):
            P.add("dve", (lambda e, j=j: e.tensor_scalar(out=dgf, in0=ident_f, scalar1=src[:, col0 + j:col0 + j + 1], scalar2=None,
                                                         op0=ALU.mult)),
                  reads=[("ident_f", 0), srckey], writes=[("dgf", 0)])
            P.add("dve", lambda e: e.tensor_copy(out=dgh, in_=dgf), reads=[("dgf", 0)], writes=[("dgh", 0)])
            P.add("dve", lambda e: e.tensor_tensor(out=dgl, in0=dgf, in1=dgh, op=ALU.subtract),
                  reads=[("dgf", 0), ("dgh", 0)], writes=[("dgl", 0)])
            bk, bkk = banks[j // 4]

            def bmm(e, bk=bk, j=j):
                o = bk[:, (j % 4) * 128:(j % 4 + 1) * 128]
                e.matmul(o, lhsT=ones_b, rhs=dgh, start=True, stop=False)
                return e.matmul(o, lhsT=ones_b, rhs=dgl, start=False, stop=True)
            P.add("pe", bmm, reads=[("ones_b", 0), ("dgh", 0), ("dgl", 0)], writes=[bkk])
        for b2 in range(2):
            bk, bkk = banks[b2]
            P.add("act", (lambda e, bk=bk, b2=b2: e.activation(out=row[:, b2 * 512:(b2 + 1) * 512], in_=bk[:, :], func=AF.Identity)),
                  reads=[bkk], writes=[(name, b2)])
        return row

    gt1row = row_bcast("gt1row", 16)
    a2row = row_bcast("a2row", 0, src=a2, srckey=("a2", 0))
    sh2row = row_bcast("sh2row", 24)
    gt2row = row_bcast("gt2row", 40)
    wout = load_cast("wout", wout_d.rearrange("(c p) n -> p c n", p=128), [8, D])
    for k in range(8):
        P.add("dve", (lambda e, k=k: e.tensor_tensor(out=wout[:, k, :], in0=wout[:, k, :], in1=gt1row, op=ALU.mult)),
              reads=[("wout", 0), ("wout", 1 + k), ("gt1row", 0), ("gt1row", 1)], writes=[("wout", 1 + k)])
    x1 = A.alloc("x1", [NT, D], F32)
    for ti in range(NT):
        P.add("sp", (lambda e, ti=ti: e.dma_start(out=x1[:, ti, :], in_=x_d[ti * 128:(ti + 1) * 128, :])),
              writes=[("x1", ti)], dma=True, grp="x1ld%d" % ti)
    wr_f = A.alloc("wr_f", [8, 36], F32)
    wr_b = A.alloc("wr_b", [8, 36], BF16)
    with nc.allow_non_contiguous_dma(reason="small router weight rows"):
        P.add("sp", lambda e: e.dma_start(out=wr_f, in_=wr_d.rearrange("(c p) n -> p c n", p=128)), writes=[("wr_f", 0)],
              dma=True, grp="c_wr")
    P.add("dve", lambda e: e.tensor_copy(out=wr_b, in_=wr_f), reads=[("wr_f", 0)], writes=[("wr_b", 0)])
    brt = load_const("brt", br_d, [36])
    h2tok = A.alloc("h2tok", [NT, D], BF16)
    xn2 = [A.alloc("xn2_%d" % i, [D], F32) for i in range(2)]
    h2T = [A.alloc("h2T%d" % i, [8, 128], BF16) for i in range(2)]
    ss2 = A.alloc("ss2", [NT], F32)
    rs2 = A.alloc("rs2", [NT], F32)
    psr = [hold_ps(), hold_ps()]
    def emit_p5(ti):
        s2 = ti % 2
        P.add("act", (lambda e, ti=ti: e.activation(out=junk, in_=x1[:, ti, :], func=AF.Square, accum_out=ss2[:, ti:ti + 1])),
              reads=[("x1", ti)], writes=[("junk", 0), ("ss2", ti)])
        P.add("act", (lambda e, ti=ti: e.activation(out=rs2[:, ti:ti + 1], in_=ss2[:, ti:ti + 1], func=AF.Sqrt, scale=1.0 / D, bias=1e-6)),
              reads=[("ss2", ti)], writes=[("rs2", ti)])
        P.add("dve", (lambda e, ti=ti: e.reciprocal(out=rs2[:, ti:ti + 1], in_=rs2[:, ti:ti + 1])),
              reads=[("rs2", ti)], writes=[("rs2", ti)])
        P.add("act", (lambda e, ti=ti, s2=s2: e.activation(out=xn2[s2], in_=x1[:, ti, :], func=AF.Identity, scale=rs2[:, ti:ti + 1])),
              reads=[("x1", ti), ("rs2", ti)], writes=[("xn2_%d" % s2, 0)])
        P.add("dve", (lambda e, s2=s2: e.tensor_tensor(out=xn2[s2], in0=xn2[s2], in1=a2row, op=ALU.mult)),
              reads=[("xn2_%d" % s2, 0), ("a2row", 0), ("a2row", 1)], writes=[("xn2_%d" % s2, 0)])
        P.add("dve", (lambda e, s2=s2, ti=ti: e.tensor_tensor(out=h2tok[:, ti, :], in0=xn2[s2], in1=sh2row, op=ALU.add)),
              reads=[("xn2_%d" % s2, 0), ("sh2row", 0), ("sh2row", 1)], writes=[("h2tok", ti)])
        pt, pkt = next_ps()
        ptb = pt.bitcast(BF16)

        def h2tr(e, ptb=ptb, ti=ti):
            ins = None
            for c in range(8):
                ins = e.transpose(out=ptb[:, c * 128:(c + 1) * 128], in_=h2tok[:, ti, c * 128:(c + 1) * 128], identity=ident_b)
            return ins
        P.add("pe", h2tr, reads=[("ident_b", 0), ("h2tok", ti)], writes=[pkt])
        P.add("act", (lambda e, ptb=ptb, s2=s2: e.activation(out=h2T[s2].rearrange("p a b -> p (a b)"), in_=ptb[:, 0:1024], func=AF.Identity)),
              reads=[pkt], writes=[("h2T%d" % s2, 0)])
        bk, bkk = psr[ti // 8]

        def rmm(e, bk=bk, ti=ti, s2=s2):
            ins = None
            o = bk[:, (ti % 8) * 36:(ti % 8 + 1) * 36]
            for k in range(8):
                ins = e.matmul(o, lhsT=h2T[s2][:, k, :], rhs=wr_b[:, k, :], start=(k == 0), stop=(k == 7))
            return ins
        P.add("pe", rmm, reads=[("h2T%d" % s2, 0), ("wr_b", 0)], writes=[bkk])

    def emit_p4(ti):
        for half in range(2):
            ps, pk = next_ps()

            def omm(e, ps=ps, ti=ti, half=half):
                ins = None
                for k in range(8):
                    ins = e.matmul(ps[:, :], lhsT=merged[:, k, ti * 128:(ti + 1) * 128], rhs=wout[:, k, half * 512:(half + 1) * 512],
                                   start=(k == 0), stop=(k == 7))
                return ins
            P.add("pe", omm, reads=[("wout", 1 + k) for k in range(8)] + [("merged", k, ti // 4) for k in range(8)], writes=[pk])
            P.add("dve", (lambda e, ps=ps, ti=ti, half=half: e.tensor_tensor(
                out=x1[:, ti, half * 512:(half + 1) * 512], in0=x1[:, ti, half * 512:(half + 1) * 512], in1=ps[:, :], op=ALU.add)),
                reads=[pk, ("x1", ti)], writes=[("x1", ti)])

    emit_p4(0)
    for ti in range(NT):
        if ti + 1 < NT:
            emit_p4(ti + 1)
        emit_p5(ti)
    dbg_out("x1", x1, [("x1", ti) for ti in range(NT)])
    A.free("merged"); A.free("wout"); A.free("gt1row")

    Lg = A.alloc("Lg", [NT, 36], F32)
    for b2 in range(2):
        bk, bkk = psr[b2]
        P.add("dve", (lambda e, bk=bk, b2=b2: e.tensor_tensor(
            out=Lg[:, b2 * 8:(b2 + 1) * 8, :], in0=bk[:, 0:288].rearrange("p (a b) -> p a b", b=36),
            in1=brt.unsqueeze(1).to_broadcast([128, 8, 36]), op=ALU.add)),
            reads=[bkk, ("brt", 0)], writes=[("Lg", b2)])
    release_ps(psr[0][1]); release_ps(psr[1][1])
    dbg_out("Lg", Lg, [("Lg", 0), ("Lg", 1)])
    dbg_out("h2tok", h2tok, [("h2tok", ti) for ti in range(NT)])
    for nm in ("xn2_0", "xn2_1", "h2T0", "h2T1", "a2row", "sh2row", "wr_f", "wr_b"):
        A.free(nm)
    if stage <= 5:
        return finish()

    NCHK0 = NG - 15
    def T_(name, shape, dt=F32):
        return A.alloc(name, shape, dt)
    LK = [("Lg", 0), ("Lg", 1)]
    lg = Lg[:, :, 0:4]
    le = Lg[:, :, 4:36]
    gmax = T_("gmax", [NT])
    G1h = T_("G1h", [NT, 4])
    egs = T_("egs", [NT, 4])
    p_g = T_("p_g", [NT])
    P.add("dve", lambda e: e.tensor_reduce(out=gmax, in_=lg, axis=AX.X, op=ALU.max), reads=LK, writes=[("gmax", 0)])
    gmb = gmax.unsqueeze(2).to_broadcast([128, NT, 4])
    P.add("dve", lambda e: e.tensor_tensor(out=G1h, in0=lg, in1=gmb, op=ALU.is_equal), reads=LK + [("gmax", 0)], writes=[("G1h", 0)])
    P.add("dve", lambda e: e.tensor_tensor(out=egs, in0=lg, in1=gmb, op=ALU.subtract), reads=LK + [("gmax", 0)], writes=[("egs", 0)])
    P.add("act", lambda e: e.activation(out=egs, in_=egs, func=AF.Exp), reads=[("egs", 0)], writes=[("egs", 0)])
    P.add("dve", lambda e: e.tensor_reduce(out=p_g, in_=egs, axis=AX.X, op=ALU.add), reads=[("egs", 0)], writes=[("p_g", 0)])
    P.add("dve", lambda e: e.reciprocal(out=p_g, in_=p_g), reads=[("p_g", 0)], writes=[("p_g", 0)])
    tmp32 = T_("tmp32", [NT, 32])
    lsel = T_("lsel", [NT, 8])
    P.add("dve", lambda e: e.tensor_tensor(
        out=tmp32.rearrange("p t (g j) -> p t g j", j=8), in0=le.rearrange("p t (g j) -> p t g j", j=8),
        in1=G1h.unsqueeze(3).to_broadcast([128, NT, 4, 8]), op=ALU.mult),
        reads=LK + [("G1h", 0)], writes=[("tmp32", 0)])
    P.add("dve", lambda e: e.tensor_reduce(out=lsel, in_=tmp32.rearrange("p t (g j) -> p t j g", j=8), axis=AX.X, op=ALU.add),
          reads=[("tmp32", 0)], writes=[("lsel", 0)])
    m1 = T_("m1", [NT])
    m2 = T_("m2", [NT])
    E1 = T_("E1", [NT, 8])
    E2 = T_("E2", [NT, 8])
    ls2 = T_("ls2", [NT, 8])
    P.add("dve", lambda e: e.tensor_reduce(out=m1, in_=lsel, axis=AX.X, op=ALU.max), reads=[("lsel", 0)], writes=[("m1", 0)])
    P.add("dve", lambda e: e.tensor_tensor(out=E1, in0=lsel, in1=m1.unsqueeze(2).to_broadcast([128, NT, 8]), op=ALU.is_equal),
          reads=[("lsel", 0), ("m1", 0)], writes=[("E1", 0)])
    P.add("dve", lambda e: e.scalar_tensor_tensor(out=ls2.rearrange("p a b -> p (a b)"), in0=E1.rearrange("p a b -> p (a b)"),
                                                  scalar=-1e30, in1=lsel.rearrange("p a b -> p (a b)"), op0=ALU.mult, op1=ALU.add),
          reads=[("E1", 0), ("lsel", 0)], writes=[("ls2", 0)])
    P.add("dve", lambda e: e.tensor_reduce(out=m2, in_=ls2, axis=AX.X, op=ALU.max), reads=[("ls2", 0)], writes=[("m2", 0)])
    P.add("dve", lambda e: e.tensor_tensor(out=E2, in0=ls2, in1=m2.unsqueeze(2).to_broadcast([128, NT, 8]), op=ALU.is_equal),
          reads=[("ls2", 0), ("m2", 0)], writes=[("E2", 0)])
    w1 = T_("w1", [NT])
    w2 = T_("w2", [NT])
    P.add("dve", lambda e: e.tensor_tensor(out=w2, in0=m1, in1=m2, op=ALU.subtract), reads=[("m1", 0), ("m2", 0)], writes=[("w2", 0)])
    P.add("act", lambda e: e.activation(out=w1, in_=w2, func=AF.Sigmoid), reads=[("w2", 0)], writes=[("w1", 0)])
    P.add("dve", lambda e: e.tensor_tensor(out=w1, in0=w1, in1=p_g, op=ALU.mult), reads=[("w1", 0), ("p_g", 0)], writes=[("w1", 0)])
    P.add("dve", lambda e: e.tensor_tensor(out=w2, in0=p_g, in1=w1, op=ALU.subtract), reads=[("w1", 0), ("p_g", 0), ("w2", 0)], writes=[("w2", 0)])
    A1 = T_("A1", [NT, 32])
    A2 = T_("A2", [NT, 32])
    A12b = T_("A12b", [NT, 32], BF16)
    for g in range(4):
        gb = G1h[:, :, g].unsqueeze(2).to_broadcast([128, NT, 8])
        P.add("dve", (lambda e, g=g, gb=gb: e.tensor_tensor(out=A1[:, :, g * 8:(g + 1) * 8], in0=E1, in1=gb, op=ALU.mult)),
              reads=[("E1", 0), ("G1h", 0)], writes=[("A1", g)])
        P.add("dve", (lambda e, g=g, gb=gb: e.tensor_tensor(out=A2[:, :, g * 8:(g + 1) * 8], in0=E2, in1=gb, op=ALU.mult)),
              reads=[("E2", 0), ("G1h", 0)], writes=[("A2", g)])
    AK = [("A1", g) for g in range(4)] + [("A2", g) for g in range(4)]
    P.add("dve", lambda e: e.tensor_tensor(out=A12b, in0=A1, in1=A2, op=ALU.add), reads=AK, writes=[("A12b", 0)])
    lstrict = T_("lstrict", [128], BF16)
    lsf = T_("lsf", [128], F32)
    P.add("pool", lambda e: e.memset(lsf, 1.0), writes=[("lsf", 0)])
    P.add("pool", lambda e: e.affine_select(out=lsf, in_=lsf, pattern=[[1, 128]], compare_op=ALU.is_ge, fill=0.0, base=-1,
                                             channel_multiplier=-1), reads=[("lsf", 0)], writes=[("lsf", 0)])
    P.add("pool", lambda e: e.tensor_copy(out=lstrict, in_=lsf), reads=[("lsf", 0)], writes=[("lstrict", 0)])
    psw, pkw = next_ps()
    pst_, pkt_ = next_ps()
    A12f = A12b.rearrange("p a b -> p (a b)")
    P.add("pe", lambda e: e.matmul(psw[:, :], lhsT=lstrict, rhs=A12f, start=True, stop=True),
          reads=[("lstrict", 0), ("A12b", 0)], writes=[pkw])
    P.add("pe", lambda e: e.matmul(pst_[:, :], lhsT=ones_b, rhs=A12f, start=True, stop=True),
          reads=[("ones_b", 0), ("A12b", 0)], writes=[pkt_])
    carry = T_("carry", [NT + 1, 32])
    P.add("dve", lambda e: e.memset(carry[:, 0, :], 0.0), writes=[("carry", 0)])
    for ti in range(NT):
        P.add("dve", (lambda e, ti=ti: e.tensor_tensor(out=carry[:, ti + 1, :], in0=carry[:, ti, :], in1=pst_[:, ti * 32:(ti + 1) * 32],
                                                      op=ALU.add)),
              reads=[("carry", ti), pkt_], writes=[("carry", ti + 1)])
    counts = carry[:, NT, :]
    CK = [("carry", ti) for ti in range(NT + 1)]
    thr = T_("thr", [64])
    thr_i = T_("thr_i", [64], I32)
    P.add("pool", lambda e: e.iota(thr_i, pattern=[[1, 64]], base=0, channel_multiplier=0), writes=[("thr_i", 0)])
    P.add("pool", lambda e: e.tensor_copy(out=thr, in_=thr_i), reads=[("thr_i", 0)], writes=[("thr", 0)])
    thr128 = T_("thr128", [8])
    P.add("pool", lambda e: e.tensor_scalar(out=thr128, in0=thr[:, 0:8], scalar1=float(GROWS), scalar2=None, op0=ALU.mult),
          reads=[("thr", 0)], writes=[("thr128", 0)])
    cmp1 = T_("cmp1", [32, 8])
    ngrp = T_("ngrp", [32])
    P.add("dve", lambda e: e.tensor_tensor(out=cmp1, in0=counts.unsqueeze(2).to_broadcast([128, 32, 8]),
                                           in1=thr128.unsqueeze(1).to_broadcast([128, 32, 8]), op=ALU.is_gt),
          reads=CK + [("thr128", 0)], writes=[("cmp1", 0)])
    P.add("dve", lambda e: e.tensor_reduce(out=ngrp, in_=cmp1, axis=AX.X, op=ALU.add), reads=[("cmp1", 0)], writes=[("ngrp", 0)])
    cs = [T_("cs0", [32]), T_("cs1", [32])]
    src, srck = ngrp, ("ngrp", 0)
    for si, sh in enumerate((1, 2, 4, 8, 16)):
        dst = cs[si % 2]
        dk = ("cs%d" % (si % 2),)
        P.add("dve", (lambda e, dst=dst, src=src, sh=sh: e.tensor_copy(out=dst[:, 0:sh], in_=src[:, 0:sh])),
              reads=[srck], writes=[dk + (0,)])
        P.add("dve", (lambda e, dst=dst, src=src, sh=sh: e.tensor_tensor(out=dst[:, sh:32], in0=src[:, sh:32], in1=src[:, 0:32 - sh],
                                                                        op=ALU.add)),
              reads=[srck], writes=[dk + (1,)])
        src, srck = dst, dk + (1,)
        if si > 0:
            pass
    pend = src
    PK = [("cs0", 0), ("cs0", 1), ("cs1", 0), ("cs1", 1)]
    pstart = T_("pstart", [32])
    P.add("dve", lambda e: e.tensor_tensor(out=pstart, in0=pend, in1=ngrp, op=ALU.subtract), reads=PK + [("ngrp", 0)],
          writes=[("pstart", 0)])
    P.add("dve", lambda e: e.tensor_scalar(out=pstart, in0=pstart, scalar1=float(GROWS), scalar2=None, op0=ALU.mult),
          reads=[("pstart", 0)], writes=[("pstart", 0)])
    cmp2 = T_("cmp2", [NG, 32])
    grpf = T_("grpf", [NG])
    grpi = T_("grpi", [NG], I32)
    P.add("dve", lambda e: e.tensor_tensor(out=cmp2, in0=pend.unsqueeze(1).to_broadcast([128, NG, 32]),
                                           in1=thr[:, 0:NG].unsqueeze(2).to_broadcast([128, NG, 32]), op=ALU.is_le),
          reads=PK + [("thr", 0)], writes=[("cmp2", 0)])
    P.add("dve", lambda e: e.tensor_reduce(out=grpf, in_=cmp2, axis=AX.X, op=ALU.add), reads=[("cmp2", 0)], writes=[("grpf", 0)])
    P.add("dve", lambda e: e.tensor_scalar(out=grpf, in0=grpf, scalar1=31.0, scalar2=None, op0=ALU.min),
          reads=[("grpf", 0)], writes=[("grpf", 0)])
    P.add("dve", lambda e: e.tensor_copy(out=grpi, in_=grpf), reads=[("grpf", 0)], writes=[("grpi", 0)])
    pidx_i = T_("pidx_i", [1], I32)
    pidx = T_("pidx", [1])
    idxf = T_("idxf", [NG])
    inval = T_("inval", [NG])
    idxw = T_("idxw", [NG], I32)
    idxs = T_("idxs", [NG], I32)
    P.add("pool", lambda e: e.iota(pidx_i, pattern=[[0, 1]], base=0, channel_multiplier=1), writes=[("pidx_i", 0)])
    P.add("pool", lambda e: e.tensor_copy(out=pidx, in_=pidx_i), reads=[("pidx_i", 0)], writes=[("pidx", 0)])
    P.add("dve", lambda e: e.tensor_scalar(out=idxf, in0=grpf, scalar1=128.0, scalar2=pidx[:, 0:1], op0=ALU.mult, op1=ALU.add),
          reads=[("grpf", 0), ("pidx", 0)], writes=[("idxf", 0)])
    P.add("dve", lambda e: e.tensor_scalar(out=inval, in0=thr[:, 0:NG], scalar1=pend[:, 31:32], scalar2=None, op0=ALU.is_ge),
          reads=PK + [("thr", 0)], writes=[("inval", 0)])
    P.add("dve", lambda e: e.tensor_copy(out=idxw, in_=idxf), reads=[("idxf", 0)], writes=[("idxw", 0)])
    P.add("dve", lambda e: e.scalar_tensor_tensor(out=idxf, in0=inval, scalar=8192.0, in1=idxf, op0=ALU.mult, op1=ALU.add),
          reads=[("inval", 0), ("idxf", 0), ("idxw", 0)], writes=[("idxf", 0)])
    P.add("dve", lambda e: e.tensor_copy(out=idxs, in_=idxf), reads=[("idxf", 0)], writes=[("idxs", 0)])
    stg = {wn: A.alloc("stg_" + wn, [4096], F32) for wn in ("wg", "wu", "wd")}
    for (wsrc_, wn_) in ((weg_d, "wg"), (weu_d, "wu"), (wed_d, "wd")):
        P.add("pool", (lambda e, wsrc_=wsrc_, wn_=wn_: e.indirect_dma_start(
            out=stg[wn_], out_offset=None, in_=wsrc_, in_offset=bass.IndirectOffsetOnAxis(ap=idxw[:, 0:1], axis=0))),
            reads=[("idxw", 0)], writes=[("stg_" + wn_, 0)], dma=True, grp="stg_" + wn_)
    slot = T_("slot", [NT, 32])
    P.add("dve", lambda e: e.tensor_tensor(out=slot, in0=psw[:, :].rearrange("p (a b) -> p a b", b=32), in1=carry[:, 0:NT, :], op=ALU.add),
          reads=[pkw] + CK, writes=[("slot", 0)])
    P.add("dve", lambda e: e.tensor_tensor(out=slot, in0=slot, in1=pstart.unsqueeze(1).to_broadcast([128, NT, 32]), op=ALU.add),
          reads=[("slot", 0), ("pstart", 0)], writes=[("slot", 0)])
    dstf = T_("dstf", [2, NT])
    dsti = T_("dsti", [2, NT], I32)
    for q, (Aq, qk) in enumerate(((A1, "A1"), (A2, "A2"))):
        P.add("dve", (lambda e, Aq=Aq: e.tensor_tensor(out=tmp32, in0=Aq, in1=slot, op=ALU.mult)),
              reads=[(qk, g) for g in range(4)] + [("slot", 0), ("tmp32", 0)], writes=[("tmp32", 0)])
        P.add("dve", (lambda e, q=q: e.tensor_reduce(out=dstf[:, q, :], in_=tmp32, axis=AX.X, op=ALU.add)),
              reads=[("tmp32", 0)], writes=[("dstf", q)])
    P.add("dve", lambda e: e.tensor_copy(out=dsti, in_=dstf), reads=[("dstf", 0), ("dstf", 1)], writes=[("dsti", 0)])
    dbg_out("dstf", dstf, [("dstf", 0), ("dstf", 1)])
    dbg_out("grpf", grpf, [("grpf", 0)])
    dbg_out("w1", w1, [("w1", 0)])
    dbg_out("w2", w2, [("w2", 0)])
    for nm in ("gmax", "G1h", "egs", "p_g", "tmp32", "lsel", "m1", "m2", "E1", "E2", "ls2", "A1", "A2", "A12b", "lstrict", "lsf",
               "carry", "thr", "thr_i", "thr128", "cmp1", "ngrp", "cs0", "cs1", "pstart", "slot", "cmp2", "Lg"):
        A.free(nm)
    if stage <= 5.5:
        return finish()

    for ti in range(NT):
        for q in range(2):
            P.add("pool", (lambda e, ti=ti, q=q: e.indirect_dma_start(
                out=XS, out_offset=bass.IndirectOffsetOnAxis(ap=dsti[:, q, ti:ti + 1], axis=0),
                in_=h2tok[:, ti, :], in_offset=None)),
                reads=[("h2tok", ti), ("dsti", 0)] + XSZ_KEYS, writes=[("XS", ti, q)], dma=True, grp="xs_sc")
    XS_KEYS = [("XS", ti, q) for ti in range(NT) for q in range(2)]
    A.free("h2tok")
    NS = 2
    wgs = [A.alloc("wg%d" % i, [8, 512], BF16) for i in range(NS)]
    wus = [A.alloc("wu%d" % i, [8, 512], BF16) for i in range(NS)]
    wds = [A.alloc("wd%d" % i, [4, D], BF16) for i in range(NS)]
    xgt = [A.alloc("xgt%d" % i, [D], BF16) for i in range(2)]
    xgT = [A.alloc("xgT%d" % i, [8, 256], BF16) for i in range(2)]
    sgl = [A.alloc("sgl%d" % i, [4, 256], BF16) for i in range(1)]
    aT = [A.alloc("aT%d" % i, [4, 256], BF16) for i in range(2)]
    ysb = [A.alloc("ysb%d" % i, [D], BF16) for i in range(4)]
    def emit_load(g, part="both"):
        sl = g % NS
        s2 = g % 2
        for (wt, wsrc, wn, ceng) in ((wgs, weg_d, "wg", "act"), (wus, weu_d, "wu", "dve"), (wds, wed_d, "wd", "pool")):
            st_ = stg[wn]
            if part in ("both", "dma") and g >= NCHK0:
                P.add("pool", (lambda e, g=g, st_=st_, wsrc=wsrc: e.indirect_dma_start(
                    out=st_, out_offset=None, in_=wsrc,
                    in_offset=bass.IndirectOffsetOnAxis(ap=idxs[:, g:g + 1], axis=0), bounds_check=32 * 128 - 1, oob_is_err=False)),
                    reads=[("idxs", 0)], writes=[("stg_" + wn, 0)], dma=True, grp="stg_" + wn)
            elif part in ("both", "dma"):
                P.add("pool", (lambda e, g=g, st_=st_, wsrc=wsrc: e.indirect_dma_start(
                    out=st_, out_offset=None, in_=wsrc,
                    in_offset=bass.IndirectOffsetOnAxis(ap=idxw[:, g:g + 1], axis=0))),
                    reads=[("idxw", 0)], writes=[("stg_" + wn, 0)], dma=True, grp="stg_" + wn)
            if part == "dma":
                continue
            dstv = wt[sl].rearrange("p a b -> p (a b)")
            if ceng == "act":
                P.add("act", (lambda e, dstv=dstv, st_=st_: e.activation(out=dstv, in_=st_, func=AF.Identity)),
                      reads=[("stg_" + wn, 0)], writes=[("%s%d" % (wn, sl), 0)])
            elif ceng == "dve":
                P.add("dve", (lambda e, dstv=dstv, st_=st_: e.tensor_copy(out=dstv, in_=st_)),
                      reads=[("stg_" + wn, 0)], writes=[("%s%d" % (wn, sl), 0)])
            else:
                P.add("act", (lambda e, dstv=dstv, st_=st_: e.activation(out=dstv[:, 0:2048], in_=st_[:, 0:2048], func=AF.Identity)),
                      reads=[("stg_" + wn, 0)], writes=[("%s%d" % (wn, sl), 0)])
                P.add("dve", (lambda e, dstv=dstv, st_=st_: e.tensor_copy(out=dstv[:, 2048:4096], in_=st_[:, 2048:4096])),
                      reads=[("stg_" + wn, 0)], writes=[("%s%d" % (wn, sl), 1)])

    def emit_compute_a(g):
        s2 = g % 2
        for hf in range(2):
            xi = (2 * g + hf) % 2
            r0 = g * GROWS + hf * 128
            P.add("sp", (lambda e, r0=r0, xi=xi: e.dma_start(out=xgt[xi], in_=XS[r0:r0 + 128, :])),
                  reads=XS_KEYS, writes=[("xgt%d" % xi, 0)], dma=True, grp="xgt%d" % xi)
            pt, pkt = next_ps()
            ptb = pt.bitcast(BF16)

            def xtr(e, ptb=ptb, xi=xi):
                ins = None
                for c in range(8):
                    ins = e.transpose(out=ptb[:, c * 128:(c + 1) * 128], in_=xgt[xi][:, c * 128:(c + 1) * 128], identity=ident_b)
                return ins
            P.add("pe", xtr, reads=[("ident_b", 0), ("xgt%d" % xi, 0)], writes=[pkt])
            P.add("act", (lambda e, ptb=ptb, s2=s2, hf=hf: e.activation(
                out=xgT[s2][:, :, hf * 128:(hf + 1) * 128], in_=ptb[:, 0:1024].rearrange("p (a b) -> p a b", b=128), func=AF.Identity)),
                reads=[pkt], writes=[("xgT%d" % s2, hf)])

    def emit_compute_b1(g):
        sl = g % NS
        s2 = g % 2
        pg_ = [next_ps(), next_ps()]
        pu_ = [next_ps(), next_ps()]

        def gumm(e, pg_=pg_, pu_=pu_, sl=sl, s2=s2):
            ins = None
            for (pp, ww) in ((pg_, wgs), (pu_, wus)):
                for fc in range(4):
                    o = pp[fc // 2][0][:, (fc % 2) * 256:(fc % 2 + 1) * 256]
                    for k in range(8):
                        ins = e.matmul(o, lhsT=ww[sl][:, k, fc * 128:(fc + 1) * 128], rhs=xgT[s2][:, k, :], start=(k == 0), stop=(k == 7))
            return ins
        P.add("pe", gumm, reads=[("wg%d" % sl, 0), ("wu%d" % sl, 0), ("xgT%d" % s2, 0), ("xgT%d" % s2, 1)],
              writes=[pg_[0][1], pg_[1][1], pu_[0][1], pu_[1][1]])
        for b2 in range(2):
            P.add("act", (lambda e, pg_=pg_, s2=s2, b2=b2: e.activation(
                out=sgl[0][:, 2 * b2:2 * b2 + 2, :].rearrange("p a b -> p (a b)"), in_=pg_[b2][0][:, :], func=AF.Silu)),
                reads=[pg_[b2][1]], writes=[("sgl0", b2)])
            P.add("dve", (lambda e, pu_=pu_, s2=s2, b2=b2: e.tensor_tensor(
                out=aT[s2][:, 2 * b2:2 * b2 + 2, :].rearrange("p a b -> p (a b)"), in0=pu_[b2][0][:, :],
                in1=sgl[0][:, 2 * b2:2 * b2 + 2, :].rearrange("p a b -> p (a b)"), op=ALU.mult)),
                reads=[pu_[b2][1], ("sgl0", b2)], writes=[("aT%d" % s2, b2)])

    def emit_compute_b2(g):
        sl = g % NS
        s2 = g % 2
        for hf in range(2):
            yi = (2 * g + hf) % 4
            py = [next_ps(), next_ps()]

            def dmm(e, py=py, sl=sl, s2=s2, hf=hf):
                ins = None
                for half in range(2):
                    for fc in range(4):
                        ins = e.matmul(py[half][0][:, :], lhsT=aT[s2][:, fc, hf * 128:(hf + 1) * 128],
                                       rhs=wds[sl][:, fc, half * 512:(half + 1) * 512], start=(fc == 0), stop=(fc == 3))
                return ins
            P.add("pe", dmm, reads=[("wd%d" % sl, 0), ("wd%d" % sl, 1), ("aT%d" % s2, 0), ("aT%d" % s2, 1)], writes=[py[0][1], py[1][1]])
            for half in range(2):
                P.add("dve", (lambda e, py=py, yi=yi, half=half: e.tensor_tensor(
                    out=ysb[yi][:, half * 512:(half + 1) * 512], in0=py[half][0][:, :], in1=gt2row[:, half * 512:(half + 1) * 512],
                    op=ALU.mult)),
                    reads=[py[half][1], ("gt2row", half)], writes=[("ysb%d" % yi, half)])
            r0 = g * GROWS + hf * 128
            P.add("sp", (lambda e, r0=r0, yi=yi: e.dma_start(out=YS[r0:r0 + 128, :], in_=ysb[yi])),
                  reads=[("ysb%d" % yi, 0), ("ysb%d" % yi, 1)], writes=[("YS", g, hf)], dma=True, grp="ys_st%d" % yi)

    emit_load(0, "cast")
    emit_compute_a(0)
    for g in range(NG):
        emit_compute_b1(g)
        if g + 1 < NG:
            emit_load(g + 1)
            emit_compute_a(g + 1)
        emit_compute_b2(g)
    YS_KEYS = [("YS", g, hf) for g in range(NG) for hf in range(2)]
    for i in range(NS):
        A.free("wg%d" % i); A.free("wu%d" % i); A.free("wd%d" % i)
    for nm in ("stg_wg", "stg_wu", "stg_wd", "xgt0", "xgt1", "xgT0", "xgT1", "sgl0", "aT0", "aT1", "ysb0", "ysb1", "ysb2", "ysb3"):
        A.free(nm)

    gfin = load_const("gfin", gfin_d, [D])
    NYG = 4
    yg = [[A.alloc("yg%d_%d" % (q, i), [D], BF16) for i in range(NYG)] for q in range(2)]
    acc = [A.alloc("acc%d" % i, [D], F32) for i in range(2)]
    outt = [A.alloc("outt%d" % i, [D], F32) for i in range(2)]
    ssf = A.alloc("ssf", [NT], F32)
    rsf = A.alloc("rsf", [NT], F32)

    def emit_g(ti):
        s4 = ti % NYG
        for q in range(2):
            P.add("pool", (lambda e, ti=ti, q=q, s4=s4: e.indirect_dma_start(
                out=yg[q][s4], out_offset=None, in_=YS,
                in_offset=bass.IndirectOffsetOnAxis(ap=dsti[:, q, ti:ti + 1], axis=0))),
                reads=YS_KEYS + [("dsti", 0)], writes=[("yg%d_%d" % (q, s4), 0)], dma=True, grp="yg%d_%d" % (q, s4))

    def emit_c1(ti):
        s4 = ti % NYG
        s2 = ti % 2
        y1, y2 = yg[0][s4], yg[1][s4]
        k1, k2 = ("yg0_%d" % s4, 0), ("yg1_%d" % s4, 0)
        ac = acc[s2]
        ak = ("acc%d" % s2, 0)
        P.add("act", (lambda e, y1=y1, ac=ac, ti=ti: e.activation(out=ac, in_=y1, func=AF.Identity, scale=w1[:, ti:ti + 1])),
              reads=[k1, ("w1", 0)], writes=[ak])
        P.add("dve", (lambda e, ac=ac, y2=y2, ti=ti: e.scalar_tensor_tensor(out=ac, in0=y2, scalar=w2[:, ti:ti + 1], in1=ac,
                                                                          op0=ALU.mult, op1=ALU.add)),
              reads=[ak, k2, ("w2", 0)], writes=[ak])
        P.add("dve", (lambda e, ac=ac, ti=ti: e.tensor_tensor(out=x1[:, ti, :], in0=x1[:, ti, :], in1=ac, op=ALU.add)),
              reads=[ak, ("x1", ti)], writes=[("x1", ti)])

    def emit_c2(ti):
        s2 = ti % 2
        P.add("act", (lambda e, ti=ti: e.activation(out=junk, in_=x1[:, ti, :], func=AF.Square, accum_out=ssf[:, ti:ti + 1])),
              reads=[("x1", ti)], writes=[("junk", 0), ("ssf", ti)])
        P.add("act", (lambda e, ti=ti: e.activation(out=rsf[:, ti:ti + 1], in_=ssf[:, ti:ti + 1], func=AF.Sqrt, scale=1.0 / D, bias=1e-6)),
              reads=[("ssf", ti)], writes=[("rsf", ti)])
        P.add("dve", (lambda e, ti=ti: e.reciprocal(out=rsf[:, ti:ti + 1], in_=rsf[:, ti:ti + 1])),
              reads=[("rsf", ti)], writes=[("rsf", ti)])
        ot = outt[s2]
        ok = ("outt%d" % s2, 0)
        P.add("act", (lambda e, ot=ot, ti=ti: e.activation(out=ot, in_=x1[:, ti, :], func=AF.Identity, scale=rsf[:, ti:ti + 1])),
              reads=[("x1", ti), ("rsf", ti)], writes=[ok])
        P.add("dve", (lambda e, ot=ot: e.tensor_tensor(out=ot, in0=ot, in1=gfin, op=ALU.mult)),
              reads=[ok, ("gfin", 0)], writes=[ok])
        P.add("sp", (lambda e, ot=ot, ti=ti: e.dma_start(out=out_d[ti * 128:(ti + 1) * 128, :], in_=ot)),
              reads=[ok], dma=True, grp="out%d" % s2)

    for ti in range(min(3, NT)):
        emit_g(ti)
    for ti in range(NT):
        if ti + 3 < NT:
            emit_g(ti + 3)
        emit_c1(ti)
        if ti > 0:
            emit_c2(ti - 1)
    emit_c2(NT - 1)
    P.emit(final_wait_groups=["out0", "out1"] + (["dbgout"] if "dbgout" in P.dma_groups else []))
    build.stats = dict(peak_kb=A.peak * 4 / 1024.0, n_ops=len(P.all_ops), n_groups=len(P.dma_groups))
    return nc, dbg_outs


def host_layout(inp, b):
    f = lambda a: np.ascontiguousarray(a, dtype=np.float32)
    col = lambda v, n: f(np.asarray(v).reshape(n, 128).T)
    m = {}
    m["x"] = f(inp["x"][b])
    m["c_col"] = col(inp["c"][b], 8)
    m["w_ada"] = f(inp["w_ada"][0])
    m["b_ada_col"] = col(inp["b_ada"][0], 48)
    m["g1_col"] = col(inp["g_norm1"][0], 8)
    m["g2_col"] = col(inp["g_norm2"][0], 8)
    m["w_in"] = f(inp["w_in"][0])
    m["b_if_bc"] = f(np.broadcast_to(inp["b_if"][0][None, :], (128, 8)))
    m["conv_w_col"] = f(inp["conv_dw_w"][0].reshape(31, 4, 128).transpose(2, 1, 0))
    m["conv_b_col"] = col(inp["conv_dw_b"][0], 4)
    m["conv_lng_col"] = col(inp["conv_ln_g"][0], 4)
    m["conv_lnb_col"] = col(inp["conv_ln_b"][0], 4)
    m["w_conv_out"] = f(inp["w_conv_out"][0])
    m["qk_w_col"] = f(inp["qk_conv_w"][0].reshape(4, 8, 128).transpose(2, 1, 0))
    m["qk_b_col"] = col(inp["qk_conv_b"][0], 8)
    m["mng_col"] = col(inp["m_norm_g"][0], 4)
    m["w_m_out"] = f(inp["w_m_out"][0])
    m["w_out"] = f(inp["w_out"][0])
    m["w_router"] = f(np.concatenate([inp["w_rg"][0], inp["w_re"][0]], axis=1))
    m["b_router_bc"] = f(np.broadcast_to(np.concatenate([inp["b_rg"][0], inp["b_re"][0]])[None, :], (128, 36)))
    m["w_e_gate_l"] = f(inp["w_e_gate"][0].reshape(32, 8, 128, 512).transpose(0, 2, 1, 3).reshape(32 * 128, 8 * 512))
    m["w_e_up_l"] = f(inp["w_e_up"][0].reshape(32, 8, 128, 512).transpose(0, 2, 1, 3).reshape(32 * 128, 8 * 512))
    m["w_e_down_l"] = f(inp["w_e_down"][0].reshape(32, 4, 128, D).transpose(0, 2, 1, 3).reshape(32 * 128, 4 * D))
    m["g_final_bc"] = f(np.broadcast_to(np.asarray(inp["g_final"])[None, :], (128, D)))
    return m


def kernel(**inputs):
    nc, _ = build()
    shared = host_layout(inputs, 0)
    in_maps = []
    for b in range(8):
        m = dict(shared)
        m["x"] = np.ascontiguousarray(inputs["x"][b], dtype=np.float32)
        m["c_col"] = np.ascontiguousarray(np.asarray(inputs["c"][b]).reshape(8, 128).T, dtype=np.float32)
        in_maps.append(m)
    res = run_bass_kernel_spmd(nc, in_maps, core_ids=list(range(8)))
    return np.stack([np.asarray(r["out"]) for r in res.results], axis=0).astype(np.float32)
```

```python
import contextlib
import numpy as np
import concourse.bass as bass
import concourse.mybir as mybir
from concourse.bass_utils import run_bass_kernel_spmd

F32 = mybir.dt.float32
BF16 = mybir.dt.bfloat16
I32 = mybir.dt.int32
AF = mybir.ActivationFunctionType
ALU = mybir.AluOpType
AX = mybir.AxisListType

T = 2048
D = 1024
NT = 16
NB = 4
DIN = 5128
ENG_NAMES = ("pe", "act", "dve", "pool", "sp")


class Op:
    __slots__ = ("eng", "fn", "is_dma", "grp", "signal", "val", "idx", "deps")

    def __init__(self, eng, fn, is_dma, grp):
        self.eng = eng
        self.fn = fn
        self.is_dma = is_dma
        self.grp = grp
        self.signal = False
        self.val = None
        self.idx = None
        self.deps = []


def _reduce_ops(ops):
    latest = {}
    dm = {}
    for o in ops:
        if o.is_dma:
            if o.grp not in dm or dm[o.grp].idx < o.idx:
                dm[o.grp] = o
        else:
            if o.eng not in latest or latest[o.eng].idx < o.idx:
                latest[o.eng] = o
    return list(latest.values()) + list(dm.values())


class Prog:
    def __init__(self, nc):
        self.nc = nc
        self.ops = {e: [] for e in ENG_NAMES}
        self.all_ops = []
        self.last_writer = {}
        self.readers = {}
        self.dma_groups = {}
        self.buf_pred = {}
        self.keys_by_buf = {}
        self.wait_all_groups = set()

    def _touch(self, k):
        if k not in self.readers:
            self.readers[k] = list(self.buf_pred.get(k[0], ()))
            self.last_writer[k] = None
            self.keys_by_buf.setdefault(k[0], set()).add(k)

    def ops_touching(self, bufname):
        s = list(self.buf_pred.get(bufname, ()))
        for k in self.keys_by_buf.get(bufname, ()):
            w = self.last_writer.get(k)
            if w is not None:
                s.append(w)
            s.extend(self.readers.get(k, ()))
        return _reduce_ops(s)

    def add(self, eng, fn, reads=(), writes=(), dma=False, grp=None):
        op = Op(eng, fn, dma, grp)
        op.idx = len(self.all_ops)
        self.all_ops.append(op)
        self.ops[eng].append(op)
        if dma:
            assert grp is not None
            self.dma_groups.setdefault(grp, []).append(op)
        deps = []
        for k in reads:
            self._touch(k)
            w = self.last_writer[k]
            if w is not None:
                deps.append((w, "raw"))
            elif self.readers[k] and k[0] in self.buf_pred:
                pass
        for k in writes:
            self._touch(k)
            w = self.last_writer[k]
            if w is not None:
                deps.append((w, "waw"))
            for r in self.readers[k]:
                deps.append((r, "war"))
        for d, kind in deps:
            if d is op:
                continue
            if (not d.is_dma) and (not dma) and d.eng == eng:
                if eng == "pe":
                    continue
            op.deps.append(d)
        for k in reads:
            self.readers[k].append(op)
        for k in writes:
            self.last_writer[k] = op
            self.readers[k] = []
        return op

    def emit(self, final_wait_groups=()):
        nc = self.nc
        for op in self.all_ops:
            op.deps = _reduce_ops(op.deps)
            for d in op.deps:
                d.signal = True
        for e in ENG_NAMES:
            c = 0
            for op in self.ops[e]:
                if (not op.is_dma) and op.signal:
                    c += 1
                    op.val = c
        gtotal = {}
        for g, lst in self.dma_groups.items():
            c = 0
            for op in lst:
                c += 16
                op.val = c
            gtotal[g] = c
        with contextlib.ExitStack() as st:
            esem = {e: st.enter_context(nc.semaphore("s_" + e)) for e in ENG_NAMES}
            gsem = {g: st.enter_context(nc.semaphore("d_%d" % i))
                    for i, g in enumerate(self.dma_groups)}
            block = st.enter_context(nc.Block())

            def run(e, engobj):
                seen = {}
                for op in self.ops[e]:
                    for d in op.deps:
                        if d.is_dma:
                            key = ("g", d.grp)
                            sem = gsem[d.grp]
                            v = gtotal[d.grp] if d.grp in self.wait_all_groups else d.val
                        else:
                            key = ("e", d.eng)
                            sem = esem[d.eng]
                            v = d.val
                        if seen.get(key, 0) >= v:
                            continue
                        seen[key] = v
                        engobj.wait_ge(sem, v)
                    ins = op.fn(engobj)
                    if op.is_dma:
                        ins.then_inc(gsem[op.grp], 16)
                    elif op.signal:
                        ins.then_inc(esem[e], 1)
                if e == "sp":
                    for g in final_wait_groups:
                        engobj.wait_ge(gsem[g], gtotal[g])

            block.tensor(lambda eng: run("pe", eng))
            block.scalar(lambda eng: run("act", eng))
            block.vector(lambda eng: run("dve", eng))
            block.gpsimd(lambda eng: run("pool", eng))
            block.sync(lambda eng: run("sp", eng))


class Arena:
    def __init__(self, nc, prog, words):
        self.t = nc.alloc_sbuf_tensor("arena", [128, words], F32)
        self.P = prog
        self.free_list = [(0, words)]
        self.live = {}
        self.dead = []
        self.peak = 0

    def alloc(self, name, shape, dt, parts=128):
        n = int(np.prod(shape))
        esz = 2 if dt == BF16 else 4
        words = (n * esz + 31) // 32 * 8
        small = words <= 1100
        order = range(len(self.free_list) - 1, -1, -1) if small else range(len(self.free_list))
        for i in order:
            o, w = self.free_list[i]
            if w >= words:
                if w == words:
                    off = o
                    self.free_list.pop(i)
                elif small:
                    off = o + w - words
                    self.free_list[i] = (o, w - words)
                else:
                    off = o
                    self.free_list[i] = (o + words, w - words)
                break
        else:
            raise RuntimeError("SBUF arena full allocating %s (%d words); live=%s" % (
                name, words, {k: v[1] for k, v in self.live.items()}))
        self.live[name] = (off, words)
        self.peak = max(self.peak, off + words)
        preds = []
        for (o, w, nm) in self.dead:
            if o < off + words and off < o + w:
                preds.extend(self.P.ops_touching(nm))
        assert name not in self.P.keys_by_buf, name
        self.P.buf_pred[name] = _reduce_ops(preds)
        v = self.t[0:parts, off:off + words]
        if dt != F32:
            v = v.bitcast(dt)
        v = v[:, 0:n]
        if len(shape) == 2:
            v = v.rearrange("p (a b) -> p a b", b=shape[1])
        elif len(shape) == 3:
            v = v.rearrange("p (a b c) -> p a b c", b=shape[1], c=shape[2])
        return v

    def free(self, name):
        off, words = self.live.pop(name)
        self.dead.append((off, words, name))
        fl = self.free_list + [(off, words)]
        fl.sort()
        merged = []
        for o, w in fl:
            if merged and merged[-1][0] + merged[-1][1] == o:
                merged[-1] = (merged[-1][0], merged[-1][1] + w)
            else:
                merged.append((o, w))
        self.free_list = merged


def build(stage=99, dbg=()):
    nc = bass.Bass("TRN2", target_bir_lowering=False)
    P = Prog(nc)
    A = Arena(nc, P, 52992)

    def din(name, shape, dt=F32):
        return nc.dram_tensor(name, list(shape), dt, kind="ExternalInput").ap()

    x_d = din("x", [T, D])
    ccol_d = din("c_col", [128, 8])
    wada_d = din("w_ada", [D, 6 * D])
    bada_d = din("b_ada_col", [128, 48])
    g1_d = din("g1_col", [128, 8])
    g2_d = din("g2_col", [128, 8])
    win_d = din("w_in", [D, DIN])
    bif_d = din("b_if_bc", [128, 8])
    cw_d = din("conv_w_col", [128, 4, 31])
    cb_d = din("conv_b_col", [128, 4])
    clg_d = din("conv_lng_col", [128, 4])
    clb_d = din("conv_lnb_col", [128, 4])
    wco_d = din("w_conv_out", [512, D])
    qkw_d = din("qk_w_col", [128, 8, 4])
    qkb_d = din("qk_b_col", [128, 8])
    mng_d = din("mng_col", [128, 4])
    wmo_d = din("w_m_out", [512, D])
    wout_d = din("w_out", [D, D])
    wr_d = din("w_router", [D, 36])
    br_d = din("b_router_bc", [128, 36])
    weg_d = din("w_e_gate_l", [32 * 128, 8 * 512])
    weu_d = din("w_e_up_l", [32 * 128, 8 * 512])
    wed_d = din("w_e_down_l", [32 * 128, 4 * D])
    gfin_d = din("g_final_bc", [128, D])
    out_d = nc.dram_tensor("out", [T, D], F32, kind="ExternalOutput").ap()

    dbg_outs = {}

    def dbg_out(name, ap, reads):
        if name not in dbg:
            return
        shape = list(ap.shape)
        dt = ap.dtype
        d = nc.dram_tensor("dbg_" + name, shape, dt, kind="ExternalOutput").ap()
        dbg_outs[name] = d
        P.add("sp", lambda e: e.dma_start(out=d, in_=ap), reads=reads, dma=True, grp="dbgout")

    psb = [nc.alloc_psum_tensor("ps%d" % i, [128, 512], F32) for i in range(8)]
    ps_rot = list(range(8))

    def next_ps():
        i = ps_rot.pop(0)
        ps_rot.append(i)
        return psb[i], ("ps%d" % i,)

    def hold_ps():
        i = ps_rot.pop(0)
        return psb[i], ("ps%d" % i,)

    def release_ps(key):
        ps_rot.append(int(key[0][2:]))

    ident_f = A.alloc("ident_f", [128], F32)
    ident_b = A.alloc("ident_b", [128], BF16)
    ones_b = A.alloc("ones_b", [128], BF16)
    ones_f = A.alloc("ones_f", [128], F32)
    mask_ut = A.alloc("mask_ut", [128], BF16)
    tri_f = A.alloc("tri_f", [128], F32)
    K_ID = ("ident_f", 0)
    P.add("pool", lambda e: e.memset(ident_f, 0.0), writes=[("ident_f", 0)])
    P.add("pool", lambda e: e.affine_select(out=ident_f, in_=ident_f, pattern=[[-1, 128]],
                                             compare_op=ALU.not_equal, fill=1.0, base=0, channel_multiplier=1),
          reads=[("ident_f", 0)], writes=[("ident_f", 0)])
    P.add("pool", lambda e: e.tensor_copy(out=ident_b, in_=ident_f), reads=[("ident_f", 0)], writes=[("ident_b", 0)])
    P.add("pool", lambda e: e.memset(ones_b, 1.0), writes=[("ones_b", 0)])
    P.add("pool", lambda e: e.memset(ones_f, 1.0), writes=[("ones_f", 0)])
    P.add("pool", lambda e: e.memset(tri_f, 1.0), writes=[("tri_f", 0)])
    P.add("pool", lambda e: e.affine_select(out=tri_f, in_=tri_f, pattern=[[1, 128]],
                                             compare_op=ALU.is_ge, fill=0.0, base=0, channel_multiplier=-1),
          reads=[("tri_f", 0)], writes=[("tri_f", 0)])
    P.add("pool", lambda e: e.tensor_copy(out=mask_ut, in_=tri_f), reads=[("tri_f", 0)], writes=[("mask_ut", 0)])

    def load_const(name, dram, shape, dt=F32):
        t = A.alloc(name, shape, dt)
        P.add("sp", lambda e: e.dma_start(out=t, in_=dram), writes=[(name, 0)], dma=True, grp="c_" + name)
        return t

    ccol = load_const("ccol", ccol_d, [8])
    bada = load_const("bada", bada_d, [48])
    g1c = load_const("g1c", g1_d, [8])
    g2c = load_const("g2c", g2_d, [8])

    silc = A.alloc("silc", [8], F32)
    silb = A.alloc("silb", [8], BF16)
    P.add("act", lambda e: e.activation(out=silc, in_=ccol, func=AF.Silu), reads=[("ccol", 0)], writes=[("silc", 0)])
    P.add("dve", lambda e: e.tensor_copy(out=silb, in_=silc), reads=[("silc", 0)], writes=[("silb", 0)])
    modT = A.alloc("modT", [48], F32)
    wada_v = wada_d.rearrange("(c p) n -> p c n", p=128)
    NWA = 2
    wab = [A.alloc("wada%d" % i, [8, 512], BF16) for i in range(NWA)]
    a1 = A.alloc("a1", [8], F32)
    a2 = A.alloc("a2", [8], F32)

    def adaln_block(blk, ps_mod, k_mod):
        s_ = blk % NWA
        buf = wab[s_]
        nm = "wada%d" % s_
        P.add("pool", (lambda e, buf=buf, blk=blk: e.dma_start(out=buf, in_=wada_v[:, :, blk * 512:(blk + 1) * 512])),
              writes=[(nm, 0)], dma=True, grp=nm)

        def mm(e, buf=buf, blk=blk):
            ins = None
            for jj in range(4):
                j = blk * 4 + jj
                for k in range(8):
                    ins = e.matmul(ps_mod[:, j:j + 1], lhsT=buf[:, k, jj * 128:(jj + 1) * 128], rhs=silb[:, k:k + 1],
                                   start=(k == 0), stop=(k == 7))
            return ins
        P.add("pe", mm, reads=[(nm, 0), ("silb", 0)], writes=[k_mod])

    def adaln_finish(ps_mod, k_mod, c0, c1, part):
        P.add("dve", lambda e: e.tensor_tensor(out=modT[:, c0:c1], in0=ps_mod[:, c0:c1], in1=bada[:, c0:c1], op=ALU.add),
              reads=[k_mod, ("bada", 0)], writes=[("modT", part)])
        release_ps(k_mod)

    pm0, km0 = hold_ps()
    for blk in range(4):
        adaln_block(blk, pm0, km0)
    adaln_finish(pm0, km0, 0, 16, 0)
    P.add("dve", lambda e: e.scalar_tensor_tensor(out=a1, in0=modT[:, 8:16], scalar=1.0, in1=g1c, op0=ALU.add, op1=ALU.mult),
          reads=[("modT", 0), ("g1c", 0)], writes=[("a1", 0)])
    p2state = {}

    def adaln_p2_block(blk):
        if "ps" not in p2state:
            p2state["ps"] = hold_ps()
        adaln_block(blk, *p2state["ps"])

    def adaln_p2_end():
        pm1, km1 = p2state["ps"]
        adaln_finish(pm1, km1, 16, 48, 1)
        P.add("dve", lambda e: e.scalar_tensor_tensor(out=a2, in0=modT[:, 32:40], scalar=1.0, in1=g2c, op0=ALU.add, op1=ALU.mult),
              reads=[("modT", 1), ("g2c", 0)], writes=[("a2", 0)])
        dbg_out("modT", modT, [("modT", 0), ("modT", 1)])
        for i in range(NWA):
            A.free("wada%d" % i)

    hT = A.alloc("hT", [8, T], BF16)
    merged = A.alloc("merged", [8, T], BF16)
    NWB = 3
    wbufs = [A.alloc("wblk%d" % i, [8, 512], BF16) for i in range(NWB)]
    NXB = 8
    xin = [A.alloc("xin%d" % i, [D], F32) for i in range(NXB)]
    xnb = [A.alloc("xnb%d" % i, [D], BF16) for i in range(NXB)]
    junk = A.alloc("junk", [D], F32)
    ss1 = A.alloc("ss1", [NT], F32)
    rs1 = A.alloc("rs1", [NT], F32)

    def p2_stats(nb):
        for tt in range(4):
            ti = nb * 4 + tt
            s = ti % NXB
            P.add("sp", (lambda e, s=s, ti=ti: e.dma_start(out=xin[s], in_=x_d[ti * 128:(ti + 1) * 128, :])),
                  writes=[("xin%d" % s, 0)], dma=True, grp="xin%d" % s)
            P.add("act", (lambda e, s=s, ti=ti: e.activation(out=junk, in_=xin[s], func=AF.Square,
                                                             accum_out=ss1[:, ti:ti + 1])),
                  reads=[("xin%d" % s, 0)], writes=[("junk", 0), ("ss1", ti)])
            P.add("act", (lambda e, ti=ti: e.activation(out=rs1[:, ti:ti + 1], in_=ss1[:, ti:ti + 1], func=AF.Sqrt,
                                                        scale=1.0 / D, bias=1e-6)),
                  reads=[("ss1", ti)], writes=[("rs1", ti)])
            P.add("dve", (lambda e, ti=ti: e.reciprocal(out=rs1[:, ti:ti + 1], in_=rs1[:, ti:ti + 1])),
                  reads=[("rs1", ti)], writes=[("rs1", ti)])
            P.add("dve", (lambda e, s=s, ti=ti: e.tensor_scalar(out=xnb[s], in0=xin[s], scalar1=rs1[:, ti:ti + 1],
                                                                scalar2=None, op0=ALU.mult)),
                  reads=[("xin%d" % s, 0), ("rs1", ti)], writes=[("xnb%d" % s, 0)])

    def p2_tr(nb):
        pst = [next_ps() for _ in range(4)]
        for tt in range(4):
            ti = nb * 4 + tt
            s = ti % NXB

            def tr(e, s=s, tt=tt, pst=pst):
                ins = None
                for c in range(8):
                    pb = pst[c // 2][0].bitcast(BF16)
                    ins = e.transpose(out=pb[:, (c % 2) * 512 + tt * 128:(c % 2) * 512 + (tt + 1) * 128],
                                      in_=xnb[s][:, c * 128:(c + 1) * 128], identity=ident_b)
                return ins
            P.add("pe", tr, reads=[("xnb%d" % s, 0), ("ident_b", 0)], writes=[pst[i][1] for i in range(4)])
        for c in range(8):
            pb = pst[c // 2][0].bitcast(BF16)
            P.add("act", (lambda e, c=c, pb=pb, nb=nb: e.activation(
                out=hT[:, c, nb * 512:(nb + 1) * 512], in_=pb[:, (c % 2) * 512:(c % 2 + 1) * 512],
                func=AF.Identity, scale=a1[:, c:c + 1], bias=modT[:, c:c + 1])),
                reads=[pst[c // 2][1], ("a1", 0), ("modT", 0)],
                writes=[("hT", c, nb)])

    p2_stats(0)
    for nb in range(NB):
        if nb + 1 < NB:
            p2_stats(nb + 1)
        p2_tr(nb)
    dbg_out("hT", hT, [("hT", c, nb) for c in range(8) for nb in range(NB)])
    for i in range(NXB):
        A.free("xin%d" % i); A.free("xnb%d" % i)

    def finish():
        P.emit(final_wait_groups=["dbgout"] if "dbgout" in P.dma_groups else [])
        return nc, dbg_outs

    GROWS = 256
    NG = -(-(2 * T + 32 * (GROWS - 1)) // GROWS)
    XS = nc.dram_tensor("xs_scratch", [NG * GROWS, D], BF16).ap()
    YS = nc.dram_tensor("ys_scratch", [NG * GROWS, D], BF16).ap()
    if stage <= 1:
        return finish()

    win_v = win_d.rearrange("(c p) n -> p c n", p=128)
    NWB = 3
    wb_ctr = [0]

    def load_wblock(col0, ncols=512):
        i = wb_ctr[0] % NWB
        wb_ctr[0] += 1
        buf = wbufs[i]
        nm = "wblk%d" % i
        P.add("pool", lambda e: e.dma_start(out=buf[:, :, 0:ncols], in_=win_v[:, :, col0:col0 + ncols]),
              writes=[(nm, 0)], dma=True, grp=nm)
        return buf, (nm, 0)

    def load_w4(dram_v):
        i = wb_ctr[0] % NWB
        wb_ctr[0] += 1
        nm = "wblk%d" % i
        v = wbufs[i].rearrange("p a b -> p (a b)").rearrange("p (a b) -> p a b", b=D)
        P.add("pool", lambda e: e.dma_start(out=v, in_=dram_v), writes=[(nm, 0)], dma=True, grp=nm)
        return v, (nm, 0)

    def load_cast(name, dram_ap, shape):
        t = A.alloc(name, shape, BF16)
        P.add("pool", lambda e: e.dma_start(out=t, in_=dram_ap), writes=[(name, 0)], dma=True, grp="c_" + name)
        return t

    hT_keys = lambda nb: [("hT", c, nb) for c in range(8)]

    def proj_fm(wb, wkey, mcol, nb):
        ps, pk = next_ps()

        def mm(e):
            ins = None
            for k in range(8):
                ins = e.matmul(ps[:, :], lhsT=wb[:, k, mcol * 128:(mcol + 1) * 128], rhs=hT[:, k, nb * 512:(nb + 1) * 512],
                               start=(k == 0), stop=(k == 7))
            return ins
        P.add("pe", mm, reads=[wkey] + hT_keys(nb), writes=[pk])
        return ps, pk

    cw = load_const("cw", cw_d, [4, 31])
    cb = load_const("cb", cb_d, [4])
    clg = load_const("clg", clg_d, [4])
    clb = load_const("clb", clb_d, [4])
    u = A.alloc("u", [4, 32 + T], BF16)
    PADU = 32
    for m in range(4):
        P.add("pool", (lambda e, m=m: e.memset(u[:, m, 0:PADU], 0.0)), writes=[("u", m, -1)])
    dg31 = A.alloc("dg31", [4, 31, 128], BF16)
    for m in range(4):
        P.add("pool", (lambda e, m=m: e.tensor_tensor(
            out=dg31[:, m], in0=ident_b.unsqueeze(1).to_broadcast([128, 31, 128]),
            in1=cw[:, m, :].unsqueeze(2).to_broadcast([128, 31, 128]), op=ALU.mult)),
            reads=[("ident_b", 0), ("cw", 0)], writes=[("dg31", m)])
    sgt = [A.alloc("sgt%d" % i, [512], BF16) for i in range(2)]
    sg_ctr = [0]

    def next_sgt():
        i = sg_ctr[0] % 2
        sg_ctr[0] += 1
        return sgt[i], ("sgt%d" % i, 0)

    wa, wak = load_wblock(0)
    wbk, wbkk = load_wblock(512)
    for m in range(4):
        for nb in range(NB):
            psa, pka = proj_fm(wa, wak, m, nb)
            psb_, pkb = proj_fm(wbk, wbkk, m, nb)
            sg, sgk = next_sgt()
            P.add("act", (lambda e, sg=sg, p=psb_: e.activation(out=sg, in_=p[:, :], func=AF.Sigmoid)),
                  reads=[pkb], writes=[sgk])
            P.add("dve", (lambda e, sg=sg, p=psa, m=m, nb=nb: e.tensor_tensor(
                out=u[:, m, PADU + nb * 512:PADU + (nb + 1) * 512], in0=p[:, :], in1=sg, op=ALU.mult)),
                reads=[pka, sgk], writes=[("u", m, nb)])
    dbg_out("u", u, [("u", m, nb) for m in range(4) for nb in range(-1, NB)])

    wco, wcok = load_w4(wco_d.rearrange("(c p) n -> p c n", p=128))
    gA_blocks = {0: load_wblock(3080)}
    cT = A.alloc("cT", [4, T], BF16)
    sqT = A.alloc("sqT", [4, T], BF16)
    for m in range(4):
        for nb in range(NB):
            ps, pk = next_ps()

            def cmm(e, ps=ps, m=m, nb=nb):
                ins = None
                for k in range(31):
                    o = PADU - 30 + nb * 512 + k
                    ins = e.matmul(ps[:, :], lhsT=dg31[:, m, k, :], rhs=u[:, m, o:o + 512], start=(k == 0), stop=(k == 30))
                return ins
            P.add("pe", cmm, reads=[("dg31", m), ("u", m, nb), ("u", m, nb - 1)], writes=[pk])
            P.add("act", (lambda e, ps=ps, m=m, nb=nb: e.activation(
                out=cT[:, m, nb * 512:(nb + 1) * 512], in_=ps[:, :], func=AF.Identity, bias=cb[:, m:m + 1])),
                reads=[pk, ("cb", 0)], writes=[("cT", m, nb)])
            P.add("act", (lambda e, ps=ps, m=m, nb=nb: e.activation(
                out=sqT[:, m, nb * 512:(nb + 1) * 512], in_=ps[:, :], func=AF.Square, bias=cb[:, m:m + 1])),
                reads=[pk, ("cb", 0)], writes=[("sqT", m, nb)])
            gi = m * NB + nb
            if gi % 2 == 1:
                adaln_p2_block(4 + gi // 2)
    adaln_p2_end()
    dbg_out("cT", cT, [("cT", m, nb) for m in range(4) for nb in range(NB)])
    A.free("u")
    A.free("dg31")

    actT = A.alloc("actT", [4, T], BF16)
    mean_t = A.alloc("mean_t", [512], F32)
    rstd_t = A.alloc("rstd_t", [512], F32)
    msq_t = A.alloc("msq_t", [512], F32)
    nrm_t = [A.alloc("nrm_t%d" % i, [512], F32) for i in range(2)]
    for nb in range(NB):
        ps1, pk1 = next_ps()
        ps2, pk2 = next_ps()

        def smm(e, ps1=ps1, ps2=ps2, nb=nb):
            ins = None
            for m in range(4):
                ins = e.matmul(ps1[:, :], lhsT=ones_b, rhs=cT[:, m, nb * 512:(nb + 1) * 512], start=(m == 0), stop=(m == 3))
            for m in range(4):
                ins = e.matmul(ps2[:, :], lhsT=ones_b, rhs=sqT[:, m, nb * 512:(nb + 1) * 512], start=(m == 0), stop=(m == 3))
            return ins
        P.add("pe", smm, reads=[("ones_b", 0)] + [("cT", m, nb) for m in range(4)] + [("sqT", m, nb) for m in range(4)],
              writes=[pk1, pk2])
        P.add("dve", (lambda e, ps1=ps1: e.tensor_scalar(out=mean_t, in0=ps1[:, :], scalar1=1.0 / 512, scalar2=None, op0=ALU.mult)),
              reads=[pk1], writes=[("mean_t", 0)])
        P.add("dve", lambda e: e.tensor_tensor(out=msq_t, in0=mean_t, in1=mean_t, op=ALU.mult),
              reads=[("mean_t", 0)], writes=[("msq_t", 0)])
        P.add("dve", (lambda e, ps2=ps2: e.scalar_tensor_tensor(out=rstd_t, in0=ps2[:, :], scalar=1.0 / 512, in1=msq_t,
                                                                op0=ALU.mult, op1=ALU.subtract)),
              reads=[pk2, ("msq_t", 0)], writes=[("rstd_t", 0)])
        P.add("act", lambda e: e.activation(out=rstd_t, in_=rstd_t, func=AF.Sqrt, bias=1e-5),
              reads=[("rstd_t", 0)], writes=[("rstd_t", 0)])
        P.add("dve", lambda e: e.reciprocal(out=rstd_t, in_=rstd_t), reads=[("rstd_t", 0)], writes=[("rstd_t", 0)])
        for m in range(4):
            nt = nrm_t[m % 2]
            ntk = ("nrm_t%d" % (m % 2), 0)
            P.add("dve", (lambda e, nt=nt, m=m, nb=nb: e.tensor_tensor(out=nt, in0=cT[:, m, nb * 512:(nb + 1) * 512], in1=mean_t,
                                                                      op=ALU.subtract)),
                  reads=[("cT", m, nb), ("mean_t", 0)], writes=[ntk])
            P.add("dve", (lambda e, nt=nt: e.tensor_tensor(out=nt, in0=nt, in1=rstd_t, op=ALU.mult)),
                  reads=[ntk, ("rstd_t", 0)], writes=[ntk])
            P.add("act", (lambda e, nt=nt, m=m, nb=nb: e.activation(
                out=actT[:, m, nb * 512:(nb + 1) * 512], in_=nt, func=AF.Silu, scale=clg[:, m:m + 1], bias=clb[:, m:m + 1])),
                reads=[ntk, ("clg", 0), ("clb", 0)], writes=[("actT", m, nb)])
    dbg_out("actT", actT, [("actT", m, nb) for m in range(4) for nb in range(NB)])
    A.free("cT"); A.free("sqT"); A.free("mean_t"); A.free("rstd_t"); A.free("msq_t"); A.free("nrm_t0"); A.free("nrm_t1")

    for jb in range(2):
        wg_, wgk = gA_blocks[jb] if jb in gA_blocks else load_wblock(3080 + jb * 512)
        for jj in range(4):
            j = jb * 4 + jj
            for nb in range(NB):
                psy, pky = next_ps()

                def ymm(e, psy=psy, j=j, nb=nb):
                    ins = None
                    for m in range(4):
                        ins = e.matmul(psy[:, :], lhsT=wco[:, m, j * 128:(j + 1) * 128], rhs=actT[:, m, nb * 512:(nb + 1) * 512],
                                       start=(m == 0), stop=(m == 3))
                    return ins
                P.add("pe", ymm, reads=[wcok] + [("actT", m, nb) for m in range(4)], writes=[pky])
                psg, pkg = proj_fm(wg_, wgk, jj, nb)
                sg, sgk = next_sgt()
                P.add("act", (lambda e, sg=sg, p=psg: e.activation(out=sg, in_=p[:, :], func=AF.Sigmoid)),
                      reads=[pkg], writes=[sgk])
                P.add("dve", (lambda e, sg=sg, p=psy, j=j, nb=nb: e.tensor_tensor(
                    out=merged[:, j, nb * 512:(nb + 1) * 512], in0=p[:, :], in1=sg, op=ALU.mult)),
                    reads=[pky, sgk], writes=[("merged", j, nb)])
    dbg_out("mergedA", merged, [("merged", j, nb) for j in range(8) for nb in range(NB)])
    A.free("actT")
    if stage <= 2:
        return finish()

    zt = A.alloc("zt", [D], BF16)
    P.add("pool", lambda e: e.memset(zt, 0.0), writes=[("zt", 0)])
    XSZ_KEYS = []
    for zi in range(NG * GROWS // 1024):
        P.add("sp", (lambda e, zi=zi: e.dma_start(out=XS[zi * 1024:(zi + 1) * 1024, :].rearrange("(n p) d -> p n d", p=128),
                                                  in_=zt.unsqueeze(1).to_broadcast([128, 8, D]))),
              reads=[("zt", 0)], writes=[("XSZ", zi)], dma=True, grp="xs_zero")
        XSZ_KEYS.append(("XSZ", zi))
    A.free("zt")
    PADQ = 4
    qkw = load_const("qkw", qkw_d, [8, 4])
    qkb = load_const("qkb", qkb_d, [8])
    bif = load_const("bif", bif_d, [8])
    mng = load_const("mng", mng_d, [4])
    qk_raw = A.alloc("qk_raw", [8, PADQ + T], BF16)
    for cc in range(8):
        P.add("pool", (lambda e, cc=cc: e.memset(qk_raw[:, cc, 0:PADQ], 0.0)), writes=[("qk_raw", cc, -1)])
    dg4 = A.alloc("dg4", [8, 4, 128], BF16)
    P.add("pool", lambda e: e.tensor_tensor(
        out=dg4.rearrange("p a b c -> p (a b) c"), in0=ident_b.unsqueeze(1).to_broadcast([128, 32, 128]),
        in1=qkw.rearrange("p a b -> p (a b)").unsqueeze(2).to_broadcast([128, 32, 128]), op=ALU.mult),
        reads=[("ident_b", 0), ("qkw", 0)], writes=[("dg4", 0)])
    for half in range(2):
        wq_, wqk = load_wblock(1024 + half * 512)
        for m in range(4):
            cc = half * 4 + m
            for nb in range(NB):
                ps, pk = proj_fm(wq_, wqk, m, nb)
                P.add("act", (lambda e, ps=ps, cc=cc, nb=nb: e.activation(
                    out=qk_raw[:, cc, PADQ + nb * 512:PADQ + (nb + 1) * 512], in_=ps[:, :], func=AF.Identity)),
                    reads=[pk], writes=[("qk_raw", cc, nb)])
    qkc = A.alloc("qkc", [8, T], BF16)
    for cc in range(8):
        for nb in range(NB):
            ps, pk = next_ps()

            def qmm(e, ps=ps, cc=cc, nb=nb):
                ins = None
                for k in range(4):
                    o = PADQ - 3 + nb * 512 + k
                    ins = e.matmul(ps[:, :], lhsT=dg4[:, cc, k, :], rhs=qk_raw[:, cc, o:o + 512], start=(k == 0), stop=(k == 3))
                return ins
            P.add("pe", qmm, reads=[("dg4", 0), ("qk_raw", cc, nb), ("qk_raw", cc, nb - 1)], writes=[pk])
            P.add("act", (lambda e, ps=ps, cc=cc, nb=nb: e.activation(
                out=qkc[:, cc, nb * 512:(nb + 1) * 512], in_=ps[:, :], func=AF.Silu, bias=qkb[:, cc:cc + 1])),
                reads=[pk, ("qkb", 0)], writes=[("qkc", cc, nb)])
    dbg_out("qkc", qkc, [("qkc", cc, nb) for cc in range(8) for nb in range(NB)])
    A.free("qk_raw"); A.free("dg4")
    if stage <= 2.2:
        return finish()

    wif = A.alloc("wif", [8, 8], BF16)
    wif_f = A.alloc("wif_f", [8, 8], F32)
    with nc.allow_non_contiguous_dma(reason="tiny gate-weight columns"):
        P.add("sp", lambda e: e.dma_start(out=wif_f, in_=win_v[:, :, 3072:3080]), writes=[("wif_f", 0)], dma=True, grp="c_wif")
    P.add("dve", lambda e: e.tensor_copy(out=wif, in_=wif_f), reads=[("wif_f", 0)], writes=[("wif", 0)])
    G = A.alloc("G", [NT, 8], F32)
    nlf = A.alloc("nlf", [NT, 4], F32)
    gtmp = A.alloc("gtmp", [NT, 4], F32)
    A_inv = A.alloc("A_inv", [NT, 4], F32)
    Bv = A.alloc("Bv", [NT, 4], F32)
    dec = A.alloc("dec", [NT, 4], F32)
    psg, pkg = hold_ps()

    def gmm(e):
        ins = None
        for ti in range(NT):
            for k in range(8):
                ins = e.matmul(psg[:, ti * 8:(ti + 1) * 8], lhsT=hT[:, k, ti * 128:(ti + 1) * 128], rhs=wif[:, k, :],
                               start=(k == 0), stop=(k == 7))
        return ins
    P.add("pe", gmm, reads=[("wif", 0)] + [("hT", c, nb) for c in range(8) for nb in range(NB)], writes=[pkg])
    P.add("dve", lambda e: e.tensor_tensor(out=G, in0=psg[:, 0:128].rearrange("p (a b) -> p a b", b=8),
                                           in1=bif.unsqueeze(1).to_broadcast([128, NT, 8]), op=ALU.add),
          reads=[pkg, ("bif", 0)], writes=[("G", 0)])
    release_ps(pkg)
    dbg_out("G", G, [("G", 0)])
    if stage <= 2.31:
        return finish()
    P.add("act", lambda e: e.activation(out=gtmp, in_=G[:, :, 4:8], func=AF.Exp, scale=-1.0),
          reads=[("G", 0)], writes=[("gtmp", 0)])
    P.add("act", lambda e: e.activation(out=nlf, in_=gtmp, func=AF.Ln, bias=1.0),
          reads=[("gtmp", 0)], writes=[("nlf", 0)])
    dbg_out("nlf", nlf, [("nlf", 0)])
    if stage <= 2.32:
        return finish()
    psc, pkc = next_ps()
    nlf2 = nlf.rearrange("p a b -> p (a b)")
    nl_hi = A.alloc("nl_hi", [64], BF16)
    nl_lo = A.alloc("nl_lo", [64], BF16)
    P.add("dve", lambda e: e.tensor_copy(out=nl_hi, in_=nlf2), reads=[("nlf", 0)], writes=[("nl_hi", 0)])
    P.add("dve", lambda e: e.tensor_tensor(out=nl_lo, in0=nlf2, in1=nl_hi, op=ALU.subtract),
          reads=[("nlf", 0), ("nl_hi", 0)], writes=[("nl_lo", 0)])

    def cmm2(e):
        e.matmul(psc[:, 0:64], lhsT=mask_ut, rhs=nl_hi, start=True, stop=False)
        e.matmul(psc[:, 0:64], lhsT=mask_ut, rhs=nl_lo, start=False, stop=True)
        e.matmul(psc[:, 64:128], lhsT=ones_b, rhs=nl_hi, start=True, stop=False)
        return e.matmul(psc[:, 64:128], lhsT=ones_b, rhs=nl_lo, start=False, stop=True)
    P.add("pe", cmm2, reads=[("mask_ut", 0), ("ones_b", 0), ("nl_hi", 0), ("nl_lo", 0)], writes=[pkc])
    if stage <= 2.33:
        P.add("dve", lambda e: e.tensor_copy(out=gtmp.rearrange("p a b -> p (a b)"), in_=psc[:, 0:64]), reads=[pkc], writes=[("gtmp", 0)])
        dbg_out("ncum", gtmp, [("gtmp", 0)])
        return finish()
    LNS = float(np.log(128.0 ** 0.5))
    cval = A.alloc("cval", [2], F32)
    P.add("pool", lambda e: e.memset(cval[:, 0:1], LNS), writes=[("cval", 0)])
    P.add("pool", lambda e: e.memset(cval[:, 1:2], -LNS), writes=[("cval", 1)])
    P.add("act", lambda e: e.activation(out=A_inv.rearrange("p a b -> p (a b)"), in_=psc[:, 0:64], func=AF.Exp, bias=cval[:, 0:1]),
          reads=[pkc, ("cval", 0)], writes=[("A_inv", 0)])
    A_ = A.alloc("A_", [NT, 4], F32)
    P.add("act", lambda e: e.activation(out=A_.rearrange("p a b -> p (a b)"), in_=psc[:, 0:64], func=AF.Exp, scale=-1.0, bias=cval[:, 1:2]),
          reads=[pkc, ("cval", 1)], writes=[("A_", 0)])
    if stage <= 2.34:
        dbg_out("A_", A_, [("A_", 0)])
        dbg_out("A_inv", A_inv, [("A_inv", 0)])
        return finish()
    P.add("dve", lambda e: e.tensor_tensor(out=gtmp, in0=psc[:, 0:64].rearrange("p (a b) -> p a b", b=4), in1=G[:, :, 0:4], op=ALU.add),
          reads=[pkc, ("G", 0), ("gtmp", 0)], writes=[("gtmp", 0)])
    P.add("act", lambda e: e.activation(out=Bv, in_=gtmp, func=AF.Exp), reads=[("gtmp", 0)], writes=[("Bv", 0)])
    if stage <= 2.36:
        dbg_out("Bv", Bv, [("Bv", 0)])
        return finish()
    P.add("act", lambda e: e.activation(out=dec.rearrange("p a b -> p (a b)"), in_=psc[:, 64:128], func=AF.Exp, scale=-1.0),
          reads=[pkc], writes=[("dec", 0)])
    dbg_out("Bv", Bv, [("Bv", 0)])
    dbg_out("A_", A_, [("A_", 0)])
    dbg_out("decay", dec, [("dec", 0)])

    if stage <= 2.4:
        return finish()
    ktok = A.alloc("ktok", [NT, 512], BF16)
    for c in range(NT):
        ps, pk = next_ps()
        pb = ps.bitcast(BF16)

        def ktr(e, pb=pb, c=c):
            ins = None
            for h in range(4):
                ins = e.transpose(out=pb[:, h * 128:(h + 1) * 128], in_=qkc[:, 4 + h, c * 128:(c + 1) * 128], identity=ident_b)
            return ins
        P.add("pe", ktr, reads=[("ident_b", 0)] + [("qkc", 4 + h, c // 4) for h in range(4)], writes=[pk])
        P.add("act", (lambda e, pb=pb, c=c: e.activation(out=ktok[:, c, :], in_=pb[:, 0:512], func=AF.Identity)),
              reads=[pk], writes=[("ktok", c)])

    vB = A.alloc("vB", [NT, 4, 129], BF16)
    wv_, wvk = load_wblock(2048)
    for c in range(NT):
        ps, pk = next_ps()

        def vmm(e, ps=ps, c=c):
            ins = None
            for k in range(8):
                ins = e.matmul(ps[:, :], lhsT=hT[:, k, c * 128:(c + 1) * 128], rhs=wv_[:, k, 0:512], start=(k == 0), stop=(k == 7))
            return ins
        P.add("pe", vmm, reads=[wvk] + hT_keys(c // 4), writes=[pk])
        P.add("dve", (lambda e, ps=ps, c=c: e.tensor_tensor(
            out=vB[:, c, :, 0:128], in0=ps[:, :].rearrange("p (a b) -> p a b", b=128),
            in1=Bv[:, c, :].unsqueeze(2).to_broadcast([128, 4, 128]), op=ALU.mult)),
            reads=[pk, ("Bv", 0)], writes=[("vB", c, 0)])
        P.add("dve", (lambda e, c=c: e.tensor_copy(out=vB[:, c, :, 128], in_=Bv[:, c, :])),
              reads=[("Bv", 0)], writes=[("vB", c, 1)])

    if stage <= 2.6:
        return finish()
    E = A.alloc("E", [4, 129], F32)
    Cb = [A.alloc("Cb%d" % i, [4, 129], BF16) for i in range(2)]
    sm = [A.alloc("sm%d" % i, [4, 128], BF16) for i in range(2)]
    hn = [A.alloc("hn%d" % i, [4, 128], BF16) for i in range(2)]
    st6 = A.alloc("st6", [4, 6], F32)
    mv = A.alloc("mv", [4, 2], F32)
    den = A.alloc("den", [4], F32)
    qq = A.alloc("qq", [4], F32)
    rstd = A.alloc("rstd", [4], F32)
    sgo = [A.alloc("sgo%d" % i, [4, 512], BF16) for i in range(2)]
    hmT = A.alloc("hmT", [4, T], BF16)
    wo_, wok = load_wblock(2560)
    CW = 256

    chs = {}

    def chunk_A(c):
        nb = c // 4
        cs = slice(c * 128, (c + 1) * 128)
        if c % 4 == 0:
            for h in range(4):
                ps, pk = proj_fm(wo_, wok, h, nb)
                P.add("act", (lambda e, ps=ps, h=h, nb=nb: e.activation(out=sgo[nb % 2][:, h, :], in_=ps[:, :], func=AF.Sigmoid)),
                      reads=[pk], writes=[("sgo%d" % (nb % 2), h)])
        pss, pks = next_ps()

        def smm2(e, pss=pss, cs=cs):
            ins = None
            for h in range(4):
                ins = e.matmul(pss[:, h * 128:(h + 1) * 128], lhsT=qkc[:, 4 + h, cs], rhs=qkc[:, h, cs], start=True, stop=True)
            return ins
        P.add("pe", smm2, reads=[("qkc", cc, nb) for cc in range(8)], writes=[pks])
        smc = sm[c % 2]
        smk = ("sm%d" % (c % 2), 0)
        P.add("dve", (lambda e, pss=pss, smc=smc: e.tensor_tensor(
            out=smc, in0=pss[:, :].rearrange("p (a b) -> p a b", b=128),
            in1=mask_ut.unsqueeze(1).to_broadcast([128, 4, 128]), op=ALU.mult)),
            reads=[pks, ("mask_ut", 0)], writes=[smk])
        pu = [hold_ps(), hold_ps()]

        def umm(e, pu=pu, c=c):
            ins = None
            for h in range(4):
                o = pu[h // 2][0][:, (h % 2) * CW:(h % 2) * CW + 129]
                ins = e.matmul(o, lhsT=ktok[:, c, h * 128:(h + 1) * 128], rhs=vB[:, c, h, :], start=True, stop=True)
            return ins
        P.add("pe", umm, reads=[("ktok", c), ("vB", c, 0), ("vB", c, 1)], writes=[pu[0][1], pu[1][1]])
        chs[c] = (smc, smk, pu)

    def chunk_B(c):
        nb = c // 4
        cs = slice(c * 128, (c + 1) * 128)
        smc, smk, pu = chs[c]
        pn = [next_ps(), next_ps()]

        def nmm(e, pn=pn, smc=smc, c=c, cs=cs):
            ins = None
            for h in range(4):
                o = pn[h // 2][0][:, (h % 2) * CW:(h % 2) * CW + 129]
                ins = e.matmul(o, lhsT=smc[:, h, :], rhs=vB[:, c, h, :], start=True, stop=(c == 0))
                if c > 0:
                    ins = e.matmul(o, lhsT=qkc[:, h, cs], rhs=Cb[(c - 1) % 2][:, h, :], start=False, stop=True)
            return ins
        rd = [smk, ("vB", c, 0), ("vB", c, 1)] + [("qkc", h, nb) for h in range(4)]
        if c > 0:
            rd += [("Cb%d" % ((c - 1) % 2), h) for h in range(4)]
        P.add("pe", nmm, reads=rd, writes=[pn[0][1], pn[1][1]])
        for h in range(4):
            src = pu[h // 2][0][:, (h % 2) * CW:(h % 2) * CW + 129]
            if c == 0:
                P.add("dve", (lambda e, src=src, h=h: e.tensor_copy(out=E[:, h, :], in_=src)),
                      reads=[pu[h // 2][1]], writes=[("E", h)])
            else:
                P.add("dve", (lambda e, src=src, h=h, c=c: e.scalar_tensor_tensor(
                    out=E[:, h, :], in0=E[:, h, :], scalar=dec[:, c - 1, h:h + 1], in1=src, op0=ALU.mult, op1=ALU.add)),
                    reads=[pu[h // 2][1], ("E", h), ("dec", 0)], writes=[("E", h)])
            if c < NT - 1:
                P.add("act", (lambda e, h=h, c=c: e.activation(out=Cb[c % 2][:, h, :], in_=E[:, h, :], func=AF.Identity,
                                                               scale=dec[:, c, h:h + 1])),
                      reads=[("E", h), ("dec", 0)], writes=[("Cb%d" % (c % 2), h)])
        release_ps(pu[0][1]); release_ps(pu[1][1])
        chs[c] = pn

    def chunk_C(c):
        nb = c // 4
        cs = slice(c * 128, (c + 1) * 128)
        pn = chs[c]
        for h in range(4):
            src = pn[h // 2][0][:, (h % 2) * CW:(h % 2) * CW + 128]
            P.add("dve", (lambda e, src=src, h=h: e.bn_stats(out=st6[:, h, :], in_=src)),
                  reads=[pn[h // 2][1]], writes=[("st6", h)])
            P.add("dve", (lambda e, h=h: e.bn_aggr(out=mv[:, h, :], in_=st6[:, h, :])),
                  reads=[("st6", h)], writes=[("mv", h)])
        for b2 in range(2):
            dsrc = pn[b2][0][:, 0:512].rearrange("p (a b) -> p a b", b=CW)[:, :, 128]
            P.add("dve", (lambda e, dsrc=dsrc, b2=b2, c=c: e.tensor_tensor(
                out=den[:, 2 * b2:2 * b2 + 2], in0=dsrc, in1=A_[:, c, 2 * b2:2 * b2 + 2], op=ALU.mult)),
                reads=[pn[b2][1], ("A_", 0)], writes=[("den", b2)])
        P.add("dve", lambda e: e.scalar_tensor_tensor(out=den, in0=den, scalar=-1.0, in1=den, op0=ALU.mult, op1=ALU.max),
              reads=[("den", 0), ("den", 1)], writes=[("den", 0), ("den", 1)])
        P.add("dve", lambda e: e.tensor_scalar(out=den, in0=den, scalar1=1.0, scalar2=None, op0=ALU.max),
              reads=[("den", 0), ("den", 1)], writes=[("den", 0), ("den", 1)])
        P.add("dve", (lambda e, c=c: e.tensor_tensor(out=qq, in0=den, in1=A_inv[:, c, :], op=ALU.mult)),
              reads=[("den", 0), ("den", 1), ("A_inv", 0)], writes=[("qq", 0)])
        P.add("dve", lambda e: e.tensor_tensor(out=qq, in0=qq, in1=qq, op=ALU.mult), reads=[("qq", 0)], writes=[("qq", 0)])
        P.add("dve", lambda e: e.scalar_tensor_tensor(out=rstd, in0=qq, scalar=1e-5, in1=mv[:, :, 1], op0=ALU.mult, op1=ALU.add),
              reads=[("qq", 0)] + [("mv", h) for h in range(4)], writes=[("rstd", 0)])
        P.add("act", lambda e: e.activation(out=rstd, in_=rstd, func=AF.Sqrt), reads=[("rstd", 0)], writes=[("rstd", 0)])
        P.add("dve", lambda e: e.reciprocal(out=rstd, in_=rstd), reads=[("rstd", 0)], writes=[("rstd", 0)])
        hnc = hn[c % 2]
        hnk = "hn%d" % (c % 2)
        for h in range(4):
            src = pn[h // 2][0][:, (h % 2) * CW:(h % 2) * CW + 128]
            P.add("dve", (lambda e, src=src, h=h, hnc=hnc: e.tensor_scalar(
                out=hnc[:, h, :], in0=src, scalar1=mv[:, h, 0:1], scalar2=rstd[:, h:h + 1], op0=ALU.subtract, op1=ALU.mult)),
                reads=[pn[h // 2][1], ("mv", h), ("rstd", 0)], writes=[(hnk, h)])
        pt, pkt = next_ps()
        ptb = pt.bitcast(BF16)

        def htr(e, ptb=ptb, hnc=hnc):
            ins = None
            for h in range(4):
                ins = e.transpose(out=ptb[:, h * 128:(h + 1) * 128], in_=hnc[:, h, :], identity=ident_b)
            return ins
        P.add("pe", htr, reads=[("ident_b", 0)] + [(hnk, h) for h in range(4)], writes=[pkt])
        P.add("dve", (lambda e, ptb=ptb, c=c, nb=nb, cs=cs: e.tensor_tensor(
            out=hmT[:, :, cs], in0=ptb[:, 0:512].rearrange("p (a b) -> p a b", b=128),
            in1=sgo[nb % 2][:, :, (c % 4) * 128:(c % 4 + 1) * 128], op=ALU.mult)),
            reads=[pkt] + [("sgo%d" % (nb % 2), h) for h in range(4)], writes=[("hmT", c)])

    chunk_A(0)
    for c in range(NT):
        if c + 1 < NT:
            chunk_A(c + 1)
        chunk_B(c)
        chunk_C(c)
    dbg_out("hmT", hmT, [("hmT", c) for c in range(NT)])
    for nm in ("qkc", "wif", "wif_f", "nl_hi", "nl_lo", "G", "nlf", "gtmp", "A_inv", "Bv", "dec", "A_", "ktok", "vB", "E", "Cb0", "Cb1", "sm0", "sm1",
               "hn0", "hn1", "st6", "mv", "den", "qq", "rstd", "sgo0", "sgo1"):
        A.free(nm)

    wmo = load_cast("wmo", wmo_d.rearrange("(c p) n -> p c n", p=128), [4, D])
    for h in range(4):
        P.add("dve", (lambda e, h=h: e.tensor_scalar(out=wmo[:, h, :], in0=wmo[:, h, :], scalar1=mng[:, h:h + 1], scalar2=None,
                                                     op0=ALU.mult)),
              reads=[("wmo", 0), ("wmo", 1 + h), ("mng", 0)], writes=[("wmo", 1 + h)])
    mtmp = [A.alloc("mtmp%d" % i, [512], BF16) for i in range(2)]
    for jb in range(2):
        wg_, wgk = load_wblock(4104 + jb * 512)
        for jj in range(4):
            j = jb * 4 + jj
            for nb in range(NB):
                psy, pky = next_ps()

                def ymm2(e, psy=psy, j=j, nb=nb):
                    ins = None
                    for h in range(4):
                        ins = e.matmul(psy[:, :], lhsT=wmo[:, h, j * 128:(j + 1) * 128], rhs=hmT[:, h, nb * 512:(nb + 1) * 512],
                                       start=(h == 0), stop=(h == 3))
                    return ins
                P.add("pe", ymm2, reads=[("wmo", 1 + h) for h in range(4)] + [("hmT", c) for c in range(nb * 4, nb * 4 + 4)],
                      writes=[pky])
                psg2, pkg2 = proj_fm(wg_, wgk, jj, nb)
                sg, sgk = next_sgt()
                P.add("act", (lambda e, sg=sg, p=psg2: e.activation(out=sg, in_=p[:, :], func=AF.Sigmoid)),
                      reads=[pkg2], writes=[sgk])
                mt = mtmp[(j * NB + nb) % 2]
                mtk = ("mtmp%d" % ((j * NB + nb) % 2), 0)
                P.add("dve", (lambda e, sg=sg, p=psy, mt=mt: e.tensor_tensor(out=mt, in0=p[:, :], in1=sg, op=ALU.mult)),
                      reads=[pky, sgk], writes=[mtk])
                P.add("dve", (lambda e, mt=mt, j=j, nb=nb: e.tensor_tensor(
                    out=merged[:, j, nb * 512:(nb + 1) * 512], in0=merged[:, j, nb * 512:(nb + 1) * 512], in1=mt, op=ALU.add)),
                    reads=[mtk, ("merged", j, nb)], writes=[("merged", j, nb)])
    dbg_out("merged", merged, [("merged", j, nb) for j in range(8) for nb in range(NB)])
    for nm in ("hmT", "wmo", "mtmp0", "mtmp1", "sgt0", "sgt1", "hT", "wblk0", "wblk1", "wblk2"):
        A.free(nm)
    if stage <= 3:
        return finish()

    dgf = A.alloc("dgf", [128], F32)
    dgh = A.alloc("dgh", [128], BF16)
    dgl = A.alloc("dgl", [128], BF16)

    def row_bcast(name, col0, src=None, srckey=("modT", 1)):
        src = modT if src is None else src
        row = A.alloc(name, [D], F32)
        banks = [next_ps(), next_ps()]
        for j in range(8):
            P.add("dve", (lambda e, j=j: e.tensor_scalar(out=dgf, in0=ident_f, scalar1=src[:, col0 + j:col0 + j + 1], scalar2=None,
                                                         op0=ALU.mult)),
                  reads=[("ident_f", 0), srckey], writes=[("dgf", 0)])
            P.add("dve", lambda e: e.tensor_copy(out=dgh, in_=dgf), reads=[("dgf", 0)], writes=[("dgh", 0)])
            P.add("dve", lambda e: e.tensor_tensor(out=dgl, in0=dgf, in1=dgh, op=ALU.subtract),
                  reads=[("dgf", 0), ("dgh", 0)], writes=[("dgl", 0)])
            bk, bkk = banks[j // 4]

            def bmm(e, bk=bk, j=j):
                o = bk[:, (j % 4) * 128:(j % 4 + 1) * 128]
                e.matmul(o, lhsT=ones_b, rhs=dgh, start=True, stop=False)
                return e.matmul(o, lhsT=ones_b, rhs=dgl, start=False, stop=True)
            P.add("pe", bmm, reads=[("ones_b", 0), ("dgh", 0), ("dgl", 0)], writes=[bkk])
        for b2 in range(2):
            bk, bkk = banks[b2]
            P.add("act", (lambda e, bk=bk, b2=b2: e.activation(out=row[:, b2 * 512:(b2 + 1) * 512], in_=bk[:, :], func=AF.Identity)),
                  reads=[bkk], writes=[(name, b2)])
        return row

    gt1row = row_bcast("gt1row", 16)
    a2row = row_bcast("a2row", 0, src=a2, srckey=("a2", 0))
    sh2row = row_bcast("sh2row", 24)
    gt2row = row_bcast("gt2row", 40)
    wout = load_cast("wout", wout_d.rearrange("(c p) n -> p c n", p=128), [8, D])
    for k in range(8):
        P.add("dve", (lambda e, k=k: e.tensor_tensor(out=wout[:, k, :], in0=wout[:, k, :], in1=gt1row, op=ALU.mult)),
              reads=[("wout", 0), ("wout", 1 + k), ("gt1row", 0), ("gt1row", 1)], writes=[("wout", 1 + k)])
    x1 = A.alloc("x1", [NT, D], F32)
    for ti in range(NT):
        P.add("sp", (lambda e, ti=ti: e.dma_start(out=x1[:, ti, :], in_=x_d[ti * 128:(ti + 1) * 128, :])),
              writes=[("x1", ti)], dma=True, grp="x1ld%d" % ti)
    wr_f = A.alloc("wr_f", [8, 36], F32)
    wr_b = A.alloc("wr_b", [8, 36], BF16)
    with nc.allow_non_contiguous_dma(reason="small router weight rows"):
        P.add("sp", lambda e: e.dma_start(out=wr_f, in_=wr_d.rearrange("(c p) n -> p c n", p=128)), writes=[("wr_f", 0)],
              dma=True, grp="c_wr")
    P.add("dve", lambda e: e.tensor_copy(out=wr_b, in_=wr_f), reads=[("wr_f", 0)], writes=[("wr_b", 0)])
    brt = load_const("brt", br_d, [36])
    h2tok = A.alloc("h2tok", [NT, D], BF16)
    xn2 = [A.alloc("xn2_%d" % i, [D], F32) for i in range(2)]
    h2T = [A.alloc("h2T%d" % i, [8, 128], BF16) for i in range(2)]
    ss2 = A.alloc("ss2", [NT], F32)
    rs2 = A.alloc("rs2", [NT], F32)
    psr = [hold_ps(), hold_ps()]
    def emit_p5(ti):
        s2 = ti % 2
        P.add("act", (lambda e, ti=ti: e.activation(out=junk, in_=x1[:, ti, :], func=AF.Square, accum_out=ss2[:, ti:ti + 1])),
              reads=[("x1", ti)], writes=[("junk", 0), ("ss2", ti)])
        P.add("act", (lambda e, ti=ti: e.activation(out=rs2[:, ti:ti + 1], in_=ss2[:, ti:ti + 1], func=AF.Sqrt, scale=1.0 / D, bias=1e-6)),
              reads=[("ss2", ti)], writes=[("rs2", ti)])
        P.add("dve", (lambda e, ti=ti: e.reciprocal(out=rs2[:, ti:ti + 1], in_=rs2[:, ti:ti + 1])),
              reads=[("rs2", ti)], writes=[("rs2", ti)])
        P.add("act", (lambda e, ti=ti, s2=s2: e.activation(out=xn2[s2], in_=x1[:, ti, :], func=AF.Identity, scale=rs2[:, ti:ti + 1])),
              reads=[("x1", ti), ("rs2", ti)], writes=[("xn2_%d" % s2, 0)])
        P.add("dve", (lambda e, s2=s2: e.tensor_tensor(out=xn2[s2], in0=xn2[s2], in1=a2row, op=ALU.mult)),
              reads=[("xn2_%d" % s2, 0), ("a2row", 0), ("a2row", 1)], writes=[("xn2_%d" % s2, 0)])
        P.add("dve", (lambda e, s2=s2, ti=ti: e.tensor_tensor(out=h2tok[:, ti, :], in0=xn2[s2], in1=sh2row, op=ALU.add)),
              reads=[("xn2_%d" % s2, 0), ("sh2row", 0), ("sh2row", 1)], writes=[("h2tok", ti)])

    def emit_p5b(ti):
        s2 = ti % 2
        pt, pkt = next_ps()
        ptb = pt.bitcast(BF16)

        def h2tr(e, ptb=ptb, ti=ti):
            ins = None
            for c in range(8):
                ins = e.transpose(out=ptb[:, c * 128:(c + 1) * 128], in_=h2tok[:, ti, c * 128:(c + 1) * 128], identity=ident_b)
            return ins
        P.add("pe", h2tr, reads=[("ident_b", 0), ("h2tok", ti)], writes=[pkt])
        P.add("act", (lambda e, ptb=ptb, s2=s2: e.activation(out=h2T[s2].rearrange("p a b -> p (a b)"), in_=ptb[:, 0:1024], func=AF.Identity)),
              reads=[pkt], writes=[("h2T%d" % s2, 0)])
        bk, bkk = psr[ti // 8]

        def rmm(e, bk=bk, ti=ti, s2=s2):
            ins = None
            o = bk[:, (ti % 8) * 36:(ti % 8 + 1) * 36]
            for k in range(8):
                ins = e.matmul(o, lhsT=h2T[s2][:, k, :], rhs=wr_b[:, k, :], start=(k == 0), stop=(k == 7))
            return ins
        P.add("pe", rmm, reads=[("h2T%d" % s2, 0), ("wr_b", 0)], writes=[bkk])

    def emit_p4(ti):
        for half in range(2):
            ps, pk = next_ps()

            def omm(e, ps=ps, ti=ti, half=half):
                ins = None
                for k in range(8):
                    ins = e.matmul(ps[:, :], lhsT=merged[:, k, ti * 128:(ti + 1) * 128], rhs=wout[:, k, half * 512:(half + 1) * 512],
                                   start=(k == 0), stop=(k == 7))
                return ins
            P.add("pe", omm, reads=[("wout", 1 + k) for k in range(8)] + [("merged", k, ti // 4) for k in range(8)], writes=[pk])
            P.add("dve", (lambda e, ps=ps, ti=ti, half=half: e.tensor_tensor(
                out=x1[:, ti, half * 512:(half + 1) * 512], in0=x1[:, ti, half * 512:(half + 1) * 512], in1=ps[:, :], op=ALU.add)),
                reads=[pk, ("x1", ti)], writes=[("x1", ti)])

    emit_p4(0)
    emit_p4(1)
    emit_p5(0)
    for ti in range(NT):
        if ti + 2 < NT:
            emit_p4(ti + 2)
        if ti + 1 < NT:
            emit_p5(ti + 1)
        emit_p5b(ti)
    dbg_out("x1", x1, [("x1", ti) for ti in range(NT)])
    A.free("merged"); A.free("wout"); A.free("gt1row")

    Lg = A.alloc("Lg", [NT, 36], F32)
    for b2 in range(2):
        bk, bkk = psr[b2]
        P.add("dve", (lambda e, bk=bk, b2=b2: e.tensor_tensor(
            out=Lg[:, b2 * 8:(b2 + 1) * 8, :], in0=bk[:, 0:288].rearrange("p (a b) -> p a b", b=36),
            in1=brt.unsqueeze(1).to_broadcast([128, 8, 36]), op=ALU.add)),
            reads=[bkk, ("brt", 0)], writes=[("Lg", b2)])
    release_ps(psr[0][1]); release_ps(psr[1][1])
    dbg_out("Lg", Lg, [("Lg", 0), ("Lg", 1)])
    dbg_out("h2tok", h2tok, [("h2tok", ti) for ti in range(NT)])
    for nm in ("xn2_0", "xn2_1", "h2T0", "h2T1", "a2row", "sh2row", "wr_f", "wr_b"):
        A.free(nm)
    if stage <= 5:
        return finish()

    NCHK0 = NG - 15
    def T_(name, shape, dt=F32):
        return A.alloc(name, shape, dt)
    LK = [("Lg", 0), ("Lg", 1)]
    lg = Lg[:, :, 0:4]
    le = Lg[:, :, 4:36]
    gmax = T_("gmax", [NT])
    G1h = T_("G1h", [NT, 4])
    egs = T_("egs", [NT, 4])
    p_g = T_("p_g", [NT])
    P.add("dve", lambda e: e.tensor_reduce(out=gmax, in_=lg, axis=AX.X, op=ALU.max), reads=LK, writes=[("gmax", 0)])
    gmb = gmax.unsqueeze(2).to_broadcast([128, NT, 4])
    P.add("dve", lambda e: e.tensor_tensor(out=G1h, in0=lg, in1=gmb, op=ALU.is_equal), reads=LK + [("gmax", 0)], writes=[("G1h", 0)])
    P.add("dve", lambda e: e.tensor_tensor(out=egs, in0=lg, in1=gmb, op=ALU.subtract), reads=LK + [("gmax", 0)], writes=[("egs", 0)])
    P.add("act", lambda e: e.activation(out=egs, in_=egs, func=AF.Exp), reads=[("egs", 0)], writes=[("egs", 0)])
    P.add("dve", lambda e: e.tensor_reduce(out=p_g, in_=egs, axis=AX.X, op=ALU.add), reads=[("egs", 0)], writes=[("p_g", 0)])
    P.add("dve", lambda e: e.reciprocal(out=p_g, in_=p_g), reads=[("p_g", 0)], writes=[("p_g", 0)])
    tmp32 = T_("tmp32", [NT, 32])
    lsel = T_("lsel", [NT, 8])
    P.add("dve", lambda e: e.tensor_tensor(
        out=tmp32.rearrange("p t (g j) -> p t g j", j=8), in0=le.rearrange("p t (g j) -> p t g j", j=8),
        in1=G1h.unsqueeze(3).to_broadcast([128, NT, 4, 8]), op=ALU.mult),
        reads=LK + [("G1h", 0)], writes=[("tmp32", 0)])
    P.add("dve", lambda e: e.tensor_reduce(out=lsel, in_=tmp32.rearrange("p t (g j) -> p t j g", j=8), axis=AX.X, op=ALU.add),
          reads=[("tmp32", 0)], writes=[("lsel", 0)])
    m1 = T_("m1", [NT])
    m2 = T_("m2", [NT])
    E1 = T_("E1", [NT, 8])
    E2 = T_("E2", [NT, 8])
    ls2 = T_("ls2", [NT, 8])
    P.add("dve", lambda e: e.tensor_reduce(out=m1, in_=lsel, axis=AX.X, op=ALU.max), reads=[("lsel", 0)], writes=[("m1", 0)])
    P.add("dve", lambda e: e.tensor_tensor(out=E1, in0=lsel, in1=m1.unsqueeze(2).to_broadcast([128, NT, 8]), op=ALU.is_equal),
          reads=[("lsel", 0), ("m1", 0)], writes=[("E1", 0)])
    P.add("dve", lambda e: e.scalar_tensor_tensor(out=ls2.rearrange("p a b -> p (a b)"), in0=E1.rearrange("p a b -> p (a b)"),
                                                  scalar=-1e30, in1=lsel.rearrange("p a b -> p (a b)"), op0=ALU.mult, op1=ALU.add),
          reads=[("E1", 0), ("lsel", 0)], writes=[("ls2", 0)])
    P.add("dve", lambda e: e.tensor_reduce(out=m2, in_=ls2, axis=AX.X, op=ALU.max), reads=[("ls2", 0)], writes=[("m2", 0)])
    P.add("dve", lambda e: e.tensor_tensor(out=E2, in0=ls2, in1=m2.unsqueeze(2).to_broadcast([128, NT, 8]), op=ALU.is_equal),
          reads=[("ls2", 0), ("m2", 0)], writes=[("E2", 0)])
    w1 = T_("w1", [NT])
    w2 = T_("w2", [NT])
    P.add("dve", lambda e: e.tensor_tensor(out=w2, in0=m1, in1=m2, op=ALU.subtract), reads=[("m1", 0), ("m2", 0)], writes=[("w2", 0)])
    P.add("act", lambda e: e.activation(out=w1, in_=w2, func=AF.Sigmoid), reads=[("w2", 0)], writes=[("w1", 0)])
    P.add("dve", lambda e: e.tensor_tensor(out=w1, in0=w1, in1=p_g, op=ALU.mult), reads=[("w1", 0), ("p_g", 0)], writes=[("w1", 0)])
    P.add("dve", lambda e: e.tensor_tensor(out=w2, in0=p_g, in1=w1, op=ALU.subtract), reads=[("w1", 0), ("p_g", 0), ("w2", 0)], writes=[("w2", 0)])
    A1 = T_("A1", [NT, 32])
    A2 = T_("A2", [NT, 32])
    A12b = T_("A12b", [NT, 32], BF16)
    for g in range(4):
        gb = G1h[:, :, g].unsqueeze(2).to_broadcast([128, NT, 8])
        P.add("dve", (lambda e, g=g, gb=gb: e.tensor_tensor(out=A1[:, :, g * 8:(g + 1) * 8], in0=E1, in1=gb, op=ALU.mult)),
              reads=[("E1", 0), ("G1h", 0)], writes=[("A1", g)])
        P.add("dve", (lambda e, g=g, gb=gb: e.tensor_tensor(out=A2[:, :, g * 8:(g + 1) * 8], in0=E2, in1=gb, op=ALU.mult)),
              reads=[("E2", 0), ("G1h", 0)], writes=[("A2", g)])
    AK = [("A1", g) for g in range(4)] + [("A2", g) for g in range(4)]
    P.add("dve", lambda e: e.tensor_tensor(out=A12b, in0=A1, in1=A2, op=ALU.add), reads=AK, writes=[("A12b", 0)])
    lstrict = T_("lstrict", [128], BF16)
    lsf = T_("lsf", [128], F32)
    P.add("pool", lambda e: e.memset(lsf, 1.0), writes=[("lsf", 0)])
    P.add("pool", lambda e: e.affine_select(out=lsf, in_=lsf, pattern=[[1, 128]], compare_op=ALU.is_ge, fill=0.0, base=-1,
                                             channel_multiplier=-1), reads=[("lsf", 0)], writes=[("lsf", 0)])
    P.add("pool", lambda e: e.tensor_copy(out=lstrict, in_=lsf), reads=[("lsf", 0)], writes=[("lstrict", 0)])
    psw, pkw = next_ps()
    pst_, pkt_ = next_ps()
    A12f = A12b.rearrange("p a b -> p (a b)")
    P.add("pe", lambda e: e.matmul(psw[:, :], lhsT=lstrict, rhs=A12f, start=True, stop=True),
          reads=[("lstrict", 0), ("A12b", 0)], writes=[pkw])
    P.add("pe", lambda e: e.matmul(pst_[:, :], lhsT=ones_b, rhs=A12f, start=True, stop=True),
          reads=[("ones_b", 0), ("A12b", 0)], writes=[pkt_])
    carry = T_("carry", [NT + 1, 32])
    P.add("dve", lambda e: e.memset(carry[:, 0, :], 0.0), writes=[("carry", 0)])
    for ti in range(NT):
        P.add("dve", (lambda e, ti=ti: e.tensor_tensor(out=carry[:, ti + 1, :], in0=carry[:, ti, :], in1=pst_[:, ti * 32:(ti + 1) * 32],
                                                      op=ALU.add)),
              reads=[("carry", ti), pkt_], writes=[("carry", ti + 1)])
    counts = carry[:, NT, :]
    CK = [("carry", ti) for ti in range(NT + 1)]
    thr = T_("thr", [64])
    thr_i = T_("thr_i", [64], I32)
    P.add("pool", lambda e: e.iota(thr_i, pattern=[[1, 64]], base=0, channel_multiplier=0), writes=[("thr_i", 0)])
    P.add("pool", lambda e: e.tensor_copy(out=thr, in_=thr_i), reads=[("thr_i", 0)], writes=[("thr", 0)])
    thr128 = T_("thr128", [8])
    P.add("pool", lambda e: e.tensor_scalar(out=thr128, in0=thr[:, 0:8], scalar1=float(GROWS), scalar2=None, op0=ALU.mult),
          reads=[("thr", 0)], writes=[("thr128", 0)])
    cmp1 = T_("cmp1", [32, 8])
    ngrp = T_("ngrp", [32])
    P.add("dve", lambda e: e.tensor_tensor(out=cmp1, in0=counts.unsqueeze(2).to_broadcast([128, 32, 8]),
                                           in1=thr128.unsqueeze(1).to_broadcast([128, 32, 8]), op=ALU.is_gt),
          reads=CK + [("thr128", 0)], writes=[("cmp1", 0)])
    P.add("dve", lambda e: e.tensor_reduce(out=ngrp, in_=cmp1, axis=AX.X, op=ALU.add), reads=[("cmp1", 0)], writes=[("ngrp", 0)])
    cs = [T_("cs0", [32]), T_("cs1", [32])]
    src, srck = ngrp, ("ngrp", 0)
    for si, sh in enumerate((1, 2, 4, 8, 16)):
        dst = cs[si % 2]
        dk = ("cs%d" % (si % 2),)
        P.add("dve", (lambda e, dst=dst, src=src, sh=sh: e.tensor_copy(out=dst[:, 0:sh], in_=src[:, 0:sh])),
              reads=[srck], writes=[dk + (0,)])
        P.add("dve", (lambda e, dst=dst, src=src, sh=sh: e.tensor_tensor(out=dst[:, sh:32], in0=src[:, sh:32], in1=src[:, 0:32 - sh],
                                                                        op=ALU.add)),
              reads=[srck], writes=[dk + (1,)])
        src, srck = dst, dk + (1,)
        if si > 0:
            pass
    pend = src
    PK = [("cs0", 0), ("cs0", 1), ("cs1", 0), ("cs1", 1)]
    pstart = T_("pstart", [32])
    P.add("dve", lambda e: e.tensor_tensor(out=pstart, in0=pend, in1=ngrp, op=ALU.subtract), reads=PK + [("ngrp", 0)],
          writes=[("pstart", 0)])
    P.add("dve", lambda e: e.tensor_scalar(out=pstart, in0=pstart, scalar1=float(GROWS), scalar2=None, op0=ALU.mult),
          reads=[("pstart", 0)], writes=[("pstart", 0)])
    cmp2 = T_("cmp2", [NG, 32])
    grpf = T_("grpf", [NG])
    grpi = T_("grpi", [NG], I32)
    P.add("dve", lambda e: e.tensor_tensor(out=cmp2, in0=pend.unsqueeze(1).to_broadcast([128, NG, 32]),
                                           in1=thr[:, 0:NG].unsqueeze(2).to_broadcast([128, NG, 32]), op=ALU.is_le),
          reads=PK + [("thr", 0)], writes=[("cmp2", 0)])
    P.add("dve", lambda e: e.tensor_reduce(out=grpf, in_=cmp2, axis=AX.X, op=ALU.add), reads=[("cmp2", 0)], writes=[("grpf", 0)])
    P.add("dve", lambda e: e.tensor_scalar(out=grpf, in0=grpf, scalar1=31.0, scalar2=None, op0=ALU.min),
          reads=[("grpf", 0)], writes=[("grpf", 0)])
    P.add("dve", lambda e: e.tensor_copy(out=grpi, in_=grpf), reads=[("grpf", 0)], writes=[("grpi", 0)])
    pidx_i = T_("pidx_i", [1], I32)
    pidx = T_("pidx", [1])
    idxf = T_("idxf", [NG])
    inval = T_("inval", [NG])
    idxw = T_("idxw", [NG], I32)
    idxs = T_("idxs", [NG], I32)
    P.add("pool", lambda e: e.iota(pidx_i, pattern=[[0, 1]], base=0, channel_multiplier=1), writes=[("pidx_i", 0)])
    P.add("pool", lambda e: e.tensor_copy(out=pidx, in_=pidx_i), reads=[("pidx_i", 0)], writes=[("pidx", 0)])
    P.add("dve", lambda e: e.tensor_scalar(out=idxf, in0=grpf, scalar1=128.0, scalar2=pidx[:, 0:1], op0=ALU.mult, op1=ALU.add),
          reads=[("grpf", 0), ("pidx", 0)], writes=[("idxf", 0)])
    P.add("dve", lambda e: e.tensor_scalar(out=inval, in0=thr[:, 0:NG], scalar1=pend[:, 31:32], scalar2=None, op0=ALU.is_ge),
          reads=PK + [("thr", 0)], writes=[("inval", 0)])
    P.add("dve", lambda e: e.tensor_copy(out=idxw, in_=idxf), reads=[("idxf", 0)], writes=[("idxw", 0)])
    P.add("dve", lambda e: e.scalar_tensor_tensor(out=idxf, in0=inval, scalar=8192.0, in1=idxf, op0=ALU.mult, op1=ALU.add),
          reads=[("inval", 0), ("idxf", 0), ("idxw", 0)], writes=[("idxf", 0)])
    P.add("dve", lambda e: e.tensor_copy(out=idxs, in_=idxf), reads=[("idxf", 0)], writes=[("idxs", 0)])
    stg = {wn: A.alloc("stg_" + wn, [4096], F32) for wn in ("wg", "wu", "wd")}
    for (wsrc_, wn_) in ((weg_d, "wg"), (weu_d, "wu"), (wed_d, "wd")):
        P.add("pool", (lambda e, wsrc_=wsrc_, wn_=wn_: e.indirect_dma_start(
            out=stg[wn_], out_offset=None, in_=wsrc_, in_offset=bass.IndirectOffsetOnAxis(ap=idxw[:, 0:1], axis=0))),
            reads=[("idxw", 0)], writes=[("stg_" + wn_, 0)], dma=True, grp="stg_" + wn_)
    slot = T_("slot", [NT, 32])
    P.add("dve", lambda e: e.tensor_tensor(out=slot, in0=psw[:, :].rearrange("p (a b) -> p a b", b=32), in1=carry[:, 0:NT, :], op=ALU.add),
          reads=[pkw] + CK, writes=[("slot", 0)])
    P.add("dve", lambda e: e.tensor_tensor(out=slot, in0=slot, in1=pstart.unsqueeze(1).to_broadcast([128, NT, 32]), op=ALU.add),
          reads=[("slot", 0), ("pstart", 0)], writes=[("slot", 0)])
    dstf = T_("dstf", [2, NT])
    dsti = T_("dsti", [2, NT], I32)
    for q, (Aq, qk) in enumerate(((A1, "A1"), (A2, "A2"))):
        P.add("dve", (lambda e, Aq=Aq: e.tensor_tensor(out=tmp32, in0=Aq, in1=slot, op=ALU.mult)),
              reads=[(qk, g) for g in range(4)] + [("slot", 0), ("tmp32", 0)], writes=[("tmp32", 0)])
        P.add("dve", (lambda e, q=q: e.tensor_reduce(out=dstf[:, q, :], in_=tmp32, axis=AX.X, op=ALU.add)),
              reads=[("tmp32", 0)], writes=[("dstf", q)])
    P.add("dve", lambda e: e.tensor_copy(out=dsti, in_=dstf), reads=[("dstf", 0), ("dstf", 1)], writes=[("dsti", 0)])
    dbg_out("dstf", dstf, [("dstf", 0), ("dstf", 1)])
    dbg_out("grpf", grpf, [("grpf", 0)])
    dbg_out("w1", w1, [("w1", 0)])
    dbg_out("w2", w2, [("w2", 0)])
    for nm in ("gmax", "G1h", "egs", "p_g", "tmp32", "lsel", "m1", "m2", "E1", "E2", "ls2", "A1", "A2", "A12b", "lstrict", "lsf",
               "carry", "thr", "thr_i", "thr128", "cmp1", "ngrp", "cs0", "cs1", "pstart", "slot", "cmp2", "Lg"):
        A.free(nm)
    if stage <= 5.5:
        return finish()

    for ti in range(NT):
        for q in range(2):
            P.add("pool", (lambda e, ti=ti, q=q: e.indirect_dma_start(
                out=XS, out_offset=bass.IndirectOffsetOnAxis(ap=dsti[:, q, ti:ti + 1], axis=0),
                in_=h2tok[:, ti, :], in_offset=None)),
                reads=[("h2tok", ti), ("dsti", 0)] + XSZ_KEYS, writes=[("XS", ti, q)], dma=True, grp="xs_sc")
    XS_KEYS = [("XS", ti, q) for ti in range(NT) for q in range(2)]
    A.free("h2tok")
    NS = 2
    wgs = [A.alloc("wg%d" % i, [8, 512], BF16) for i in range(NS)]
    wus = [A.alloc("wu%d" % i, [8, 512], BF16) for i in range(NS)]
    wds = [A.alloc("wd%d" % i, [4, D], BF16) for i in range(NS)]
    xgt = [A.alloc("xgt%d" % i, [D], BF16) for i in range(2)]
    xgT = [A.alloc("xgT%d" % i, [8, 256], BF16) for i in range(2)]
    sgl = [A.alloc("sgl%d" % i, [4, 256], BF16) for i in range(1)]
    aT = [A.alloc("aT%d" % i, [4, 256], BF16) for i in range(2)]
    ysb = [A.alloc("ysb%d" % i, [D], BF16) for i in range(4)]
    def emit_load(g, part="both"):
        sl = g % NS
        s2 = g % 2
        for (wt, wsrc, wn, ceng) in ((wgs, weg_d, "wg", "act"), (wus, weu_d, "wu", "dve"), (wds, wed_d, "wd", "pool")):
            st_ = stg[wn]
            if part in ("both", "dma") and g >= NCHK0:
                P.add("pool", (lambda e, g=g, st_=st_, wsrc=wsrc: e.indirect_dma_start(
                    out=st_, out_offset=None, in_=wsrc,
                    in_offset=bass.IndirectOffsetOnAxis(ap=idxs[:, g:g + 1], axis=0), bounds_check=32 * 128 - 1, oob_is_err=False)),
                    reads=[("idxs", 0)], writes=[("stg_" + wn, 0)], dma=True, grp="stg_" + wn)
            elif part in ("both", "dma"):
                P.add("pool", (lambda e, g=g, st_=st_, wsrc=wsrc: e.indirect_dma_start(
                    out=st_, out_offset=None, in_=wsrc,
                    in_offset=bass.IndirectOffsetOnAxis(ap=idxw[:, g:g + 1], axis=0))),
                    reads=[("idxw", 0)], writes=[("stg_" + wn, 0)], dma=True, grp="stg_" + wn)
            if part == "dma":
                continue
            dstv = wt[sl].rearrange("p a b -> p (a b)")
            if ceng == "act":
                P.add("act", (lambda e, dstv=dstv, st_=st_: e.activation(out=dstv, in_=st_, func=AF.Identity)),
                      reads=[("stg_" + wn, 0)], writes=[("%s%d" % (wn, sl), 0)])
            elif ceng == "dve":
                P.add("dve", (lambda e, dstv=dstv, st_=st_: e.tensor_copy(out=dstv, in_=st_)),
                      reads=[("stg_" + wn, 0)], writes=[("%s%d" % (wn, sl), 0)])
            else:
                P.add("act", (lambda e, dstv=dstv, st_=st_: e.activation(out=dstv[:, 0:2048], in_=st_[:, 0:2048], func=AF.Identity)),
                      reads=[("stg_" + wn, 0)], writes=[("%s%d" % (wn, sl), 0)])
                P.add("dve", (lambda e, dstv=dstv, st_=st_: e.tensor_copy(out=dstv[:, 2048:4096], in_=st_[:, 2048:4096])),
                      reads=[("stg_" + wn, 0)], writes=[("%s%d" % (wn, sl), 1)])

    def emit_compute_a(g):
        s2 = g % 2
        for hf in range(2):
            xi = (2 * g + hf) % 2
            r0 = g * GROWS + hf * 128
            P.add("sp", (lambda e, r0=r0, xi=xi: e.dma_start(out=xgt[xi], in_=XS[r0:r0 + 128, :])),
                  reads=XS_KEYS, writes=[("xgt%d" % xi, 0)], dma=True, grp="xgt%d" % xi)
            pt, pkt = next_ps()
            ptb = pt.bitcast(BF16)

            def xtr(e, ptb=ptb, xi=xi):
                ins = None
                for c in range(8):
                    ins = e.transpose(out=ptb[:, c * 128:(c + 1) * 128], in_=xgt[xi][:, c * 128:(c + 1) * 128], identity=ident_b)
                return ins
            P.add("pe", xtr, reads=[("ident_b", 0), ("xgt%d" % xi, 0)], writes=[pkt])
            P.add("act", (lambda e, ptb=ptb, s2=s2, hf=hf: e.activation(
                out=xgT[s2][:, :, hf * 128:(hf + 1) * 128], in_=ptb[:, 0:1024].rearrange("p (a b) -> p a b", b=128), func=AF.Identity)),
                reads=[pkt], writes=[("xgT%d" % s2, hf)])

    def emit_compute_b1(g):
        sl = g % NS
        s2 = g % 2
        pg_ = [next_ps(), next_ps()]
        pu_ = [next_ps(), next_ps()]

        def gumm(e, pg_=pg_, pu_=pu_, sl=sl, s2=s2):
            ins = None
            for (pp, ww) in ((pg_, wgs), (pu_, wus)):
                for fc in range(4):
                    o = pp[fc // 2][0][:, (fc % 2) * 256:(fc % 2 + 1) * 256]
                    for k in range(8):
                        ins = e.matmul(o, lhsT=ww[sl][:, k, fc * 128:(fc + 1) * 128], rhs=xgT[s2][:, k, :], start=(k == 0), stop=(k == 7))
            return ins
        P.add("pe", gumm, reads=[("wg%d" % sl, 0), ("wu%d" % sl, 0), ("xgT%d" % s2, 0), ("xgT%d" % s2, 1)],
              writes=[pg_[0][1], pg_[1][1], pu_[0][1], pu_[1][1]])
        for b2 in range(2):
            P.add("act", (lambda e, pg_=pg_, s2=s2, b2=b2: e.activation(
                out=sgl[0][:, 2 * b2:2 * b2 + 2, :].rearrange("p a b -> p (a b)"), in_=pg_[b2][0][:, :], func=AF.Silu)),
                reads=[pg_[b2][1]], writes=[("sgl0", b2)])
            P.add("dve", (lambda e, pu_=pu_, s2=s2, b2=b2: e.tensor_tensor(
                out=aT[s2][:, 2 * b2:2 * b2 + 2, :].rearrange("p a b -> p (a b)"), in0=pu_[b2][0][:, :],
                in1=sgl[0][:, 2 * b2:2 * b2 + 2, :].rearrange("p a b -> p (a b)"), op=ALU.mult)),
                reads=[pu_[b2][1], ("sgl0", b2)], writes=[("aT%d" % s2, b2)])

    def emit_compute_b2(g):
        sl = g % NS
        s2 = g % 2
        for hf in range(2):
            yi = (2 * g + hf) % 4
            py = [next_ps(), next_ps()]

            def dmm(e, py=py, sl=sl, s2=s2, hf=hf):
                ins = None
                for half in range(2):
                    for fc in range(4):
                        ins = e.matmul(py[half][0][:, :], lhsT=aT[s2][:, fc, hf * 128:(hf + 1) * 128],
                                       rhs=wds[sl][:, fc, half * 512:(half + 1) * 512], start=(fc == 0), stop=(fc == 3))
                return ins
            P.add("pe", dmm, reads=[("wd%d" % sl, 0), ("wd%d" % sl, 1), ("aT%d" % s2, 0), ("aT%d" % s2, 1)], writes=[py[0][1], py[1][1]])
            for half in range(2):
                P.add("dve", (lambda e, py=py, yi=yi, half=half: e.tensor_tensor(
                    out=ysb[yi][:, half * 512:(half + 1) * 512], in0=py[half][0][:, :], in1=gt2row[:, half * 512:(half + 1) * 512],
                    op=ALU.mult)),
                    reads=[py[half][1], ("gt2row", half)], writes=[("ysb%d" % yi, half)])
            r0 = g * GROWS + hf * 128
            P.add("sp", (lambda e, r0=r0, yi=yi: e.dma_start(out=YS[r0:r0 + 128, :], in_=ysb[yi])),
                  reads=[("ysb%d" % yi, 0), ("ysb%d" % yi, 1)], writes=[("YS", g, hf)], dma=True, grp="ys_st%d" % yi)

    emit_load(0, "cast")
    emit_compute_a(0)
    for g in range(NG):
        emit_compute_b1(g)
        if g + 1 < NG:
            emit_load(g + 1)
            emit_compute_a(g + 1)
        emit_compute_b2(g)
    YS_KEYS = [("YS", g, hf) for g in range(NG) for hf in range(2)]
    for i in range(NS):
        A.free("wg%d" % i); A.free("wu%d" % i); A.free("wd%d" % i)
    for nm in ("stg_wg", "stg_wu", "stg_wd", "xgt0", "xgt1", "xgT0", "xgT1", "sgl0", "aT0", "aT1", "ysb0", "ysb1", "ysb2", "ysb3"):
        A.free(nm)

    gfin = load_const("gfin", gfin_d, [D])
    NYG = 4
    yg = [[A.alloc("yg%d_%d" % (q, i), [D], BF16) for i in range(NYG)] for q in range(2)]
    acc = [A.alloc("acc%d" % i, [D], F32) for i in range(2)]
    outt = [A.alloc("outt%d" % i, [D], F32) for i in range(2)]
    ssf = A.alloc("ssf", [NT], F32)
    rsf = A.alloc("rsf", [NT], F32)

    def emit_g(ti):
        s4 = ti % NYG
        for q in range(2):
            P.add("pool", (lambda e, ti=ti, q=q, s4=s4: e.indirect_dma_start(
                out=yg[q][s4], out_offset=None, in_=YS,
                in_offset=bass.IndirectOffsetOnAxis(ap=dsti[:, q, ti:ti + 1], axis=0))),
                reads=YS_KEYS + [("dsti", 0)], writes=[("yg%d_%d" % (q, s4), 0)], dma=True, grp="yg%d_%d" % (q, s4))

    def emit_c1(ti):
        s4 = ti % NYG
        s2 = ti % 2
        y1, y2 = yg[0][s4], yg[1][s4]
        k1, k2 = ("yg0_%d" % s4, 0), ("yg1_%d" % s4, 0)
        ac = acc[s2]
        ak = ("acc%d" % s2, 0)
        P.add("act", (lambda e, y1=y1, ac=ac, ti=ti: e.activation(out=ac, in_=y1, func=AF.Identity, scale=w1[:, ti:ti + 1])),
              reads=[k1, ("w1", 0)], writes=[ak])
        P.add("dve", (lambda e, ac=ac, y2=y2, ti=ti: e.scalar_tensor_tensor(out=ac, in0=y2, scalar=w2[:, ti:ti + 1], in1=ac,
                                                                          op0=ALU.mult, op1=ALU.add)),
              reads=[ak, k2, ("w2", 0)], writes=[ak])
        P.add("dve", (lambda e, ac=ac, ti=ti: e.tensor_tensor(out=x1[:, ti, :], in0=x1[:, ti, :], in1=ac, op=ALU.add)),
              reads=[ak, ("x1", ti)], writes=[("x1", ti)])

    def emit_c2(ti):
        s2 = ti % 2
        P.add("act", (lambda e, ti=ti: e.activation(out=junk, in_=x1[:, ti, :], func=AF.Square, accum_out=ssf[:, ti:ti + 1])),
              reads=[("x1", ti)], writes=[("junk", 0), ("ssf", ti)])
        P.add("act", (lambda e, ti=ti: e.activation(out=rsf[:, ti:ti + 1], in_=ssf[:, ti:ti + 1], func=AF.Sqrt, scale=1.0 / D, bias=1e-6)),
              reads=[("ssf", ti)], writes=[("rsf", ti)])
        P.add("dve", (lambda e, ti=ti: e.reciprocal(out=rsf[:, ti:ti + 1], in_=rsf[:, ti:ti + 1])),
              reads=[("rsf", ti)], writes=[("rsf", ti)])
        ot = outt[s2]
        ok = ("outt%d" % s2, 0)
        P.add("act", (lambda e, ot=ot, ti=ti: e.activation(out=ot, in_=x1[:, ti, :], func=AF.Identity, scale=rsf[:, ti:ti + 1])),
              reads=[("x1", ti), ("rsf", ti)], writes=[ok])
        P.add("dve", (lambda e, ot=ot: e.tensor_tensor(out=ot, in0=ot, in1=gfin, op=ALU.mult)),
              reads=[ok, ("gfin", 0)], writes=[ok])
        P.add("sp", (lambda e, ot=ot, ti=ti: e.dma_start(out=out_d[ti * 128:(ti + 1) * 128, :], in_=ot)),
              reads=[ok], dma=True, grp="out%d" % s2)

    for ti in range(min(3, NT)):
        emit_g(ti)
    for ti in range(NT):
        if ti + 3 < NT:
            emit_g(ti + 3)
        emit_c1(ti)
        if ti > 0:
            emit_c2(ti - 1)
    emit_c2(NT - 1)
    P.emit(final_wait_groups=["out0", "out1"] + (["dbgout"] if "dbgout" in P.dma_groups else []))
    build.stats = dict(peak_kb=A.peak * 4 / 1024.0, n_ops=len(P.all_ops), n_groups=len(P.dma_groups))
    return nc, dbg_outs


def host_layout(inp, b):
    f = lambda a: np.ascontiguousarray(a, dtype=np.float32)
    col = lambda v, n: f(np.asarray(v).reshape(n, 128).T)
    m = {}
    m["x"] = f(inp["x"][b])
    m["c_col"] = col(inp["c"][b], 8)
    m["w_ada"] = f(inp["w_ada"][0])
    m["b_ada_col"] = col(inp["b_ada"][0], 48)
    m["g1_col"] = col(inp["g_norm1"][0], 8)
    m["g2_col"] = col(inp["g_norm2"][0], 8)
    m["w_in"] = f(inp["w_in"][0])
    m["b_if_bc"] = f(np.broadcast_to(inp["b_if"][0][None, :], (128, 8)))
    m["conv_w_col"] = f(inp["conv_dw_w"][0].reshape(31, 4, 128).transpose(2, 1, 0))
    m["conv_b_col"] = col(inp["conv_dw_b"][0], 4)
    m["conv_lng_col"] = col(inp["conv_ln_g"][0], 4)
    m["conv_lnb_col"] = col(inp["conv_ln_b"][0], 4)
    m["w_conv_out"] = f(inp["w_conv_out"][0])
    m["qk_w_col"] = f(inp["qk_conv_w"][0].reshape(4, 8, 128).transpose(2, 1, 0))
    m["qk_b_col"] = col(inp["qk_conv_b"][0], 8)
    m["mng_col"] = col(inp["m_norm_g"][0], 4)
    m["w_m_out"] = f(inp["w_m_out"][0])
    m["w_out"] = f(inp["w_out"][0])
    m["w_router"] = f(np.concatenate([inp["w_rg"][0], inp["w_re"][0]], axis=1))
    m["b_router_bc"] = f(np.broadcast_to(np.concatenate([inp["b_rg"][0], inp["b_re"][0]])[None, :], (128, 36)))
    m["w_e_gate_l"] = f(inp["w_e_gate"][0].reshape(32, 8, 128, 512).transpose(0, 2, 1, 3).reshape(32 * 128, 8 * 512))
    m["w_e_up_l"] = f(inp["w_e_up"][0].reshape(32, 8, 128, 512).transpose(0, 2, 1, 3).reshape(32 * 128, 8 * 512))
    m["w_e_down_l"] = f(inp["w_e_down"][0].reshape(32, 4, 128, D).transpose(0, 2, 1, 3).reshape(32 * 128, 4 * D))
    m["g_final_bc"] = f(np.broadcast_to(np.asarray(inp["g_final"])[None, :], (128, D)))
    return m


def kernel(**inputs):
    nc, _ = build()
    shared = host_layout(inputs, 0)
    in_maps = []
    for b in range(8):
        m = dict(shared)
        m["x"] = np.ascontiguousarray(inputs["x"][b], dtype=np.float32)
        m["c_col"] = np.ascontiguousarray(np.asarray(inputs["c"][b]).reshape(8, 128).T, dtype=np.float32)
        in_maps.append(m)
    res = run_bass_kernel_spmd(nc, in_maps, core_ids=list(range(8)))
    return np.stack([np.asarray(r["out"]) for r in res.results], axis=0).astype(np.float32)
```
